# Optimizing a Trainium2 kernel written in Bass

```python
import math
import jax, jax.numpy as jnp
from jax import lax
import numpy as np

D_MODEL = 1024
BATCH = 8
SEQ = 4096
DEPTH = 4

CHUNK = 64
N_MIXERS = 2
N_CONV_LAYERS = (DEPTH + 1) // 2
N_RET_LAYERS = DEPTH // 2

CONV_WIDTH = 31

RET_HEADS = 4
RET_QK_DIM = D_MODEL
RET_V_DIM = 2 * D_MODEL
RET_HEAD_QK = RET_QK_DIM // RET_HEADS
RET_HEAD_V = RET_V_DIM // RET_HEADS
ROPE_BASE = 10000.0

N_GROUPS = 4
EXPERTS_PER_GROUP = 8
N_EXPERTS = N_GROUPS * EXPERTS_PER_GROUP
TOP_K_IN_GROUP = 2
EXPERT_FF = D_MODEL // 2

DEEPNORM_ALPHA = (2.0 * DEPTH) ** 0.25
DEEPNORM_BETA = (8.0 * DEPTH) ** -0.25
LN_EPS = 1e-5

kernel_name = "hybrid_conv_retention_hmoe_deepnorm"


def layer_norm(x, g, b):
    xf = x.astype(jnp.float32)
    mu = jnp.mean(xf, axis=-1, keepdims=True)
    var = jnp.mean(jnp.square(xf - mu), axis=-1, keepdims=True)
    y = (xf - mu) * lax.rsqrt(var + LN_EPS) * g.astype(jnp.float32) + b.astype(jnp.float32)
    return y.astype(x.dtype)


def conformer_conv(x, w_pw1, b_pw1, w_dw, b_dw, ln_g, ln_b, w_pw2, b_pw2):
    h = x @ w_pw1 + b_pw1
    a, gate = jnp.split(h, 2, axis=-1)
    h = a * jax.nn.sigmoid(gate)
    h = lax.conv_general_dilated(
        h, w_dw[:, None, :], window_strides=(1,),
        padding=[(CONV_WIDTH - 1, 0)],
        dimension_numbers=("NWC", "WIO", "NWC"),
        feature_group_count=D_MODEL) + b_dw
    h = jax.nn.silu(layer_norm(h, ln_g, ln_b))
    return h @ w_pw2 + b_pw2


def rotary(t, positions):
    half = t.shape[-1] // 2
    inv_freq = ROPE_BASE ** (-jnp.arange(half, dtype=jnp.float32) / half)
    ang = positions.astype(jnp.float32)[..., None] * inv_freq
    cos = jnp.cos(ang)[:, :, None, :]
    sin = jnp.sin(ang)[:, :, None, :]
    t1, t2 = t[..., :half], t[..., half:]
    return jnp.concatenate([t1 * cos - t2 * sin, t1 * sin + t2 * cos], axis=-1)


def retention(x, positions, w_qkvg, gn_g, gn_b, w_o):
    B, S, _ = x.shape
    H, dk, dv, C = RET_HEADS, RET_HEAD_QK, RET_HEAD_V, CHUNK
    NC = S // C
    qkvg = x @ w_qkvg
    q, k, v, g = jnp.split(qkvg, [RET_QK_DIM, 2 * RET_QK_DIM, 2 * RET_QK_DIM + RET_V_DIM], axis=-1)
    q = rotary(q.reshape(B, S, H, dk).astype(jnp.float32), positions)
    k = rotary(k.reshape(B, S, H, dk).astype(jnp.float32), positions) * (dk ** -0.5)
    v = v.reshape(B, S, H, dv).astype(jnp.float32)

    log_gamma = jnp.log(1.0 - 2.0 ** (-5.0 - jnp.arange(H, dtype=jnp.float32)))
    idx = jnp.arange(C, dtype=jnp.float32)
    d_mask = jnp.exp(log_gamma[:, None, None] * jnp.abs(idx[:, None] - idx[None, :]))
    xi = jnp.exp(log_gamma[None, :] * (idx[:, None] + 1.0))
    zeta = jnp.exp(log_gamma[None, :] * (C - 1.0 - idx[:, None]))
    chunk_decay = jnp.exp(log_gamma * C)

    qc = q.reshape(B, NC, C, H, dk)
    kc = k.reshape(B, NC, C, H, dk)
    vc = v.reshape(B, NC, C, H, dv)

    scores = jnp.einsum("bnihd,bnjhd->bnhij", qc, kc) * d_mask
    o_inner = jnp.einsum("bnhij,bnjhe->bnihe", scores, vc)

    def step(state, inp):
        q_n, k_n, v_n = inp
        o = jnp.einsum("bihd,bhde->bihe", q_n, state) * xi[None, :, :, None]
        state = state * chunk_decay[None, :, None, None] + jnp.einsum(
            "bihd,bihe->bhde", k_n * zeta[None, :, :, None], v_n)
        return state, o

    xs = (qc.transpose(1, 0, 2, 3, 4), kc.transpose(1, 0, 2, 3, 4), vc.transpose(1, 0, 2, 3, 4))
    state0 = jnp.zeros((B, H, dk, dv), jnp.float32)
    _, o_cross = lax.scan(step, state0, xs)
    o = (o_inner + o_cross.transpose(1, 0, 2, 3, 4)).reshape(B, S, H, dv)

    mu = jnp.mean(o, axis=-1, keepdims=True)
    var = jnp.mean(jnp.square(o - mu), axis=-1, keepdims=True)
    o = ((o - mu) * lax.rsqrt(var + LN_EPS)).reshape(B, S, RET_V_DIM)
    o = o * gn_g.astype(jnp.float32) + gn_b.astype(jnp.float32)
    y = (jax.nn.silu(g.astype(jnp.float32)) * o).astype(x.dtype)
    return y @ w_o


def hier_moe(x, w_grp, b_grp, w_route, b_route, w_gate, w_up, w_down):
    B, S, D = x.shape
    T = B * S
    xt = x.reshape(T, D)
    grp_prob = jax.nn.softmax((xt @ w_grp + b_grp).astype(jnp.float32), axis=-1)
    p_g, g_idx = lax.top_k(grp_prob, 1)
    exp_logits = (xt @ w_route + b_route).astype(jnp.float32).reshape(T, N_GROUPS, EXPERTS_PER_GROUP)
    sel = jnp.take_along_axis(exp_logits, g_idx[:, :, None], axis=1)[:, 0]
    top_val, top_idx = lax.top_k(sel, TOP_K_IN_GROUP)
    gates = jax.nn.softmax(top_val, axis=-1) * p_g
    expert_id = g_idx * EXPERTS_PER_GROUP + top_idx
    combine = jnp.sum(jax.nn.one_hot(expert_id, N_EXPERTS, dtype=jnp.float32) * gates[..., None], axis=1)
    combine = combine.astype(x.dtype)
    y = jnp.zeros((T, D), x.dtype)
    for e in range(N_EXPERTS):
        h = jax.nn.silu(xt @ w_gate[e]) * (xt @ w_up[e])
        y = y + combine[:, e:e + 1] * (h @ w_down[e])
    return y.reshape(B, S, D)


def setup_inputs(seed: int = 0) -> dict:
    key = jax.random.key(seed)
    ks = jax.random.split(key, 26)
    D, F = D_MODEL, EXPERT_FF
    nrm = jax.random.normal
    f32 = jnp.float32
    x = nrm(ks[0], (BATCH, SEQ, D), f32)
    offset = jax.random.randint(ks[1], (BATCH, 1), 0, 4096, dtype=jnp.int32)
    positions = (offset + jnp.arange(SEQ, dtype=jnp.int32)[None, :]).astype(jnp.int32)
    Lc, Lr = N_CONV_LAYERS, N_RET_LAYERS
    return {
        "x": x,
        "positions": positions,
        "conv_w_pw1": nrm(ks[2], (Lc, D, 2 * D), f32) * D ** -0.5,
        "conv_b_pw1": nrm(ks[3], (Lc, 2 * D), f32) * 0.02,
        "conv_w_dw": nrm(ks[4], (Lc, CONV_WIDTH, D), f32) * CONV_WIDTH ** -0.5,
        "conv_b_dw": nrm(ks[5], (Lc, D), f32) * 0.02,
        "conv_ln_g": 1.0 + 0.05 * nrm(ks[6], (Lc, D), f32),
        "conv_ln_b": nrm(ks[7], (Lc, D), f32) * 0.02,
        "conv_w_pw2": nrm(ks[8], (Lc, D, D), f32) * D ** -0.5 * DEEPNORM_BETA,
        "conv_b_pw2": nrm(ks[9], (Lc, D), f32) * 0.02,
        "ret_w_qkvg": nrm(ks[10], (Lr, D, 2 * RET_QK_DIM + 2 * RET_V_DIM), f32) * D ** -0.5,
        "ret_gn_g": 1.0 + 0.05 * nrm(ks[11], (Lr, RET_V_DIM), f32),
        "ret_gn_b": nrm(ks[12], (Lr, RET_V_DIM), f32) * 0.02,
        "ret_w_o": nrm(ks[13], (Lr, RET_V_DIM, D), f32) * RET_V_DIM ** -0.5 * DEEPNORM_BETA,
        "ln1_g": 1.0 + 0.05 * nrm(ks[14], (DEPTH, D), f32),
        "ln1_b": nrm(ks[15], (DEPTH, D), f32) * 0.02,
        "ln2_g": 1.0 + 0.05 * nrm(ks[16], (DEPTH, D), f32),
        "ln2_b": nrm(ks[17], (DEPTH, D), f32) * 0.02,
        "moe_w_grp": nrm(ks[18], (DEPTH, D, N_GROUPS), f32) * D ** -0.5,
        "moe_b_grp": nrm(ks[19], (DEPTH, N_GROUPS), f32) * 0.01,
        "moe_w_route": nrm(ks[20], (DEPTH, D, N_EXPERTS), f32) * D ** -0.5,
        "moe_b_route": nrm(ks[21], (DEPTH, N_EXPERTS), f32) * 0.01,
        "moe_w_gate": nrm(ks[22], (DEPTH, N_EXPERTS, D, F), f32) * D ** -0.5,
        "moe_w_up": nrm(ks[23], (DEPTH, N_EXPERTS, D, F), f32) * D ** -0.5,
        "moe_w_down": nrm(ks[24], (DEPTH, N_EXPERTS, F, D), f32) * F ** -0.5 * DEEPNORM_BETA,
    }


def reference(x, positions, conv_w_pw1, conv_b_pw1, conv_w_dw, conv_b_dw, conv_ln_g, conv_ln_b,
              conv_w_pw2, conv_b_pw2, ret_w_qkvg, ret_gn_g, ret_gn_b, ret_w_o,
              ln1_g, ln1_b, ln2_g, ln2_b, moe_w_grp, moe_b_grp, moe_w_route, moe_b_route,
              moe_w_gate, moe_w_up, moe_w_down):
    for i in range(DEPTH):
        j = i // N_MIXERS
        if i % N_MIXERS == 0:
            mix = conformer_conv(x, conv_w_pw1[j], conv_b_pw1[j], conv_w_dw[j], conv_b_dw[j],
                                 conv_ln_g[j], conv_ln_b[j], conv_w_pw2[j], conv_b_pw2[j])
        else:
            mix = retention(x, positions, ret_w_qkvg[j], ret_gn_g[j], ret_gn_b[j], ret_w_o[j])
        x = layer_norm(DEEPNORM_ALPHA * x + mix, ln1_g[i], ln1_b[i])
        ffn = hier_moe(x, moe_w_grp[i], moe_b_grp[i], moe_w_route[i], moe_b_route[i],
                       moe_w_gate[i], moe_w_up[i], moe_w_down[i])
        x = layer_norm(DEEPNORM_ALPHA * x + ffn, ln2_g[i], ln2_b[i])
    return x
```

```python
import contextlib
import numpy as np
import ml_dtypes
import concourse.bass as bass
import concourse.mybir as mybir
from concourse.bass_utils import run_bass_kernel_spmd

F32 = mybir.dt.float32
BF16 = mybir.dt.bfloat16
I32 = mybir.dt.int32
ALU = mybir.AluOpType
AF = mybir.ActivationFunctionType
AX = mybir.AxisListType

PE, ACT, DVE, POOL, SP = "pe", "act", "dve", "pool", "sp"
ENGS = (PE, ACT, DVE, POOL, SP)

D = 1024
SEQ = 4096
NT = SEQ // 128
DEPTH = 4
NE = 32
FF = 512
SLOT = 512
NSLOT_T = 2 * SEQ // SLOT + NE
ALPHA = (2.0 * DEPTH) ** 0.25
EPS = 1e-5
CW = 31
RH = 4


class _Op:
    __slots__ = ("eng", "fn", "deps", "dma", "sig", "dsem", "dval", "dprev", "pos", "need_sig")


class Prog:
    def __init__(self, nc, n_dma_sems=8, same_eng_dist=3):
        self.nc = nc
        self.ops = []
        self.last_writer = {}
        self.readers = {}
        self.eng_count = {e: 0 for e in ENGS}
        self.n_dma_sems = n_dma_sems
        self.same_eng_dist = same_eng_dist

    def add(self, eng, fn, reads=(), writes=(), dma=False):
        op = _Op()
        op.eng, op.fn, op.dma = eng, fn, dma
        op.sig = None
        op.need_sig = False
        op.pos = self.eng_count[eng]
        self.eng_count[eng] += 1
        idx = len(self.ops)
        deps = set()
        for k in reads:
            w = self.last_writer.get(k)
            if w is not None:
                deps.add((w, True))
        for k in writes:
            w = self.last_writer.get(k)
            if w is not None:
                deps.add((w, False))
            for r in self.readers.get(k, ()):
                if r != idx:
                    deps.add((r, False))
        for k in reads:
            self.readers.setdefault(k, []).append(idx)
        for k in writes:
            self.last_writer[k] = idx
            self.readers[k] = []
        real = {}
        for d, raw in deps:
            dop = self.ops[d]
            if dop.dma or dop.eng != eng or dma:
                real[d] = True
            elif raw and eng != PE and (op.pos - dop.pos) < self.same_eng_dist:
                real[d] = True
        op.deps = sorted(real)
        for d in op.deps:
            if not self.ops[d].dma:
                self.ops[d].need_sig = True
        self.ops.append(op)
        return idx

    def pe(self, fn, reads=(), writes=()):
        return self.add(PE, fn, reads, writes)

    def act(self, fn, reads=(), writes=()):
        return self.add(ACT, fn, reads, writes)

    def dve(self, fn, reads=(), writes=()):
        return self.add(DVE, fn, reads, writes)

    def pool(self, fn, reads=(), writes=()):
        return self.add(POOL, fn, reads, writes)

    def ve(self, eng, fn, reads=(), writes=()):
        return self.add(eng, fn, reads, writes)

    def dma(self, fn, reads=(), writes=(), q=SP):
        return self.add(q, fn, reads, writes, dma=True)

    def emit(self):
        nc = self.nc
        ops = self.ops
        pool = _sem_pool(nc, self.n_dma_sems)
        sigc = dict(pool["eval"])
        sig0 = dict(pool["eval"])
        dcount = dict(pool["dcount"])
        dsem_val = dict(pool["dval"])
        used_d = set()
        last_of = {}
        for i, op in enumerate(ops):
            last_of[op.eng] = i
        for e, i in last_of.items():
            if not ops[i].dma:
                ops[i].need_sig = True
        for op in ops:
            if op.dma:
                j = dcount[op.eng] % self.n_dma_sems
                dcount[op.eng] += 1
                key = (op.eng, j)
                prev = dsem_val.get(key, 0)
                op.dsem, op.dprev, op.dval = key, prev, prev + 16
                dsem_val[key] = op.dval
                used_d.add(key)
            elif op.need_sig:
                sigc[op.eng] += 1
                op.sig = sigc[op.eng]

        esem = pool["esem"]
        dsem = pool["dsem"]
        pool["eval"] = dict(sigc)
        pool["dcount"] = dict(dcount)
        pool["dval"] = dict(dsem_val)
        with contextlib.ExitStack() as st:
            block = st.enter_context(nc.Block())

            def run_engine(ename, eh):
                waited = {}

                def wait(sem_key, semh, val):
                    if waited.get(sem_key, 0) >= val:
                        return
                    eh.wait_ge(semh, val)
                    waited[sem_key] = val

                for op in ops:
                    if op.eng != ename:
                        continue
                    for d in op.deps:
                        dop = ops[d]
                        if dop.dma:
                            wait(dop.dsem, dsem[dop.dsem], dop.dval)
                        else:
                            wait(dop.eng, esem[dop.eng], dop.sig)
                    if op.dma:
                        if op.dprev > 0:
                            wait(op.dsem, dsem[op.dsem], op.dprev)
                        op.fn(eh).then_inc(dsem[op.dsem], 16)
                    else:
                        ins = op.fn(eh)
                        if op.sig is not None:
                            ins.then_inc(esem[ename], 1)
                for key in sorted(used_d):
                    wait(key, dsem[key], dsem_val[key])
                for e in ENGS:
                    if sigc[e] > sig0[e]:
                        wait(e, esem[e], sigc[e])

            @block.tensor
            def _(eh):
                run_engine(PE, eh)

            @block.scalar
            def _(eh):
                run_engine(ACT, eh)

            @block.vector
            def _(eh):
                run_engine(DVE, eh)

            @block.gpsimd
            def _(eh):
                run_engine(POOL, eh)

            @block.sync
            def _(eh):
                run_engine(SP, eh)


_SEM_POOLS = {}


def _sem_pool(nc, n_dma_sems):
    key = id(nc)
    if key not in _SEM_POOLS:
        pool = {"esem": {e: nc.alloc_semaphore(name="s_" + e) for e in ENGS}, "dsem": {}, "dval": {}}
        pool["eval"] = {e: 0 for e in ENGS}
        pool["dcount"] = {e: 0 for e in ENGS}
        for e in (SP, ACT, POOL):
            for j in range(n_dma_sems):
                pool["dsem"][(e, j)] = nc.alloc_semaphore(name="d_%s_%d" % (e, j))
        _SEM_POOLS[key] = pool
    return _SEM_POOLS[key]


class Phase:
    def __init__(self, nc, name):
        self.nc = nc
        self.name = name
        self.st = contextlib.ExitStack()
        self.P = Prog(nc)
        self.n = 0

    def sb(self, shape, dt, tag="t"):
        self.n += 1
        return self.st.enter_context(self.nc.sbuf_tensor("%s_%s%d" % (self.name, tag, self.n), list(shape), dt))

    def psum(self, tag="ps"):
        self.n += 1
        return self.st.enter_context(self.nc.psum_tensor("%s_%s%d" % (self.name, tag, self.n), [128, 512], F32))

    def finish(self):
        self.P.emit()
        self.st.close()


CF_IDENT = 0
CF_ONES = 128
CF_MT = 256
CF_THR = 768
CF_IOTA = 816
CF_INVF = 824
CF_ZETA = 825
CF_EPS = 829
CF_N = 832
CB_IDENT = 0
CB_ONES = 128
CB_TRIU = 256
CB_XI = 384
CB_N = 384 + 1024


def _host_consts():
    cf = np.zeros((128, CF_N), np.float32)
    cf[:, CF_IDENT:CF_IDENT + 128] = np.eye(128, dtype=np.float32)
    cf[:, CF_ONES:CF_ONES + 128] = 1.0
    gam = 1.0 - 2.0 ** (-5.0 - np.arange(RH, dtype=np.float64))
    i = np.arange(128)
    for h in range(RH):
        ci, cj = i[:, None] // 64, i[None, :] // 64
        dist = (i[:, None] - i[None, :]).astype(np.float64)
        M = np.where(ci == cj, gam[h] ** np.abs(dist), np.where(ci > cj, gam[h] ** dist, 0.0))
        cf[:, CF_MT + h * 128: CF_MT + (h + 1) * 128] = (M.T * (256.0 ** -0.5)).astype(np.float32)
        cf[:, CF_ZETA + h] = (gam[h] ** (127.0 - i) * (256.0 ** -0.5)).astype(np.float32)
    cf[:, CF_THR:CF_THR + NSLOT_T] = (np.arange(NSLOT_T) * SLOT).astype(np.float32)[None, :]
    cf[:, CF_IOTA] = i
    cf[:, CF_EPS] = EPS
    for c in range(4):
        cf[:, CF_IOTA + 1 + c] = c * 128 + i
    cf[:, CF_INVF] = (np.float32(10000.0) ** (-(np.arange(128, dtype=np.float32)) / np.float32(128))).astype(np.float32)
    cb = np.zeros((128, CB_N), np.float32)
    cb[:, CB_IDENT:CB_IDENT + 128] = np.eye(128)
    cb[:, CB_ONES:CB_ONES + 128] = 1.0
    cb[:, CB_TRIU:CB_TRIU + 128] = (i[:, None] < i[None, :]).astype(np.float32)
    for kc in range(8):
        h = kc // 2
        cb[:, CB_XI + kc * 128: CB_XI + (kc + 1) * 128] = (gam[h] ** (i + 1.0))[None, :]
    return cf, cb.astype(ml_dtypes.bfloat16), gam


GAMMA = 1.0 - 2.0 ** (-5.0 - np.arange(RH, dtype=np.float64))

PB_LN1G, PB_LN1B, PB_LN2G, PB_LN2B, PB_RB = 0, 1024, 2048, 3072, 4096
PB_N = 4160
PM_N = 4096
PP_B1, PP_WDW, PP_BDW, PP_LNG, PP_LNB = 0, 16, 16 + 248, 16 + 248 + 8, 16 + 248 + 16
PP_N = 16 + 248 + 24


class G:
    pass


EPS_AP = [None]


def _ln_token_major(P, eng2, r, x1, g_ap, b_ap, tmp_stats, tmp_mv, tmp_rs, key_r, key_out, tagk):
    P.dve(lambda e: e.bn_stats(out=tmp_stats[:, 0:6], in_=r[:, 0:512]), reads=[key_r], writes=[tagk + "st"])
    P.dve(lambda e: e.bn_stats(out=tmp_stats[:, 6:12], in_=r[:, 512:1024]), reads=[key_r], writes=[tagk + "st"])
    P.dve(lambda e: e.bn_aggr(out=tmp_mv[:, 0:2], in_=tmp_stats[:, 0:12]), reads=[tagk + "st"], writes=[tagk + "mv"])
    P.act(lambda e: e.activation(out=tmp_rs[:, 0:1], in_=tmp_mv[:, 1:2], func=AF.Sqrt, bias=EPS_AP[0], scale=1.0),
          reads=[tagk + "mv"], writes=[tagk + "rs"])
    P.dve(lambda e: e.reciprocal(out=tmp_rs[:, 0:1], in_=tmp_rs[:, 0:1]), reads=[tagk + "rs"], writes=[tagk + "rs"])
    P.dve(lambda e: e.tensor_scalar(out=r[:, :], in0=r[:, :], scalar1=tmp_mv[:, 0:1], scalar2=tmp_rs[:, 0:1],
                                    op0=ALU.subtract, op1=ALU.mult), reads=[key_r, tagk + "mv", tagk + "rs"], writes=[key_r])
    P.ve(eng2, lambda e: e.tensor_tensor(out=r[:, :], in0=r[:, :], in1=g_ap, op=ALU.mult), reads=[key_r, "PB"], writes=[key_r])
    P.ve(eng2, lambda e: e.tensor_tensor(out=x1[:, :], in0=r[:, :], in1=b_ap, op=ALU.add), reads=[key_r, "PB"], writes=[key_out])


def _router_tile(ph, g, ti, x1, key_x1, W, psR, psT2):
    P = ph.P
    cf = g.cf
    for half in range(2):
        for q in range(4):
            kc = half * 4 + q
            P.pe(lambda e, kc=kc, q=q, half=half: e.transpose(out=psT2[half][:, q * 128:(q + 1) * 128],
                                                              in_=x1[:, kc * 128:(kc + 1) * 128],
                                                              identity=cf[:, CF_IDENT:CF_IDENT + 128]),
                 reads=[key_x1], writes=["psT2_%d" % half])
        P.act(lambda e, half=half: e.copy(out=W.x1T[:, half * 4:(half + 1) * 4, :],
                                          in_=psT2[half][:, :].rearrange("p (k t) -> p k t", k=4)),
              reads=["psT2_%d" % half], writes=["x1T"])
    for kc in range(8):
        P.pe(lambda e, kc=kc: e.matmul(psR[:, 0:36], lhsT=W.x1T[:, kc, :], rhs=g.wr[:, kc, :],
                                       start=(kc == 0), stop=(kc == 7)), reads=["x1T", "WR"], writes=["psR"])
    L = W.L
    P.dve(lambda e: e.tensor_tensor(out=L[:, 0:36], in0=psR[:, 0:36], in1=g.pb[:, PB_RB:PB_RB + 36], op=ALU.add),
          reads=["psR", "PB"], writes=["L"])
    sm = W.sm
    P.dve(lambda e: e.tensor_reduce(out=sm[:, 0:1], in_=L[:, 0:4], axis=AX.X, op=ALU.max), reads=["L"], writes=["sm0"])
    P.dve(lambda e: e.tensor_scalar(out=sm[:, 1:2], in0=sm[:, 0:1], scalar1=-1.0, scalar2=None, op0=ALU.mult),
          reads=["sm0"], writes=["sm1"])
    P.act(lambda e: e.activation(out=W.ge[:, 0:4], in_=L[:, 0:4], func=AF.Exp, bias=sm[:, 1:2], scale=1.0,
                                 accum_out=sm[:, 2:3]), reads=["L", "sm1"], writes=["ge", "sm2"])
    P.dve(lambda e: e.reciprocal(out=sm[:, 3:4], in_=sm[:, 2:3]), reads=["sm2"], writes=["sm3"])
    P.dve(lambda e: e.tensor_scalar(out=W.m4[:, 0:4], in0=L[:, 0:4], scalar1=sm[:, 0:1], scalar2=None, op0=ALU.is_equal),
          reads=["L", "sm0"], writes=["m4"])
    P.dve(lambda e: e.tensor_scalar(out=W.m4[:, 0:4], in0=W.m4[:, 0:4], scalar1=-1.0, scalar2=1e30, op0=ALU.add, op1=ALU.mult),
          reads=["m4"], writes=["m4"])
    P.dve(lambda e: e.tensor_tensor(out=W.ml[:, :, :], in0=L[:, 4:36].rearrange("p (g j) -> p g j", g=4),
                                    in1=W.m4[:, 0:4].unsqueeze(2).to_broadcast([128, 4, 8]), op=ALU.add),
          reads=["L", "m4"], writes=["ml"])
    mlf = W.ml[:, :, :].rearrange("p g j -> p (g j)")
    P.dve(lambda e: e.max(out=W.t8[:, 0:8], in_=mlf), reads=["ml"], writes=["t8"])
    P.dve(lambda e: e.tensor_scalar(out=g.RE1[:, ti, :], in0=mlf, scalar1=W.t8[:, 0:1], scalar2=None, op0=ALU.is_equal),
          reads=["ml", "t8"], writes=["RE1"])
    P.dve(lambda e: e.tensor_scalar(out=g.RE2[:, ti, :], in0=mlf, scalar1=W.t8[:, 1:2], scalar2=None, op0=ALU.is_equal),
          reads=["ml", "t8"], writes=["RE2"])
    P.dve(lambda e: e.tensor_tensor(out=sm[:, 4:5], in0=W.t8[:, 1:2], in1=W.t8[:, 0:1], op=ALU.subtract),
          reads=["t8"], writes=["sm4"])
    P.act(lambda e: e.activation(out=sm[:, 5:6], in_=sm[:, 4:5], func=AF.Exp), reads=["sm4"], writes=["sm5"])
    P.dve(lambda e: e.tensor_scalar(out=sm[:, 6:7], in0=sm[:, 5:6], scalar1=1.0, scalar2=None, op0=ALU.add),
          reads=["sm5"], writes=["sm6"])
    P.dve(lambda e: e.reciprocal(out=sm[:, 7:8], in_=sm[:, 6:7]), reads=["sm6"], writes=["sm7"])
    P.dve(lambda e: e.tensor_tensor(out=g.RG[:, ti, 0:1], in0=sm[:, 7:8], in1=sm[:, 3:4], op=ALU.mult),
          reads=["sm7", "sm3"], writes=["RG"])
    P.dve(lambda e: e.tensor_tensor(out=g.RG[:, ti, 1:2], in0=g.RG[:, ti, 0:1], in1=sm[:, 5:6], op=ALU.mult),
          reads=["RG", "sm5"], writes=["RG"])
    P.dve(lambda e: e.tensor_tensor(out=W.Mb[:, 0:32], in0=g.RE1[:, ti, :], in1=g.RE2[:, ti, :], op=ALU.add),
          reads=["RE1", "RE2"], writes=["Mb"])
    cb = g.cb
    P.pe(lambda e: e.matmul(psR[:, 64:96], lhsT=cb[:, CB_TRIU:CB_TRIU + 128], rhs=W.Mb[:, 0:32], start=True, stop=True),
         reads=["Mb"], writes=["psR"])
    P.pe(lambda e: e.matmul(psR[:, 128:160], lhsT=cb[:, CB_ONES:CB_ONES + 128], rhs=W.Mb[:, 0:32], start=True, stop=True),
         reads=["Mb"], writes=["psR"])
    P.dve(lambda e: e.tensor_tensor(out=g.RPF[:, ti, :], in0=psR[:, 64:96], in1=g.CAR[:, 0:32], op=ALU.add),
          reads=["psR", "CAR"], writes=["RPF"])
    P.dve(lambda e: e.tensor_tensor(out=g.CAR[:, 0:32], in0=psR[:, 128:160], in1=g.CAR[:, 0:32], op=ALU.add),
          reads=["psR", "CAR"], writes=["CAR"])


class _RW:
    pass


def _router_work(ph):
    W = _RW()
    W.x1T = ph.sb([128, 8, 128], F32, "x1T")
    W.L = ph.sb([128, 36], F32, "L")
    W.sm = ph.sb([128, 16], F32, "sm")
    W.ge = ph.sb([128, 4], F32, "ge")
    W.m4 = ph.sb([128, 4], F32, "m4")
    W.ml = ph.sb([128, 4, 8], F32, "ml")
    W.t8 = ph.sb([128, 8], F32, "t8")
    W.Mb = ph.sb([128, 32], BF16, "Mb")
    return W


def _post_mixer(ph, g, ti, r, key_r, W, bufs, psR, psT2, xdst):
    P = ph.P
    x1 = bufs.x1[ti % 2]
    kx1 = "x1_%d" % (ti % 2)
    _ln_token_major(P, POOL, r, x1, g.pb[:, PB_LN1G:PB_LN1G + 1024], g.pb[:, PB_LN1B:PB_LN1B + 1024],
                    bufs.st, bufs.mv, bufs.rs, key_r, kx1, "ln1")
    P.dma(lambda e: e.dma_start(out=xdst[ti * 128:(ti + 1) * 128, :], in_=x1[:, :]), reads=[kx1], writes=["XB_%d" % ti])
    xb = bufs.x1bf[ti % 2]
    kxb = "x1bf_%d" % (ti % 2)
    P.act(lambda e: e.copy(out=xb[:, :].rearrange("t (kk p) -> t kk p", p=128),
                           in_=x1[:, :].rearrange("t (p kk) -> t kk p", kk=8)), reads=[kx1], writes=[kxb])
    P.dma(lambda e: e.dma_start(out=g.xb16[ti * 128:(ti + 1) * 128, :], in_=xb[:, :]), reads=[kxb], writes=["xb16_%d" % ti])
    _router_tile(ph, g, ti, x1, kx1, W, psR, psT2)


def _load_layer_params(ph, g, li):
    P = ph.P
    P.dma(lambda e: e.dma_start(out=g.pb[:, :], in_=g.d_pbln[li]), writes=["PB"])
    P.dma(lambda e: e.dma_start(out=g.wr[:, :, :], in_=g.d_wr[li]), writes=["WR"])
    P.dve(lambda e: e.memset(g.CAR[:, :], 0.0), writes=["CAR"])


def conv_phase(nc, g, li, xsrc, xdst):
    j = li // 2
    ph = Phase(nc, "cv%d" % li)
    P = ph.P
    cf, cb = g.cf, g.cb
    _load_layer_params(ph, g, li)
    W1b = ph.sb([128, 8, 2048], BF16, "W1")
    W2b = ph.sb([128, 8, 1024], BF16, "W2")
    pm = ph.sb([128, 1024], F32, "pm")
    pp = ph.sb([128, PP_N], F32, "pp")
    for q in range(4):
        P.dma(lambda e, q=q: e.dma_start(out=W1b[:, :, q * 512:(q + 1) * 512],
                                         in_=g.d_conv_w_pw1[j].rearrange("(k p) f -> p k f", p=128)[:, :, q * 512:(q + 1) * 512]),
              writes=["W1_%d" % q], q=POOL)
    P.dma(lambda e: e.dma_start(out=W2b[:, :, :], in_=g.d_conv_w_pw2[j].rearrange("(k p) f -> p k f", p=128)),
          writes=["W2"], q=POOL)
    P.dma(lambda e: e.dma_start(out=pm[:, :], in_=g.d_pbmix[li][:, 0:1024]), writes=["PM"])
    P.dma(lambda e: e.dma_start(out=pp[:, :], in_=g.d_pp[j]), writes=["PP"])

    xin = [ph.sb([128, 1024], F32, "xin") for _ in range(2)]
    xbf = ph.sb([128, 1024], BF16, "xbf")
    xT = ph.sb([128, 8, 512], BF16, "xT")
    hg = ph.sb([128, 8, 512 + CW - 1], F32, "hg")
    sig = [ph.sb([128, 512], F32, "sig") for _ in range(2)]
    cv = ph.sb([128, 8, 512], F32, "cv")
    sq = [ph.sb([128, 512], F32, "sq") for _ in range(2)]
    mean = ph.sb([128, 512], F32, "mean")
    rstd = ph.sb([128, 512], F32, "rstd")
    nmr = ph.sb([128, 512], F32, "nmr")
    hT = ph.sb([128, 8, 512], BF16, "hT")
    rbuf = [ph.sb([128, 1024], F32, "r") for _ in range(2)]
    bufs = _RW()
    bufs.x1 = [ph.sb([128, 1024], F32, "x1") for _ in range(2)]
    bufs.x1bf = [ph.sb([128, 1024], BF16, "x1bf") for _ in range(2)]
    bufs.st = ph.sb([128, 12], F32, "st")
    bufs.mv = ph.sb([128, 2], F32, "mv")
    bufs.rs = ph.sb([128, 1], F32, "rs")
    RWk = _router_work(ph)
    psT = ph.psum("psT")
    psA = [ph.psum("psA") for _ in range(2)]
    psG = [ph.psum("psG") for _ in range(2)]
    psR = ph.psum("psR")
    psT2 = [ph.psum("psT2") for _ in range(2)]
    psS = [psG[0], psG[1]]
    psT_bf = psT[:, :].bitcast(BF16)

    P.dve(lambda e: e.memset(hg[:, :, 0:CW - 1], 0.0), writes=["hg"])
    conv_eng = [DVE] * 8
    norm_eng = [DVE, POOL, DVE, POOL, DVE, POOL, DVE, POOL]
    for st_i in range(NT // 4):
        for sub in range(4):
            ti = st_i * 4 + sub
            xi = xin[ti % 2]
            kxi = "xin_%d" % (ti % 2)
            P.dma(lambda e, xi=xi, ti=ti: e.dma_start(out=xi[:, :], in_=xsrc[ti * 128:(ti + 1) * 128, :]), writes=[kxi])
            P.act(lambda e, xi=xi: e.copy(out=xbf[:, :], in_=xi[:, :]), reads=[kxi], writes=["xbf"])
            for kc in range(8):
                P.pe(lambda e, kc=kc: e.transpose(out=psT_bf[:, kc * 128:(kc + 1) * 128], in_=xbf[:, kc * 128:(kc + 1) * 128],
                                                  identity=cb[:, CB_IDENT:CB_IDENT + 128]), reads=["xbf"], writes=["psT"])
            P.dve(lambda e, sub=sub: e.tensor_copy(out=xT[:, :, sub * 128:(sub + 1) * 128],
                                                   in_=psT_bf.rearrange("p (k t) -> p k t", k=8)), reads=["psT"], writes=["xT"])
        for c in range(8):
            pa, pg = psA[c % 2], psG[c % 2]
            ka, kg = "psA_%d" % (c % 2), "psG_%d" % (c % 2)
            for kc in range(8):
                P.pe(lambda e, kc=kc, c=c, pa=pa: e.matmul(pa[:, :], lhsT=W1b[:, kc, c * 128:(c + 1) * 128], rhs=xT[:, kc, :],
                                                           start=(kc == 0), stop=(kc == 7)),
                     reads=["xT", "W1_%d" % (c // 4)], writes=[ka])
            for kc in range(8):
                P.pe(lambda e, kc=kc, c=c, pg=pg: e.matmul(pg[:, :], lhsT=W1b[:, kc, 1024 + c * 128:1024 + (c + 1) * 128], rhs=xT[:, kc, :],
                                                           start=(kc == 0), stop=(kc == 7)),
                     reads=["xT", "W1_%d" % (2 + c // 4)], writes=[kg])
            sg = sig[c % 2]
            ks = "sig_%d" % (c % 2)
            P.act(lambda e, c=c, pg=pg, sg=sg: e.activation(out=sg[:, :], in_=pg[:, :], func=AF.Sigmoid,
                                                            bias=pp[:, PP_B1 + 8 + c:PP_B1 + 9 + c], scale=1.0),
                  reads=[kg, "PP"], writes=[ks])
            P.dve(lambda e, c=c, pa=pa, sg=sg: e.scalar_tensor_tensor(out=hg[:, c, CW - 1:CW - 1 + 512], in0=pa[:, :],
                                                                      scalar=pp[:, PP_B1 + c:PP_B1 + c + 1], in1=sg[:, :],
                                                                      op0=ALU.add, op1=ALU.mult),
                  reads=[ka, ks, "PP"], writes=["hg"])
        for jj in range(CW):
            for c in range(8):
                eng = conv_eng[c]
                if jj == 0:
                    P.ve(eng, lambda e, c=c: e.tensor_scalar(out=cv[:, c, :], in0=hg[:, c, 0:512],
                                                             scalar1=pp[:, PP_WDW + c * CW:PP_WDW + c * CW + 1],
                                                             scalar2=pp[:, PP_BDW + c:PP_BDW + c + 1], op0=ALU.mult, op1=ALU.add),
                         reads=["hg", "PP"], writes=["cv_%d" % c])
                else:
                    P.ve(eng, lambda e, c=c, jj=jj: e.scalar_tensor_tensor(out=cv[:, c, :], in0=hg[:, c, jj:jj + 512],
                                                                           scalar=pp[:, PP_WDW + c * CW + jj:PP_WDW + c * CW + jj + 1],
                                                                           in1=cv[:, c, :], op0=ALU.mult, op1=ALU.add),
                         reads=["hg", "PP", "cv_%d" % c], writes=["cv_%d" % c])
        P.act(lambda e: e.copy(out=hg[:, :, 0:CW - 1], in_=hg[:, :, 512:512 + CW - 1]), reads=["hg"], writes=["hg"])
        for c in range(8):
            s_ = sq[c % 2]
            ksq = "sq_%d" % (c % 2)
            P.act(lambda e, c=c, s_=s_: e.activation(out=s_[:, :], in_=cv[:, c, :], func=AF.Square), reads=["cv_%d" % c], writes=[ksq])
            P.pe(lambda e, c=c: e.matmul(psS[0][:, :], lhsT=cf[:, CF_ONES:CF_ONES + 128], rhs=cv[:, c, :], start=(c == 0), stop=(c == 7)),
                 reads=["cv_%d" % c], writes=["psG_0"])
            P.pe(lambda e, c=c, s_=s_: e.matmul(psS[1][:, :], lhsT=cf[:, CF_ONES:CF_ONES + 128], rhs=s_[:, :], start=(c == 0), stop=(c == 7)),
                 reads=[ksq], writes=["psG_1"])
        P.act(lambda e: e.mul(out=mean[:, :], in_=psS[0][:, :], mul=1.0 / D), reads=["psG_0"], writes=["mean"])
        P.dve(lambda e: e.tensor_tensor(out=nmr[:, :], in0=mean[:, :], in1=mean[:, :], op=ALU.mult), reads=["mean"], writes=["nmr"])
        P.dve(lambda e: e.scalar_tensor_tensor(out=rstd[:, :], in0=psS[1][:, :], scalar=1.0 / D, in1=nmr[:, :],
                                               op0=ALU.mult, op1=ALU.subtract), reads=["psG_1", "nmr"], writes=["rstd"])
        P.act(lambda e: e.activation(out=rstd[:, :], in_=rstd[:, :], func=AF.Sqrt, bias=EPS_AP[0], scale=1.0),
              reads=["rstd"], writes=["rstd"])
        P.dve(lambda e: e.reciprocal(out=rstd[:, :], in_=rstd[:, :]), reads=["rstd"], writes=["rstd"])
        P.dve(lambda e: e.scalar_tensor_tensor(out=nmr[:, :], in0=mean[:, :], scalar=-1.0, in1=rstd[:, :],
                                               op0=ALU.mult, op1=ALU.mult), reads=["mean", "rstd"], writes=["nmr"])
        for c in range(8):
            eng = norm_eng[c]
            P.ve(eng, lambda e, c=c: e.tensor_tensor(out=cv[:, c, :], in0=cv[:, c, :], in1=rstd[:, :], op=ALU.mult),
                 reads=["cv_%d" % c, "rstd"], writes=["cv_%d" % c])
        for c in range(8):
            eng = norm_eng[c]
            P.ve(eng, lambda e, c=c: e.tensor_tensor(out=cv[:, c, :], in0=cv[:, c, :], in1=nmr[:, :], op=ALU.add),
                 reads=["cv_%d" % c, "nmr"], writes=["cv_%d" % c])
        for c in range(8):
            P.act(lambda e, c=c: e.activation(out=hT[:, c, :], in_=cv[:, c, :], func=AF.Silu,
                                              bias=pp[:, PP_LNB + c:PP_LNB + c + 1], scale=pp[:, PP_LNG + c:PP_LNG + c + 1]),
                  reads=["cv_%d" % c, "PP"], writes=["hT"])
        for sub in range(4):
            ti = st_i * 4 + sub
            xi = xin[ti % 2]
            kxi = "xin_%d" % (ti % 2)
            P.dma(lambda e, xi=xi, ti=ti: e.dma_start(out=xi[:, :], in_=xsrc[ti * 128:(ti + 1) * 128, :]), writes=[kxi])
            r = rbuf[ti % 2]
            kr = "r_%d" % (ti % 2)
            for half in range(2):
                pa = psA[half]
                ka = "psA_%d" % half
                for c in range(8):
                    P.pe(lambda e, c=c, half=half, sub=sub, pa=pa: e.matmul(pa[:, :], lhsT=hT[:, c, sub * 128:(sub + 1) * 128],
                                                                            rhs=W2b[:, c, half * 512:(half + 1) * 512],
                                                                            start=(c == 0), stop=(c == 7)),
                         reads=["hT", "W2"], writes=[ka])
                P.dve(lambda e, half=half, pa=pa, xi=xi, r=r: e.scalar_tensor_tensor(out=r[:, half * 512:(half + 1) * 512],
                                                                                     in0=xi[:, half * 512:(half + 1) * 512], scalar=ALPHA,
                                                                                     in1=pa[:, :], op0=ALU.mult, op1=ALU.add),
                      reads=[ka, kxi], writes=[kr])
            P.pool(lambda e, r=r: e.tensor_tensor(out=r[:, :], in0=r[:, :], in1=pm[:, 0:1024], op=ALU.add), reads=[kr, "PM"], writes=[kr])
            _post_mixer(ph, g, ti, r, kr, RWk, bufs, psR, psT2, xdst)
    ph.finish()


def offsets_scatter_phase(nc, g, li):
    ph = Phase(nc, "os%d" % li)
    P = ph.P
    cf = g.cf
    cnt = g.CAR
    r_ = ph.sb([128, 32], F32, "r")
    nz = ph.sb([128, 32], F32, "nz")
    pad = ph.sb([128, 32], F32, "pad")
    ca = ph.sb([128, 32], F32, "ca")
    cbuf = ph.sb([128, 32], F32, "cb")
    off = ph.sb([128, 32], F32, "off")
    big = ph.sb([128, NT, 32], F32, "big")
    g.tmpbig = ph.sb([128, NT, 32], F32, "tmpbig")
    posf = ph.sb([128, NT, 2], F32, "posf")
    ej = ph.sb([128, NSLOT_T], F32, "ej")
    tmpw = ph.sb([128, NSLOT_T], F32, "tmpw")
    P.dve(lambda e: e.memset(nz[:, :], 0.0), writes=["nz"])
    for m in range(2 * SEQ // SLOT):
        P.dve(lambda e, m=m: e.scalar_tensor_tensor(out=nz[:, :], in0=cnt[:, 0:32], scalar=float(m * SLOT), in1=nz[:, :],
                                                    op0=ALU.is_gt, op1=ALU.add), reads=["CAR", "nz"], writes=["nz"])
    P.dve(lambda e: e.tensor_scalar(out=pad[:, :], in0=nz[:, :], scalar1=float(SLOT), scalar2=None, op0=ALU.mult), reads=["nz"], writes=["pad"])
    src, dst = pad, ca
    ksrc, kdst = "pad", "ca"
    step = 1
    while step < 32:
        P.dve(lambda e, src=src, dst=dst, step=step: e.tensor_copy(out=dst[:, 0:step], in_=src[:, 0:step]), reads=[ksrc], writes=[kdst])
        P.dve(lambda e, src=src, dst=dst, step=step: e.tensor_tensor(out=dst[:, step:32], in0=src[:, step:32], in1=src[:, 0:32 - step], op=ALU.add),
              reads=[ksrc], writes=[kdst])
        if dst is ca:
            src, dst, ksrc, kdst = ca, cbuf, "ca", "cb"
        else:
            src, dst, ksrc, kdst = cbuf, ca, "cb", "ca"
        step *= 2
    cum, kcum = src, ksrc
    P.dve(lambda e: e.tensor_tensor(out=off[:, :], in0=cum[:, :], in1=pad[:, :], op=ALU.subtract), reads=[kcum, "pad"], writes=["off"])
    P.dve(lambda e: e.tensor_tensor(out=big[:, :, :], in0=g.RPF[:, :, :], in1=off[:, :].unsqueeze(1).to_broadcast([128, NT, 32]), op=ALU.add),
          reads=["RPF", "off"], writes=["big"])
    for k, RE, kre in ((0, g.RE1, "RE1"), (1, g.RE2, "RE2")):
        P.dve(lambda e, RE=RE: e.tensor_tensor(out=g.tmpbig[:, :, :], in0=big[:, :, :], in1=RE[:, :, :], op=ALU.mult),
              reads=["big", kre], writes=["tmpbig"])
        P.dve(lambda e, k=k: e.tensor_reduce(out=posf[:, :, k], in_=g.tmpbig[:, :, :], axis=AX.X, op=ALU.add),
              reads=["tmpbig"], writes=["posf"])
    P.dve(lambda e: e.tensor_copy(out=g.POS[:, :, :], in_=posf[:, :, :]), reads=["posf"], writes=["POS"])
    P.dve(lambda e: e.memset(ej[:, :], 0.0), writes=["ej"])
    for ex in range(NE):
        P.dve(lambda e, ex=ex: e.scalar_tensor_tensor(out=ej[:, :], in0=cf[:, CF_THR:CF_THR + NSLOT_T], scalar=cum[:, ex:ex + 1], in1=ej[:, :],
                                                      op0=ALU.is_ge, op1=ALU.add), reads=[kcum, "ej"], writes=["ej"])
    P.dve(lambda e: e.tensor_scalar(out=ej[:, :], in0=ej[:, :], scalar1=float(NE - 1), scalar2=None, op0=ALU.min), reads=["ej"], writes=["ej"])
    P.dve(lambda e: e.tensor_scalar(out=tmpw[:, :], in0=ej[:, :], scalar1=128.0, scalar2=cf[:, CF_IOTA:CF_IOTA + 1], op0=ALU.mult, op1=ALU.add),
          reads=["ej"], writes=["tmpw"])
    P.dve(lambda e: e.tensor_scalar(out=tmpw[:, :], in0=tmpw[:, :], scalar1=float(li * NE * 128), scalar2=None, op0=ALU.add),
          reads=["tmpw"], writes=["tmpw"])
    P.dve(lambda e: e.tensor_copy(out=g.WG[:, :], in_=tmpw[:, :]), reads=["tmpw"], writes=["WG"])
    for c in range(4):
        P.dve(lambda e, c=c: e.tensor_scalar(out=tmpw[:, :], in0=ej[:, :], scalar1=512.0, scalar2=cf[:, CF_IOTA + 1 + c:CF_IOTA + 2 + c],
                                             op0=ALU.mult, op1=ALU.add), reads=["ej"], writes=["tmpw"])
        P.dve(lambda e: e.tensor_scalar(out=tmpw[:, :], in0=tmpw[:, :], scalar1=float(li * NE * FF), scalar2=None, op0=ALU.add),
              reads=["tmpw"], writes=["tmpw"])
        P.dve(lambda e, c=c: e.tensor_copy(out=g.WD[:, c, :], in_=tmpw[:, :]), reads=["tmpw"], writes=["WD"])
    xbt = [ph.sb([128, 1024], BF16, "xbt") for _ in range(4)]
    for ti in range(NT):
        xb = xbt[ti % 4]
        kx = "xbt_%d" % (ti % 4)
        P.dma(lambda e, xb=xb, ti=ti: e.dma_start(out=xb[:, :], in_=g.xb16[ti * 128:(ti + 1) * 128, :]), writes=[kx])
        for k in range(2):
            P.dma(lambda e, xb=xb, ti=ti, k=k: e.indirect_dma_start(out=g.xs, out_offset=bass.IndirectOffsetOnAxis(ap=g.POS[:, ti, k:k + 1], axis=0),
                                                                    in_=xb[:, :], in_offset=None),
                  reads=[kx, "POS"], writes=["xs_%d_%d" % (ti, k)], q=POOL)
    ph.finish()


def expert_phase(nc, g, li, use_dma_transpose=True):
    ph = Phase(nc, "ex%d" % li)
    P = ph.P
    cb = g.cb
    NB = 2
    Wg = [ph.sb([128, 8, FF], BF16, "Wg") for _ in range(NB)]
    Wu = [ph.sb([128, 8, FF], BF16, "Wu") for _ in range(NB)]
    Wd = [ph.sb([128, 4, D], BF16, "Wd") for _ in range(NB)]
    xTp = [ph.sb([128, 8, SLOT], BF16, "xTp") for _ in range(NB)]
    xrow = [ph.sb([128, D], BF16, "xrow") for _ in range(2)]
    sgt = [ph.sb([128, SLOT], F32, "sg") for _ in range(2)]
    hT = [ph.sb([128, 4, SLOT], BF16, "hT") for _ in range(2)]
    ysb = [ph.sb([128, D], F32, "ysb") for _ in range(2)]
    psg = [ph.psum("psg") for _ in range(2)]
    psu = [ph.psum("psu") for _ in range(2)]
    psy = [ph.psum("psy") for _ in range(2)]
    pst = [ph.psum("pst") for _ in range(2)]
    wgv = g.d_moe_w_gate.rearrange("l e (p kk) f -> (l e p) (kk f)", kk=8)
    wuv = g.d_moe_w_up.rearrange("l e (p kk) f -> (l e p) (kk f)", kk=8)
    wdv = g.d_moe_w_down.rearrange("l e k d -> (l e k) d")
    for jt in range(NSLOT_T):
        b = jt % NB
        P.dma(lambda e, b=b, jt=jt: e.indirect_dma_start(out=Wg[b][:, :, :].rearrange("p k f -> p (k f)"), out_offset=None, in_=wgv,
                                                         in_offset=bass.IndirectOffsetOnAxis(ap=g.WG[:, jt:jt + 1], axis=0)),
              reads=["WG"], writes=["Wg_%d" % b], q=POOL)
        P.dma(lambda e, b=b, jt=jt: e.indirect_dma_start(out=Wu[b][:, :, :].rearrange("p k f -> p (k f)"), out_offset=None, in_=wuv,
                                                         in_offset=bass.IndirectOffsetOnAxis(ap=g.WG[:, jt:jt + 1], axis=0)),
              reads=["WG"], writes=["Wu_%d" % b], q=POOL)
        for c in range(4):
            P.dma(lambda e, b=b, jt=jt, c=c: e.indirect_dma_start(out=Wd[b][:, c, :], out_offset=None, in_=wdv,
                                                                  in_offset=bass.IndirectOffsetOnAxis(ap=g.WD[:, c, jt:jt + 1], axis=0)),
                  reads=["WD"], writes=["Wd_%d_%d" % (b, c)], q=POOL)
        if use_dma_transpose:
            for kk in range(8):
                P.dma(lambda e, b=b, jt=jt, kk=kk: e.dma_start_transpose(out=xTp[b][:, kk, :],
                                                                         in_=g.xs[jt * SLOT:(jt + 1) * SLOT, kk * 128:(kk + 1) * 128]),
                      reads=["xs"], writes=["xTp_%d" % b])
        else:
            for sub in range(4):
                xr = xrow[sub % 2]
                kxr = "xrow_%d" % (sub % 2)
                P.dma(lambda e, xr=xr, jt=jt, sub=sub: e.dma_start(out=xr[:, :], in_=g.xs[jt * SLOT + sub * 128: jt * SLOT + (sub + 1) * 128, :]),
                      reads=["xs"], writes=[kxr])
                pt = pst[sub % 2]
                kpt = "pst_%d" % (sub % 2)
                ptb = pt[:, :].bitcast(BF16)
                for kk in range(8):
                    P.pe(lambda e, xr=xr, kk=kk, ptb=ptb: e.transpose(out=ptb[:, kk * 128:(kk + 1) * 128], in_=xr[:, kk * 128:(kk + 1) * 128],
                                                                      identity=cb[:, CB_IDENT:CB_IDENT + 128]), reads=[kxr], writes=[kpt])
                P.ve(ACT if sub % 2 == 0 else DVE,
                     (lambda e, b=b, sub=sub, ptb=ptb: e.copy(out=xTp[b][:, :, sub * 128:(sub + 1) * 128], in_=ptb.rearrange("p (k t) -> p k t", k=8)))
                     if sub % 2 == 0 else
                     (lambda e, b=b, sub=sub, ptb=ptb: e.tensor_copy(out=xTp[b][:, :, sub * 128:(sub + 1) * 128], in_=ptb.rearrange("p (k t) -> p k t", k=8))),
                     reads=[kpt], writes=["xTp_%d" % b])
        h = hT[jt % 2]
        kh = "hT_%d" % (jt % 2)
        for fc in range(4):
            pg_, pu_ = psg[fc % 2], psu[fc % 2]
            kg, ku = "psg_%d" % (fc % 2), "psu_%d" % (fc % 2)
            for kk in range(8):
                P.pe(lambda e, b=b, kk=kk, fc=fc, pg_=pg_: e.matmul(pg_[:, :], lhsT=Wg[b][:, kk, fc * 128:(fc + 1) * 128], rhs=xTp[b][:, kk, :],
                                                                    start=(kk == 0), stop=(kk == 7)),
                     reads=["Wg_%d" % b, "xTp_%d" % b], writes=[kg])
            for kk in range(8):
                P.pe(lambda e, b=b, kk=kk, fc=fc, pu_=pu_: e.matmul(pu_[:, :], lhsT=Wu[b][:, kk, fc * 128:(fc + 1) * 128], rhs=xTp[b][:, kk, :],
                                                                    start=(kk == 0), stop=(kk == 7)),
                     reads=["Wu_%d" % b, "xTp_%d" % b], writes=[ku])
            sg = sgt[fc % 2]
            ksg = "sgt_%d" % (fc % 2)
            P.act(lambda e, pg_=pg_, sg=sg: e.activation(out=sg[:, :], in_=pg_[:, :], func=AF.Silu), reads=[kg], writes=[ksg])
            P.dve(lambda e, pu_=pu_, sg=sg, h=h, fc=fc: e.tensor_tensor(out=h[:, fc, :], in0=pu_[:, :], in1=sg[:, :], op=ALU.mult),
                  reads=[ku, ksg], writes=[kh])
        for sub in range(4):
            yb = ysb[sub % 2]
            kyb = "ysb_%d" % (sub % 2)
            for half in range(2):
                py = psy[half]
                kpy = "psy_%d" % half
                for fc in range(4):
                    P.pe(lambda e, b=b, fc=fc, sub=sub, half=half, py=py, h=h: e.matmul(py[:, :], lhsT=h[:, fc, sub * 128:(sub + 1) * 128],
                                                                                      rhs=Wd[b][:, fc, half * 512:(half + 1) * 512],
                                                                                      start=(fc == 0), stop=(fc == 3)),
                         reads=[kh] + ["Wd_%d_%d" % (b, c) for c in range(4)], writes=[kpy])
                if half == 0:
                    P.act(lambda e, py=py, yb=yb: e.copy(out=yb[:, 0:512], in_=py[:, :]), reads=[kpy], writes=[kyb + "a"])
                else:
                    P.dve(lambda e, py=py, yb=yb: e.tensor_copy(out=yb[:, 512:1024], in_=py[:, :]), reads=[kpy], writes=[kyb + "b"])
            P.dma(lambda e, yb=yb, jt=jt, sub=sub: e.dma_start(out=g.ys[jt * SLOT + sub * 128: jt * SLOT + (sub + 1) * 128, :], in_=yb[:, :]),
                  reads=[kyb + "a", kyb + "b"], writes=["ys_%d_%d" % (jt, sub)])
    ph.finish()


def combine_phase(nc, g, li, x1src, xdst):
    ph = Phase(nc, "cm%d" % li)
    P = ph.P
    NB = 2
    y1 = [ph.sb([128, D], F32, "y1") for _ in range(NB)]
    y2 = [ph.sb([128, D], F32, "y2") for _ in range(NB)]
    x1 = [ph.sb([128, D], F32, "x1") for _ in range(NB)]
    x2 = [ph.sb([128, D], F32, "x2") for _ in range(NB)]
    st = ph.sb([128, 12], F32, "st")
    mv = ph.sb([128, 2], F32, "mv")
    rs = ph.sb([128, 1], F32, "rs")
    for ti in range(NT):
        b = ti % NB
        P.dma(lambda e, b=b, ti=ti: e.dma_start(out=x1[b][:, :], in_=x1src[ti * 128:(ti + 1) * 128, :]), reads=["XB"], writes=["x1_%d" % b])
        P.dma(lambda e, b=b, ti=ti: e.indirect_dma_start(out=y1[b][:, :], out_offset=None, in_=g.ys,
                                                         in_offset=bass.IndirectOffsetOnAxis(ap=g.POS[:, ti, 0:1], axis=0)),
              reads=["ys", "POS"], writes=["y1_%d" % b], q=POOL)
        P.dma(lambda e, b=b, ti=ti: e.indirect_dma_start(out=y2[b][:, :], out_offset=None, in_=g.ys,
                                                         in_offset=bass.IndirectOffsetOnAxis(ap=g.POS[:, ti, 1:2], axis=0)),
              reads=["ys", "POS"], writes=["y2_%d" % b], q=POOL)
        P.pool(lambda e, b=b, ti=ti: e.tensor_scalar(out=y1[b][:, :], in0=y1[b][:, :], scalar1=g.RG[:, ti, 0:1], scalar2=None, op0=ALU.mult),
               reads=["y1_%d" % b, "RG"], writes=["y1_%d" % b])
        P.dve(lambda e, b=b, ti=ti: e.scalar_tensor_tensor(out=y2[b][:, :], in0=y2[b][:, :], scalar=g.RG[:, ti, 1:2], in1=y1[b][:, :],
                                                           op0=ALU.mult, op1=ALU.add), reads=["y2_%d" % b, "y1_%d" % b, "RG"], writes=["y2_%d" % b])
        P.dve(lambda e, b=b: e.scalar_tensor_tensor(out=x1[b][:, :], in0=x1[b][:, :], scalar=ALPHA, in1=y2[b][:, :],
                                                    op0=ALU.mult, op1=ALU.add), reads=["x1_%d" % b, "y2_%d" % b], writes=["x1_%d" % b])
        _ln_token_major(P, POOL, x1[b], x2[b], g.pb[:, PB_LN2G:PB_LN2G + 1024], g.pb[:, PB_LN2B:PB_LN2B + 1024],
                        st, mv, rs, "x1_%d" % b, "x2_%d" % b, "ln2")
        P.dma(lambda e, b=b, ti=ti: e.dma_start(out=xdst[ti * 128:(ti + 1) * 128, :], in_=x2[b][:, :]), reads=["x2_%d" % b], writes=["XOUT_%d" % ti])
    ph.finish()


def build_program(n_layers=DEPTH, stop_after=None, use_dma_transpose=True):
    nc = bass.Bass("TRN2", target_bir_lowering=False)
    g = G()

    def din(name, shape, dt):
        return nc.dram_tensor(name, list(shape), dt, kind="ExternalInput").ap()

    def dscr(name, shape, dt):
        return nc.dram_tensor(name, list(shape), dt, kind="Internal").ap()

    g.d_x = din("x", [SEQ, D], F32)
    g.d_posb = din("posb", [128, SEQ], I32)
    g.d_cf = din("constf", [128, CF_N], F32)
    g.d_cb = din("constb", [128, CB_N], BF16)
    g.d_conv_w_pw1 = din("conv_w_pw1", [2, D, 2 * D], F32)
    g.d_conv_w_pw2 = din("conv_w_pw2", [2, D, D], F32)
    g.d_ret_w_qkvg = din("ret_w_qkvg", [2, D, 6 * D], F32)
    g.d_ret_w_o = din("ret_w_o", [2, 2 * D, D], F32)
    g.d_moe_w_gate = din("moe_w_gate", [DEPTH, NE, D, FF], F32)
    g.d_moe_w_up = din("moe_w_up", [DEPTH, NE, D, FF], F32)
    g.d_moe_w_down = din("moe_w_down", [DEPTH, NE, FF, D], F32)
    g.d_pbln = din("pbln", [DEPTH, 128, PB_N], F32)
    g.d_pbmix = din("pbmix", [DEPTH, 128, PM_N], F32)
    g.d_pp = din("pp", [2, 128, PP_N], F32)
    g.d_wr = din("wr", [DEPTH, 128, 8, 36], F32)
    g.d_out = nc.dram_tensor("out", [SEQ, D], F32, kind="ExternalOutput").ap()
    g.XA = dscr("XA", [SEQ, D], F32)
    g.XB = dscr("XB", [SEQ, D], F32)
    g.xb16 = dscr("xb16", [SEQ, D], BF16)
    g.xs = dscr("xs", [NSLOT_T * SLOT, D], BF16)
    g.ys = dscr("ys", [NSLOT_T * SLOT, D], F32)
    g.QS = dscr("QS", [NT, 128, 8, 128], BF16)
    g.KS = dscr("KS", [NT, 128, 8, 128], BF16)
    g.VS = dscr("VS", [SEQ, 2 * D], BF16)
    g.SG = dscr("SG", [SEQ, 2 * D], BF16)

    with contextlib.ExitStack() as st:
        def sb(name, shape, dt):
            return st.enter_context(nc.sbuf_tensor(name, list(shape), dt))
        g.cf = sb("cf", [128, CF_N], F32)
        g.cb = sb("cb", [128, CB_N], BF16)
        g.pb = sb("pb", [128, PB_N], F32)
        g.wr = sb("wr_sb", [128, 8, 36], F32)
        g.RE1 = sb("RE1", [128, NT, 32], F32)
        g.RE2 = sb("RE2", [128, NT, 32], F32)
        g.RPF = sb("RPF", [128, NT, 32], F32)
        g.RG = sb("RG", [128, NT, 2], F32)
        g.POS = sb("POS", [128, NT, 2], I32)
        g.CAR = sb("CAR", [128, 32], F32)
        g.WG = sb("WG", [128, NSLOT_T], I32)
        g.WD = sb("WD", [128, 4, NSLOT_T], I32)

        EPS_AP[0] = g.cf[:, CF_EPS:CF_EPS + 1]
        ph = Phase(nc, "init")
        ph.P.dma(lambda e: e.dma_start(out=g.cf[:, :], in_=g.d_cf), writes=["cf"])
        ph.P.dma(lambda e: e.dma_start(out=g.cb[:, :], in_=g.d_cb), writes=["cb"])
        ph.finish()

        xcur = g.d_x
        for li in range(n_layers):
            if li % 2 == 0:
                conv_phase(nc, g, li, xcur, g.XB)
            else:
                retention_phase(nc, g, li, xcur, g.XB)
            if stop_after == ("mix", li):
                _copy_out(nc, g, g.XB)
                break
            offsets_scatter_phase(nc, g, li)
            expert_phase(nc, g, li, use_dma_transpose)
            last = (li == n_layers - 1)
            xnext = g.d_out if last else g.XA
            combine_phase(nc, g, li, g.XB, xnext)
            xcur = xnext
    return nc


def _copy_out(nc, g, src):
    ph = Phase(nc, "cpy")
    t = [ph.sb([128, D], F32, "t") for _ in range(2)]
    for ti in range(NT):
        b = ti % 2
        ph.P.dma(lambda e, b=b, ti=ti: e.dma_start(out=t[b][:, :], in_=src[ti * 128:(ti + 1) * 128, :]), reads=["src"], writes=["t%d" % b])
        ph.P.dma(lambda e, b=b, ti=ti: e.dma_start(out=g.d_out[ti * 128:(ti + 1) * 128, :], in_=t[b][:, :]), reads=["t%d" % b], writes=["o%d" % ti])
    ph.finish()


TWO_PI_HI = 6.28125
TWO_PI_LO = 2.0 * np.pi - 6.28125
PI = float(np.pi)


def retention_phase(nc, g, li, xsrc, xdst):
    _ret_pass1(nc, g, li, xsrc)
    _ret_pass2(nc, g, li, xsrc, xdst)


def _ret_pass1(nc, g, li, xsrc):
    j = li // 2
    ph = Phase(nc, "rp%d" % li)
    P = ph.P
    cf, cb = g.cf, g.cb
    _load_layer_params(ph, g, li)
    Wq = ph.sb([128, 8, 6 * D], BF16, "Wq")
    wsrc = g.d_ret_w_qkvg[j].rearrange("(k p) f -> p k f", p=128)
    for q in range(12):
        P.dma(lambda e, q=q: e.dma_start(out=Wq[:, :, q * 512:(q + 1) * 512], in_=wsrc[:, :, q * 512:(q + 1) * 512]),
              writes=["Wq_%d" % q], q=POOL)
    posi = ph.sb([128, 512], I32, "posi")
    posf = ph.sb([128, 512], F32, "posf")
    xin = [ph.sb([128, D], F32, "xin") for _ in range(2)]
    xbf = ph.sb([128, D], BF16, "xbf")
    xT = ph.sb([128, 8, 512], BF16, "xT")
    ang = ph.sb([128, 512], F32, "ang")
    ni = ph.sb([128, 512], I32, "ni")
    nf = ph.sb([128, 512], F32, "nf")
    rr = ph.sb([128, 512], F32, "rr")
    cc = ph.sb([128, 512], F32, "cc")
    mm = ph.sb([128, 512], F32, "mm")
    sinT = ph.sb([128, 512], F32, "sinT")
    cosT = ph.sb([128, 512], F32, "cosT")
    ta = [ph.sb([128, 512], F32, "ta")] * 2
    tb = [ph.sb([128, 512], F32, "tb")] * 2
    tc_ = [ph.sb([128, 512], F32, "tc")] * 2
    td = [ph.sb([128, 512], F32, "td")] * 2
    qkT = ph.sb([128, 16, 512], BF16, "qkT")
    vrow = [ph.sb([128, 2 * D], BF16, "vrow")] * 2
    srow = [ph.sb([128, 2 * D], BF16, "srow")] * 2
    psT = ph.psum("psT")
    psT_bf = psT[:, :].bitcast(BF16)
    psq = [ph.psum("psq") for _ in range(4)]
    psv = [ph.psum("psv") for _ in range(2)]
    invf = cf[:, CF_INVF:CF_INVF + 1]
    for st_i in range(NT // 4):
        t0 = st_i * 512
        for sub in range(4):
            ti = st_i * 4 + sub
            xi = xin[ti % 2]
            kxi = "xin_%d" % (ti % 2)
            P.dma(lambda e, xi=xi, ti=ti: e.dma_start(out=xi[:, :], in_=xsrc[ti * 128:(ti + 1) * 128, :]), writes=[kxi])
            P.act(lambda e, xi=xi: e.copy(out=xbf[:, :], in_=xi[:, :]), reads=[kxi], writes=["xbf"])
            for kc in range(8):
                P.pe(lambda e, kc=kc: e.transpose(out=psT_bf[:, kc * 128:(kc + 1) * 128], in_=xbf[:, kc * 128:(kc + 1) * 128],
                                                  identity=cb[:, CB_IDENT:CB_IDENT + 128]), reads=["xbf"], writes=["psT"])
            P.dve(lambda e, sub=sub: e.tensor_copy(out=xT[:, :, sub * 128:(sub + 1) * 128],
                                                   in_=psT_bf.rearrange("p (k t) -> p k t", k=8)), reads=["psT"], writes=["xT"])
        P.dma(lambda e, t0=t0: e.dma_start(out=posi[:, :], in_=g.d_posb[:, t0:t0 + 512]), writes=["posi"])
        P.pool(lambda e: e.tensor_copy(out=posf[:, :], in_=posi[:, :]), reads=["posi"], writes=["posf"])
        P.pool(lambda e: e.tensor_scalar(out=ang[:, :], in0=posf[:, :], scalar1=invf, scalar2=None, op0=ALU.mult),
               reads=["posf"], writes=["ang"])
        P.pool(lambda e: e.tensor_scalar(out=ni[:, :], in0=ang[:, :], scalar1=float(1.0 / (2.0 * np.pi)), scalar2=None, op0=ALU.mult),
               reads=["ang"], writes=["ni"])
        P.pool(lambda e: e.tensor_copy(out=nf[:, :], in_=ni[:, :]), reads=["ni"], writes=["nf"])
        P.dve(lambda e: e.scalar_tensor_tensor(out=rr[:, :], in0=nf[:, :], scalar=-TWO_PI_HI, in1=ang[:, :], op0=ALU.mult, op1=ALU.add),
              reads=["nf", "ang"], writes=["rr"])
        P.dve(lambda e: e.scalar_tensor_tensor(out=rr[:, :], in0=nf[:, :], scalar=-TWO_PI_LO, in1=rr[:, :], op0=ALU.mult, op1=ALU.add),
              reads=["nf", "rr"], writes=["rr"])
        P.pool(lambda e: e.tensor_scalar(out=mm[:, :], in0=rr[:, :], scalar1=PI, scalar2=-2.0 * PI, op0=ALU.is_gt, op1=ALU.mult),
               reads=["rr"], writes=["mm"])
        P.pool(lambda e: e.tensor_tensor(out=rr[:, :], in0=rr[:, :], in1=mm[:, :], op=ALU.add), reads=["rr", "mm"], writes=["rr"])
        P.pool(lambda e: e.tensor_scalar(out=mm[:, :], in0=rr[:, :], scalar1=-PI, scalar2=2.0 * PI, op0=ALU.is_lt, op1=ALU.mult),
               reads=["rr"], writes=["mm"])
        P.pool(lambda e: e.tensor_tensor(out=rr[:, :], in0=rr[:, :], in1=mm[:, :], op=ALU.add), reads=["rr", "mm"], writes=["rr"])
        P.pool(lambda e: e.tensor_scalar(out=cc[:, :], in0=rr[:, :], scalar1=0.5 * PI, scalar2=None, op0=ALU.add), reads=["rr"], writes=["cc"])
        P.pool(lambda e: e.tensor_scalar(out=mm[:, :], in0=cc[:, :], scalar1=PI, scalar2=-2.0 * PI, op0=ALU.is_gt, op1=ALU.mult),
               reads=["cc"], writes=["mm"])
        P.pool(lambda e: e.tensor_tensor(out=cc[:, :], in0=cc[:, :], in1=mm[:, :], op=ALU.add), reads=["cc", "mm"], writes=["cc"])
        P.act(lambda e: e.activation(out=sinT[:, :], in_=rr[:, :], func=AF.Sin), reads=["rr"], writes=["sinT"])
        P.act(lambda e: e.activation(out=cosT[:, :], in_=cc[:, :], func=AF.Sin), reads=["cc"], writes=["cosT"])
        for hp in range(8):
            b2 = hp % 2
            p1, p2 = psq[2 * b2], psq[2 * b2 + 1]
            k1, k2 = "psq_%d" % (2 * b2), "psq_%d" % (2 * b2 + 1)
            for half, pq, kq in ((0, p1, k1), (1, p2, k2)):
                c0 = (2 * hp + half) * 128
                for kc in range(8):
                    P.pe(lambda e, kc=kc, c0=c0, pq=pq: e.matmul(pq[:, :], lhsT=Wq[:, kc, c0:c0 + 128], rhs=xT[:, kc, :],
                                                                 start=(kc == 0), stop=(kc == 7)),
                         reads=["xT", "Wq_%d" % (c0 // 512)], writes=[kq])
            a_, b_, c_, d_ = ta[b2], tb[b2], tc_[b2], td[b2]
            sfx = "_0"
            P.dve(lambda e, p1=p1, a_=a_: e.tensor_tensor(out=a_[:, :], in0=p1[:, :], in1=cosT[:, :], op=ALU.mult), reads=[k1, "cosT"], writes=["ta" + sfx])
            P.dve(lambda e, p2=p2, b_=b_: e.tensor_tensor(out=b_[:, :], in0=p2[:, :], in1=sinT[:, :], op=ALU.mult), reads=[k2, "sinT"], writes=["tb" + sfx])
            P.dve(lambda e, p1=p1, c_=c_: e.tensor_tensor(out=c_[:, :], in0=p1[:, :], in1=sinT[:, :], op=ALU.mult), reads=[k1, "sinT"], writes=["tc" + sfx])
            P.dve(lambda e, p2=p2, d_=d_: e.tensor_tensor(out=d_[:, :], in0=p2[:, :], in1=cosT[:, :], op=ALU.mult), reads=[k2, "cosT"], writes=["td" + sfx])
            P.pool(lambda e, hp=hp, a_=a_, b_=b_: e.tensor_tensor(out=qkT[:, 2 * hp, :], in0=a_[:, :], in1=b_[:, :], op=ALU.subtract),
                   reads=["ta" + sfx, "tb" + sfx], writes=["qkT"])
            P.pool(lambda e, hp=hp, c_=c_, d_=d_: e.tensor_tensor(out=qkT[:, 2 * hp + 1, :], in0=c_[:, :], in1=d_[:, :], op=ALU.add),
                   reads=["tc" + sfx, "td" + sfx], writes=["qkT"])
        for sub in range(4):
            ti = st_i * 4 + sub
            P.dma(lambda e, ti=ti, sub=sub: e.dma_start(out=g.QS[ti], in_=qkT[:, 0:8, sub * 128:(sub + 1) * 128]), reads=["qkT"], writes=["QS_%d" % ti])
            P.dma(lambda e, ti=ti, sub=sub: e.dma_start(out=g.KS[ti], in_=qkT[:, 8:16, sub * 128:(sub + 1) * 128]), reads=["qkT"], writes=["KS_%d" % ti])
        for sub in range(4):
            ti = st_i * 4 + sub
            vr, sr = vrow[ti % 2], srow[ti % 2]
            kv, ks = "vrow_0", "srow_0"
            for cbk in range(8):
                pv = psv[cbk % 2]
                kpv = "psv_%d" % (cbk % 2)
                c0 = 2048 + cbk * 512
                for kc in range(8):
                    P.pe(lambda e, kc=kc, c0=c0, pv=pv, sub=sub: e.matmul(pv[:, :], lhsT=xT[:, kc, sub * 128:(sub + 1) * 128], rhs=Wq[:, kc, c0:c0 + 512],
                                                                          start=(kc == 0), stop=(kc == 7)),
                         reads=["xT", "Wq_%d" % (c0 // 512)], writes=[kpv])
                if cbk < 4:
                    P.dve(lambda e, pv=pv, vr=vr, cbk=cbk: e.tensor_copy(out=vr[:, cbk * 512:(cbk + 1) * 512], in_=pv[:, :]), reads=[kpv], writes=[kv])
                else:
                    P.act(lambda e, pv=pv, sr=sr, cbk=cbk: e.activation(out=sr[:, (cbk - 4) * 512:(cbk - 3) * 512], in_=pv[:, :], func=AF.Silu),
                          reads=[kpv], writes=[ks])
            P.dma(lambda e, vr=vr, ti=ti: e.dma_start(out=g.VS[ti * 128:(ti + 1) * 128, :], in_=vr[:, :]), reads=[kv], writes=["VS_%d" % ti])
            P.dma(lambda e, sr=sr, ti=ti: e.dma_start(out=g.SG[ti * 128:(ti + 1) * 128, :], in_=sr[:, :]), reads=[ks], writes=["SG_%d" % ti])
    ph.finish()


def _ret_pass2(nc, g, li, xsrc, xdst):
    j = li // 2
    ph = Phase(nc, "rq%d" % li)
    P = ph.P
    cf, cb = g.cf, g.cb
    Wo = ph.sb([128, 16, D], BF16, "Wo")
    wsrc = g.d_ret_w_o[j].rearrange("(k p) f -> p k f", p=128)
    for q in range(2):
        P.dma(lambda e, q=q: e.dma_start(out=Wo[:, q * 8:(q + 1) * 8, :], in_=wsrc[:, q * 8:(q + 1) * 8, :]), writes=["Wo"], q=POOL)
    pm = ph.sb([128, PM_N], F32, "pm")
    P.dma(lambda e: e.dma_start(out=pm[:, :], in_=g.d_pbmix[li]), writes=["PM"])
    qT = [ph.sb([128, 8, 128], BF16, "qT") for _ in range(2)]
    kT = [ph.sb([128, 8, 128], BF16, "kT") for _ in range(2)]
    vrow = ph.sb([128, 2 * D], BF16, "vrow")
    srow = ph.sb([128, 2 * D], BF16, "srow")
    xin = [ph.sb([128, D], F32, "xin") for _ in range(2)]
    S = ph.sb([128, RH, 2, 512], F32, "S")
    Sbf = ph.sb([128, RH, 2, 512], BF16, "Sbf")
    qxT = ph.sb([128, 8, 128], BF16, "qxT")
    kz = ph.sb([128, D], BF16, "kz")
    PT = [ph.sb([128, 128], BF16, "PT") for _ in range(2)]
    on = [ph.sb([128, 512], F32, "on") for _ in range(2)]
    y = ph.sb([128, 2 * D], BF16, "y")
    yT = ph.sb([128, 16, 128], BF16, "yT")
    rbuf = [ph.sb([128, D], F32, "r") for _ in range(2)]
    gst = ph.sb([128, 6], F32, "gst")
    gmv = ph.sb([128, 2], F32, "gmv")
    grs = ph.sb([128, 1], F32, "grs")
    gnm = ph.sb([128, 1], F32, "gnm")
    bufs = _RW()
    bufs.x1 = [ph.sb([128, D], F32, "x1") for _ in range(2)]
    bufs.x1bf = [ph.sb([128, D], BF16, "x1bf") for _ in range(2)]
    bufs.st = ph.sb([128, 12], F32, "st")
    bufs.mv = ph.sb([128, 2], F32, "mv")
    bufs.rs = ph.sb([128, 1], F32, "rs")
    RWk = _router_work(ph)
    pss = ph.psum("pss")
    pso = [ph.psum("pso") for _ in range(2)]
    pst = [ph.psum("pst") for _ in range(2)]
    psX = [ph.psum("psX") for _ in range(2)]
    psR = ph.psum("psR")
    psX_bf = [p[:, :].bitcast(BF16) for p in psX]
    P.dve(lambda e: e.memset(S[:, :, :, :], 0.0), writes=["S"])
    P.dve(lambda e: e.memset(Sbf[:, :, :, :], 0.0), writes=["Sbf"])
    for ti in range(NT):
        b = ti % 2
        kq, kk_ = "qT_%d" % b, "kT_%d" % b
        xi = xin[b]
        kxi = "xin_%d" % b
        P.dma(lambda e, b=b, ti=ti: e.dma_start(out=qT[b][:, :, :], in_=g.QS[ti]), writes=[kq])
        P.dma(lambda e, b=b, ti=ti: e.dma_start(out=kT[b][:, :, :], in_=g.KS[ti]), writes=[kk_])
        P.dma(lambda e, ti=ti: e.dma_start(out=vrow[:, :], in_=g.VS[ti * 128:(ti + 1) * 128, :]), writes=["vrow"])
        P.dma(lambda e, ti=ti: e.dma_start(out=srow[:, :], in_=g.SG[ti * 128:(ti + 1) * 128, :]), writes=["srow"])
        P.dma(lambda e, xi=xi, ti=ti: e.dma_start(out=xi[:, :], in_=xsrc[ti * 128:(ti + 1) * 128, :]), writes=[kxi])
        P.dve(lambda e, b=b: e.tensor_tensor(out=qxT[:, :, :], in0=qT[b][:, :, :],
                                             in1=cb[:, CB_XI:CB_XI + 1024].rearrange("p (k t) -> p k t", k=8), op=ALU.mult),
              reads=[kq], writes=["qxT"])
        for kc in range(8):
            P.pe(lambda e, b=b, kc=kc: e.transpose(out=psX_bf[0][:, kc * 128:(kc + 1) * 128], in_=kT[b][:, kc, :],
                                                   identity=cb[:, CB_IDENT:CB_IDENT + 128]), reads=[kk_], writes=["psT2_0"])
        for h in range(RH):
            P.act(lambda e, h=h: e.activation(out=kz[:, h * 256:(h + 1) * 256], in_=psX_bf[0][:, h * 256:(h + 1) * 256], func=AF.Copy,
                                              scale=cf[:, CF_ZETA + h:CF_ZETA + h + 1]), reads=["psT2_0"], writes=["kz"])
        for h in range(RH):
            pb2 = h % 2
            po = pso[pb2]
            kpo = "pso_%d" % pb2
            for c in range(2):
                P.pe(lambda e, b=b, h=h, c=c: e.matmul(pss[:, 0:128], lhsT=kT[b][:, 2 * h + c, :], rhs=qT[b][:, 2 * h + c, :],
                                                       start=(c == 0), stop=(c == 1)), reads=[kq, kk_], writes=["pss"])
            pt = PT[pb2]
            kpt = "PT_%d" % pb2
            P.dve(lambda e, h=h, pt=pt: e.tensor_tensor(out=pt[:, :], in0=pss[:, 0:128], in1=cf[:, CF_MT + h * 128:CF_MT + (h + 1) * 128], op=ALU.mult),
                  reads=["pss"], writes=[kpt])
            P.pe(lambda e, h=h, pt=pt, po=po: e.matmul(po[:, :], lhsT=pt[:, :], rhs=vrow[:, h * 512:(h + 1) * 512], start=True, stop=False),
                 reads=[kpt, "vrow"], writes=[kpo])
            for c in range(2):
                P.pe(lambda e, h=h, c=c, po=po: e.matmul(po[:, :], lhsT=qxT[:, 2 * h + c, :], rhs=Sbf[:, h, c, :], start=False, stop=(c == 1)),
                     reads=["qxT", "Sbf_%d" % h], writes=[kpo])
            for c in range(2):
                P.pe(lambda e, h=h, c=c: e.matmul(pst[c][:, :], lhsT=kz[:, h * 256 + c * 128:h * 256 + (c + 1) * 128], rhs=vrow[:, h * 512:(h + 1) * 512],
                                                  start=True, stop=True), reads=["kz", "vrow"], writes=["pst_%d" % c])
                P.dve(lambda e, h=h, c=c: e.scalar_tensor_tensor(out=S[:, h, c, :], in0=S[:, h, c, :], scalar=float(GAMMA[h] ** 128.0), in1=pst[c][:, :],
                                                                 op0=ALU.mult, op1=ALU.add), reads=["pst_%d" % c, "S"], writes=["S"])
                P.act(lambda e, h=h, c=c: e.copy(out=Sbf[:, h, c, :], in_=S[:, h, c, :]), reads=["S"], writes=["Sbf_%d" % h])
            P.dve(lambda e, po=po: e.bn_stats(out=gst[:, 0:6], in_=po[:, :]), reads=[kpo], writes=["gst"])
            P.dve(lambda e: e.bn_aggr(out=gmv[:, 0:2], in_=gst[:, 0:6]), reads=["gst"], writes=["gmv"])
            P.act(lambda e: e.activation(out=grs[:, 0:1], in_=gmv[:, 1:2], func=AF.Sqrt, bias=EPS_AP[0], scale=1.0), reads=["gmv"], writes=["grs"])
            P.dve(lambda e: e.reciprocal(out=grs[:, 0:1], in_=grs[:, 0:1]), reads=["grs"], writes=["grs"])
            P.dve(lambda e: e.scalar_tensor_tensor(out=gnm[:, 0:1], in0=gmv[:, 0:1], scalar=-1.0, in1=grs[:, 0:1], op0=ALU.mult, op1=ALU.mult),
                  reads=["gmv", "grs"], writes=["gnm"])
            o_ = on[pb2]
            kon = "on_%d" % pb2
            P.act(lambda e, po=po, o_=o_: e.activation(out=o_[:, :], in_=po[:, :], func=AF.Identity, bias=gnm[:, 0:1], scale=grs[:, 0:1]),
                  reads=[kpo, "grs", "gnm"], writes=[kon])
            P.pool(lambda e, h=h, o_=o_: e.tensor_tensor(out=o_[:, :], in0=o_[:, :], in1=pm[:, h * 512:(h + 1) * 512], op=ALU.mult),
                   reads=[kon, "PM"], writes=[kon])
            P.pool(lambda e, h=h, o_=o_: e.tensor_tensor(out=o_[:, :], in0=o_[:, :], in1=pm[:, 2 * D + h * 512:2 * D + (h + 1) * 512], op=ALU.add),
                   reads=[kon, "PM"], writes=[kon])
            P.dve(lambda e, h=h, o_=o_: e.tensor_tensor(out=y[:, h * 512:(h + 1) * 512], in0=o_[:, :], in1=srow[:, h * 512:(h + 1) * 512], op=ALU.mult),
                  reads=[kon, "srow"], writes=["y"])
        for half in range(2):
            for q in range(8):
                kc = half * 8 + q
                P.pe(lambda e, kc=kc, q=q, half=half: e.transpose(out=psX_bf[half][:, q * 128:(q + 1) * 128], in_=y[:, kc * 128:(kc + 1) * 128],
                                                                  identity=cb[:, CB_IDENT:CB_IDENT + 128]), reads=["y"], writes=["psT2_%d" % half])
            if half == 0:
                P.act(lambda e: e.copy(out=yT[:, 0:8, :], in_=psX_bf[0].rearrange("p (k t) -> p k t", k=8)), reads=["psT2_0"], writes=["yT"])
            else:
                P.dve(lambda e: e.tensor_copy(out=yT[:, 8:16, :], in_=psX_bf[1].rearrange("p (k t) -> p k t", k=8)), reads=["psT2_1"], writes=["yT"])
        r = rbuf[b]
        kr = "r_%d" % b
        for half in range(2):
            po = pso[half]
            kpo = "pso_%d" % half
            for kc in range(16):
                P.pe(lambda e, kc=kc, half=half, po=po: e.matmul(po[:, :], lhsT=yT[:, kc, :], rhs=Wo[:, kc, half * 512:(half + 1) * 512],
                                                                 start=(kc == 0), stop=(kc == 15)), reads=["yT", "Wo"], writes=[kpo])
            P.dve(lambda e, half=half, po=po, xi=xi, r=r: e.scalar_tensor_tensor(out=r[:, half * 512:(half + 1) * 512], in0=xi[:, half * 512:(half + 1) * 512],
                                                                                 scalar=ALPHA, in1=po[:, :], op0=ALU.mult, op1=ALU.add),
                  reads=[kpo, kxi], writes=[kr])
        _post_mixer(ph, g, ti, r, kr, RWk, bufs, psR, psX, xdst)
    ph.finish()


def _rep(v, n=128):
    return np.ascontiguousarray(np.broadcast_to(np.asarray(v, np.float32).reshape(1, -1), (n, np.asarray(v).size)))


def prepare_shared(inputs):
    cf, cb, _ = _host_consts()
    sh = {"constf": cf, "constb": cb}
    for k in ("conv_w_pw1", "conv_w_pw2", "ret_w_qkvg", "ret_w_o", "moe_w_gate", "moe_w_up", "moe_w_down"):
        sh[k] = np.ascontiguousarray(inputs[k], dtype=np.float32)
    pbln = np.zeros((DEPTH, 128, PB_N), np.float32)
    pbmix = np.zeros((DEPTH, 128, PM_N), np.float32)
    wr = np.zeros((DEPTH, 128, 8, 36), np.float32)
    for i in range(DEPTH):
        pbln[i, :, PB_LN1G:PB_LN1G + D] = _rep(inputs["ln1_g"][i])
        pbln[i, :, PB_LN1B:PB_LN1B + D] = _rep(inputs["ln1_b"][i])
        pbln[i, :, PB_LN2G:PB_LN2G + D] = _rep(inputs["ln2_g"][i])
        pbln[i, :, PB_LN2B:PB_LN2B + D] = _rep(inputs["ln2_b"][i])
        pbln[i, :, PB_RB:PB_RB + 4] = _rep(inputs["moe_b_grp"][i])
        pbln[i, :, PB_RB + 4:PB_RB + 36] = _rep(inputs["moe_b_route"][i])
        wcat = np.concatenate([inputs["moe_w_grp"][i], inputs["moe_w_route"][i]], axis=1)
        wr[i] = wcat.reshape(8, 128, 36).transpose(1, 0, 2)
        j = i // 2
        if i % 2 == 0:
            pbmix[i, :, 0:D] = _rep(inputs["conv_b_pw2"][j])
        else:
            pbmix[i, :, 0:2 * D] = _rep(inputs["ret_gn_g"][j])
            pbmix[i, :, 2 * D:4 * D] = _rep(inputs["ret_gn_b"][j])
    pp = np.zeros((2, 128, PP_N), np.float32)
    for j in range(2):
        pp[j, :, PP_B1:PP_B1 + 16] = np.asarray(inputs["conv_b_pw1"][j]).reshape(16, 128).T
        wdw = np.asarray(inputs["conv_w_dw"][j])
        pp[j, :, PP_WDW:PP_WDW + 248] = wdw.reshape(CW, 8, 128).transpose(2, 1, 0).reshape(128, 248)
        pp[j, :, PP_BDW:PP_BDW + 8] = np.asarray(inputs["conv_b_dw"][j]).reshape(8, 128).T
        pp[j, :, PP_LNG:PP_LNG + 8] = np.asarray(inputs["conv_ln_g"][j]).reshape(8, 128).T
        pp[j, :, PP_LNB:PP_LNB + 8] = np.asarray(inputs["conv_ln_b"][j]).reshape(8, 128).T
    sh["pbln"], sh["pbmix"], sh["pp"], sh["wr"] = pbln, pbmix, pp, wr
    return sh


_NC_CACHE = {}


def kernel(**inputs):
    x = np.asarray(inputs["x"], np.float32)
    pos = np.asarray(inputs["positions"], np.int32)
    sh = prepare_shared(inputs)
    if "nc" not in _NC_CACHE:
        _NC_CACHE["nc"] = build_program()
    nc = _NC_CACHE["nc"]
    in_maps = []
    for c in range(8):
        m = dict(sh)
        m["x"] = np.ascontiguousarray(x[c])
        m["posb"] = np.ascontiguousarray(np.broadcast_to(pos[c][None, :], (128, SEQ)))
        in_maps.append(m)
    res = run_bass_kernel_spmd(nc, in_maps, core_ids=list(range(8)))
    return np.stack([np.asarray(r["out"], np.float32) for r in res.results], axis=0)
```

```python
import contextlib
import numpy as np
import ml_dtypes
import concourse.bass as bass
import concourse.mybir as mybir
from concourse.bass_utils import run_bass_kernel_spmd

F32 = mybir.dt.float32
BF16 = mybir.dt.bfloat16
I32 = mybir.dt.int32
ALU = mybir.AluOpType
AF = mybir.ActivationFunctionType
AX = mybir.AxisListType

PE, ACT, DVE, POOL, SP = "pe", "act", "dve", "pool", "sp"
ENGS = (PE, ACT, DVE, POOL, SP)

D = 1024
SEQ = 4096
NT = SEQ // 128
DEPTH = 4
NE = 32
FF = 512
SLOT = 512
NSLOT_T = 2 * SEQ // SLOT + NE
ALPHA = (2.0 * DEPTH) ** 0.25
EPS = 1e-5
CW = 31
RH = 4


class _Op:
    __slots__ = ("eng", "fn", "deps", "dma", "sig", "dsem", "dval", "dprev", "pos", "need_sig")


class Prog:
    def __init__(self, nc, n_dma_sems=8, same_eng_dist=3):
        self.nc = nc
        self.ops = []
        self.last_writer = {}
        self.readers = {}
        self.eng_count = {e: 0 for e in ENGS}
        self.n_dma_sems = n_dma_sems
        self.same_eng_dist = same_eng_dist

    def add(self, eng, fn, reads=(), writes=(), dma=False):
        op = _Op()
        op.eng, op.fn, op.dma = eng, fn, dma
        op.sig = None
        op.need_sig = False
        op.pos = self.eng_count[eng]
        self.eng_count[eng] += 1
        idx = len(self.ops)
        deps = set()
        for k in reads:
            w = self.last_writer.get(k)
            if w is not None:
                deps.add((w, True))
        for k in writes:
            w = self.last_writer.get(k)
            if w is not None:
                deps.add((w, False))
            for r in self.readers.get(k, ()):
                if r != idx:
                    deps.add((r, False))
        for k in reads:
            self.readers.setdefault(k, []).append(idx)
        for k in writes:
            self.last_writer[k] = idx
            self.readers[k] = []
        real = {}
        for d, raw in deps:
            dop = self.ops[d]
            if dop.dma or dop.eng != eng or dma:
                real[d] = True
            elif raw and eng != PE and (op.pos - dop.pos) < self.same_eng_dist:
                real[d] = True
        latest = {}
        keep = []
        for d in real:
            dop = self.ops[d]
            if dop.dma:
                keep.append(d)
            elif d > latest.get(dop.eng, -1):
                latest[dop.eng] = d
        op.deps = sorted(keep + list(latest.values()))
        for d in op.deps:
            if not self.ops[d].dma:
                self.ops[d].need_sig = True
        self.ops.append(op)
        return idx

    def pe(self, fn, reads=(), writes=()):
        return self.add(PE, fn, reads, writes)

    def act(self, fn, reads=(), writes=()):
        return self.add(ACT, fn, reads, writes)

    def dve(self, fn, reads=(), writes=()):
        return self.add(DVE, fn, reads, writes)

    def pool(self, fn, reads=(), writes=()):
        return self.add(POOL, fn, reads, writes)

    def ve(self, eng, fn, reads=(), writes=()):
        return self.add(eng, fn, reads, writes)

    def dma(self, fn, reads=(), writes=(), q=SP):
        return self.add(q, fn, reads, writes, dma=True)

    def emit(self):
        nc = self.nc
        ops = self.ops
        pool = _sem_pool(nc, self.n_dma_sems)
        sigc = dict(pool["eval"])
        sig0 = dict(pool["eval"])
        dcount = dict(pool["dcount"])
        dsem_val = dict(pool["dval"])
        used_d = set()
        last_of = {}
        for i, op in enumerate(ops):
            last_of[op.eng] = i
        for e, i in last_of.items():
            if not ops[i].dma:
                ops[i].need_sig = True
        for op in ops:
            if op.dma:
                j = dcount[op.eng] % self.n_dma_sems
                dcount[op.eng] += 1
                key = (op.eng, j)
                prev = dsem_val.get(key, 0)
                op.dsem, op.dprev, op.dval = key, prev, prev + 16
                dsem_val[key] = op.dval
                used_d.add(key)
            elif op.need_sig:
                sigc[op.eng] += 1
                op.sig = sigc[op.eng]

        esem = pool["esem"]
        dsem = pool["dsem"]
        pool["eval"] = dict(sigc)
        pool["dcount"] = dict(dcount)
        pool["dval"] = dict(dsem_val)
        with contextlib.ExitStack() as st:
            block = st.enter_context(nc.Block())

            def run_engine(ename, eh):
                waited = {}

                def wait(sem_key, semh, val):
                    if waited.get(sem_key, 0) >= val:
                        return
                    eh.wait_ge(semh, val)
                    waited[sem_key] = val

                for op in ops:
                    if op.eng != ename:
                        continue
                    for d in op.deps:
                        dop = ops[d]
                        if dop.dma:
                            wait(dop.dsem, dsem[dop.dsem], dop.dval)
                        else:
                            wait(dop.eng, esem[dop.eng], dop.sig)
                    if op.dma:
                        if op.dprev > 0:
                            wait(op.dsem, dsem[op.dsem], op.dprev)
                        op.fn(eh).then_inc(dsem[op.dsem], 16)
                    else:
                        ins = op.fn(eh)
                        if op.sig is not None:
                            ins.then_inc(esem[ename], 1)
                for key in sorted(used_d):
                    wait(key, dsem[key], dsem_val[key])
                for e in ENGS:
                    if sigc[e] > sig0[e]:
                        wait(e, esem[e], sigc[e])

            @block.tensor
            def _(eh):
                run_engine(PE, eh)

            @block.scalar
            def _(eh):
                run_engine(ACT, eh)

            @block.vector
            def _(eh):
                run_engine(DVE, eh)

            @block.gpsimd
            def _(eh):
                run_engine(POOL, eh)

            @block.sync
            def _(eh):
                run_engine(SP, eh)


_SEM_POOLS = {}


def _sem_pool(nc, n_dma_sems):
    key = id(nc)
    if key not in _SEM_POOLS:
        pool = {"esem": {e: nc.alloc_semaphore(name="s_" + e) for e in ENGS}, "dsem": {}, "dval": {}}
        pool["eval"] = {e: 0 for e in ENGS}
        pool["dcount"] = {e: 0 for e in ENGS}
        for e in (SP, ACT, POOL):
            for j in range(n_dma_sems):
                pool["dsem"][(e, j)] = nc.alloc_semaphore(name="d_%s_%d" % (e, j))
        _SEM_POOLS[key] = pool
    return _SEM_POOLS[key]


class Phase:
    def __init__(self, nc, name):
        self.nc = nc
        self.name = name
        self.st = contextlib.ExitStack()
        self.P = Prog(nc)
        self.n = 0

    def sb(self, shape, dt, tag="t"):
        self.n += 1
        return self.st.enter_context(self.nc.sbuf_tensor("%s_%s%d" % (self.name, tag, self.n), list(shape), dt))

    def psum(self, tag="ps"):
        self.n += 1
        return self.st.enter_context(self.nc.psum_tensor("%s_%s%d" % (self.name, tag, self.n), [128, 512], F32))

    def finish(self):
        self.P.emit()
        self.st.close()


CF_IDENT = 0
CF_ONES = 128
CF_MT = 256
CF_THR = 768
CF_IOTA = 816
CF_INVF = 824
CF_ZETA = 825
CF_EPS = 829
CF_N = 832
CB_IDENT = 0
CB_ONES = 128
CB_TRIU = 256
CB_XI = 384
CB_N = 384 + 1024


def _host_consts():
    cf = np.zeros((128, CF_N), np.float32)
    cf[:, CF_IDENT:CF_IDENT + 128] = np.eye(128, dtype=np.float32)
    cf[:, CF_ONES:CF_ONES + 128] = 1.0
    gam = 1.0 - 2.0 ** (-5.0 - np.arange(RH, dtype=np.float64))
    i = np.arange(128)
    for h in range(RH):
        ci, cj = i[:, None] // 64, i[None, :] // 64
        dist = (i[:, None] - i[None, :]).astype(np.float64)
        M = np.where(ci == cj, gam[h] ** np.abs(dist), np.where(ci > cj, gam[h] ** dist, 0.0))
        cf[:, CF_MT + h * 128: CF_MT + (h + 1) * 128] = (M.T * (256.0 ** -0.5)).astype(np.float32)
        cf[:, CF_ZETA + h] = (gam[h] ** (127.0 - i) * (256.0 ** -0.5)).astype(np.float32)
    cf[:, CF_THR:CF_THR + NSLOT_T] = (np.arange(NSLOT_T) * SLOT).astype(np.float32)[None, :]
    cf[:, CF_IOTA] = i
    cf[:, CF_EPS] = EPS
    for c in range(4):
        cf[:, CF_IOTA + 1 + c] = c * 128 + i
    cf[:, CF_INVF] = (np.float32(10000.0) ** (-(np.arange(128, dtype=np.float32)) / np.float32(128))).astype(np.float32)
    cb = np.zeros((128, CB_N), np.float32)
    cb[:, CB_IDENT:CB_IDENT + 128] = np.eye(128)
    cb[:, CB_ONES:CB_ONES + 128] = 1.0
    cb[:, CB_TRIU:CB_TRIU + 128] = (i[:, None] < i[None, :]).astype(np.float32)
    for kc in range(8):
        h = kc // 2
        cb[:, CB_XI + kc * 128: CB_XI + (kc + 1) * 128] = (gam[h] ** (i + 1.0))[None, :]
    return cf, cb.astype(ml_dtypes.bfloat16), gam


GAMMA = 1.0 - 2.0 ** (-5.0 - np.arange(RH, dtype=np.float64))

PB_LN1G, PB_LN1B, PB_LN2G, PB_LN2B, PB_RB = 0, 1024, 2048, 3072, 4096
PB_N = 4160
PM_N = 4096
PP_B1, PP_WDW, PP_BDW, PP_LNG, PP_LNB = 0, 16, 16 + 248, 16 + 248 + 8, 16 + 248 + 16
PP_N = 16 + 248 + 24


class G:
    pass


EPS_AP = [None]


def _ln_token_major(P, eng2, r, x1, g_ap, b_ap, tmp_stats, tmp_mv, tmp_rs, key_r, key_out, tagk):
    P.dve(lambda e: e.bn_stats(out=tmp_stats[:, 0:6], in_=r[:, 0:512]), reads=[key_r], writes=[tagk + "st"])
    P.dve(lambda e: e.bn_stats(out=tmp_stats[:, 6:12], in_=r[:, 512:1024]), reads=[key_r], writes=[tagk + "st"])
    P.dve(lambda e: e.bn_aggr(out=tmp_mv[:, 0:2], in_=tmp_stats[:, 0:12]), reads=[tagk + "st"], writes=[tagk + "mv"])
    P.act(lambda e: e.activation(out=tmp_rs[:, 0:1], in_=tmp_mv[:, 1:2], func=AF.Sqrt, bias=EPS_AP[0], scale=1.0),
          reads=[tagk + "mv"], writes=[tagk + "rs"])
    P.dve(lambda e: e.reciprocal(out=tmp_rs[:, 0:1], in_=tmp_rs[:, 0:1]), reads=[tagk + "rs"], writes=[tagk + "rs"])
    P.dve(lambda e: e.scalar_tensor_tensor(out=tmp_rs[:, 1:2], in0=tmp_mv[:, 0:1], scalar=-1.0, in1=tmp_rs[:, 0:1], op0=ALU.mult, op1=ALU.mult),
          reads=[tagk + "mv", tagk + "rs"], writes=[tagk + "rs"])
    P.act(lambda e: e.activation(out=r[:, :], in_=r[:, :], func=AF.Identity, bias=tmp_rs[:, 1:2], scale=tmp_rs[:, 0:1]),
          reads=[key_r, tagk + "rs"], writes=[key_r])
    P.ve(eng2, lambda e: e.tensor_tensor(out=r[:, :], in0=r[:, :], in1=g_ap, op=ALU.mult), reads=[key_r, "PB"], writes=[key_r])
    P.ve(eng2, lambda e: e.tensor_tensor(out=x1[:, :], in0=r[:, :], in1=b_ap, op=ALU.add), reads=[key_r, "PB"], writes=[key_out])


def _router_tile(ph, g, ti, x1, key_x1, W, psR, psT2):
    P = ph.P
    cf = g.cf
    for half in range(2):
        for q in range(4):
            kc = half * 4 + q
            P.pe(lambda e, kc=kc, q=q, half=half: e.transpose(out=psT2[half][:, q * 128:(q + 1) * 128],
                                                              in_=x1[:, kc * 128:(kc + 1) * 128],
                                                              identity=cf[:, CF_IDENT:CF_IDENT + 128]),
                 reads=[key_x1], writes=["psT2_%d" % half])
        P.act(lambda e, half=half: e.copy(out=W.x1T[:, half * 4:(half + 1) * 4, :],
                                          in_=psT2[half][:, :].rearrange("p (k t) -> p k t", k=4)),
              reads=["psT2_%d" % half], writes=["x1T"])
    for kc in range(8):
        P.pe(lambda e, kc=kc: e.matmul(psR[:, 0:36], lhsT=W.x1T[:, kc, :], rhs=g.wr[:, kc, :],
                                       start=(kc == 0), stop=(kc == 7)), reads=["x1T", "WR"], writes=["psR"])
    L = W.L
    P.dve(lambda e: e.tensor_tensor(out=L[:, 0:36], in0=psR[:, 0:36], in1=g.pb[:, PB_RB:PB_RB + 36], op=ALU.add),
          reads=["psR", "PB"], writes=["L"])
    sm = W.sm
    P.dve(lambda e: e.tensor_reduce(out=sm[:, 0:1], in_=L[:, 0:4], axis=AX.X, op=ALU.max), reads=["L"], writes=["sm0"])
    P.dve(lambda e: e.tensor_scalar(out=sm[:, 1:2], in0=sm[:, 0:1], scalar1=-1.0, scalar2=None, op0=ALU.mult),
          reads=["sm0"], writes=["sm1"])
    P.act(lambda e: e.activation(out=W.ge[:, 0:4], in_=L[:, 0:4], func=AF.Exp, bias=sm[:, 1:2], scale=1.0,
                                 accum_out=sm[:, 2:3]), reads=["L", "sm1"], writes=["ge", "sm2"])
    P.dve(lambda e: e.reciprocal(out=sm[:, 3:4], in_=sm[:, 2:3]), reads=["sm2"], writes=["sm3"])
    P.dve(lambda e: e.tensor_scalar(out=W.m4[:, 0:4], in0=L[:, 0:4], scalar1=sm[:, 0:1], scalar2=None, op0=ALU.is_equal),
          reads=["L", "sm0"], writes=["m4"])
    P.dve(lambda e: e.tensor_scalar(out=W.m4[:, 0:4], in0=W.m4[:, 0:4], scalar1=-1.0, scalar2=1e30, op0=ALU.add, op1=ALU.mult),
          reads=["m4"], writes=["m4"])
    P.dve(lambda e: e.tensor_tensor(out=W.ml[:, :, :], in0=L[:, 4:36].rearrange("p (g j) -> p g j", g=4),
                                    in1=W.m4[:, 0:4].unsqueeze(2).to_broadcast([128, 4, 8]), op=ALU.add),
          reads=["L", "m4"], writes=["ml"])
    mlf = W.ml[:, :, :].rearrange("p g j -> p (g j)")
    P.dve(lambda e: e.max(out=W.t8[:, 0:8], in_=mlf), reads=["ml"], writes=["t8"])
    P.dve(lambda e: e.tensor_scalar(out=g.RE1[:, ti, :], in0=mlf, scalar1=W.t8[:, 0:1], scalar2=None, op0=ALU.is_equal),
          reads=["ml", "t8"], writes=["RE1"])
    P.dve(lambda e: e.tensor_scalar(out=g.RE2[:, ti, :], in0=mlf, scalar1=W.t8[:, 1:2], scalar2=None, op0=ALU.is_equal),
          reads=["ml", "t8"], writes=["RE2"])
    P.dve(lambda e: e.tensor_tensor(out=sm[:, 4:5], in0=W.t8[:, 1:2], in1=W.t8[:, 0:1], op=ALU.subtract),
          reads=["t8"], writes=["sm4"])
    P.act(lambda e: e.activation(out=sm[:, 5:6], in_=sm[:, 4:5], func=AF.Exp), reads=["sm4"], writes=["sm5"])
    P.dve(lambda e: e.tensor_scalar(out=sm[:, 6:7], in0=sm[:, 5:6], scalar1=1.0, scalar2=None, op0=ALU.add),
          reads=["sm5"], writes=["sm6"])
    P.dve(lambda e: e.reciprocal(out=sm[:, 7:8], in_=sm[:, 6:7]), reads=["sm6"], writes=["sm7"])
    P.dve(lambda e: e.tensor_tensor(out=g.RG[:, ti, 0:1], in0=sm[:, 7:8], in1=sm[:, 3:4], op=ALU.mult),
          reads=["sm7", "sm3"], writes=["RG"])
    P.dve(lambda e: e.tensor_tensor(out=g.RG[:, ti, 1:2], in0=g.RG[:, ti, 0:1], in1=sm[:, 5:6], op=ALU.mult),
          reads=["RG", "sm5"], writes=["RG"])
    P.dve(lambda e: e.tensor_tensor(out=W.Mb[:, 0:32], in0=g.RE1[:, ti, :], in1=g.RE2[:, ti, :], op=ALU.add),
          reads=["RE1", "RE2"], writes=["Mb"])
    cb = g.cb
    P.pe(lambda e: e.matmul(psR[:, 64:96], lhsT=cb[:, CB_TRIU:CB_TRIU + 128], rhs=W.Mb[:, 0:32], start=True, stop=True),
         reads=["Mb"], writes=["psR"])
    P.pe(lambda e: e.matmul(psR[:, 128:160], lhsT=cb[:, CB_ONES:CB_ONES + 128], rhs=W.Mb[:, 0:32], start=True, stop=True),
         reads=["Mb"], writes=["psR"])
    P.dve(lambda e: e.tensor_tensor(out=g.RPF[:, ti, :], in0=psR[:, 64:96], in1=g.CAR[:, 0:32], op=ALU.add),
          reads=["psR", "CAR"], writes=["RPF"])
    P.dve(lambda e: e.tensor_tensor(out=g.CAR[:, 0:32], in0=psR[:, 128:160], in1=g.CAR[:, 0:32], op=ALU.add),
          reads=["psR", "CAR"], writes=["CAR"])


class _RW:
    pass


def _router_work(ph):
    W = _RW()
    W.x1T = ph.sb([128, 8, 128], F32, "x1T")
    W.L = ph.sb([128, 36], F32, "L")
    W.sm = ph.sb([128, 16], F32, "sm")
    W.ge = ph.sb([128, 4], F32, "ge")
    W.m4 = ph.sb([128, 4], F32, "m4")
    W.ml = ph.sb([128, 4, 8], F32, "ml")
    W.t8 = ph.sb([128, 8], F32, "t8")
    W.Mb = ph.sb([128, 32], BF16, "Mb")
    return W


def _post_mixer(ph, g, ti, r, key_r, W, bufs, psR, psT2, xdst):
    P = ph.P
    x1 = bufs.x1[ti % 2]
    kx1 = "x1_%d" % (ti % 2)
    _ln_token_major(P, POOL, r, x1, g.pb[:, PB_LN1G:PB_LN1G + 1024], g.pb[:, PB_LN1B:PB_LN1B + 1024],
                    bufs.st, bufs.mv, bufs.rs, key_r, kx1, "ln1")
    P.dma(lambda e: e.dma_start(out=xdst[ti * 128:(ti + 1) * 128, :], in_=x1[:, :]), reads=[kx1], writes=["XB_%d" % ti])
    xb = bufs.x1bf[ti % 2]
    kxb = "x1bf_%d" % (0 if bufs.x1bf[0] is bufs.x1bf[1] else ti % 2)
    P.act(lambda e: e.copy(out=xb[:, :].rearrange("t (kk p) -> t kk p", p=128),
                           in_=x1[:, :].rearrange("t (p kk) -> t kk p", kk=8)), reads=[kx1], writes=[kxb])
    P.dma(lambda e: e.dma_start(out=g.xb16[ti * 128:(ti + 1) * 128, :], in_=xb[:, :]), reads=[kxb], writes=["xb16_%d" % ti])
    _router_tile(ph, g, ti, x1, kx1, W, psR, psT2)


def _load_layer_params(ph, g, li):
    P = ph.P
    P.dma(lambda e: e.dma_start(out=g.pb[:, :], in_=g.d_pbln[li]), writes=["PB"])
    P.dma(lambda e: e.dma_start(out=g.wr[:, :, :], in_=g.d_wr[li]), writes=["WR"])
    P.dve(lambda e: e.memset(g.CAR[:, :], 0.0), writes=["CAR"])


def conv_phase(nc, g, li, xsrc, xdst):
    j = li // 2
    ph = Phase(nc, "cv%d" % li)
    P = ph.P
    cf, cb = g.cf, g.cb
    _load_layer_params(ph, g, li)
    W1b = ph.sb([128, 8, 2048], BF16, "W1")
    W2b = ph.sb([128, 8, 1024], BF16, "W2")
    pm = ph.sb([128, 1024], F32, "pm")
    pp = ph.sb([128, PP_N], F32, "pp")
    for q in range(4):
        P.dma(lambda e, q=q: e.dma_start(out=W1b[:, :, q * 512:(q + 1) * 512],
                                         in_=g.d_conv_w_pw1[j].rearrange("(k p) f -> p k f", p=128)[:, :, q * 512:(q + 1) * 512]),
              writes=["W1_%d" % q], q=POOL)
    P.dma(lambda e: e.dma_start(out=W2b[:, :, :], in_=g.d_conv_w_pw2[j].rearrange("(k p) f -> p k f", p=128)),
          writes=["W2"], q=POOL)
    P.dma(lambda e: e.dma_start(out=pm[:, :], in_=g.d_pbmix[li][:, 0:1024]), writes=["PM"])
    P.dma(lambda e: e.dma_start(out=pp[:, :], in_=g.d_pp[j]), writes=["PP"])

    xin = [ph.sb([128, 1024], F32, "xin") for _ in range(2)]
    xbf = ph.sb([128, 1024], BF16, "xbf")
    xT = ph.sb([128, 8, 512], BF16, "xT")
    hg = ph.sb([128, 8, 512 + CW - 1], BF16, "hg")
    NPE = 16
    diag = ph.sb([128, 8 * NPE, 128], BF16, "diag")
    for c in range(8):
        for jj in range(NPE):
            P.act(lambda e, c=c, jj=jj: e.activation(out=diag[:, c * NPE + jj, :], in_=cf[:, CF_IDENT:CF_IDENT + 128], func=AF.Copy,
                                                     scale=pp[:, PP_WDW + c * CW + jj:PP_WDW + c * CW + jj + 1]),
                  reads=["PP", "cf"], writes=["diag"])
    sig = [ph.sb([128, 512], F32, "sig")] * 2
    cv = ph.sb([128, 8, 512], F32, "cv")
    sq = [ph.sb([128, 512], F32, "sq")] * 2
    mean = ph.sb([128, 512], F32, "mean")
    rstd = ph.sb([128, 512], F32, "rstd")
    nmr = ph.sb([128, 512], F32, "nmr")
    hT = ph.sb([128, 8, 512], BF16, "hT")
    rbuf = [ph.sb([128, 1024], F32, "r") for _ in range(2)]
    bufs = _RW()
    bufs.x1 = [ph.sb([128, 1024], F32, "x1") for _ in range(2)]
    bufs.x1bf = [ph.sb([128, 1024], BF16, "x1bf")] * 2
    bufs.st = ph.sb([128, 12], F32, "st")
    bufs.mv = ph.sb([128, 2], F32, "mv")
    bufs.rs = ph.sb([128, 2], F32, "rs")
    RWk = _router_work(ph)
    psT = ph.psum("psT")
    psA = [ph.psum("psA") for _ in range(2)]
    psG = [ph.psum("psG") for _ in range(2)]
    psR = ph.psum("psR")
    psT2 = [ph.psum("psT2") for _ in range(2)]
    psS = [psG[0], psG[1]]
    psT_bf = psT[:, :].bitcast(BF16)

    P.dve(lambda e: e.memset(hg[:, :, 0:CW - 1], 0.0), writes=["hg"])
    conv_eng = [DVE] * 8
    norm_eng = [DVE, POOL, DVE, POOL, DVE, POOL, DVE, POOL]
    for st_i in range(NT // 4):
        for sub in range(4):
            ti = st_i * 4 + sub
            xi = xin[ti % 2]
            kxi = "xin_%d" % (ti % 2)
            P.dma(lambda e, xi=xi, ti=ti: e.dma_start(out=xi[:, :], in_=xsrc[ti * 128:(ti + 1) * 128, :]), writes=[kxi])
            P.act(lambda e, xi=xi: e.copy(out=xbf[:, :], in_=xi[:, :]), reads=[kxi], writes=["xbf"])
            for kc in range(8):
                P.pe(lambda e, kc=kc: e.transpose(out=psT_bf[:, kc * 128:(kc + 1) * 128], in_=xbf[:, kc * 128:(kc + 1) * 128],
                                                  identity=cb[:, CB_IDENT:CB_IDENT + 128]), reads=["xbf"], writes=["psT"])
            P.dve(lambda e, sub=sub: e.tensor_copy(out=xT[:, :, sub * 128:(sub + 1) * 128],
                                                   in_=psT_bf.rearrange("p (k t) -> p k t", k=8)), reads=["psT"], writes=["xT"])
        for c in range(8):
            pa, pg = psA[c % 2], psG[c % 2]
            ka, kg = "psA_%d" % (c % 2), "psG_%d" % (c % 2)
            for kc in range(8):
                P.pe(lambda e, kc=kc, c=c, pa=pa: e.matmul(pa[:, :], lhsT=W1b[:, kc, c * 128:(c + 1) * 128], rhs=xT[:, kc, :],
                                                           start=(kc == 0), stop=(kc == 7)),
                     reads=["xT", "W1_%d" % (c // 4)], writes=[ka])
            for kc in range(8):
                P.pe(lambda e, kc=kc, c=c, pg=pg: e.matmul(pg[:, :], lhsT=W1b[:, kc, 1024 + c * 128:1024 + (c + 1) * 128], rhs=xT[:, kc, :],
                                                           start=(kc == 0), stop=(kc == 7)),
                     reads=["xT", "W1_%d" % (2 + c // 4)], writes=[kg])
            sg = sig[c % 2]
            ks = "sig_0"
            P.act(lambda e, c=c, pg=pg, sg=sg: e.activation(out=sg[:, :], in_=pg[:, :], func=AF.Sigmoid,
                                                            bias=pp[:, PP_B1 + 8 + c:PP_B1 + 9 + c], scale=1.0),
                  reads=[kg, "PP"], writes=[ks])
            P.dve(lambda e, c=c, pa=pa, sg=sg: e.scalar_tensor_tensor(out=hg[:, c, CW - 1:CW - 1 + 512], in0=pa[:, :],
                                                                      scalar=pp[:, PP_B1 + c:PP_B1 + c + 1], in1=sg[:, :],
                                                                      op0=ALU.add, op1=ALU.mult),
                  reads=[ka, ks, "PP"], writes=["hg"])
        cps = [psA[0], psG[0], psA[1], psG[1]]
        cpk = ["psA_0", "psG_0", "psA_1", "psG_1"]
        for c in range(8):
            pc, kpc = cps[c % 4], cpk[c % 4]
            for jj in range(NPE):
                P.pe(lambda e, c=c, jj=jj, pc=pc: e.matmul(pc[:, :], lhsT=diag[:, c * NPE + jj, :], rhs=hg[:, c, jj:jj + 512],
                                                           start=(jj == 0), stop=(jj == NPE - 1)), reads=["hg", "diag"], writes=[kpc])
            P.act(lambda e, c=c, pc=pc: e.activation(out=cv[:, c, :], in_=pc[:, :], func=AF.Identity,
                                                     bias=pp[:, PP_BDW + c:PP_BDW + c + 1], scale=1.0),
                  reads=[kpc, "PP"], writes=["cv_%d" % c])
        for jj in range(NPE, CW):
            for c in range(8):
                P.dve(lambda e, c=c, jj=jj: e.scalar_tensor_tensor(out=cv[:, c, :], in0=hg[:, c, jj:jj + 512],
                                                                   scalar=pp[:, PP_WDW + c * CW + jj:PP_WDW + c * CW + jj + 1],
                                                                   in1=cv[:, c, :], op0=ALU.mult, op1=ALU.add),
                      reads=["hg", "PP", "cv_%d" % c], writes=["cv_%d" % c])
        P.act(lambda e: e.copy(out=hg[:, :, 0:CW - 1], in_=hg[:, :, 512:512 + CW - 1]), reads=["hg"], writes=["hg"])
        for c in range(8):
            s_ = sq[c % 2]
            ksq = "sq_0"
            P.act(lambda e, c=c, s_=s_: e.activation(out=s_[:, :], in_=cv[:, c, :], func=AF.Square), reads=["cv_%d" % c], writes=[ksq])
            P.pe(lambda e, c=c: e.matmul(psS[0][:, :], lhsT=cf[:, CF_ONES:CF_ONES + 128], rhs=cv[:, c, :], start=(c == 0), stop=(c == 7)),
                 reads=["cv_%d" % c], writes=["psG_0"])
            P.pe(lambda e, c=c, s_=s_: e.matmul(psS[1][:, :], lhsT=cf[:, CF_ONES:CF_ONES + 128], rhs=s_[:, :], start=(c == 0), stop=(c == 7)),
                 reads=[ksq], writes=["psG_1"])
        P.act(lambda e: e.mul(out=mean[:, :], in_=psS[0][:, :], mul=1.0 / D), reads=["psG_0"], writes=["mean"])
        P.dve(lambda e: e.tensor_tensor(out=nmr[:, :], in0=mean[:, :], in1=mean[:, :], op=ALU.mult), reads=["mean"], writes=["nmr"])
        P.dve(lambda e: e.scalar_tensor_tensor(out=rstd[:, :], in0=psS[1][:, :], scalar=1.0 / D, in1=nmr[:, :],
                                               op0=ALU.mult, op1=ALU.subtract), reads=["psG_1", "nmr"], writes=["rstd"])
        P.act(lambda e: e.activation(out=rstd[:, :], in_=rstd[:, :], func=AF.Sqrt, bias=EPS_AP[0], scale=1.0),
              reads=["rstd"], writes=["rstd"])
        P.dve(lambda e: e.reciprocal(out=rstd[:, :], in_=rstd[:, :]), reads=["rstd"], writes=["rstd"])
        P.dve(lambda e: e.scalar_tensor_tensor(out=nmr[:, :], in0=mean[:, :], scalar=-1.0, in1=rstd[:, :],
                                               op0=ALU.mult, op1=ALU.mult), reads=["mean", "rstd"], writes=["nmr"])
        for c in range(8):
            eng = norm_eng[c]
            P.ve(eng, lambda e, c=c: e.tensor_tensor(out=cv[:, c, :], in0=cv[:, c, :], in1=rstd[:, :], op=ALU.mult),
                 reads=["cv_%d" % c, "rstd"], writes=["cv_%d" % c])
        for c in range(8):
            eng = norm_eng[c]
            P.ve(eng, lambda e, c=c: e.tensor_tensor(out=cv[:, c, :], in0=cv[:, c, :], in1=nmr[:, :], op=ALU.add),
                 reads=["cv_%d" % c, "nmr"], writes=["cv_%d" % c])
        for c in range(8):
            P.act(lambda e, c=c: e.activation(out=hT[:, c, :], in_=cv[:, c, :], func=AF.Silu,
                                              bias=pp[:, PP_LNB + c:PP_LNB + c + 1], scale=pp[:, PP_LNG + c:PP_LNG + c + 1]),
                  reads=["cv_%d" % c, "PP"], writes=["hT"])
        for sub in range(4):
            ti = st_i * 4 + sub
            xi = xin[ti % 2]
            kxi = "xin_%d" % (ti % 2)
            P.dma(lambda e, xi=xi, ti=ti: e.dma_start(out=xi[:, :], in_=xsrc[ti * 128:(ti + 1) * 128, :]), writes=[kxi])
            r = rbuf[ti % 2]
            kr = "r_%d" % (ti % 2)
            for half in range(2):
                pa = psA[half]
                ka = "psA_%d" % half
                for c in range(8):
                    P.pe(lambda e, c=c, half=half, sub=sub, pa=pa: e.matmul(pa[:, :], lhsT=hT[:, c, sub * 128:(sub + 1) * 128],
                                                                            rhs=W2b[:, c, half * 512:(half + 1) * 512],
                                                                            start=(c == 0), stop=(c == 7)),
                         reads=["hT", "W2"], writes=[ka])
                P.dve(lambda e, half=half, pa=pa, xi=xi, r=r: e.scalar_tensor_tensor(out=r[:, half * 512:(half + 1) * 512],
                                                                                     in0=xi[:, half * 512:(half + 1) * 512], scalar=ALPHA,
                                                                                     in1=pa[:, :], op0=ALU.mult, op1=ALU.add),
                      reads=[ka, kxi], writes=[kr])
            P.pool(lambda e, r=r: e.tensor_tensor(out=r[:, :], in0=r[:, :], in1=pm[:, 0:1024], op=ALU.add), reads=[kr, "PM"], writes=[kr])
            _post_mixer(ph, g, ti, r, kr, RWk, bufs, psR, psT2, xdst)
    ph.finish()


def offsets_scatter_phase(nc, g, li):
    ph = Phase(nc, "os%d" % li)
    P = ph.P
    cf = g.cf
    cnt = g.CAR
    r_ = ph.sb([128, 32], F32, "r")
    nz = ph.sb([128, 32], F32, "nz")
    pad = ph.sb([128, 32], F32, "pad")
    ca = ph.sb([128, 32], F32, "ca")
    cbuf = ph.sb([128, 32], F32, "cb")
    off = ph.sb([128, 32], F32, "off")
    big = ph.sb([128, NT, 32], F32, "big")
    g.tmpbig = ph.sb([128, NT, 32], F32, "tmpbig")
    posf = ph.sb([128, NT, 2], F32, "posf")
    ej = ph.sb([128, NSLOT_T], F32, "ej")
    tmpw = ph.sb([128, NSLOT_T], F32, "tmpw")
    P.dve(lambda e: e.memset(nz[:, :], 0.0), writes=["nz"])
    for m in range(2 * SEQ // SLOT):
        P.dve(lambda e, m=m: e.scalar_tensor_tensor(out=nz[:, :], in0=cnt[:, 0:32], scalar=float(m * SLOT), in1=nz[:, :],
                                                    op0=ALU.is_gt, op1=ALU.add), reads=["CAR", "nz"], writes=["nz"])
    P.dve(lambda e: e.tensor_scalar(out=pad[:, :], in0=nz[:, :], scalar1=float(SLOT), scalar2=None, op0=ALU.mult), reads=["nz"], writes=["pad"])
    src, dst = pad, ca
    ksrc, kdst = "pad", "ca"
    step = 1
    while step < 32:
        P.dve(lambda e, src=src, dst=dst, step=step: e.tensor_copy(out=dst[:, 0:step], in_=src[:, 0:step]), reads=[ksrc], writes=[kdst])
        P.dve(lambda e, src=src, dst=dst, step=step: e.tensor_tensor(out=dst[:, step:32], in0=src[:, step:32], in1=src[:, 0:32 - step], op=ALU.add),
              reads=[ksrc], writes=[kdst])
        if dst is ca:
            src, dst, ksrc, kdst = ca, cbuf, "ca", "cb"
        else:
            src, dst, ksrc, kdst = cbuf, ca, "cb", "ca"
        step *= 2
    cum, kcum = src, ksrc
    P.dve(lambda e: e.tensor_tensor(out=off[:, :], in0=cum[:, :], in1=pad[:, :], op=ALU.subtract), reads=[kcum, "pad"], writes=["off"])
    P.dve(lambda e: e.tensor_tensor(out=big[:, :, :], in0=g.RPF[:, :, :], in1=off[:, :].unsqueeze(1).to_broadcast([128, NT, 32]), op=ALU.add),
          reads=["RPF", "off"], writes=["big"])
    for k, RE, kre in ((0, g.RE1, "RE1"), (1, g.RE2, "RE2")):
        P.dve(lambda e, RE=RE: e.tensor_tensor(out=g.tmpbig[:, :, :], in0=big[:, :, :], in1=RE[:, :, :], op=ALU.mult),
              reads=["big", kre], writes=["tmpbig"])
        P.dve(lambda e, k=k: e.tensor_reduce(out=posf[:, :, k], in_=g.tmpbig[:, :, :], axis=AX.X, op=ALU.add),
              reads=["tmpbig"], writes=["posf"])
    P.dve(lambda e: e.tensor_copy(out=g.POS[:, :, :], in_=posf[:, :, :]), reads=["posf"], writes=["POS"])
    P.dve(lambda e: e.memset(ej[:, :], 0.0), writes=["ej"])
    for ex in range(NE):
        P.dve(lambda e, ex=ex: e.scalar_tensor_tensor(out=ej[:, :], in0=cf[:, CF_THR:CF_THR + NSLOT_T], scalar=cum[:, ex:ex + 1], in1=ej[:, :],
                                                      op0=ALU.is_ge, op1=ALU.add), reads=[kcum, "ej"], writes=["ej"])
    P.dve(lambda e: e.tensor_scalar(out=ej[:, :], in0=ej[:, :], scalar1=float(NE - 1), scalar2=None, op0=ALU.min), reads=["ej"], writes=["ej"])
    P.dve(lambda e: e.tensor_scalar(out=tmpw[:, :], in0=ej[:, :], scalar1=128.0, scalar2=cf[:, CF_IOTA:CF_IOTA + 1], op0=ALU.mult, op1=ALU.add),
          reads=["ej"], writes=["tmpw"])
    P.dve(lambda e: e.tensor_scalar(out=tmpw[:, :], in0=tmpw[:, :], scalar1=float(li * NE * 128), scalar2=None, op0=ALU.add),
          reads=["tmpw"], writes=["tmpw"])
    P.dve(lambda e: e.tensor_copy(out=g.WG[:, :], in_=tmpw[:, :]), reads=["tmpw"], writes=["WG"])
    for c in range(4):
        P.dve(lambda e, c=c: e.tensor_scalar(out=tmpw[:, :], in0=ej[:, :], scalar1=512.0, scalar2=cf[:, CF_IOTA + 1 + c:CF_IOTA + 2 + c],
                                             op0=ALU.mult, op1=ALU.add), reads=["ej"], writes=["tmpw"])
        P.dve(lambda e: e.tensor_scalar(out=tmpw[:, :], in0=tmpw[:, :], scalar1=float(li * NE * FF), scalar2=None, op0=ALU.add),
              reads=["tmpw"], writes=["tmpw"])
        P.dve(lambda e, c=c: e.tensor_copy(out=g.WD[:, c, :], in_=tmpw[:, :]), reads=["tmpw"], writes=["WD"])
    xbt = [ph.sb([128, 1024], BF16, "xbt") for _ in range(4)]
    for ti in range(NT):
        xb = xbt[ti % 4]
        kx = "xbt_%d" % (ti % 4)
        P.dma(lambda e, xb=xb, ti=ti: e.dma_start(out=xb[:, :], in_=g.xb16[ti * 128:(ti + 1) * 128, :]), writes=[kx])
        for k in range(2):
            P.dma(lambda e, xb=xb, ti=ti, k=k: e.indirect_dma_start(out=g.xs, out_offset=bass.IndirectOffsetOnAxis(ap=g.POS[:, ti, k:k + 1], axis=0),
                                                                    in_=xb[:, :], in_offset=None),
                  reads=[kx, "POS"], writes=["xs_%d_%d" % (ti, k)], q=POOL)
    ph.finish()


def expert_phase(nc, g, li, use_dma_transpose=True):
    ph = Phase(nc, "ex%d" % li)
    P = ph.P
    cb = g.cb
    NB = 3
    Wg = [ph.sb([128, 8, FF], BF16, "Wg") for _ in range(NB)]
    Wu = [ph.sb([128, 8, FF], BF16, "Wu") for _ in range(NB)]
    Wd = [ph.sb([128, 4, D], BF16, "Wd") for _ in range(NB)]
    xTp = [ph.sb([128, 8, SLOT], BF16, "xTp") for _ in range(NB)]
    sgt = [ph.sb([128, SLOT], F32, "sg") for _ in range(2)]
    hT = [ph.sb([128, 4, SLOT], BF16, "hT") for _ in range(2)]
    ysb = [ph.sb([128, D], F32, "ysb") for _ in range(4)]
    psg = [ph.psum("psg") for _ in range(2)]
    psu = [ph.psum("psu") for _ in range(2)]
    psy = [ph.psum("psy") for _ in range(4)]
    wgv = g.d_moe_w_gate.rearrange("l e (p kk) f -> (l e p) (kk f)", kk=8)
    wuv = g.d_moe_w_up.rearrange("l e (p kk) f -> (l e p) (kk f)", kk=8)
    wdv = g.d_moe_w_down.rearrange("l e k d -> (l e k) d")

    def load(jt):
        b = jt % NB
        for kk in range(8):
            P.dma(lambda e, b=b, jt=jt, kk=kk: e.dma_start_transpose(out=xTp[b][:, kk, :],
                                                                     in_=g.xs[jt * SLOT:(jt + 1) * SLOT, kk * 128:(kk + 1) * 128]),
                  reads=["xs"], writes=["xTp_%d" % b])
        P.dma(lambda e, b=b, jt=jt: e.indirect_dma_start(out=Wg[b][:, :, :].rearrange("p k f -> p (k f)"), out_offset=None, in_=wgv,
                                                         in_offset=bass.IndirectOffsetOnAxis(ap=g.WG[:, jt:jt + 1], axis=0)),
              reads=["WG"], writes=["Wg_%d" % b], q=POOL)
        P.dma(lambda e, b=b, jt=jt: e.indirect_dma_start(out=Wu[b][:, :, :].rearrange("p k f -> p (k f)"), out_offset=None, in_=wuv,
                                                         in_offset=bass.IndirectOffsetOnAxis(ap=g.WG[:, jt:jt + 1], axis=0)),
              reads=["WG"], writes=["Wu_%d" % b], q=POOL)
        for c in range(4):
            P.dma(lambda e, b=b, jt=jt, c=c: e.indirect_dma_start(out=Wd[b][:, c, :], out_offset=None, in_=wdv,
                                                                  in_offset=bass.IndirectOffsetOnAxis(ap=g.WD[:, c, jt:jt + 1], axis=0)),
                  reads=["WD"], writes=["Wd_%d_%d" % (b, c)], q=POOL)

    for jt in range(min(NB - 1, NSLOT_T)):
        load(jt)
    for jt in range(NSLOT_T):
        b = jt % NB
        if jt + NB - 1 < NSLOT_T:
            load(jt + NB - 1)
        h = hT[jt % 2]
        kh = "hT_%d" % (jt % 2)
        for fc in range(4):
            pg_, pu_ = psg[fc % 2], psu[fc % 2]
            kg, ku = "psg_%d" % (fc % 2), "psu_%d" % (fc % 2)
            for kk in range(8):
                P.pe(lambda e, b=b, kk=kk, fc=fc, pg_=pg_: e.matmul(pg_[:, :], lhsT=Wg[b][:, kk, fc * 128:(fc + 1) * 128], rhs=xTp[b][:, kk, :],
                                                                    start=(kk == 0), stop=(kk == 7)),
                     reads=["Wg_%d" % b, "xTp_%d" % b], writes=[kg])
            for kk in range(8):
                P.pe(lambda e, b=b, kk=kk, fc=fc, pu_=pu_: e.matmul(pu_[:, :], lhsT=Wu[b][:, kk, fc * 128:(fc + 1) * 128], rhs=xTp[b][:, kk, :],
                                                                    start=(kk == 0), stop=(kk == 7)),
                     reads=["Wu_%d" % b, "xTp_%d" % b], writes=[ku])
            sg = sgt[fc % 2]
            ksg = "sgt_%d" % (fc % 2)
            P.act(lambda e, pg_=pg_, sg=sg: e.activation(out=sg[:, :], in_=pg_[:, :], func=AF.Silu), reads=[kg], writes=[ksg])
            P.dve(lambda e, pu_=pu_, sg=sg, h=h, fc=fc: e.tensor_tensor(out=h[:, fc, :], in0=pu_[:, :], in1=sg[:, :], op=ALU.mult),
                  reads=[ku, ksg], writes=[kh])
        for sub in range(4):
            yb = ysb[sub]
            kyb = "ysb_%d" % sub
            for half in range(2):
                pi_ = (sub % 2) * 2 + half
                py = psy[pi_]
                kpy = "psy_%d" % pi_
                for fc in range(4):
                    P.pe(lambda e, b=b, fc=fc, sub=sub, half=half, py=py, h=h: e.matmul(py[:, :], lhsT=h[:, fc, sub * 128:(sub + 1) * 128],
                                                                                      rhs=Wd[b][:, fc, half * 512:(half + 1) * 512],
                                                                                      start=(fc == 0), stop=(fc == 3)),
                         reads=[kh] + ["Wd_%d_%d" % (b, c) for c in range(4)], writes=[kpy])
                if half == 0:
                    P.act(lambda e, py=py, yb=yb: e.copy(out=yb[:, 0:512], in_=py[:, :]), reads=[kpy], writes=[kyb + "a"])
                else:
                    P.dve(lambda e, py=py, yb=yb: e.tensor_copy(out=yb[:, 512:1024], in_=py[:, :]), reads=[kpy], writes=[kyb + "b"])
            P.dma(lambda e, yb=yb, jt=jt, sub=sub: e.dma_start(out=g.ys[jt * SLOT + sub * 128: jt * SLOT + (sub + 1) * 128, :], in_=yb[:, :]),
                  reads=[kyb + "a", kyb + "b"], writes=["ys_%d_%d" % (jt, sub)])
    ph.finish()


def combine_phase(nc, g, li, x1src, xdst):
    ph = Phase(nc, "cm%d" % li)
    P = ph.P
    NB = 3
    y1 = [ph.sb([128, D], F32, "y1") for _ in range(NB)]
    y2 = [ph.sb([128, D], F32, "y2") for _ in range(NB)]
    x1 = [ph.sb([128, D], F32, "x1") for _ in range(NB)]
    x2 = [ph.sb([128, D], F32, "x2") for _ in range(NB)]
    st = ph.sb([128, 12], F32, "st")
    mv = ph.sb([128, 2], F32, "mv")
    rs = ph.sb([128, 2], F32, "rs")

    def load(ti):
        b = ti % NB
        P.dma(lambda e, b=b, ti=ti: e.dma_start(out=x1[b][:, :], in_=x1src[ti * 128:(ti + 1) * 128, :]), reads=["XB"], writes=["x1_%d" % b])
        P.dma(lambda e, b=b, ti=ti: e.indirect_dma_start(out=y1[b][:, :], out_offset=None, in_=g.ys,
                                                         in_offset=bass.IndirectOffsetOnAxis(ap=g.POS[:, ti, 0:1], axis=0)),
              reads=["ys", "POS"], writes=["y1_%d" % b], q=POOL)
        P.dma(lambda e, b=b, ti=ti: e.indirect_dma_start(out=y2[b][:, :], out_offset=None, in_=g.ys,
                                                         in_offset=bass.IndirectOffsetOnAxis(ap=g.POS[:, ti, 1:2], axis=0)),
              reads=["ys", "POS"], writes=["y2_%d" % b], q=POOL)

    for ti in range(NB - 1):
        load(ti)
    for ti in range(NT):
        b = ti % NB
        if ti + NB - 1 < NT:
            load(ti + NB - 1)
        P.act(lambda e, b=b, ti=ti: e.activation(out=y1[b][:, :], in_=y1[b][:, :], func=AF.Copy, scale=g.RG[:, ti, 0:1]),
              reads=["y1_%d" % b, "RG"], writes=["y1_%d" % b])
        P.dve(lambda e, b=b, ti=ti: e.scalar_tensor_tensor(out=y2[b][:, :], in0=y2[b][:, :], scalar=g.RG[:, ti, 1:2], in1=y1[b][:, :],
                                                           op0=ALU.mult, op1=ALU.add), reads=["y2_%d" % b, "y1_%d" % b, "RG"], writes=["y2_%d" % b])
        P.dve(lambda e, b=b: e.scalar_tensor_tensor(out=x1[b][:, :], in0=x1[b][:, :], scalar=ALPHA, in1=y2[b][:, :],
                                                    op0=ALU.mult, op1=ALU.add), reads=["x1_%d" % b, "y2_%d" % b], writes=["x1_%d" % b])
        _ln_token_major(P, DVE, x1[b], x2[b], g.pb[:, PB_LN2G:PB_LN2G + 1024], g.pb[:, PB_LN2B:PB_LN2B + 1024],
                        st, mv, rs, "x1_%d" % b, "x2_%d" % b, "ln2")
        P.dma(lambda e, b=b, ti=ti: e.dma_start(out=xdst[ti * 128:(ti + 1) * 128, :], in_=x2[b][:, :]), reads=["x2_%d" % b], writes=["XOUT_%d" % ti])
    ph.finish()


def build_program(n_layers=DEPTH, stop_after=None, use_dma_transpose=True):
    nc = bass.Bass("TRN2", target_bir_lowering=False)
    g = G()

    def din(name, shape, dt):
        return nc.dram_tensor(name, list(shape), dt, kind="ExternalInput").ap()

    def dscr(name, shape, dt):
        return nc.dram_tensor(name, list(shape), dt, kind="Internal").ap()

    g.d_x = din("x", [SEQ, D], F32)
    g.d_posb = din("posb", [128, SEQ], I32)
    g.d_cf = din("constf", [128, CF_N], F32)
    g.d_cb = din("constb", [128, CB_N], BF16)
    g.d_conv_w_pw1 = din("conv_w_pw1", [2, D, 2 * D], F32)
    g.d_conv_w_pw2 = din("conv_w_pw2", [2, D, D], F32)
    g.d_ret_w_qkvg = din("ret_w_qkvg", [2, D, 6 * D], F32)
    g.d_ret_w_o = din("ret_w_o", [2, 2 * D, D], F32)
    g.d_moe_w_gate = din("moe_w_gate", [DEPTH, NE, D, FF], F32)
    g.d_moe_w_up = din("moe_w_up", [DEPTH, NE, D, FF], F32)
    g.d_moe_w_down = din("moe_w_down", [DEPTH, NE, FF, D], F32)
    g.d_pbln = din("pbln", [DEPTH, 128, PB_N], F32)
    g.d_pbmix = din("pbmix", [DEPTH, 128, PM_N], F32)
    g.d_pp = din("pp", [2, 128, PP_N], F32)
    g.d_wr = din("wr", [DEPTH, 128, 8, 36], F32)
    g.d_out = nc.dram_tensor("out", [SEQ, D], F32, kind="ExternalOutput").ap()
    g.XA = dscr("XA", [SEQ, D], F32)
    g.XB = dscr("XB", [SEQ, D], F32)
    g.xb16 = dscr("xb16", [SEQ, D], BF16)
    g.xs = dscr("xs", [NSLOT_T * SLOT, D], BF16)
    g.ys = dscr("ys", [NSLOT_T * SLOT, D], F32)
    g.QS = dscr("QS", [NT, 128, 8, 128], BF16)
    g.KS = dscr("KS", [NT, 128, 8, 128], BF16)
    g.VS = dscr("VS", [SEQ, 2 * D], BF16)
    g.SG = dscr("SG", [SEQ, 2 * D], BF16)

    with contextlib.ExitStack() as st:
        def sb(name, shape, dt):
            return st.enter_context(nc.sbuf_tensor(name, list(shape), dt))
        g.cf = sb("cf", [128, CF_N], F32)
        g.cb = sb("cb", [128, CB_N], BF16)
        g.pb = sb("pb", [128, PB_N], F32)
        g.wr = sb("wr_sb", [128, 8, 36], F32)
        g.RE1 = sb("RE1", [128, NT, 32], F32)
        g.RE2 = sb("RE2", [128, NT, 32], F32)
        g.RPF = sb("RPF", [128, NT, 32], F32)
        g.RG = sb("RG", [128, NT, 2], F32)
        g.POS = sb("POS", [128, NT, 2], I32)
        g.CAR = sb("CAR", [128, 32], F32)
        g.WG = sb("WG", [128, NSLOT_T], I32)
        g.WD = sb("WD", [128, 4, NSLOT_T], I32)

        EPS_AP[0] = g.cf[:, CF_EPS:CF_EPS + 1]
        ph = Phase(nc, "init")
        ph.P.dma(lambda e: e.dma_start(out=g.cf[:, :], in_=g.d_cf), writes=["cf"])
        ph.P.dma(lambda e: e.dma_start(out=g.cb[:, :], in_=g.d_cb), writes=["cb"])
        ph.finish()

        xcur = g.d_x
        for li in range(n_layers):
            if li % 2 == 0:
                conv_phase(nc, g, li, xcur, g.XB)
            else:
                retention_phase(nc, g, li, xcur, g.XB)
            if stop_after == ("mix", li):
                _copy_out(nc, g, g.XB)
                break
            offsets_scatter_phase(nc, g, li)
            expert_phase(nc, g, li, use_dma_transpose)
            last = (li == n_layers - 1)
            xnext = g.d_out if last else g.XA
            combine_phase(nc, g, li, g.XB, xnext)
            xcur = xnext
    return nc


def _copy_out(nc, g, src):
    ph = Phase(nc, "cpy")
    t = [ph.sb([128, D], F32, "t") for _ in range(2)]
    for ti in range(NT):
        b = ti % 2
        ph.P.dma(lambda e, b=b, ti=ti: e.dma_start(out=t[b][:, :], in_=src[ti * 128:(ti + 1) * 128, :]), reads=["src"], writes=["t%d" % b])
        ph.P.dma(lambda e, b=b, ti=ti: e.dma_start(out=g.d_out[ti * 128:(ti + 1) * 128, :], in_=t[b][:, :]), reads=["t%d" % b], writes=["o%d" % ti])
    ph.finish()


TWO_PI_HI = 6.28125
TWO_PI_LO = 2.0 * np.pi - 6.28125
PI = float(np.pi)


def retention_phase(nc, g, li, xsrc, xdst):
    _ret_pass1(nc, g, li, xsrc)
    _ret_pass2(nc, g, li, xsrc, xdst)


def _ret_pass1(nc, g, li, xsrc):
    j = li // 2
    ph = Phase(nc, "rp%d" % li)
    P = ph.P
    cf, cb = g.cf, g.cb
    _load_layer_params(ph, g, li)
    Wq = ph.sb([128, 8, 6 * D], BF16, "Wq")
    wsrc = g.d_ret_w_qkvg[j].rearrange("(k p) f -> p k f", p=128)
    for q in range(12):
        P.dma(lambda e, q=q: e.dma_start(out=Wq[:, :, q * 512:(q + 1) * 512], in_=wsrc[:, :, q * 512:(q + 1) * 512]),
              writes=["Wq_%d" % q], q=POOL)
    posi = ph.sb([128, 512], I32, "posi")
    posf = ph.sb([128, 512], F32, "posf")
    xin = [ph.sb([128, D], F32, "xin") for _ in range(2)]
    xbf = ph.sb([128, D], BF16, "xbf")
    xT = ph.sb([128, 8, 512], BF16, "xT")
    ang = ph.sb([128, 512], F32, "ang")
    ni = ph.sb([128, 512], I32, "ni")
    nf = ph.sb([128, 512], F32, "nf")
    rr = ph.sb([128, 512], F32, "rr")
    cc = ph.sb([128, 512], F32, "cc")
    mm = ph.sb([128, 512], F32, "mm")
    sinT = ph.sb([128, 512], F32, "sinT")
    cosT = ph.sb([128, 512], F32, "cosT")
    ta = [ph.sb([128, 512], F32, "ta")] * 2
    tb = [ph.sb([128, 512], F32, "tb")] * 2
    tc_ = [ph.sb([128, 512], F32, "tc")] * 2
    td = [ph.sb([128, 512], F32, "td")] * 2
    qkT = ph.sb([128, 16, 512], BF16, "qkT")
    vrow = [ph.sb([128, 2 * D], BF16, "vrow")] * 2
    srow = [ph.sb([128, 2 * D], BF16, "srow")] * 2
    psT = ph.psum("psT")
    psT_bf = psT[:, :].bitcast(BF16)
    psq = [ph.psum("psq") for _ in range(4)]
    psv = [ph.psum("psv") for _ in range(2)]
    invf = cf[:, CF_INVF:CF_INVF + 1]
    for st_i in range(NT // 4):
        t0 = st_i * 512
        for sub in range(4):
            ti = st_i * 4 + sub
            xi = xin[ti % 2]
            kxi = "xin_%d" % (ti % 2)
            P.dma(lambda e, xi=xi, ti=ti: e.dma_start(out=xi[:, :], in_=xsrc[ti * 128:(ti + 1) * 128, :]), writes=[kxi])
            P.act(lambda e, xi=xi: e.copy(out=xbf[:, :], in_=xi[:, :]), reads=[kxi], writes=["xbf"])
            for kc in range(8):
                P.pe(lambda e, kc=kc: e.transpose(out=psT_bf[:, kc * 128:(kc + 1) * 128], in_=xbf[:, kc * 128:(kc + 1) * 128],
                                                  identity=cb[:, CB_IDENT:CB_IDENT + 128]), reads=["xbf"], writes=["psT"])
            P.dve(lambda e, sub=sub: e.tensor_copy(out=xT[:, :, sub * 128:(sub + 1) * 128],
                                                   in_=psT_bf.rearrange("p (k t) -> p k t", k=8)), reads=["psT"], writes=["xT"])
        for sub in range(4):
            ti = st_i * 4 + sub
            vr, sr = vrow[ti % 2], srow[ti % 2]
            kv, ks = "vrow_0", "srow_0"
            for cbk in range(8):
                pv = psv[cbk % 2]
                kpv = "psv_%d" % (cbk % 2)
                c0 = 2048 + cbk * 512
                for kc in range(8):
                    P.pe(lambda e, kc=kc, c0=c0, pv=pv, sub=sub: e.matmul(pv[:, :], lhsT=xT[:, kc, sub * 128:(sub + 1) * 128], rhs=Wq[:, kc, c0:c0 + 512],
                                                                          start=(kc == 0), stop=(kc == 7)),
                         reads=["xT", "Wq_%d" % (c0 // 512)], writes=[kpv])
                if cbk < 4:
                    P.dve(lambda e, pv=pv, vr=vr, cbk=cbk: e.tensor_copy(out=vr[:, cbk * 512:(cbk + 1) * 512], in_=pv[:, :]), reads=[kpv], writes=[kv])
                else:
                    P.act(lambda e, pv=pv, sr=sr, cbk=cbk: e.activation(out=sr[:, (cbk - 4) * 512:(cbk - 3) * 512], in_=pv[:, :], func=AF.Silu),
                          reads=[kpv], writes=[ks])
            P.dma(lambda e, vr=vr, ti=ti: e.dma_start(out=g.VS[ti * 128:(ti + 1) * 128, :], in_=vr[:, :]), reads=[kv], writes=["VS_%d" % ti])
            P.dma(lambda e, sr=sr, ti=ti: e.dma_start(out=g.SG[ti * 128:(ti + 1) * 128, :], in_=sr[:, :]), reads=[ks], writes=["SG_%d" % ti])
        P.dma(lambda e, t0=t0: e.dma_start(out=posi[:, :], in_=g.d_posb[:, t0:t0 + 512]), writes=["posi"])
        P.dve(lambda e: e.tensor_copy(out=posf[:, :], in_=posi[:, :]), reads=["posi"], writes=["posf"])
        P.dve(lambda e: e.tensor_scalar(out=ang[:, :], in0=posf[:, :], scalar1=invf, scalar2=None, op0=ALU.mult),
               reads=["posf"], writes=["ang"])
        P.dve(lambda e: e.tensor_scalar(out=ni[:, :], in0=ang[:, :], scalar1=float(1.0 / (2.0 * np.pi)), scalar2=None, op0=ALU.mult),
               reads=["ang"], writes=["ni"])
        P.dve(lambda e: e.tensor_copy(out=nf[:, :], in_=ni[:, :]), reads=["ni"], writes=["nf"])
        P.dve(lambda e: e.scalar_tensor_tensor(out=rr[:, :], in0=nf[:, :], scalar=-TWO_PI_HI, in1=ang[:, :], op0=ALU.mult, op1=ALU.add),
              reads=["nf", "ang"], writes=["rr"])
        P.dve(lambda e: e.scalar_tensor_tensor(out=rr[:, :], in0=nf[:, :], scalar=-TWO_PI_LO, in1=rr[:, :], op0=ALU.mult, op1=ALU.add),
              reads=["nf", "rr"], writes=["rr"])
        P.dve(lambda e: e.tensor_scalar(out=mm[:, :], in0=rr[:, :], scalar1=PI, scalar2=-2.0 * PI, op0=ALU.is_gt, op1=ALU.mult),
               reads=["rr"], writes=["mm"])
        P.dve(lambda e: e.tensor_tensor(out=rr[:, :], in0=rr[:, :], in1=mm[:, :], op=ALU.add), reads=["rr", "mm"], writes=["rr"])
        P.dve(lambda e: e.tensor_scalar(out=mm[:, :], in0=rr[:, :], scalar1=-PI, scalar2=2.0 * PI, op0=ALU.is_lt, op1=ALU.mult),
               reads=["rr"], writes=["mm"])
        P.dve(lambda e: e.tensor_tensor(out=rr[:, :], in0=rr[:, :], in1=mm[:, :], op=ALU.add), reads=["rr", "mm"], writes=["rr"])
        P.dve(lambda e: e.tensor_scalar(out=cc[:, :], in0=rr[:, :], scalar1=0.5 * PI, scalar2=None, op0=ALU.add), reads=["rr"], writes=["cc"])
        P.dve(lambda e: e.tensor_scalar(out=mm[:, :], in0=cc[:, :], scalar1=PI, scalar2=-2.0 * PI, op0=ALU.is_gt, op1=ALU.mult),
               reads=["cc"], writes=["mm"])
        P.dve(lambda e: e.tensor_tensor(out=cc[:, :], in0=cc[:, :], in1=mm[:, :], op=ALU.add), reads=["cc", "mm"], writes=["cc"])
        P.act(lambda e: e.activation(out=sinT[:, :], in_=rr[:, :], func=AF.Sin), reads=["rr"], writes=["sinT"])
        P.act(lambda e: e.activation(out=cosT[:, :], in_=cc[:, :], func=AF.Sin), reads=["cc"], writes=["cosT"])
        for hp in range(8):
            b2 = hp % 2
            p1, p2 = psq[2 * b2], psq[2 * b2 + 1]
            k1, k2 = "psq_%d" % (2 * b2), "psq_%d" % (2 * b2 + 1)
            for half, pq, kq in ((0, p1, k1), (1, p2, k2)):
                c0 = (2 * hp + half) * 128
                for kc in range(8):
                    P.pe(lambda e, kc=kc, c0=c0, pq=pq: e.matmul(pq[:, :], lhsT=Wq[:, kc, c0:c0 + 128], rhs=xT[:, kc, :],
                                                                 start=(kc == 0), stop=(kc == 7)),
                         reads=["xT", "Wq_%d" % (c0 // 512)], writes=[kq])
            a_, b_, c_, d_ = ta[b2], tb[b2], tc_[b2], td[b2]
            sfx = "_0"
            P.dve(lambda e, p1=p1, a_=a_: e.tensor_tensor(out=a_[:, :], in0=p1[:, :], in1=cosT[:, :], op=ALU.mult), reads=[k1, "cosT"], writes=["ta" + sfx])
            P.dve(lambda e, p2=p2, b_=b_: e.tensor_tensor(out=b_[:, :], in0=p2[:, :], in1=sinT[:, :], op=ALU.mult), reads=[k2, "sinT"], writes=["tb" + sfx])
            P.dve(lambda e, p1=p1, c_=c_: e.tensor_tensor(out=c_[:, :], in0=p1[:, :], in1=sinT[:, :], op=ALU.mult), reads=[k1, "sinT"], writes=["tc" + sfx])
            P.dve(lambda e, p2=p2, d_=d_: e.tensor_tensor(out=d_[:, :], in0=p2[:, :], in1=cosT[:, :], op=ALU.mult), reads=[k2, "cosT"], writes=["td" + sfx])
            P.pool(lambda e, hp=hp, a_=a_, b_=b_: e.tensor_tensor(out=qkT[:, 2 * hp, :], in0=a_[:, :], in1=b_[:, :], op=ALU.subtract),
                   reads=["ta" + sfx, "tb" + sfx], writes=["qkT"])
            P.pool(lambda e, hp=hp, c_=c_, d_=d_: e.tensor_tensor(out=qkT[:, 2 * hp + 1, :], in0=c_[:, :], in1=d_[:, :], op=ALU.add),
                   reads=["tc" + sfx, "td" + sfx], writes=["qkT"])
        for sub in range(4):
            ti = st_i * 4 + sub
            P.dma(lambda e, ti=ti, sub=sub: e.dma_start(out=g.QS[ti], in_=qkT[:, 0:8, sub * 128:(sub + 1) * 128]), reads=["qkT"], writes=["QS_%d" % ti])
            P.dma(lambda e, ti=ti, sub=sub: e.dma_start(out=g.KS[ti], in_=qkT[:, 8:16, sub * 128:(sub + 1) * 128]), reads=["qkT"], writes=["KS_%d" % ti])
    ph.finish()


def _ret_pass2(nc, g, li, xsrc, xdst):
    j = li // 2
    ph = Phase(nc, "rq%d" % li)
    P = ph.P
    cf, cb = g.cf, g.cb
    Wo = ph.sb([128, 16, D], BF16, "Wo")
    wsrc = g.d_ret_w_o[j].rearrange("(k p) f -> p k f", p=128)
    for q in range(2):
        P.dma(lambda e, q=q: e.dma_start(out=Wo[:, q * 8:(q + 1) * 8, :], in_=wsrc[:, q * 8:(q + 1) * 8, :]), writes=["Wo"], q=POOL)
    pm = ph.sb([128, PM_N], F32, "pm")
    P.dma(lambda e: e.dma_start(out=pm[:, :], in_=g.d_pbmix[li]), writes=["PM"])
    qT = [ph.sb([128, 8, 128], BF16, "qT") for _ in range(2)]
    kT = [ph.sb([128, 8, 128], BF16, "kT") for _ in range(2)]
    vrow_ = [ph.sb([128, 2 * D], BF16, "vrow") for _ in range(2)]
    srow_ = [ph.sb([128, 2 * D], BF16, "srow") for _ in range(2)]
    xin = [ph.sb([128, D], F32, "xin") for _ in range(2)]
    S = ph.sb([128, RH, 2, 512], F32, "S")
    Sbf = ph.sb([128, RH, 2, 512], BF16, "Sbf")
    qxT = ph.sb([128, 8, 128], BF16, "qxT")
    kz = ph.sb([128, D], BF16, "kz")
    PT = [ph.sb([128, 128], BF16, "PT") for _ in range(2)]
    on = [ph.sb([128, 512], F32, "on") for _ in range(2)]
    y = ph.sb([128, 2 * D], BF16, "y")
    yT = ph.sb([128, 16, 128], BF16, "yT")
    rbuf = [ph.sb([128, D], F32, "r") for _ in range(2)]
    gst_ = ph.sb([128, RH, 6], F32, "gst")
    gmv_ = ph.sb([128, RH, 2], F32, "gmv")
    grs_ = ph.sb([128, RH, 1], F32, "grs")
    gnm_ = ph.sb([128, RH, 1], F32, "gnm")
    bufs = _RW()
    bufs.x1 = [ph.sb([128, D], F32, "x1") for _ in range(2)]
    bufs.x1bf = [ph.sb([128, D], BF16, "x1bf") for _ in range(2)]
    bufs.st = ph.sb([128, 12], F32, "st")
    bufs.mv = ph.sb([128, 2], F32, "mv")
    bufs.rs = ph.sb([128, 2], F32, "rs")
    RWk = _router_work(ph)
    pss = ph.psum("pss")
    pso = [ph.psum("pso") for _ in range(2)]
    pst = [ph.psum("pst") for _ in range(2)]
    psX = [ph.psum("psX") for _ in range(2)]
    psR = ph.psum("psR")
    psX_bf = [p[:, :].bitcast(BF16) for p in psX]
    P.dve(lambda e: e.memset(S[:, :, :, :], 0.0), writes=["S"])
    P.dve(lambda e: e.memset(Sbf[:, :, :, :], 0.0), writes=["Sbf"])
    def load2(ti):
        b = ti % 2
        P.dma(lambda e, b=b, ti=ti: e.dma_start(out=qT[b][:, :, :], in_=g.QS[ti]), writes=["qT_%d" % b])
        P.dma(lambda e, b=b, ti=ti: e.dma_start(out=kT[b][:, :, :], in_=g.KS[ti]), writes=["kT_%d" % b])
        P.dma(lambda e, b=b, ti=ti: e.dma_start(out=vrow_[b][:, :], in_=g.VS[ti * 128:(ti + 1) * 128, :]), writes=["vrow_%d" % b])
        P.dma(lambda e, b=b, ti=ti: e.dma_start(out=srow_[b][:, :], in_=g.SG[ti * 128:(ti + 1) * 128, :]), writes=["srow_%d" % b])
        P.dma(lambda e, b=b, ti=ti: e.dma_start(out=xin[b][:, :], in_=xsrc[ti * 128:(ti + 1) * 128, :]), writes=["xin_%d" % b])

    load2(0)
    for ti in range(NT):
        b = ti % 2
        kq, kk_ = "qT_%d" % b, "kT_%d" % b
        xi = xin[b]
        kxi = "xin_%d" % b
        vrow, srow = vrow_[b], srow_[b]
        kvr, ksr = "vrow_%d" % b, "srow_%d" % b
        if ti + 1 < NT:
            load2(ti + 1)
        P.dve(lambda e, b=b: e.tensor_tensor(out=qxT[:, :, :], in0=qT[b][:, :, :],
                                             in1=cb[:, CB_XI:CB_XI + 1024].rearrange("p (k t) -> p k t", k=8), op=ALU.mult),
              reads=[kq], writes=["qxT"])
        for kc in range(8):
            P.pe(lambda e, b=b, kc=kc: e.transpose(out=psX_bf[0][:, kc * 128:(kc + 1) * 128], in_=kT[b][:, kc, :],
                                                   identity=cb[:, CB_IDENT:CB_IDENT + 128]), reads=[kk_], writes=["psT2_0"])
        for h in range(RH):
            P.act(lambda e, h=h: e.activation(out=kz[:, h * 256:(h + 1) * 256], in_=psX_bf[0][:, h * 256:(h + 1) * 256], func=AF.Copy,
                                              scale=cf[:, CF_ZETA + h:CF_ZETA + h + 1]), reads=["psT2_0"], writes=["kz"])
        for h in range(RH):
            pb2 = h % 2
            po = pso[pb2]
            kpo = "pso_%d" % pb2
            for c in range(2):
                P.pe(lambda e, b=b, h=h, c=c: e.matmul(pss[:, (h % 4) * 128:(h % 4 + 1) * 128], lhsT=kT[b][:, 2 * h + c, :], rhs=qT[b][:, 2 * h + c, :],
                                                       start=(c == 0), stop=(c == 1)), reads=[kq, kk_], writes=["pss_%d" % h])
            pt = PT[pb2]
            kpt = "PT_%d" % pb2
            P.dve(lambda e, h=h, pt=pt: e.tensor_tensor(out=pt[:, :], in0=pss[:, (h % 4) * 128:(h % 4 + 1) * 128],
                                                        in1=cf[:, CF_MT + h * 128:CF_MT + (h + 1) * 128], op=ALU.mult),
                  reads=["pss_%d" % h], writes=[kpt])
            P.pe(lambda e, h=h, pt=pt, po=po, vrow=vrow: e.matmul(po[:, :], lhsT=pt[:, :], rhs=vrow[:, h * 512:(h + 1) * 512], start=True, stop=False),
                 reads=[kpt, kvr], writes=[kpo])
            for c in range(2):
                P.pe(lambda e, h=h, c=c, po=po: e.matmul(po[:, :], lhsT=qxT[:, 2 * h + c, :], rhs=Sbf[:, h, c, :], start=False, stop=(c == 1)),
                     reads=["qxT", "Sbf_%d" % h], writes=[kpo])
            for c in range(2):
                P.pe(lambda e, h=h, c=c, vrow=vrow: e.matmul(pst[c][:, :], lhsT=kz[:, h * 256 + c * 128:h * 256 + (c + 1) * 128], rhs=vrow[:, h * 512:(h + 1) * 512],
                                                  start=True, stop=True), reads=["kz", kvr], writes=["pst_%d" % c])
                P.dve(lambda e, h=h, c=c: e.scalar_tensor_tensor(out=S[:, h, c, :], in0=S[:, h, c, :], scalar=float(GAMMA[h] ** 128.0), in1=pst[c][:, :],
                                                                 op0=ALU.mult, op1=ALU.add), reads=["pst_%d" % c, "S"], writes=["S"])
                P.act(lambda e, h=h, c=c: e.copy(out=Sbf[:, h, c, :], in_=S[:, h, c, :]), reads=["S"], writes=["Sbf_%d" % h])
            gst, gmv, grs, gnm = gst_[:, h, :], gmv_[:, h, :], grs_[:, h, :], gnm_[:, h, :]
            kh_ = "_%d" % h
            P.dve(lambda e, po=po, gst=gst: e.bn_stats(out=gst[:, 0:6], in_=po[:, :]), reads=[kpo], writes=["gst" + kh_])
            P.dve(lambda e, gst=gst, gmv=gmv: e.bn_aggr(out=gmv[:, 0:2], in_=gst[:, 0:6]), reads=["gst" + kh_], writes=["gmv" + kh_])
            P.act(lambda e, grs=grs, gmv=gmv: e.activation(out=grs[:, 0:1], in_=gmv[:, 1:2], func=AF.Sqrt, bias=EPS_AP[0], scale=1.0),
                  reads=["gmv" + kh_], writes=["grs" + kh_])
            P.dve(lambda e, grs=grs: e.reciprocal(out=grs[:, 0:1], in_=grs[:, 0:1]), reads=["grs" + kh_], writes=["grs" + kh_])
            P.dve(lambda e, gnm=gnm, gmv=gmv, grs=grs: e.scalar_tensor_tensor(out=gnm[:, 0:1], in0=gmv[:, 0:1], scalar=-1.0, in1=grs[:, 0:1],
                                                                              op0=ALU.mult, op1=ALU.mult),
                  reads=["gmv" + kh_, "grs" + kh_], writes=["gnm" + kh_])
            o_ = on[pb2]
            kon = "on_%d" % pb2
            P.act(lambda e, po=po, o_=o_, gnm=gnm, grs=grs: e.activation(out=o_[:, :], in_=po[:, :], func=AF.Identity, bias=gnm[:, 0:1], scale=grs[:, 0:1]),
                  reads=[kpo, "grs" + kh_, "gnm" + kh_], writes=[kon])
            P.dve(lambda e, h=h, o_=o_: e.tensor_tensor(out=o_[:, :], in0=o_[:, :], in1=pm[:, h * 512:(h + 1) * 512], op=ALU.mult),
                  reads=[kon, "PM"], writes=[kon])
            P.dve(lambda e, h=h, o_=o_: e.tensor_tensor(out=o_[:, :], in0=o_[:, :], in1=pm[:, 2 * D + h * 512:2 * D + (h + 1) * 512], op=ALU.add),
                  reads=[kon, "PM"], writes=[kon])
            P.dve(lambda e, h=h, o_=o_, srow=srow: e.tensor_tensor(out=y[:, h * 512:(h + 1) * 512], in0=o_[:, :], in1=srow[:, h * 512:(h + 1) * 512], op=ALU.mult),
                  reads=[kon, ksr], writes=["y"])
        for half in range(2):
            for q in range(8):
                kc = half * 8 + q
                P.pe(lambda e, kc=kc, q=q, half=half: e.transpose(out=psX_bf[half][:, q * 128:(q + 1) * 128], in_=y[:, kc * 128:(kc + 1) * 128],
                                                                  identity=cb[:, CB_IDENT:CB_IDENT + 128]), reads=["y"], writes=["psT2_%d" % half])
            if half == 0:
                P.act(lambda e: e.copy(out=yT[:, 0:8, :], in_=psX_bf[0].rearrange("p (k t) -> p k t", k=8)), reads=["psT2_0"], writes=["yT"])
            else:
                P.dve(lambda e: e.tensor_copy(out=yT[:, 8:16, :], in_=psX_bf[1].rearrange("p (k t) -> p k t", k=8)), reads=["psT2_1"], writes=["yT"])
        r = rbuf[b]
        kr = "r_%d" % b
        for half in range(2):
            po = pso[half]
            kpo = "pso_%d" % half
            for kc in range(16):
                P.pe(lambda e, kc=kc, half=half, po=po: e.matmul(po[:, :], lhsT=yT[:, kc, :], rhs=Wo[:, kc, half * 512:(half + 1) * 512],
                                                                 start=(kc == 0), stop=(kc == 15)), reads=["yT", "Wo"], writes=[kpo])
            P.dve(lambda e, half=half, po=po, xi=xi, r=r: e.scalar_tensor_tensor(out=r[:, half * 512:(half + 1) * 512], in0=xi[:, half * 512:(half + 1) * 512],
                                                                                 scalar=ALPHA, in1=po[:, :], op0=ALU.mult, op1=ALU.add),
                  reads=[kpo, kxi], writes=[kr])
        if ti > 0:
            _post_mixer(ph, g, ti - 1, rbuf[(ti - 1) % 2], "r_%d" % ((ti - 1) % 2), RWk, bufs, psR, psX, xdst)
    _post_mixer(ph, g, NT - 1, rbuf[(NT - 1) % 2], "r_%d" % ((NT - 1) % 2), RWk, bufs, psR, psX, xdst)
    ph.finish()


def _rep(v, n=128):
    return np.ascontiguousarray(np.broadcast_to(np.asarray(v, np.float32).reshape(1, -1), (n, np.asarray(v).size)))


def prepare_shared(inputs):
    cf, cb, _ = _host_consts()
    sh = {"constf": cf, "constb": cb}
    for k in ("conv_w_pw1", "conv_w_pw2", "ret_w_qkvg", "ret_w_o", "moe_w_gate", "moe_w_up", "moe_w_down"):
        sh[k] = np.ascontiguousarray(inputs[k], dtype=np.float32)
    pbln = np.zeros((DEPTH, 128, PB_N), np.float32)
    pbmix = np.zeros((DEPTH, 128, PM_N), np.float32)
    wr = np.zeros((DEPTH, 128, 8, 36), np.float32)
    for i in range(DEPTH):
        pbln[i, :, PB_LN1G:PB_LN1G + D] = _rep(inputs["ln1_g"][i])
        pbln[i, :, PB_LN1B:PB_LN1B + D] = _rep(inputs["ln1_b"][i])
        pbln[i, :, PB_LN2G:PB_LN2G + D] = _rep(inputs["ln2_g"][i])
        pbln[i, :, PB_LN2B:PB_LN2B + D] = _rep(inputs["ln2_b"][i])
        pbln[i, :, PB_RB:PB_RB + 4] = _rep(inputs["moe_b_grp"][i])
        pbln[i, :, PB_RB + 4:PB_RB + 36] = _rep(inputs["moe_b_route"][i])
        wcat = np.concatenate([inputs["moe_w_grp"][i], inputs["moe_w_route"][i]], axis=1)
        wr[i] = wcat.reshape(8, 128, 36).transpose(1, 0, 2)
        j = i // 2
        if i % 2 == 0:
            pbmix[i, :, 0:D] = _rep(inputs["conv_b_pw2"][j])
        else:
            pbmix[i, :, 0:2 * D] = _rep(inputs["ret_gn_g"][j])
            pbmix[i, :, 2 * D:4 * D] = _rep(inputs["ret_gn_b"][j])
    pp = np.zeros((2, 128, PP_N), np.float32)
    for j in range(2):
        pp[j, :, PP_B1:PP_B1 + 16] = np.asarray(inputs["conv_b_pw1"][j]).reshape(16, 128).T
        wdw = np.asarray(inputs["conv_w_dw"][j])
        pp[j, :, PP_WDW:PP_WDW + 248] = wdw.reshape(CW, 8, 128).transpose(2, 1, 0).reshape(128, 248)
        pp[j, :, PP_BDW:PP_BDW + 8] = np.asarray(inputs["conv_b_dw"][j]).reshape(8, 128).T
        pp[j, :, PP_LNG:PP_LNG + 8] = np.asarray(inputs["conv_ln_g"][j]).reshape(8, 128).T
        pp[j, :, PP_LNB:PP_LNB + 8] = np.asarray(inputs["conv_ln_b"][j]).reshape(8, 128).T
    sh["pbln"], sh["pbmix"], sh["pp"], sh["wr"] = pbln, pbmix, pp, wr
    return sh


_NC_CACHE = {}


def kernel(**inputs):
    x = np.asarray(inputs["x"], np.float32)
    pos = np.asarray(inputs["positions"], np.int32)
    sh = prepare_shared(inputs)
    if "nc" not in _NC_CACHE:
        _NC_CACHE["nc"] = build_program()
    nc = _NC_CACHE["nc"]
    in_maps = []
    for c in range(8):
        m = dict(sh)
        m["x"] = np.ascontiguousarray(x[c])
        m["posb"] = np.ascontiguousarray(np.broadcast_to(pos[c][None, :], (128, SEQ)))
        in_maps.append(m)
    res = run_bass_kernel_spmd(nc, in_maps, core_ids=list(range(8)))
    return np.stack([np.asarray(r["out"], np.float32) for r in res.results], axis=0)
```

```python
import contextlib
import numpy as np
import ml_dtypes
import concourse.bass as bass
import concourse.mybir as mybir
from concourse.bass_utils import run_bass_kernel_spmd

F32 = mybir.dt.float32
BF16 = mybir.dt.bfloat16
I32 = mybir.dt.int32
ALU = mybir.AluOpType
AF = mybir.ActivationFunctionType
AX = mybir.AxisListType

PE, ACT, DVE, POOL, SP = "pe", "act", "dve", "pool", "sp"
ENGS = (PE, ACT, DVE, POOL, SP)

D = 1024
SEQ = 4096
NT = SEQ // 128
DEPTH = 4
NE = 32
FF = 512
SLOT = 512
NSLOT_T = 2 * SEQ // SLOT + NE
ALPHA = (2.0 * DEPTH) ** 0.25
EPS = 1e-5
CW = 31
NPE_TAPS = 22
RH = 4


class _Op:
    __slots__ = ("eng", "fn", "deps", "dma", "sig", "dsem", "dval", "dprev", "pos", "need_sig")


class Prog:
    def __init__(self, nc, n_dma_sems=8, same_eng_dist=3):
        self.nc = nc
        self.ops = []
        self.last_writer = {}
        self.readers = {}
        self.eng_count = {e: 0 for e in ENGS}
        self.n_dma_sems = n_dma_sems
        self.same_eng_dist = same_eng_dist

    def add(self, eng, fn, reads=(), writes=(), dma=False):
        op = _Op()
        op.eng, op.fn, op.dma = eng, fn, dma
        op.sig = None
        op.need_sig = False
        op.pos = self.eng_count[eng]
        self.eng_count[eng] += 1
        idx = len(self.ops)
        deps = set()
        for k in reads:
            w = self.last_writer.get(k)
            if w is not None:
                deps.add((w, True))
        for k in writes:
            w = self.last_writer.get(k)
            if w is not None:
                deps.add((w, False))
            for r in self.readers.get(k, ()):
                if r != idx:
                    deps.add((r, False))
        for k in reads:
            self.readers.setdefault(k, []).append(idx)
        for k in writes:
            self.last_writer[k] = idx
            self.readers[k] = []
        real = {}
        for d, raw in deps:
            dop = self.ops[d]
            if dop.dma or dop.eng != eng or dma:
                real[d] = True
            elif raw and eng != PE and (op.pos - dop.pos) < self.same_eng_dist:
                real[d] = True
        latest = {}
        keep = []
        for d in real:
            dop = self.ops[d]
            if dop.dma:
                keep.append(d)
            elif d > latest.get(dop.eng, -1):
                latest[dop.eng] = d
        op.deps = sorted(keep + list(latest.values()))
        for d in op.deps:
            if not self.ops[d].dma:
                self.ops[d].need_sig = True
        self.ops.append(op)
        return idx

    def pe(self, fn, reads=(), writes=()):
        return self.add(PE, fn, reads, writes)

    def act(self, fn, reads=(), writes=()):
        return self.add(ACT, fn, reads, writes)

    def dve(self, fn, reads=(), writes=()):
        return self.add(DVE, fn, reads, writes)

    def pool(self, fn, reads=(), writes=()):
        return self.add(POOL, fn, reads, writes)

    def ve(self, eng, fn, reads=(), writes=()):
        return self.add(eng, fn, reads, writes)

    def dma(self, fn, reads=(), writes=(), q=SP):
        return self.add(q, fn, reads, writes, dma=True)

    def emit(self):
        nc = self.nc
        ops = self.ops
        pool = _sem_pool(nc, self.n_dma_sems)
        sigc = dict(pool["eval"])
        sig0 = dict(pool["eval"])
        dcount = dict(pool["dcount"])
        dsem_val = dict(pool["dval"])
        used_d = set()
        last_of = {}
        for i, op in enumerate(ops):
            last_of[op.eng] = i
        for e, i in last_of.items():
            if not ops[i].dma:
                ops[i].need_sig = True
        for op in ops:
            if op.dma:
                j = dcount[op.eng] % self.n_dma_sems
                dcount[op.eng] += 1
                key = (op.eng, j)
                prev = dsem_val.get(key, 0)
                op.dsem, op.dprev, op.dval = key, prev, prev + 16
                dsem_val[key] = op.dval
                used_d.add(key)
            elif op.need_sig:
                sigc[op.eng] += 1
                op.sig = sigc[op.eng]

        esem = pool["esem"]
        dsem = pool["dsem"]
        pool["eval"] = dict(sigc)
        pool["dcount"] = dict(dcount)
        pool["dval"] = dict(dsem_val)
        with contextlib.ExitStack() as st:
            block = st.enter_context(nc.Block())

            def run_engine(ename, eh):
                waited = {}

                def wait(sem_key, semh, val):
                    if waited.get(sem_key, 0) >= val:
                        return
                    eh.wait_ge(semh, val)
                    waited[sem_key] = val

                for op in ops:
                    if op.eng != ename:
                        continue
                    for d in op.deps:
                        dop = ops[d]
                        if dop.dma:
                            wait(dop.dsem, dsem[dop.dsem], dop.dval)
                        else:
                            wait(dop.eng, esem[dop.eng], dop.sig)
                    if op.dma:
                        if op.dprev > 0:
                            wait(op.dsem, dsem[op.dsem], op.dprev)
                        op.fn(eh).then_inc(dsem[op.dsem], 16)
                    else:
                        ins = op.fn(eh)
                        if op.sig is not None:
                            ins.then_inc(esem[ename], 1)
                for key in sorted(used_d):
                    wait(key, dsem[key], dsem_val[key])
                for e in ENGS:
                    if sigc[e] > sig0[e]:
                        wait(e, esem[e], sigc[e])

            @block.tensor
            def _(eh):
                run_engine(PE, eh)

            @block.scalar
            def _(eh):
                run_engine(ACT, eh)

            @block.vector
            def _(eh):
                run_engine(DVE, eh)

            @block.gpsimd
            def _(eh):
                run_engine(POOL, eh)

            @block.sync
            def _(eh):
                run_engine(SP, eh)


_SEM_POOLS = {}


def _sem_pool(nc, n_dma_sems):
    key = id(nc)
    if key not in _SEM_POOLS:
        pool = {"esem": {e: nc.alloc_semaphore(name="s_" + e) for e in ENGS}, "dsem": {}, "dval": {}}
        pool["eval"] = {e: 0 for e in ENGS}
        pool["dcount"] = {e: 0 for e in ENGS}
        for e in (SP, ACT, POOL):
            for j in range(n_dma_sems):
                pool["dsem"][(e, j)] = nc.alloc_semaphore(name="d_%s_%d" % (e, j))
        _SEM_POOLS[key] = pool
    return _SEM_POOLS[key]


class Phase:
    def __init__(self, nc, name):
        self.nc = nc
        self.name = name
        self.st = contextlib.ExitStack()
        self.P = Prog(nc)
        self.n = 0

    def sb(self, shape, dt, tag="t"):
        self.n += 1
        return self.st.enter_context(self.nc.sbuf_tensor("%s_%s%d" % (self.name, tag, self.n), list(shape), dt))

    def psum(self, tag="ps"):
        self.n += 1
        return self.st.enter_context(self.nc.psum_tensor("%s_%s%d" % (self.name, tag, self.n), [128, 512], F32))

    def finish(self):
        self.P.emit()
        self.st.close()


CF_IDENT = 0
CF_ONES = 128
CF_MT = 256
CF_THR = 768
CF_IOTA = 816
CF_INVF = 824
CF_ZETA = 825
CF_EPS = 829
CF_N = 832
CB_IDENT = 0
CB_ONES = 128
CB_TRIU = 256
CB_XI = 384
CB_N = 384 + 1024


def _host_consts():
    cf = np.zeros((128, CF_N), np.float32)
    cf[:, CF_IDENT:CF_IDENT + 128] = np.eye(128, dtype=np.float32)
    cf[:, CF_ONES:CF_ONES + 128] = 1.0
    gam = 1.0 - 2.0 ** (-5.0 - np.arange(RH, dtype=np.float64))
    i = np.arange(128)
    for h in range(RH):
        ci, cj = i[:, None] // 64, i[None, :] // 64
        dist = (i[:, None] - i[None, :]).astype(np.float64)
        M = np.where(ci == cj, gam[h] ** np.abs(dist), np.where(ci > cj, gam[h] ** dist, 0.0))
        cf[:, CF_MT + h * 128: CF_MT + (h + 1) * 128] = (M.T * (256.0 ** -0.5)).astype(np.float32)
        cf[:, CF_ZETA + h] = (gam[h] ** (127.0 - i) * (256.0 ** -0.5)).astype(np.float32)
    cf[:, CF_THR:CF_THR + NSLOT_T] = (np.arange(NSLOT_T) * SLOT).astype(np.float32)[None, :]
    cf[:, CF_IOTA] = i
    cf[:, CF_EPS] = EPS
    for c in range(4):
        cf[:, CF_IOTA + 1 + c] = c * 128 + i
    cf[:, CF_INVF] = (np.float32(10000.0) ** (-(np.arange(128, dtype=np.float32)) / np.float32(128))).astype(np.float32)
    cb = np.zeros((128, CB_N), np.float32)
    cb[:, CB_IDENT:CB_IDENT + 128] = np.eye(128)
    cb[:, CB_ONES:CB_ONES + 128] = 1.0
    cb[:, CB_TRIU:CB_TRIU + 128] = (i[:, None] < i[None, :]).astype(np.float32)
    for kc in range(8):
        h = kc // 2
        cb[:, CB_XI + kc * 128: CB_XI + (kc + 1) * 128] = (gam[h] ** (i + 1.0))[None, :]
    return cf, cb.astype(ml_dtypes.bfloat16), gam


GAMMA = 1.0 - 2.0 ** (-5.0 - np.arange(RH, dtype=np.float64))

PB_LN1G, PB_LN1B, PB_LN2G, PB_LN2B, PB_RB = 0, 1024, 2048, 3072, 4096
PB_N = 4160
PM_N = 4096
PP_B1, PP_WDW, PP_BDW, PP_LNG, PP_LNB = 0, 16, 16 + 248, 16 + 248 + 8, 16 + 248 + 16
PP_N = 16 + 248 + 24


class G:
    pass


EPS_AP = [None]


def _ln_token_major(P, eng2, r, x1, g_ap, b_ap, tmp_stats, tmp_mv, tmp_rs, key_r, key_out, tagk):
    P.dve(lambda e: e.bn_stats(out=tmp_stats[:, 0:6], in_=r[:, 0:512]), reads=[key_r], writes=[tagk + "st"])
    P.dve(lambda e: e.bn_stats(out=tmp_stats[:, 6:12], in_=r[:, 512:1024]), reads=[key_r], writes=[tagk + "st"])
    P.dve(lambda e: e.bn_aggr(out=tmp_mv[:, 0:2], in_=tmp_stats[:, 0:12]), reads=[tagk + "st"], writes=[tagk + "mv"])
    P.act(lambda e: e.activation(out=tmp_rs[:, 0:1], in_=tmp_mv[:, 1:2], func=AF.Sqrt, bias=EPS_AP[0], scale=1.0),
          reads=[tagk + "mv"], writes=[tagk + "rs"])
    P.dve(lambda e: e.reciprocal(out=tmp_rs[:, 0:1], in_=tmp_rs[:, 0:1]), reads=[tagk + "rs"], writes=[tagk + "rs"])
    P.dve(lambda e: e.scalar_tensor_tensor(out=tmp_rs[:, 1:2], in0=tmp_mv[:, 0:1], scalar=-1.0, in1=tmp_rs[:, 0:1], op0=ALU.mult, op1=ALU.mult),
          reads=[tagk + "mv", tagk + "rs"], writes=[tagk + "rs"])
    P.act(lambda e: e.activation(out=r[:, :], in_=r[:, :], func=AF.Identity, bias=tmp_rs[:, 1:2], scale=tmp_rs[:, 0:1]),
          reads=[key_r, tagk + "rs"], writes=[key_r])
    P.ve(eng2, lambda e: e.tensor_tensor(out=r[:, :], in0=r[:, :], in1=g_ap, op=ALU.mult), reads=[key_r, "PB"], writes=[key_r])
    P.ve(eng2, lambda e: e.tensor_tensor(out=x1[:, :], in0=r[:, :], in1=b_ap, op=ALU.add), reads=[key_r, "PB"], writes=[key_out])


def _router_tile(ph, g, ti, x1, key_x1, W, psR, psT2):
    P = ph.P
    cf = g.cf
    for half in range(2):
        for q in range(4):
            kc = half * 4 + q
            P.pe(lambda e, kc=kc, q=q, half=half: e.transpose(out=psT2[half][:, q * 128:(q + 1) * 128],
                                                              in_=x1[:, kc * 128:(kc + 1) * 128],
                                                              identity=cf[:, CF_IDENT:CF_IDENT + 128]),
                 reads=[key_x1], writes=["psT2_%d" % half])
        P.act(lambda e, half=half: e.copy(out=W.x1T[:, half * 4:(half + 1) * 4, :],
                                          in_=psT2[half][:, :].rearrange("p (k t) -> p k t", k=4)),
              reads=["psT2_%d" % half], writes=["x1T"])
    for kc in range(8):
        P.pe(lambda e, kc=kc: e.matmul(psR[:, 0:36], lhsT=W.x1T[:, kc, :], rhs=g.wr[:, kc, :],
                                       start=(kc == 0), stop=(kc == 7)), reads=["x1T", "WR"], writes=["psR"])
    L = W.L
    P.dve(lambda e: e.tensor_tensor(out=L[:, 0:36], in0=psR[:, 0:36], in1=g.pb[:, PB_RB:PB_RB + 36], op=ALU.add),
          reads=["psR", "PB"], writes=["L"])
    sm = W.sm
    P.dve(lambda e: e.tensor_reduce(out=sm[:, 0:1], in_=L[:, 0:4], axis=AX.X, op=ALU.max), reads=["L"], writes=["sm0"])
    P.dve(lambda e: e.tensor_scalar(out=sm[:, 1:2], in0=sm[:, 0:1], scalar1=-1.0, scalar2=None, op0=ALU.mult),
          reads=["sm0"], writes=["sm1"])
    P.act(lambda e: e.activation(out=W.ge[:, 0:4], in_=L[:, 0:4], func=AF.Exp, bias=sm[:, 1:2], scale=1.0,
                                 accum_out=sm[:, 2:3]), reads=["L", "sm1"], writes=["ge", "sm2"])
    P.dve(lambda e: e.reciprocal(out=sm[:, 3:4], in_=sm[:, 2:3]), reads=["sm2"], writes=["sm3"])
    P.dve(lambda e: e.tensor_scalar(out=W.m4[:, 0:4], in0=L[:, 0:4], scalar1=sm[:, 0:1], scalar2=None, op0=ALU.is_equal),
          reads=["L", "sm0"], writes=["m4"])
    P.dve(lambda e: e.tensor_scalar(out=W.m4[:, 0:4], in0=W.m4[:, 0:4], scalar1=-1.0, scalar2=1e30, op0=ALU.add, op1=ALU.mult),
          reads=["m4"], writes=["m4"])
    P.dve(lambda e: e.tensor_tensor(out=W.ml[:, :, :], in0=L[:, 4:36].rearrange("p (g j) -> p g j", g=4),
                                    in1=W.m4[:, 0:4].unsqueeze(2).to_broadcast([128, 4, 8]), op=ALU.add),
          reads=["L", "m4"], writes=["ml"])
    mlf = W.ml[:, :, :].rearrange("p g j -> p (g j)")
    P.dve(lambda e: e.max(out=W.t8[:, 0:8], in_=mlf), reads=["ml"], writes=["t8"])
    P.dve(lambda e: e.tensor_scalar(out=g.RE1[:, ti, :], in0=mlf, scalar1=W.t8[:, 0:1], scalar2=None, op0=ALU.is_equal),
          reads=["ml", "t8"], writes=["RE1"])
    P.dve(lambda e: e.tensor_scalar(out=g.RE2[:, ti, :], in0=mlf, scalar1=W.t8[:, 1:2], scalar2=None, op0=ALU.is_equal),
          reads=["ml", "t8"], writes=["RE2"])
    P.dve(lambda e: e.tensor_tensor(out=sm[:, 4:5], in0=W.t8[:, 1:2], in1=W.t8[:, 0:1], op=ALU.subtract),
          reads=["t8"], writes=["sm4"])
    P.act(lambda e: e.activation(out=sm[:, 5:6], in_=sm[:, 4:5], func=AF.Exp), reads=["sm4"], writes=["sm5"])
    P.dve(lambda e: e.tensor_scalar(out=sm[:, 6:7], in0=sm[:, 5:6], scalar1=1.0, scalar2=None, op0=ALU.add),
          reads=["sm5"], writes=["sm6"])
    P.dve(lambda e: e.reciprocal(out=sm[:, 7:8], in_=sm[:, 6:7]), reads=["sm6"], writes=["sm7"])
    P.dve(lambda e: e.tensor_tensor(out=g.RG[:, ti, 0:1], in0=sm[:, 7:8], in1=sm[:, 3:4], op=ALU.mult),
          reads=["sm7", "sm3"], writes=["RG"])
    P.dve(lambda e: e.tensor_tensor(out=g.RG[:, ti, 1:2], in0=g.RG[:, ti, 0:1], in1=sm[:, 5:6], op=ALU.mult),
          reads=["RG", "sm5"], writes=["RG"])
    P.dve(lambda e: e.tensor_tensor(out=W.Mb[:, 0:32], in0=g.RE1[:, ti, :], in1=g.RE2[:, ti, :], op=ALU.add),
          reads=["RE1", "RE2"], writes=["Mb"])
    cb = g.cb
    P.pe(lambda e: e.matmul(psR[:, 64:96], lhsT=cb[:, CB_TRIU:CB_TRIU + 128], rhs=W.Mb[:, 0:32], start=True, stop=True),
         reads=["Mb"], writes=["psR"])
    P.pe(lambda e: e.matmul(psR[:, 128:160], lhsT=cb[:, CB_ONES:CB_ONES + 128], rhs=W.Mb[:, 0:32], start=True, stop=True),
         reads=["Mb"], writes=["psR"])
    P.dve(lambda e: e.tensor_tensor(out=g.RPF[:, ti, :], in0=psR[:, 64:96], in1=g.CAR[:, 0:32], op=ALU.add),
          reads=["psR", "CAR"], writes=["RPF"])
    P.dve(lambda e: e.tensor_tensor(out=g.CAR[:, 0:32], in0=psR[:, 128:160], in1=g.CAR[:, 0:32], op=ALU.add),
          reads=["psR", "CAR"], writes=["CAR"])


class _RW:
    pass


def _router_work(ph):
    W = _RW()
    W.x1T = ph.sb([128, 8, 128], F32, "x1T")
    W.L = ph.sb([128, 36], F32, "L")
    W.sm = ph.sb([128, 16], F32, "sm")
    W.ge = ph.sb([128, 4], F32, "ge")
    W.m4 = ph.sb([128, 4], F32, "m4")
    W.ml = ph.sb([128, 4, 8], F32, "ml")
    W.t8 = ph.sb([128, 8], F32, "t8")
    W.Mb = ph.sb([128, 32], BF16, "Mb")
    return W


def _post_mixer(ph, g, ti, r, key_r, W, bufs, psR, psT2, xdst):
    P = ph.P
    x1 = bufs.x1[ti % 2]
    kx1 = "x1_%d" % (0 if bufs.x1[0] is bufs.x1[1] else ti % 2)
    _ln_token_major(P, DVE, r, x1, g.pb[:, PB_LN1G:PB_LN1G + 1024], g.pb[:, PB_LN1B:PB_LN1B + 1024],
                    bufs.st, bufs.mv, bufs.rs, key_r, kx1, "ln1")
    P.dma(lambda e: e.dma_start(out=xdst[ti * 128:(ti + 1) * 128, :], in_=x1[:, :]), reads=[kx1], writes=["XB_%d" % ti])
    xb = bufs.x1bf[ti % 2]
    kxb = "x1bf_%d" % (0 if bufs.x1bf[0] is bufs.x1bf[1] else ti % 2)
    P.act(lambda e: e.copy(out=xb[:, :].rearrange("t (kk p) -> t kk p", p=128),
                           in_=x1[:, :].rearrange("t (p kk) -> t kk p", kk=8)), reads=[kx1], writes=[kxb])
    P.dma(lambda e: e.dma_start(out=g.xb16[ti * 128:(ti + 1) * 128, :], in_=xb[:, :]), reads=[kxb], writes=["xb16_%d" % ti])
    _router_tile(ph, g, ti, x1, kx1, W, psR, psT2)


def _load_layer_params(ph, g, li):
    P = ph.P
    P.dma(lambda e: e.dma_start(out=g.pb[:, :], in_=g.d_pbln[li]), writes=["PB"])
    P.dma(lambda e: e.dma_start(out=g.wr[:, :, :], in_=g.d_wr[li]), writes=["WR"])
    P.dve(lambda e: e.memset(g.CAR[:, :], 0.0), writes=["CAR"])


def conv_phase(nc, g, li, xsrc, xdst):
    j = li // 2
    ph = Phase(nc, "cv%d" % li)
    P = ph.P
    cf, cb = g.cf, g.cb
    _load_layer_params(ph, g, li)
    W1b = ph.sb([128, 8, 2048], BF16, "W1")
    W2b = ph.sb([128, 8, 1024], BF16, "W2")
    pm = ph.sb([128, 1024], F32, "pm")
    pp = ph.sb([128, PP_N], F32, "pp")
    for q in range(4):
        P.dma(lambda e, q=q: e.dma_start(out=W1b[:, :, q * 512:(q + 1) * 512],
                                         in_=g.d_conv_w_pw1[j].rearrange("(k p) f -> p k f", p=128)[:, :, q * 512:(q + 1) * 512]),
              writes=["W1_%d" % q], q=POOL)
    P.dma(lambda e: e.dma_start(out=W2b[:, :, :], in_=g.d_conv_w_pw2[j].rearrange("(k p) f -> p k f", p=128)),
          writes=["W2"], q=POOL)
    P.dma(lambda e: e.dma_start(out=pm[:, :], in_=g.d_pbmix[li][:, 0:1024]), writes=["PM"])
    P.dma(lambda e: e.dma_start(out=pp[:, :], in_=g.d_pp[j]), writes=["PP"])

    xin = [ph.sb([128, 1024], F32, "xin") for _ in range(2)]
    xbf = ph.sb([128, 1024], BF16, "xbf")
    xT = ph.sb([128, 8, 512], BF16, "xT")
    hg = ph.sb([128, 8, 512 + CW - 1], BF16, "hg")
    NPE = NPE_TAPS
    diag = ph.sb([128, 8 * NPE, 128], BF16, "diag")
    for c in range(8):
        for jj in range(NPE):
            P.act(lambda e, c=c, jj=jj: e.activation(out=diag[:, c * NPE + jj, :], in_=cf[:, CF_IDENT:CF_IDENT + 128], func=AF.Copy,
                                                     scale=pp[:, PP_WDW + c * CW + jj:PP_WDW + c * CW + jj + 1]),
                  reads=["PP", "cf"], writes=["diag"])
    sig = [ph.sb([128, 512], F32, "sig")] * 2
    cv = ph.sb([128, 8, 512], F32, "cv")
    sq = [ph.sb([128, 512], F32, "sq")] * 2
    mean = ph.sb([128, 512], F32, "mean")
    rstd = ph.sb([128, 512], F32, "rstd")
    nmr = ph.sb([128, 512], F32, "nmr")
    hT = xT
    rbuf = [ph.sb([128, 1024], F32, "r") for _ in range(2)]
    bufs = _RW()
    bufs.x1 = [ph.sb([128, 1024], F32, "x1")] * 2
    bufs.x1bf = [ph.sb([128, 1024], BF16, "x1bf")] * 2
    bufs.st = ph.sb([128, 12], F32, "st")
    bufs.mv = ph.sb([128, 2], F32, "mv")
    bufs.rs = ph.sb([128, 2], F32, "rs")
    RWk = _router_work(ph)
    psT = ph.psum("psT")
    psA = [ph.psum("psA") for _ in range(2)]
    psG = [ph.psum("psG") for _ in range(2)]
    psR = ph.psum("psR")
    psT2 = [ph.psum("psT2") for _ in range(2)]
    psS = [psG[0], psG[1]]
    psT_bf = psT[:, :].bitcast(BF16)

    P.dve(lambda e: e.memset(hg[:, :, 0:CW - 1], 0.0), writes=["hg"])
    conv_eng = [DVE] * 8
    pending = []
    norm_eng = [DVE] * 8
    for st_i in range(NT // 4):
        for sub in range(4):
            ti = st_i * 4 + sub
            xi = xin[ti % 2]
            kxi = "xin_%d" % (ti % 2)
            P.dma(lambda e, xi=xi, ti=ti: e.dma_start(out=xi[:, :], in_=xsrc[ti * 128:(ti + 1) * 128, :]), writes=[kxi])
            P.act(lambda e, xi=xi: e.copy(out=xbf[:, :], in_=xi[:, :]), reads=[kxi], writes=["xbf"])
            for kc in range(8):
                P.pe(lambda e, kc=kc: e.transpose(out=psT_bf[:, kc * 128:(kc + 1) * 128], in_=xbf[:, kc * 128:(kc + 1) * 128],
                                                  identity=cb[:, CB_IDENT:CB_IDENT + 128]), reads=["xbf"], writes=["psT"])
            P.dve(lambda e, sub=sub: e.tensor_copy(out=xT[:, :, sub * 128:(sub + 1) * 128],
                                                   in_=psT_bf.rearrange("p (k t) -> p k t", k=8)), reads=["psT"], writes=["xT"])
        for c in range(8):
            pa, pg = psA[c % 2], psG[c % 2]
            ka, kg = "psA_%d" % (c % 2), "psG_%d" % (c % 2)
            for kc in range(8):
                P.pe(lambda e, kc=kc, c=c, pa=pa: e.matmul(pa[:, :], lhsT=W1b[:, kc, c * 128:(c + 1) * 128], rhs=xT[:, kc, :],
                                                           start=(kc == 0), stop=(kc == 7)),
                     reads=["xT", "W1_%d" % (c // 4)], writes=[ka])
            for kc in range(8):
                P.pe(lambda e, kc=kc, c=c, pg=pg: e.matmul(pg[:, :], lhsT=W1b[:, kc, 1024 + c * 128:1024 + (c + 1) * 128], rhs=xT[:, kc, :],
                                                           start=(kc == 0), stop=(kc == 7)),
                     reads=["xT", "W1_%d" % (2 + c // 4)], writes=[kg])
            sg = sig[c % 2]
            ks = "sig_0"
            P.act(lambda e, c=c, pg=pg, sg=sg: e.activation(out=sg[:, :], in_=pg[:, :], func=AF.Sigmoid,
                                                            bias=pp[:, PP_B1 + 8 + c:PP_B1 + 9 + c], scale=1.0),
                  reads=[kg, "PP"], writes=[ks])
            P.dve(lambda e, c=c, pa=pa, sg=sg: e.scalar_tensor_tensor(out=hg[:, c, CW - 1:CW - 1 + 512], in0=pa[:, :],
                                                                      scalar=pp[:, PP_B1 + c:PP_B1 + c + 1], in1=sg[:, :],
                                                                      op0=ALU.add, op1=ALU.mult),
                  reads=[ka, ks, "PP"], writes=["hg"])
        cps = [psA[0], psG[0], psA[1], psG[1]]
        cpk = ["psA_0", "psG_0", "psA_1", "psG_1"]
        for c in range(8):
            pc, kpc = cps[c % 4], cpk[c % 4]
            for jj in range(NPE):
                P.pe(lambda e, c=c, jj=jj, pc=pc: e.matmul(pc[:, :], lhsT=diag[:, c * NPE + jj, :], rhs=hg[:, c, jj:jj + 512],
                                                           start=(jj == 0), stop=(jj == NPE - 1)), reads=["hg", "diag"], writes=[kpc])
            P.act(lambda e, c=c, pc=pc: e.activation(out=cv[:, c, :], in_=pc[:, :], func=AF.Identity,
                                                     bias=pp[:, PP_BDW + c:PP_BDW + c + 1], scale=1.0),
                  reads=[kpc, "PP"], writes=["cv_%d" % c])
        for jj in range(NPE, CW):
            for c in range(8):
                P.dve(lambda e, c=c, jj=jj: e.scalar_tensor_tensor(out=cv[:, c, :], in0=hg[:, c, jj:jj + 512],
                                                                   scalar=pp[:, PP_WDW + c * CW + jj:PP_WDW + c * CW + jj + 1],
                                                                   in1=cv[:, c, :], op0=ALU.mult, op1=ALU.add),
                      reads=["hg", "PP", "cv_%d" % c], writes=["cv_%d" % c])
        P.act(lambda e: e.copy(out=hg[:, :, 0:CW - 1], in_=hg[:, :, 512:512 + CW - 1]), reads=["hg"], writes=["hg"])
        for c in range(8):
            s_ = sq[c % 2]
            ksq = "sq_0"
            P.act(lambda e, c=c, s_=s_: e.activation(out=s_[:, :], in_=cv[:, c, :], func=AF.Square), reads=["cv_%d" % c], writes=[ksq])
            P.pe(lambda e, c=c: e.matmul(psS[0][:, :], lhsT=cf[:, CF_ONES:CF_ONES + 128], rhs=cv[:, c, :], start=(c == 0), stop=(c == 7)),
                 reads=["cv_%d" % c], writes=["psG_0"])
            P.pe(lambda e, c=c, s_=s_: e.matmul(psS[1][:, :], lhsT=cf[:, CF_ONES:CF_ONES + 128], rhs=s_[:, :], start=(c == 0), stop=(c == 7)),
                 reads=[ksq], writes=["psG_1"])
        P.act(lambda e: e.mul(out=mean[:, :], in_=psS[0][:, :], mul=1.0 / D), reads=["psG_0"], writes=["mean"])
        P.dve(lambda e: e.tensor_tensor(out=nmr[:, :], in0=mean[:, :], in1=mean[:, :], op=ALU.mult), reads=["mean"], writes=["nmr"])
        P.dve(lambda e: e.scalar_tensor_tensor(out=rstd[:, :], in0=psS[1][:, :], scalar=1.0 / D, in1=nmr[:, :],
                                               op0=ALU.mult, op1=ALU.subtract), reads=["psG_1", "nmr"], writes=["rstd"])
        P.act(lambda e: e.activation(out=rstd[:, :], in_=rstd[:, :], func=AF.Sqrt, bias=EPS_AP[0], scale=1.0),
              reads=["rstd"], writes=["rstd"])
        P.dve(lambda e: e.reciprocal(out=rstd[:, :], in_=rstd[:, :]), reads=["rstd"], writes=["rstd"])
        P.dve(lambda e: e.scalar_tensor_tensor(out=nmr[:, :], in0=mean[:, :], scalar=-1.0, in1=rstd[:, :],
                                               op0=ALU.mult, op1=ALU.mult), reads=["mean", "rstd"], writes=["nmr"])
        for c in range(8):
            eng = norm_eng[c]
            P.ve(eng, lambda e, c=c: e.tensor_tensor(out=cv[:, c, :], in0=cv[:, c, :], in1=rstd[:, :], op=ALU.mult),
                 reads=["cv_%d" % c, "rstd"], writes=["cv_%d" % c])
        for c in range(8):
            eng = norm_eng[c]
            P.ve(eng, lambda e, c=c: e.tensor_tensor(out=cv[:, c, :], in0=cv[:, c, :], in1=nmr[:, :], op=ALU.add),
                 reads=["cv_%d" % c, "nmr"], writes=["cv_%d" % c])
        for c in range(8):
            P.act(lambda e, c=c: e.activation(out=hT[:, c, :], in_=cv[:, c, :], func=AF.Silu,
                                              bias=pp[:, PP_LNB + c:PP_LNB + c + 1], scale=pp[:, PP_LNG + c:PP_LNG + c + 1]),
                  reads=["cv_%d" % c, "PP"], writes=["xT"])
        for sub in range(4):
            ti = st_i * 4 + sub
            xi = xin[ti % 2]
            kxi = "xin_%d" % (ti % 2)
            P.dma(lambda e, xi=xi, ti=ti: e.dma_start(out=xi[:, :], in_=xsrc[ti * 128:(ti + 1) * 128, :]), writes=[kxi])
            r = rbuf[ti % 2]
            kr = "r_%d" % (ti % 2)
            for half in range(2):
                pa = (psA if sub % 2 == 0 else psG)[half]
                ka = ("psA_%d" if sub % 2 == 0 else "psG_%d") % half
                for c in range(8):
                    P.pe(lambda e, c=c, half=half, sub=sub, pa=pa: e.matmul(pa[:, :], lhsT=hT[:, c, sub * 128:(sub + 1) * 128],
                                                                            rhs=W2b[:, c, half * 512:(half + 1) * 512],
                                                                            start=(c == 0), stop=(c == 7)),
                         reads=["xT", "W2"], writes=[ka])
                P.dve(lambda e, half=half, pa=pa, xi=xi, r=r: e.scalar_tensor_tensor(out=r[:, half * 512:(half + 1) * 512],
                                                                                     in0=xi[:, half * 512:(half + 1) * 512], scalar=ALPHA,
                                                                                     in1=pa[:, :], op0=ALU.mult, op1=ALU.add),
                      reads=[ka, kxi], writes=[kr])
            P.dve(lambda e, r=r: e.tensor_tensor(out=r[:, :], in0=r[:, :], in1=pm[:, 0:1024], op=ALU.add), reads=[kr, "PM"], writes=[kr])
            if pending:
                pt_ = pending.pop(0)
                _post_mixer(ph, g, pt_, rbuf[pt_ % 2], "r_%d" % (pt_ % 2), RWk, bufs, psR, psT2, xdst)
            pending.append(ti)
    while pending:
        pt_ = pending.pop(0)
        _post_mixer(ph, g, pt_, rbuf[pt_ % 2], "r_%d" % (pt_ % 2), RWk, bufs, psR, psT2, xdst)
    ph.finish()


def offsets_scatter_phase(nc, g, li):
    ph = Phase(nc, "os%d" % li)
    P = ph.P
    cf = g.cf
    cnt = g.CAR
    r_ = ph.sb([128, 32], F32, "r")
    nz = ph.sb([128, 32], F32, "nz")
    pad = ph.sb([128, 32], F32, "pad")
    ca = ph.sb([128, 32], F32, "ca")
    cbuf = ph.sb([128, 32], F32, "cb")
    off = ph.sb([128, 32], F32, "off")
    big = ph.sb([128, NT, 32], F32, "big")
    g.tmpbig = ph.sb([128, NT, 32], F32, "tmpbig")
    posf = ph.sb([128, NT, 2], F32, "posf")
    ej = ph.sb([128, NSLOT_T], F32, "ej")
    tmpw = ph.sb([128, NSLOT_T], F32, "tmpw")
    P.dve(lambda e: e.memset(nz[:, :], 0.0), writes=["nz"])
    for m in range(2 * SEQ // SLOT):
        P.dve(lambda e, m=m: e.scalar_tensor_tensor(out=nz[:, :], in0=cnt[:, 0:32], scalar=float(m * SLOT), in1=nz[:, :],
                                                    op0=ALU.is_gt, op1=ALU.add), reads=["CAR", "nz"], writes=["nz"])
    P.dve(lambda e: e.tensor_scalar(out=pad[:, :], in0=nz[:, :], scalar1=float(SLOT), scalar2=None, op0=ALU.mult), reads=["nz"], writes=["pad"])
    src, dst = pad, ca
    ksrc, kdst = "pad", "ca"
    step = 1
    while step < 32:
        P.dve(lambda e, src=src, dst=dst, step=step: e.tensor_copy(out=dst[:, 0:step], in_=src[:, 0:step]), reads=[ksrc], writes=[kdst])
        P.dve(lambda e, src=src, dst=dst, step=step: e.tensor_tensor(out=dst[:, step:32], in0=src[:, step:32], in1=src[:, 0:32 - step], op=ALU.add),
              reads=[ksrc], writes=[kdst])
        if dst is ca:
            src, dst, ksrc, kdst = ca, cbuf, "ca", "cb"
        else:
            src, dst, ksrc, kdst = cbuf, ca, "cb", "ca"
        step *= 2
    cum, kcum = src, ksrc
    P.dve(lambda e: e.tensor_tensor(out=off[:, :], in0=cum[:, :], in1=pad[:, :], op=ALU.subtract), reads=[kcum, "pad"], writes=["off"])
    P.dve(lambda e: e.tensor_tensor(out=big[:, :, :], in0=g.RPF[:, :, :], in1=off[:, :].unsqueeze(1).to_broadcast([128, NT, 32]), op=ALU.add),
          reads=["RPF", "off"], writes=["big"])
    for k, RE, kre in ((0, g.RE1, "RE1"), (1, g.RE2, "RE2")):
        P.dve(lambda e, RE=RE: e.tensor_tensor(out=g.tmpbig[:, :, :], in0=big[:, :, :], in1=RE[:, :, :], op=ALU.mult),
              reads=["big", kre], writes=["tmpbig"])
        P.dve(lambda e, k=k: e.tensor_reduce(out=posf[:, :, k], in_=g.tmpbig[:, :, :], axis=AX.X, op=ALU.add),
              reads=["tmpbig"], writes=["posf"])
    P.dve(lambda e: e.tensor_copy(out=g.POS[:, :, :], in_=posf[:, :, :]), reads=["posf"], writes=["POS"])
    P.dve(lambda e: e.memset(ej[:, :], 0.0), writes=["ej"])
    for ex in range(NE):
        P.dve(lambda e, ex=ex: e.scalar_tensor_tensor(out=ej[:, :], in0=cf[:, CF_THR:CF_THR + NSLOT_T], scalar=cum[:, ex:ex + 1], in1=ej[:, :],
                                                      op0=ALU.is_ge, op1=ALU.add), reads=[kcum, "ej"], writes=["ej"])
    P.dve(lambda e: e.tensor_scalar(out=ej[:, :], in0=ej[:, :], scalar1=float(NE - 1), scalar2=None, op0=ALU.min), reads=["ej"], writes=["ej"])
    P.dve(lambda e: e.tensor_scalar(out=tmpw[:, :], in0=ej[:, :], scalar1=128.0, scalar2=cf[:, CF_IOTA:CF_IOTA + 1], op0=ALU.mult, op1=ALU.add),
          reads=["ej"], writes=["tmpw"])
    P.dve(lambda e: e.tensor_scalar(out=tmpw[:, :], in0=tmpw[:, :], scalar1=float(li * NE * 128), scalar2=None, op0=ALU.add),
          reads=["tmpw"], writes=["tmpw"])
    P.dve(lambda e: e.tensor_copy(out=g.WG[:, :], in_=tmpw[:, :]), reads=["tmpw"], writes=["WG"])
    for c in range(4):
        P.dve(lambda e, c=c: e.tensor_scalar(out=tmpw[:, :], in0=ej[:, :], scalar1=512.0, scalar2=cf[:, CF_IOTA + 1 + c:CF_IOTA + 2 + c],
                                             op0=ALU.mult, op1=ALU.add), reads=["ej"], writes=["tmpw"])
        P.dve(lambda e: e.tensor_scalar(out=tmpw[:, :], in0=tmpw[:, :], scalar1=float(li * NE * FF), scalar2=None, op0=ALU.add),
              reads=["tmpw"], writes=["tmpw"])
        P.dve(lambda e, c=c: e.tensor_copy(out=g.WD[:, c, :], in_=tmpw[:, :]), reads=["tmpw"], writes=["WD"])
    xbt = [ph.sb([128, 1024], BF16, "xbt") for _ in range(4)]
    for ti in range(NT):
        xb = xbt[ti % 4]
        kx = "xbt_%d" % (ti % 4)
        P.dma(lambda e, xb=xb, ti=ti: e.dma_start(out=xb[:, :], in_=g.xb16[ti * 128:(ti + 1) * 128, :]), writes=[kx])
        for k in range(2):
            P.dma(lambda e, xb=xb, ti=ti, k=k: e.indirect_dma_start(out=g.xs, out_offset=bass.IndirectOffsetOnAxis(ap=g.POS[:, ti, k:k + 1], axis=0),
                                                                    in_=xb[:, :], in_offset=None),
                  reads=[kx, "POS"], writes=["xs_%d_%d" % (ti, k)], q=POOL)
    ph.finish()


def expert_phase(nc, g, li, use_dma_transpose=True):
    ph = Phase(nc, "ex%d" % li)
    P = ph.P
    cb = g.cb
    NB = 3
    Wg = [ph.sb([128, 8, FF], BF16, "Wg") for _ in range(NB)]
    Wu = [ph.sb([128, 8, FF], BF16, "Wu") for _ in range(NB)]
    Wd = [ph.sb([128, 4, D], BF16, "Wd") for _ in range(NB)]
    xTp = [ph.sb([128, 8, SLOT], BF16, "xTp") for _ in range(2)]
    xrow = [[ph.sb([128, D], BF16, "xrow") for _ in range(4)] for _ in range(2)]
    sgt = [ph.sb([128, SLOT], F32, "sg") for _ in range(2)]
    hT = [ph.sb([128, 4, SLOT], BF16, "hT") for _ in range(2)]
    ysb = [ph.sb([128, D], F32, "ysb") for _ in range(4)]
    psg = [ph.psum("psg") for _ in range(2)]
    psu = [ph.psum("psu") for _ in range(2)]
    psy = [ph.psum("psy") for _ in range(2)]
    pst = [ph.psum("pst") for _ in range(2)]
    pst_bf = [p[:, :].bitcast(BF16) for p in pst]
    wgv = g.d_moe_w_gate.rearrange("l e (p kk) f -> (l e p) (kk f)", kk=8)
    wuv = g.d_moe_w_up.rearrange("l e (p kk) f -> (l e p) (kk f)", kk=8)
    wdv = g.d_moe_w_down.rearrange("l e k d -> (l e k) d")

    def load(jt):
        b = jt % NB
        xb = jt % 2
        for sub in range(4):
            P.dma(lambda e, xb=xb, jt=jt, sub=sub: e.dma_start(out=xrow[xb][sub][:, :], in_=g.xs[jt * SLOT + sub * 128: jt * SLOT + (sub + 1) * 128, :]),
                  reads=["xs"], writes=["xrow_%d_%d" % (xb, sub)])
        P.dma(lambda e, b=b, jt=jt: e.indirect_dma_start(out=Wg[b][:, :, :].rearrange("p k f -> p (k f)"), out_offset=None, in_=wgv,
                                                         in_offset=bass.IndirectOffsetOnAxis(ap=g.WG[:, jt:jt + 1], axis=0)),
              reads=["WG"], writes=["Wg_%d" % b], q=POOL)
        P.dma(lambda e, b=b, jt=jt: e.indirect_dma_start(out=Wu[b][:, :, :].rearrange("p k f -> p (k f)"), out_offset=None, in_=wuv,
                                                         in_offset=bass.IndirectOffsetOnAxis(ap=g.WG[:, jt:jt + 1], axis=0)),
              reads=["WG"], writes=["Wu_%d" % b], q=POOL)
        for c in range(4):
            P.dma(lambda e, b=b, jt=jt, c=c: e.indirect_dma_start(out=Wd[b][:, c, :], out_offset=None, in_=wdv,
                                                                  in_offset=bass.IndirectOffsetOnAxis(ap=g.WD[:, c, jt:jt + 1], axis=0)),
                  reads=["WD"], writes=["Wd_%d_%d" % (b, c)], q=POOL)

    def transposes(jt):
        xb = jt % 2
        for sub in range(4):
            ptb = pst_bf[sub % 2]
            kpt = "pst_%d" % (sub % 2)
            xr = xrow[xb][sub]
            for kk in range(8):
                P.pe(lambda e, xr=xr, kk=kk, ptb=ptb: e.transpose(out=ptb[:, kk * 128:(kk + 1) * 128], in_=xr[:, kk * 128:(kk + 1) * 128],
                                                                  identity=cb[:, CB_IDENT:CB_IDENT + 128]),
                     reads=["xrow_%d_%d" % (xb, sub)], writes=[kpt])
            if sub % 2 == 0:
                P.act(lambda e, xb=xb, sub=sub, ptb=ptb: e.copy(out=xTp[xb][:, :, sub * 128:(sub + 1) * 128], in_=ptb.rearrange("p (k t) -> p k t", k=8)),
                      reads=[kpt], writes=["xTp_%d" % xb])
            else:
                P.dve(lambda e, xb=xb, sub=sub, ptb=ptb: e.tensor_copy(out=xTp[xb][:, :, sub * 128:(sub + 1) * 128], in_=ptb.rearrange("p (k t) -> p k t", k=8)),
                      reads=[kpt], writes=["xTp_%d" % xb])

    load(0)
    load(1)
    transposes(0)
    for jt in range(NSLOT_T):
        b = jt % NB
        xb = jt % 2
        if jt + 1 < NSLOT_T:
            transposes(jt + 1)
        if jt + 2 < NSLOT_T:
            load(jt + 2)
        h = hT[jt % 2]
        kh = "hT_%d" % (jt % 2)
        for fc in range(4):
            pg_, pu_ = psg[fc % 2], psu[fc % 2]
            kg, ku = "psg_%d" % (fc % 2), "psu_%d" % (fc % 2)
            for kk in range(8):
                P.pe(lambda e, b=b, xb=xb, kk=kk, fc=fc, pg_=pg_: e.matmul(pg_[:, :], lhsT=Wg[b][:, kk, fc * 128:(fc + 1) * 128], rhs=xTp[xb][:, kk, :],
                                                                           start=(kk == 0), stop=(kk == 7)),
                     reads=["Wg_%d" % b, "xTp_%d" % xb], writes=[kg])
            for kk in range(8):
                P.pe(lambda e, b=b, xb=xb, kk=kk, fc=fc, pu_=pu_: e.matmul(pu_[:, :], lhsT=Wu[b][:, kk, fc * 128:(fc + 1) * 128], rhs=xTp[xb][:, kk, :],
                                                                           start=(kk == 0), stop=(kk == 7)),
                     reads=["Wu_%d" % b, "xTp_%d" % xb], writes=[ku])
            sg = sgt[fc % 2]
            ksg = "sgt_%d" % (fc % 2)
            P.act(lambda e, pg_=pg_, sg=sg: e.activation(out=sg[:, :], in_=pg_[:, :], func=AF.Silu), reads=[kg], writes=[ksg])
            P.dve(lambda e, pu_=pu_, sg=sg, h=h, fc=fc: e.tensor_tensor(out=h[:, fc, :], in0=pu_[:, :], in1=sg[:, :], op=ALU.mult),
                  reads=[ku, ksg], writes=[kh])
        for sub in range(4):
            yb = ysb[sub]
            kyb = "ysb_%d" % sub
            for half in range(2):
                py = psy[half]
                kpy = "psy_%d" % half
                for fc in range(4):
                    P.pe(lambda e, b=b, fc=fc, sub=sub, half=half, py=py, h=h: e.matmul(py[:, :], lhsT=h[:, fc, sub * 128:(sub + 1) * 128],
                                                                                      rhs=Wd[b][:, fc, half * 512:(half + 1) * 512],
                                                                                      start=(fc == 0), stop=(fc == 3)),
                         reads=[kh] + ["Wd_%d_%d" % (b, c) for c in range(4)], writes=[kpy])
                if half == 0:
                    P.act(lambda e, py=py, yb=yb: e.copy(out=yb[:, 0:512], in_=py[:, :]), reads=[kpy], writes=[kyb + "a"])
                else:
                    P.dve(lambda e, py=py, yb=yb: e.tensor_copy(out=yb[:, 512:1024], in_=py[:, :]), reads=[kpy], writes=[kyb + "b"])
            P.dma(lambda e, yb=yb, jt=jt, sub=sub: e.dma_start(out=g.ys[jt * SLOT + sub * 128: jt * SLOT + (sub + 1) * 128, :], in_=yb[:, :]),
                  reads=[kyb + "a", kyb + "b"], writes=["ys_%d_%d" % (jt, sub)])
    ph.finish()


def combine_phase(nc, g, li, x1src, xdst):
    ph = Phase(nc, "cm%d" % li)
    P = ph.P
    NB = 3
    y1 = [ph.sb([128, D], F32, "y1") for _ in range(NB)]
    y2 = [ph.sb([128, D], F32, "y2") for _ in range(NB)]
    x1 = [ph.sb([128, D], F32, "x1") for _ in range(NB)]
    x2 = [ph.sb([128, D], F32, "x2") for _ in range(NB)]
    st = ph.sb([128, 12], F32, "st")
    mv = ph.sb([128, 2], F32, "mv")
    rs = ph.sb([128, 2], F32, "rs")

    def load(ti):
        b = ti % NB
        P.dma(lambda e, b=b, ti=ti: e.dma_start(out=x1[b][:, :], in_=x1src[ti * 128:(ti + 1) * 128, :]), reads=["XB"], writes=["x1_%d" % b])
        P.dma(lambda e, b=b, ti=ti: e.indirect_dma_start(out=y1[b][:, :], out_offset=None, in_=g.ys,
                                                         in_offset=bass.IndirectOffsetOnAxis(ap=g.POS[:, ti, 0:1], axis=0)),
              reads=["ys", "POS"], writes=["y1_%d" % b], q=POOL)
        P.dma(lambda e, b=b, ti=ti: e.indirect_dma_start(out=y2[b][:, :], out_offset=None, in_=g.ys,
                                                         in_offset=bass.IndirectOffsetOnAxis(ap=g.POS[:, ti, 1:2], axis=0)),
              reads=["ys", "POS"], writes=["y2_%d" % b], q=POOL)

    for ti in range(NB - 1):
        load(ti)
    for ti in range(NT):
        b = ti % NB
        if ti + NB - 1 < NT:
            load(ti + NB - 1)
        P.act(lambda e, b=b, ti=ti: e.activation(out=y1[b][:, :], in_=y1[b][:, :], func=AF.Copy, scale=g.RG[:, ti, 0:1]),
              reads=["y1_%d" % b, "RG"], writes=["y1_%d" % b])
        P.dve(lambda e, b=b, ti=ti: e.scalar_tensor_tensor(out=y2[b][:, :], in0=y2[b][:, :], scalar=g.RG[:, ti, 1:2], in1=y1[b][:, :],
                                                           op0=ALU.mult, op1=ALU.add), reads=["y2_%d" % b, "y1_%d" % b, "RG"], writes=["y2_%d" % b])
        P.dve(lambda e, b=b: e.scalar_tensor_tensor(out=x1[b][:, :], in0=x1[b][:, :], scalar=ALPHA, in1=y2[b][:, :],
                                                    op0=ALU.mult, op1=ALU.add), reads=["x1_%d" % b, "y2_%d" % b], writes=["x1_%d" % b])
        _ln_token_major(P, DVE, x1[b], x2[b], g.pb[:, PB_LN2G:PB_LN2G + 1024], g.pb[:, PB_LN2B:PB_LN2B + 1024],
                        st, mv, rs, "x1_%d" % b, "x2_%d" % b, "ln2")
        P.dma(lambda e, b=b, ti=ti: e.dma_start(out=xdst[ti * 128:(ti + 1) * 128, :], in_=x2[b][:, :]), reads=["x2_%d" % b], writes=["XOUT_%d" % ti])
    ph.finish()


def build_program(n_layers=DEPTH, stop_after=None, use_dma_transpose=True):
    nc = bass.Bass("TRN2", target_bir_lowering=False)
    g = G()

    def din(name, shape, dt):
        return nc.dram_tensor(name, list(shape), dt, kind="ExternalInput").ap()

    def dscr(name, shape, dt):
        return nc.dram_tensor(name, list(shape), dt, kind="Internal").ap()

    g.d_x = din("x", [SEQ, D], F32)
    g.d_posb = din("posb", [128, SEQ], I32)
    g.d_cf = din("constf", [128, CF_N], F32)
    g.d_cb = din("constb", [128, CB_N], BF16)
    g.d_conv_w_pw1 = din("conv_w_pw1", [2, D, 2 * D], F32)
    g.d_conv_w_pw2 = din("conv_w_pw2", [2, D, D], F32)
    g.d_ret_w_qkvg = din("ret_w_qkvg", [2, D, 6 * D], F32)
    g.d_ret_w_o = din("ret_w_o", [2, 2 * D, D], F32)
    g.d_moe_w_gate = din("moe_w_gate", [DEPTH, NE, D, FF], F32)
    g.d_moe_w_up = din("moe_w_up", [DEPTH, NE, D, FF], F32)
    g.d_moe_w_down = din("moe_w_down", [DEPTH, NE, FF, D], F32)
    g.d_pbln = din("pbln", [DEPTH, 128, PB_N], F32)
    g.d_pbmix = din("pbmix", [DEPTH, 128, PM_N], F32)
    g.d_pp = din("pp", [2, 128, PP_N], F32)
    g.d_wr = din("wr", [DEPTH, 128, 8, 36], F32)
    g.d_out = nc.dram_tensor("out", [SEQ, D], F32, kind="ExternalOutput").ap()
    g.XA = dscr("XA", [SEQ, D], F32)
    g.XB = dscr("XB", [SEQ, D], F32)
    g.xb16 = dscr("xb16", [SEQ, D], BF16)
    g.xs = dscr("xs", [NSLOT_T * SLOT, D], BF16)
    g.ys = dscr("ys", [NSLOT_T * SLOT, D], F32)
    g.QS = dscr("QS", [NT, 128, 8, 128], BF16)
    g.KS = dscr("KS", [NT, 128, 8, 128], BF16)
    g.VS = dscr("VS", [SEQ, 2 * D], BF16)
    g.SG = dscr("SG", [SEQ, 2 * D], BF16)

    with contextlib.ExitStack() as st:
        def sb(name, shape, dt):
            return st.enter_context(nc.sbuf_tensor(name, list(shape), dt))
        g.cf = sb("cf", [128, CF_N], F32)
        g.cb = sb("cb", [128, CB_N], BF16)
        g.pb = sb("pb", [128, PB_N], F32)
        g.wr = sb("wr_sb", [128, 8, 36], F32)
        g.RE1 = sb("RE1", [128, NT, 32], F32)
        g.RE2 = sb("RE2", [128, NT, 32], F32)
        g.RPF = sb("RPF", [128, NT, 32], F32)
        g.RG = sb("RG", [128, NT, 2], F32)
        g.POS = sb("POS", [128, NT, 2], I32)
        g.CAR = sb("CAR", [128, 32], F32)
        g.WG = sb("WG", [128, NSLOT_T], I32)
        g.WD = sb("WD", [128, 4, NSLOT_T], I32)

        EPS_AP[0] = g.cf[:, CF_EPS:CF_EPS + 1]
        ph = Phase(nc, "init")
        ph.P.dma(lambda e: e.dma_start(out=g.cf[:, :], in_=g.d_cf), writes=["cf"])
        ph.P.dma(lambda e: e.dma_start(out=g.cb[:, :], in_=g.d_cb), writes=["cb"])
        ph.finish()

        xcur = g.d_x
        for li in range(n_layers):
            if li % 2 == 0:
                conv_phase(nc, g, li, xcur, g.XB)
            else:
                retention_phase(nc, g, li, xcur, g.XB)
            if stop_after == ("mix", li):
                _copy_out(nc, g, g.XB)
                break
            offsets_scatter_phase(nc, g, li)
            expert_phase(nc, g, li, use_dma_transpose)
            last = (li == n_layers - 1)
            xnext = g.d_out if last else g.XA
            combine_phase(nc, g, li, g.XB, xnext)
            xcur = xnext
    return nc


def _copy_out(nc, g, src):
    ph = Phase(nc, "cpy")
    t = [ph.sb([128, D], F32, "t") for _ in range(2)]
    for ti in range(NT):
        b = ti % 2
        ph.P.dma(lambda e, b=b, ti=ti: e.dma_start(out=t[b][:, :], in_=src[ti * 128:(ti + 1) * 128, :]), reads=["src"], writes=["t%d" % b])
        ph.P.dma(lambda e, b=b, ti=ti: e.dma_start(out=g.d_out[ti * 128:(ti + 1) * 128, :], in_=t[b][:, :]), reads=["t%d" % b], writes=["o%d" % ti])
    ph.finish()


TWO_PI_HI = 6.28125
TWO_PI_LO = 2.0 * np.pi - 6.28125
PI = float(np.pi)


def retention_phase(nc, g, li, xsrc, xdst):
    _ret_pass1(nc, g, li, xsrc)
    _ret_pass2(nc, g, li, xsrc, xdst)


def _ret_pass1(nc, g, li, xsrc):
    j = li // 2
    ph = Phase(nc, "rp%d" % li)
    P = ph.P
    cf, cb = g.cf, g.cb
    _load_layer_params(ph, g, li)
    Wq = ph.sb([128, 8, 6 * D], BF16, "Wq")
    wsrc = g.d_ret_w_qkvg[j].rearrange("(k p) f -> p k f", p=128)
    for q in range(12):
        P.dma(lambda e, q=q: e.dma_start(out=Wq[:, :, q * 512:(q + 1) * 512], in_=wsrc[:, :, q * 512:(q + 1) * 512]),
              writes=["Wq_%d" % q], q=POOL)
    posi = ph.sb([128, 512], I32, "posi")
    posf = ph.sb([128, 512], F32, "posf")
    xin = [ph.sb([128, D], F32, "xin") for _ in range(2)]
    xbf = ph.sb([128, D], BF16, "xbf")
    xT = ph.sb([128, 8, 512], BF16, "xT")
    ang = ph.sb([128, 512], F32, "ang")
    ni = ph.sb([128, 512], I32, "ni")
    nf = ph.sb([128, 512], F32, "nf")
    rr = ph.sb([128, 512], F32, "rr")
    cc = ph.sb([128, 512], F32, "cc")
    mm = ph.sb([128, 512], F32, "mm")
    sinT = ph.sb([128, 512], F32, "sinT")
    cosT = ph.sb([128, 512], F32, "cosT")
    ta = [ph.sb([128, 512], F32, "ta")] * 2
    tb = [ph.sb([128, 512], F32, "tb")] * 2
    tc_ = [ph.sb([128, 512], F32, "tc")] * 2
    td = [ph.sb([128, 512], F32, "td")] * 2
    qkT = ph.sb([128, 16, 512], BF16, "qkT")
    vrow = [ph.sb([128, 2 * D], BF16, "vrow")] * 2
    srow = [ph.sb([128, 2 * D], BF16, "srow")] * 2
    psT = ph.psum("psT")
    psT_bf = psT[:, :].bitcast(BF16)
    psq = [ph.psum("psq") for _ in range(4)]
    psv = [ph.psum("psv") for _ in range(2)]
    invf = cf[:, CF_INVF:CF_INVF + 1]
    for st_i in range(NT // 4):
        t0 = st_i * 512
        for sub in range(4):
            ti = st_i * 4 + sub
            xi = xin[ti % 2]
            kxi = "xin_%d" % (ti % 2)
            P.dma(lambda e, xi=xi, ti=ti: e.dma_start(out=xi[:, :], in_=xsrc[ti * 128:(ti + 1) * 128, :]), writes=[kxi])
            P.act(lambda e, xi=xi: e.copy(out=xbf[:, :], in_=xi[:, :]), reads=[kxi], writes=["xbf"])
            for kc in range(8):
                P.pe(lambda e, kc=kc: e.transpose(out=psT_bf[:, kc * 128:(kc + 1) * 128], in_=xbf[:, kc * 128:(kc + 1) * 128],
                                                  identity=cb[:, CB_IDENT:CB_IDENT + 128]), reads=["xbf"], writes=["psT"])
            P.dve(lambda e, sub=sub: e.tensor_copy(out=xT[:, :, sub * 128:(sub + 1) * 128],
                                                   in_=psT_bf.rearrange("p (k t) -> p k t", k=8)), reads=["psT"], writes=["xT"])
        for sub in range(4):
            ti = st_i * 4 + sub
            vr, sr = vrow[ti % 2], srow[ti % 2]
            kv, ks = "vrow_0", "srow_0"
            for cbk in range(8):
                pv = psv[cbk % 2]
                kpv = "psv_%d" % (cbk % 2)
                c0 = 2048 + cbk * 512
                for kc in range(8):
                    P.pe(lambda e, kc=kc, c0=c0, pv=pv, sub=sub: e.matmul(pv[:, :], lhsT=xT[:, kc, sub * 128:(sub + 1) * 128], rhs=Wq[:, kc, c0:c0 + 512],
                                                                          start=(kc == 0), stop=(kc == 7)),
                         reads=["xT", "Wq_%d" % (c0 // 512)], writes=[kpv])
                if cbk < 4:
                    P.dve(lambda e, pv=pv, vr=vr, cbk=cbk: e.tensor_copy(out=vr[:, cbk * 512:(cbk + 1) * 512], in_=pv[:, :]), reads=[kpv], writes=[kv])
                else:
                    P.act(lambda e, pv=pv, sr=sr, cbk=cbk: e.activation(out=sr[:, (cbk - 4) * 512:(cbk - 3) * 512], in_=pv[:, :], func=AF.Silu),
                          reads=[kpv], writes=[ks])
            P.dma(lambda e, vr=vr, ti=ti: e.dma_start(out=g.VS[ti * 128:(ti + 1) * 128, :], in_=vr[:, :]), reads=[kv], writes=["VS_%d" % ti])
            P.dma(lambda e, sr=sr, ti=ti: e.dma_start(out=g.SG[ti * 128:(ti + 1) * 128, :], in_=sr[:, :]), reads=[ks], writes=["SG_%d" % ti])
        P.dma(lambda e, t0=t0: e.dma_start(out=posi[:, :], in_=g.d_posb[:, t0:t0 + 512]), writes=["posi"])
        P.dve(lambda e: e.tensor_copy(out=posf[:, :], in_=posi[:, :]), reads=["posi"], writes=["posf"])
        P.dve(lambda e: e.tensor_scalar(out=ang[:, :], in0=posf[:, :], scalar1=invf, scalar2=None, op0=ALU.mult),
               reads=["posf"], writes=["ang"])
        P.dve(lambda e: e.tensor_scalar(out=ni[:, :], in0=ang[:, :], scalar1=float(1.0 / (2.0 * np.pi)), scalar2=None, op0=ALU.mult),
               reads=["ang"], writes=["ni"])
        P.dve(lambda e: e.tensor_copy(out=nf[:, :], in_=ni[:, :]), reads=["ni"], writes=["nf"])
        P.dve(lambda e: e.scalar_tensor_tensor(out=rr[:, :], in0=nf[:, :], scalar=-TWO_PI_HI, in1=ang[:, :], op0=ALU.mult, op1=ALU.add),
              reads=["nf", "ang"], writes=["rr"])
        P.dve(lambda e: e.scalar_tensor_tensor(out=rr[:, :], in0=nf[:, :], scalar=-TWO_PI_LO, in1=rr[:, :], op0=ALU.mult, op1=ALU.add),
              reads=["nf", "rr"], writes=["rr"])
        P.dve(lambda e: e.tensor_scalar(out=mm[:, :], in0=rr[:, :], scalar1=PI, scalar2=-2.0 * PI, op0=ALU.is_gt, op1=ALU.mult),
               reads=["rr"], writes=["mm"])
        P.dve(lambda e: e.tensor_tensor(out=rr[:, :], in0=rr[:, :], in1=mm[:, :], op=ALU.add), reads=["rr", "mm"], writes=["rr"])
        P.dve(lambda e: e.tensor_scalar(out=mm[:, :], in0=rr[:, :], scalar1=-PI, scalar2=2.0 * PI, op0=ALU.is_lt, op1=ALU.mult),
               reads=["rr"], writes=["mm"])
        P.dve(lambda e: e.tensor_tensor(out=rr[:, :], in0=rr[:, :], in1=mm[:, :], op=ALU.add), reads=["rr", "mm"], writes=["rr"])
        P.dve(lambda e: e.tensor_scalar(out=cc[:, :], in0=rr[:, :], scalar1=0.5 * PI, scalar2=None, op0=ALU.add), reads=["rr"], writes=["cc"])
        P.dve(lambda e: e.tensor_scalar(out=mm[:, :], in0=cc[:, :], scalar1=PI, scalar2=-2.0 * PI, op0=ALU.is_gt, op1=ALU.mult),
               reads=["cc"], writes=["mm"])
        P.dve(lambda e: e.tensor_tensor(out=cc[:, :], in0=cc[:, :], in1=mm[:, :], op=ALU.add), reads=["cc", "mm"], writes=["cc"])
        P.act(lambda e: e.activation(out=sinT[:, :], in_=rr[:, :], func=AF.Sin), reads=["rr"], writes=["sinT"])
        P.act(lambda e: e.activation(out=cosT[:, :], in_=cc[:, :], func=AF.Sin), reads=["cc"], writes=["cosT"])
        for hp in range(8):
            b2 = hp % 2
            p1, p2 = psq[2 * b2], psq[2 * b2 + 1]
            k1, k2 = "psq_%d" % (2 * b2), "psq_%d" % (2 * b2 + 1)
            for half, pq, kq in ((0, p1, k1), (1, p2, k2)):
                c0 = (2 * hp + half) * 128
                for kc in range(8):
                    P.pe(lambda e, kc=kc, c0=c0, pq=pq: e.matmul(pq[:, :], lhsT=Wq[:, kc, c0:c0 + 128], rhs=xT[:, kc, :],
                                                                 start=(kc == 0), stop=(kc == 7)),
                         reads=["xT", "Wq_%d" % (c0 // 512)], writes=[kq])
            a_, b_, c_, d_ = ta[b2], tb[b2], tc_[b2], td[b2]
            sfx = "_0"
            P.dve(lambda e, p1=p1, a_=a_: e.tensor_tensor(out=a_[:, :], in0=p1[:, :], in1=cosT[:, :], op=ALU.mult), reads=[k1, "cosT"], writes=["ta" + sfx])
            P.dve(lambda e, p2=p2, b_=b_: e.tensor_tensor(out=b_[:, :], in0=p2[:, :], in1=sinT[:, :], op=ALU.mult), reads=[k2, "sinT"], writes=["tb" + sfx])
            P.dve(lambda e, p1=p1, c_=c_: e.tensor_tensor(out=c_[:, :], in0=p1[:, :], in1=sinT[:, :], op=ALU.mult), reads=[k1, "sinT"], writes=["tc" + sfx])
            P.dve(lambda e, p2=p2, d_=d_: e.tensor_tensor(out=d_[:, :], in0=p2[:, :], in1=cosT[:, :], op=ALU.mult), reads=[k2, "cosT"], writes=["td" + sfx])
            P.pool(lambda e, hp=hp, a_=a_, b_=b_: e.tensor_tensor(out=qkT[:, 2 * hp, :], in0=a_[:, :], in1=b_[:, :], op=ALU.subtract),
                   reads=["ta" + sfx, "tb" + sfx], writes=["qkT"])
            P.pool(lambda e, hp=hp, c_=c_, d_=d_: e.tensor_tensor(out=qkT[:, 2 * hp + 1, :], in0=c_[:, :], in1=d_[:, :], op=ALU.add),
                   reads=["tc" + sfx, "td" + sfx], writes=["qkT"])
        for sub in range(4):
            ti = st_i * 4 + sub
            P.dma(lambda e, ti=ti, sub=sub: e.dma_start(out=g.QS[ti], in_=qkT[:, 0:8, sub * 128:(sub + 1) * 128]), reads=["qkT"], writes=["QS_%d" % ti])
            P.dma(lambda e, ti=ti, sub=sub: e.dma_start(out=g.KS[ti], in_=qkT[:, 8:16, sub * 128:(sub + 1) * 128]), reads=["qkT"], writes=["KS_%d" % ti])
    ph.finish()


def _ret_pass2(nc, g, li, xsrc, xdst):
    j = li // 2
    ph = Phase(nc, "rq%d" % li)
    P = ph.P
    cf, cb = g.cf, g.cb
    Wo = ph.sb([128, 16, D], BF16, "Wo")
    wsrc = g.d_ret_w_o[j].rearrange("(k p) f -> p k f", p=128)
    for q in range(2):
        P.dma(lambda e, q=q: e.dma_start(out=Wo[:, q * 8:(q + 1) * 8, :], in_=wsrc[:, q * 8:(q + 1) * 8, :]), writes=["Wo"], q=POOL)
    pm = ph.sb([128, PM_N], F32, "pm")
    P.dma(lambda e: e.dma_start(out=pm[:, :], in_=g.d_pbmix[li]), writes=["PM"])
    qT = [ph.sb([128, 8, 128], BF16, "qT") for _ in range(2)]
    kT = [ph.sb([128, 8, 128], BF16, "kT") for _ in range(2)]
    vrow_ = [ph.sb([128, 2 * D], BF16, "vrow") for _ in range(2)]
    srow_ = [ph.sb([128, 2 * D], BF16, "srow") for _ in range(2)]
    xin = [ph.sb([128, D], F32, "xin") for _ in range(2)]
    S = ph.sb([128, RH, 2, 512], F32, "S")
    Sbf = ph.sb([128, RH, 2, 512], BF16, "Sbf")
    qxT = ph.sb([128, 8, 128], BF16, "qxT")
    kz = ph.sb([128, D], BF16, "kz")
    PT = [ph.sb([128, 128], BF16, "PT") for _ in range(2)]
    on = [ph.sb([128, 512], F32, "on") for _ in range(2)]
    y = ph.sb([128, 2 * D], BF16, "y")
    yT = ph.sb([128, 16, 128], BF16, "yT")
    rbuf = [ph.sb([128, D], F32, "r") for _ in range(2)]
    gst_ = ph.sb([128, RH, 6], F32, "gst")
    gmv_ = ph.sb([128, RH, 2], F32, "gmv")
    grs_ = ph.sb([128, RH, 1], F32, "grs")
    gnm_ = ph.sb([128, RH, 1], F32, "gnm")
    bufs = _RW()
    bufs.x1 = [ph.sb([128, D], F32, "x1") for _ in range(2)]
    bufs.x1bf = [ph.sb([128, D], BF16, "x1bf") for _ in range(2)]
    bufs.st = ph.sb([128, 12], F32, "st")
    bufs.mv = ph.sb([128, 2], F32, "mv")
    bufs.rs = ph.sb([128, 2], F32, "rs")
    RWk = _router_work(ph)
    pss = ph.psum("pss")
    pso = [ph.psum("pso") for _ in range(2)]
    pst = [ph.psum("pst") for _ in range(2)]
    psX = [ph.psum("psX") for _ in range(2)]
    psR = ph.psum("psR")
    psX_bf = [p[:, :].bitcast(BF16) for p in psX]
    P.dve(lambda e: e.memset(S[:, :, :, :], 0.0), writes=["S"])
    P.dve(lambda e: e.memset(Sbf[:, :, :, :], 0.0), writes=["Sbf"])
    def load2(ti):
        b = ti % 2
        P.dma(lambda e, b=b, ti=ti: e.dma_start(out=qT[b][:, :, :], in_=g.QS[ti]), writes=["qT_%d" % b])
        P.dma(lambda e, b=b, ti=ti: e.dma_start(out=kT[b][:, :, :], in_=g.KS[ti]), writes=["kT_%d" % b])
        P.dma(lambda e, b=b, ti=ti: e.dma_start(out=vrow_[b][:, :], in_=g.VS[ti * 128:(ti + 1) * 128, :]), writes=["vrow_%d" % b])
        P.dma(lambda e, b=b, ti=ti: e.dma_start(out=srow_[b][:, :], in_=g.SG[ti * 128:(ti + 1) * 128, :]), writes=["srow_%d" % b])
        P.dma(lambda e, b=b, ti=ti: e.dma_start(out=xin[b][:, :], in_=xsrc[ti * 128:(ti + 1) * 128, :]), writes=["xin_%d" % b])

    load2(0)
    for ti in range(NT):
        b = ti % 2
        kq, kk_ = "qT_%d" % b, "kT_%d" % b
        xi = xin[b]
        kxi = "xin_%d" % b
        vrow, srow = vrow_[b], srow_[b]
        kvr, ksr = "vrow_%d" % b, "srow_%d" % b
        if ti + 1 < NT:
            load2(ti + 1)
        P.dve(lambda e, b=b: e.tensor_tensor(out=qxT[:, :, :], in0=qT[b][:, :, :],
                                             in1=cb[:, CB_XI:CB_XI + 1024].rearrange("p (k t) -> p k t", k=8), op=ALU.mult),
              reads=[kq], writes=["qxT"])
        for kc in range(8):
            P.pe(lambda e, b=b, kc=kc: e.transpose(out=psX_bf[0][:, kc * 128:(kc + 1) * 128], in_=kT[b][:, kc, :],
                                                   identity=cb[:, CB_IDENT:CB_IDENT + 128]), reads=[kk_], writes=["psT2_0"])
        for h in range(RH):
            P.act(lambda e, h=h: e.activation(out=kz[:, h * 256:(h + 1) * 256], in_=psX_bf[0][:, h * 256:(h + 1) * 256], func=AF.Copy,
                                              scale=cf[:, CF_ZETA + h:CF_ZETA + h + 1]), reads=["psT2_0"], writes=["kz"])
        for h in range(RH):
            pb2 = h % 2
            po = pso[pb2]
            kpo = "pso_%d" % pb2
            for c in range(2):
                P.pe(lambda e, b=b, h=h, c=c: e.matmul(pss[:, (h % 4) * 128:(h % 4 + 1) * 128], lhsT=kT[b][:, 2 * h + c, :], rhs=qT[b][:, 2 * h + c, :],
                                                       start=(c == 0), stop=(c == 1)), reads=[kq, kk_], writes=["pss_%d" % h])
            pt = PT[pb2]
            kpt = "PT_%d" % pb2
            P.dve(lambda e, h=h, pt=pt: e.tensor_tensor(out=pt[:, :], in0=pss[:, (h % 4) * 128:(h % 4 + 1) * 128],
                                                        in1=cf[:, CF_MT + h * 128:CF_MT + (h + 1) * 128], op=ALU.mult),
                  reads=["pss_%d" % h], writes=[kpt])
            P.pe(lambda e, h=h, pt=pt, po=po, vrow=vrow: e.matmul(po[:, :], lhsT=pt[:, :], rhs=vrow[:, h * 512:(h + 1) * 512], start=True, stop=False),
                 reads=[kpt, kvr], writes=[kpo])
            for c in range(2):
                P.pe(lambda e, h=h, c=c, po=po: e.matmul(po[:, :], lhsT=qxT[:, 2 * h + c, :], rhs=Sbf[:, h, c, :], start=False, stop=(c == 1)),
                     reads=["qxT", "Sbf_%d" % h], writes=[kpo])
            for c in range(2):
                P.pe(lambda e, h=h, c=c, vrow=vrow: e.matmul(pst[c][:, :], lhsT=kz[:, h * 256 + c * 128:h * 256 + (c + 1) * 128], rhs=vrow[:, h * 512:(h + 1) * 512],
                                                  start=True, stop=True), reads=["kz", kvr], writes=["pst_%d" % c])
                P.dve(lambda e, h=h, c=c: e.scalar_tensor_tensor(out=S[:, h, c, :], in0=S[:, h, c, :], scalar=float(GAMMA[h] ** 128.0), in1=pst[c][:, :],
                                                                 op0=ALU.mult, op1=ALU.add), reads=["pst_%d" % c, "S"], writes=["S"])
                P.act(lambda e, h=h, c=c: e.copy(out=Sbf[:, h, c, :], in_=S[:, h, c, :]), reads=["S"], writes=["Sbf_%d" % h])
            gst, gmv, grs, gnm = gst_[:, h, :], gmv_[:, h, :], grs_[:, h, :], gnm_[:, h, :]
            kh_ = "_%d" % h
            P.dve(lambda e, po=po, gst=gst: e.bn_stats(out=gst[:, 0:6], in_=po[:, :]), reads=[kpo], writes=["gst" + kh_])
            P.dve(lambda e, gst=gst, gmv=gmv: e.bn_aggr(out=gmv[:, 0:2], in_=gst[:, 0:6]), reads=["gst" + kh_], writes=["gmv" + kh_])
            P.act(lambda e, grs=grs, gmv=gmv: e.activation(out=grs[:, 0:1], in_=gmv[:, 1:2], func=AF.Sqrt, bias=EPS_AP[0], scale=1.0),
                  reads=["gmv" + kh_], writes=["grs" + kh_])
            P.dve(lambda e, grs=grs: e.reciprocal(out=grs[:, 0:1], in_=grs[:, 0:1]), reads=["grs" + kh_], writes=["grs" + kh_])
            P.dve(lambda e, gnm=gnm, gmv=gmv, grs=grs: e.scalar_tensor_tensor(out=gnm[:, 0:1], in0=gmv[:, 0:1], scalar=-1.0, in1=grs[:, 0:1],
                                                                              op0=ALU.mult, op1=ALU.mult),
                  reads=["gmv" + kh_, "grs" + kh_], writes=["gnm" + kh_])
            o_ = on[pb2]
            kon = "on_%d" % pb2
            P.act(lambda e, po=po, o_=o_, gnm=gnm, grs=grs: e.activation(out=o_[:, :], in_=po[:, :], func=AF.Identity, bias=gnm[:, 0:1], scale=grs[:, 0:1]),
                  reads=[kpo, "grs" + kh_, "gnm" + kh_], writes=[kon])
            P.dve(lambda e, h=h, o_=o_: e.tensor_tensor(out=o_[:, :], in0=o_[:, :], in1=pm[:, h * 512:(h + 1) * 512], op=ALU.mult),
                  reads=[kon, "PM"], writes=[kon])
            P.dve(lambda e, h=h, o_=o_: e.tensor_tensor(out=o_[:, :], in0=o_[:, :], in1=pm[:, 2 * D + h * 512:2 * D + (h + 1) * 512], op=ALU.add),
                  reads=[kon, "PM"], writes=[kon])
            P.dve(lambda e, h=h, o_=o_, srow=srow: e.tensor_tensor(out=y[:, h * 512:(h + 1) * 512], in0=o_[:, :], in1=srow[:, h * 512:(h + 1) * 512], op=ALU.mult),
                  reads=[kon, ksr], writes=["y"])
        for half in range(2):
            for q in range(8):
                kc = half * 8 + q
                P.pe(lambda e, kc=kc, q=q, half=half: e.transpose(out=psX_bf[half][:, q * 128:(q + 1) * 128], in_=y[:, kc * 128:(kc + 1) * 128],
                                                                  identity=cb[:, CB_IDENT:CB_IDENT + 128]), reads=["y"], writes=["psT2_%d" % half])
            if half == 0:
                P.act(lambda e: e.copy(out=yT[:, 0:8, :], in_=psX_bf[0].rearrange("p (k t) -> p k t", k=8)), reads=["psT2_0"], writes=["yT"])
            else:
                P.dve(lambda e: e.tensor_copy(out=yT[:, 8:16, :], in_=psX_bf[1].rearrange("p (k t) -> p k t", k=8)), reads=["psT2_1"], writes=["yT"])
        r = rbuf[b]
        kr = "r_%d" % b
        for half in range(2):
            po = pso[half]
            kpo = "pso_%d" % half
            for kc in range(16):
                P.pe(lambda e, kc=kc, half=half, po=po: e.matmul(po[:, :], lhsT=yT[:, kc, :], rhs=Wo[:, kc, half * 512:(half + 1) * 512],
                                                                 start=(kc == 0), stop=(kc == 15)), reads=["yT", "Wo"], writes=[kpo])
            P.dve(lambda e, half=half, po=po, xi=xi, r=r: e.scalar_tensor_tensor(out=r[:, half * 512:(half + 1) * 512], in0=xi[:, half * 512:(half + 1) * 512],
                                                                                 scalar=ALPHA, in1=po[:, :], op0=ALU.mult, op1=ALU.add),
                  reads=[kpo, kxi], writes=[kr])
        if ti > 0:
            _post_mixer(ph, g, ti - 1, rbuf[(ti - 1) % 2], "r_%d" % ((ti - 1) % 2), RWk, bufs, psR, psX, xdst)
    _post_mixer(ph, g, NT - 1, rbuf[(NT - 1) % 2], "r_%d" % ((NT - 1) % 2), RWk, bufs, psR, psX, xdst)
    ph.finish()


def _rep(v, n=128):
    return np.ascontiguousarray(np.broadcast_to(np.asarray(v, np.float32).reshape(1, -1), (n, np.asarray(v).size)))


def prepare_shared(inputs):
    cf, cb, _ = _host_consts()
    sh = {"constf": cf, "constb": cb}
    for k in ("conv_w_pw1", "conv_w_pw2", "ret_w_qkvg", "ret_w_o", "moe_w_gate", "moe_w_up", "moe_w_down"):
        sh[k] = np.ascontiguousarray(inputs[k], dtype=np.float32)
    pbln = np.zeros((DEPTH, 128, PB_N), np.float32)
    pbmix = np.zeros((DEPTH, 128, PM_N), np.float32)
    wr = np.zeros((DEPTH, 128, 8, 36), np.float32)
    for i in range(DEPTH):
        pbln[i, :, PB_LN1G:PB_LN1G + D] = _rep(inputs["ln1_g"][i])
        pbln[i, :, PB_LN1B:PB_LN1B + D] = _rep(inputs["ln1_b"][i])
        pbln[i, :, PB_LN2G:PB_LN2G + D] = _rep(inputs["ln2_g"][i])
        pbln[i, :, PB_LN2B:PB_LN2B + D] = _rep(inputs["ln2_b"][i])
        pbln[i, :, PB_RB:PB_RB + 4] = _rep(inputs["moe_b_grp"][i])
        pbln[i, :, PB_RB + 4:PB_RB + 36] = _rep(inputs["moe_b_route"][i])
        wcat = np.concatenate([inputs["moe_w_grp"][i], inputs["moe_w_route"][i]], axis=1)
        wr[i] = wcat.reshape(8, 128, 36).transpose(1, 0, 2)
        j = i // 2
        if i % 2 == 0:
            pbmix[i, :, 0:D] = _rep(inputs["conv_b_pw2"][j])
        else:
            pbmix[i, :, 0:2 * D] = _rep(inputs["ret_gn_g"][j])
            pbmix[i, :, 2 * D:4 * D] = _rep(inputs["ret_gn_b"][j])
    pp = np.zeros((2, 128, PP_N), np.float32)
    for j in range(2):
        pp[j, :, PP_B1:PP_B1 + 16] = np.asarray(inputs["conv_b_pw1"][j]).reshape(16, 128).T
        wdw = np.asarray(inputs["conv_w_dw"][j])
        pp[j, :, PP_WDW:PP_WDW + 248] = wdw.reshape(CW, 8, 128).transpose(2, 1, 0).reshape(128, 248)
        pp[j, :, PP_BDW:PP_BDW + 8] = np.asarray(inputs["conv_b_dw"][j]).reshape(8, 128).T
        pp[j, :, PP_LNG:PP_LNG + 8] = np.asarray(inputs["conv_ln_g"][j]).reshape(8, 128).T
        pp[j, :, PP_LNB:PP_LNB + 8] = np.asarray(inputs["conv_ln_b"][j]).reshape(8, 128).T
    sh["pbln"], sh["pbmix"], sh["pp"], sh["wr"] = pbln, pbmix, pp, wr
    return sh


_NC_CACHE = {}


def kernel(**inputs):
    x = np.asarray(inputs["x"], np.float32)
    pos = np.asarray(inputs["positions"], np.int32)
    sh = prepare_shared(inputs)
    if "nc" not in _NC_CACHE:
        _NC_CACHE["nc"] = build_program()
    nc = _NC_CACHE["nc"]
    in_maps = []
    for c in range(8):
        m = dict(sh)
        m["x"] = np.ascontiguousarray(x[c])
        m["posb"] = np.ascontiguousarray(np.broadcast_to(pos[c][None, :], (128, SEQ)))
        in_maps.append(m)
    res = run_bass_kernel_spmd(nc, in_maps, core_ids=list(range(8)))
    return np.stack([np.asarray(r["out"], np.float32) for r in res.results], axis=0)
```

```python
import contextlib
import numpy as np
import ml_dtypes
import concourse.bass as bass
import concourse.mybir as mybir
from concourse.bass_utils import run_bass_kernel_spmd

F32 = mybir.dt.float32
BF16 = mybir.dt.bfloat16
I32 = mybir.dt.int32
ALU = mybir.AluOpType
AF = mybir.ActivationFunctionType
AX = mybir.AxisListType

PE, ACT, DVE, POOL, SP = "pe", "act", "dve", "pool", "sp"
ENGS = (PE, ACT, DVE, POOL, SP)

D = 1024
SEQ = 4096
NT = SEQ // 128
DEPTH = 4
NE = 32
FF = 512
SLOT = 512
NSLOT_T = 2 * SEQ // SLOT + NE
ALPHA = (2.0 * DEPTH) ** 0.25
EPS = 1e-5
CW = 31
NPE_TAPS = 24
RH = 4


class _Op:
    __slots__ = ("eng", "fn", "deps", "dma", "sig", "dsem", "dval", "dprev", "pos", "need_sig")


class Prog:
    def __init__(self, nc, n_dma_sems=8, same_eng_dist=3):
        self.nc = nc
        self.ops = []
        self.last_writer = {}
        self.readers = {}
        self.eng_count = {e: 0 for e in ENGS}
        self.n_dma_sems = n_dma_sems
        self.same_eng_dist = same_eng_dist

    def add(self, eng, fn, reads=(), writes=(), dma=False):
        op = _Op()
        op.eng, op.fn, op.dma = eng, fn, dma
        op.sig = None
        op.need_sig = False
        op.pos = self.eng_count[eng]
        self.eng_count[eng] += 1
        idx = len(self.ops)
        deps = set()
        for k in reads:
            w = self.last_writer.get(k)
            if w is not None:
                deps.add((w, True))
        for k in writes:
            w = self.last_writer.get(k)
            if w is not None:
                deps.add((w, False))
            for r in self.readers.get(k, ()):
                if r != idx:
                    deps.add((r, False))
        for k in reads:
            self.readers.setdefault(k, []).append(idx)
        for k in writes:
            self.last_writer[k] = idx
            self.readers[k] = []
        real = {}
        for d, raw in deps:
            dop = self.ops[d]
            if dop.dma or dop.eng != eng or dma:
                real[d] = True
            elif raw and eng != PE and (op.pos - dop.pos) < self.same_eng_dist:
                real[d] = True
        latest = {}
        keep = []
        for d in real:
            dop = self.ops[d]
            if dop.dma:
                keep.append(d)
            elif d > latest.get(dop.eng, -1):
                latest[dop.eng] = d
        op.deps = sorted(keep + list(latest.values()))
        for d in op.deps:
            if not self.ops[d].dma:
                self.ops[d].need_sig = True
        self.ops.append(op)
        return idx

    def pe(self, fn, reads=(), writes=()):
        return self.add(PE, fn, reads, writes)

    def act(self, fn, reads=(), writes=()):
        return self.add(ACT, fn, reads, writes)

    def dve(self, fn, reads=(), writes=()):
        return self.add(DVE, fn, reads, writes)

    def pool(self, fn, reads=(), writes=()):
        return self.add(POOL, fn, reads, writes)

    def ve(self, eng, fn, reads=(), writes=()):
        return self.add(eng, fn, reads, writes)

    def dma(self, fn, reads=(), writes=(), q=SP):
        return self.add(q, fn, reads, writes, dma=True)

    def emit(self):
        nc = self.nc
        ops = self.ops
        pool = _sem_pool(nc, self.n_dma_sems)
        sigc = dict(pool["eval"])
        sig0 = dict(pool["eval"])
        dcount = dict(pool["dcount"])
        dsem_val = dict(pool["dval"])
        used_d = set()
        last_of = {}
        for i, op in enumerate(ops):
            last_of[op.eng] = i
        for e, i in last_of.items():
            if not ops[i].dma:
                ops[i].need_sig = True
        for op in ops:
            if op.dma:
                j = dcount[op.eng] % self.n_dma_sems
                dcount[op.eng] += 1
                key = (op.eng, j)
                prev = dsem_val.get(key, 0)
                op.dsem, op.dprev, op.dval = key, prev, prev + 16
                dsem_val[key] = op.dval
                used_d.add(key)
            elif op.need_sig:
                sigc[op.eng] += 1
                op.sig = sigc[op.eng]

        esem = pool["esem"]
        dsem = pool["dsem"]
        pool["eval"] = dict(sigc)
        pool["dcount"] = dict(dcount)
        pool["dval"] = dict(dsem_val)
        with contextlib.ExitStack() as st:
            block = st.enter_context(nc.Block())

            def run_engine(ename, eh):
                waited = {}

                def wait(sem_key, semh, val):
                    if waited.get(sem_key, 0) >= val:
                        return
                    eh.wait_ge(semh, val)
                    waited[sem_key] = val

                for op in ops:
                    if op.eng != ename:
                        continue
                    for d in op.deps:
                        dop = ops[d]
                        if dop.dma:
                            wait(dop.dsem, dsem[dop.dsem], dop.dval)
                        else:
                            wait(dop.eng, esem[dop.eng], dop.sig)
                    if op.dma:
                        if op.dprev > 0:
                            wait(op.dsem, dsem[op.dsem], op.dprev)
                        op.fn(eh).then_inc(dsem[op.dsem], 16)
                    else:
                        ins = op.fn(eh)
                        if op.sig is not None:
                            ins.then_inc(esem[ename], 1)
                for key in sorted(used_d):
                    wait(key, dsem[key], dsem_val[key])
                for e in ENGS:
                    if sigc[e] > sig0[e]:
                        wait(e, esem[e], sigc[e])

            @block.tensor
            def _(eh):
                run_engine(PE, eh)

            @block.scalar
            def _(eh):
                run_engine(ACT, eh)

            @block.vector
            def _(eh):
                run_engine(DVE, eh)

            @block.gpsimd
            def _(eh):
                run_engine(POOL, eh)

            @block.sync
            def _(eh):
                run_engine(SP, eh)


_SEM_POOLS = {}


def _sem_pool(nc, n_dma_sems):
    key = id(nc)
    if key not in _SEM_POOLS:
        pool = {"esem": {e: nc.alloc_semaphore(name="s_" + e) for e in ENGS}, "dsem": {}, "dval": {}}
        pool["eval"] = {e: 0 for e in ENGS}
        pool["dcount"] = {e: 0 for e in ENGS}
        for e in (SP, ACT, POOL):
            for j in range(n_dma_sems):
                pool["dsem"][(e, j)] = nc.alloc_semaphore(name="d_%s_%d" % (e, j))
        _SEM_POOLS[key] = pool
    return _SEM_POOLS[key]


class Phase:
    def __init__(self, nc, name):
        self.nc = nc
        self.name = name
        self.st = contextlib.ExitStack()
        self.P = Prog(nc)
        self.n = 0

    def sb(self, shape, dt, tag="t"):
        self.n += 1
        return self.st.enter_context(self.nc.sbuf_tensor("%s_%s%d" % (self.name, tag, self.n), list(shape), dt))

    def psum(self, tag="ps"):
        self.n += 1
        return self.st.enter_context(self.nc.psum_tensor("%s_%s%d" % (self.name, tag, self.n), [128, 512], F32))

    def finish(self):
        self.P.emit()
        self.st.close()


CF_IDENT = 0
CF_ONES = 128
CF_MT = 256
CF_THR = 768
CF_IOTA = 816
CF_INVF = 824
CF_ZETA = 825
CF_EPS = 829
CF_N = 832
CB_IDENT = 0
CB_ONES = 128
CB_TRIU = 256
CB_XI = 384
CB_N = 384 + 1024


def _host_consts():
    cf = np.zeros((128, CF_N), np.float32)
    cf[:, CF_IDENT:CF_IDENT + 128] = np.eye(128, dtype=np.float32)
    cf[:, CF_ONES:CF_ONES + 128] = 1.0
    gam = 1.0 - 2.0 ** (-5.0 - np.arange(RH, dtype=np.float64))
    i = np.arange(128)
    for h in range(RH):
        ci, cj = i[:, None] // 64, i[None, :] // 64
        dist = (i[:, None] - i[None, :]).astype(np.float64)
        M = np.where(ci == cj, gam[h] ** np.abs(dist), np.where(ci > cj, gam[h] ** dist, 0.0))
        cf[:, CF_MT + h * 128: CF_MT + (h + 1) * 128] = (M.T * (256.0 ** -0.5)).astype(np.float32)
        cf[:, CF_ZETA + h] = (gam[h] ** (127.0 - i) * (256.0 ** -0.5)).astype(np.float32)
    cf[:, CF_THR:CF_THR + NSLOT_T] = (np.arange(NSLOT_T) * SLOT).astype(np.float32)[None, :]
    cf[:, CF_IOTA] = i
    cf[:, CF_EPS] = EPS
    for c in range(4):
        cf[:, CF_IOTA + 1 + c] = c * 128 + i
    cf[:, CF_INVF] = (np.float32(10000.0) ** (-(np.arange(128, dtype=np.float32)) / np.float32(128))).astype(np.float32)
    cb = np.zeros((128, CB_N), np.float32)
    cb[:, CB_IDENT:CB_IDENT + 128] = np.eye(128)
    cb[:, CB_ONES:CB_ONES + 128] = 1.0
    cb[:, CB_TRIU:CB_TRIU + 128] = (i[:, None] < i[None, :]).astype(np.float32)
    for kc in range(8):
        h = kc // 2
        cb[:, CB_XI + kc * 128: CB_XI + (kc + 1) * 128] = (gam[h] ** (i + 1.0))[None, :]
    return cf, cb.astype(ml_dtypes.bfloat16), gam


GAMMA = 1.0 - 2.0 ** (-5.0 - np.arange(RH, dtype=np.float64))

PB_LN1G, PB_LN1B, PB_LN2G, PB_LN2B, PB_RB = 0, 1024, 2048, 3072, 4096
PB_N = 4160
PM_N = 4096
PP_B1, PP_WDW, PP_BDW, PP_LNG, PP_LNB = 0, 16, 16 + 248, 16 + 248 + 8, 16 + 248 + 16
PP_N = 16 + 248 + 24


class G:
    pass


EPS_AP = [None]


def _ln_token_major(P, eng2, r, x1, g_ap, b_ap, tmp_stats, tmp_mv, tmp_rs, key_r, key_out, tagk):
    P.dve(lambda e: e.bn_stats(out=tmp_stats[:, 0:6], in_=r[:, 0:512]), reads=[key_r], writes=[tagk + "st"])
    P.dve(lambda e: e.bn_stats(out=tmp_stats[:, 6:12], in_=r[:, 512:1024]), reads=[key_r], writes=[tagk + "st"])
    P.dve(lambda e: e.bn_aggr(out=tmp_mv[:, 0:2], in_=tmp_stats[:, 0:12]), reads=[tagk + "st"], writes=[tagk + "mv"])
    P.act(lambda e: e.activation(out=tmp_rs[:, 0:1], in_=tmp_mv[:, 1:2], func=AF.Sqrt, bias=EPS_AP[0], scale=1.0),
          reads=[tagk + "mv"], writes=[tagk + "rs"])
    P.dve(lambda e: e.reciprocal(out=tmp_rs[:, 0:1], in_=tmp_rs[:, 0:1]), reads=[tagk + "rs"], writes=[tagk + "rs"])
    P.dve(lambda e: e.scalar_tensor_tensor(out=tmp_rs[:, 1:2], in0=tmp_mv[:, 0:1], scalar=-1.0, in1=tmp_rs[:, 0:1], op0=ALU.mult, op1=ALU.mult),
          reads=[tagk + "mv", tagk + "rs"], writes=[tagk + "rs"])
    P.act(lambda e: e.activation(out=r[:, :], in_=r[:, :], func=AF.Identity, bias=tmp_rs[:, 1:2], scale=tmp_rs[:, 0:1]),
          reads=[key_r, tagk + "rs"], writes=[key_r])
    P.ve(eng2, lambda e: e.tensor_tensor(out=r[:, :], in0=r[:, :], in1=g_ap, op=ALU.mult), reads=[key_r, "PB"], writes=[key_r])
    P.ve(eng2, lambda e: e.tensor_tensor(out=x1[:, :], in0=r[:, :], in1=b_ap, op=ALU.add), reads=[key_r, "PB"], writes=[key_out])


def _router_tile(ph, g, ti, x1, key_x1, W, psR, psT2):
    P = ph.P
    cf = g.cf
    for half in range(2):
        for q in range(4):
            kc = half * 4 + q
            P.pe(lambda e, kc=kc, q=q, half=half: e.transpose(out=psT2[half][:, q * 128:(q + 1) * 128],
                                                              in_=x1[:, kc * 128:(kc + 1) * 128],
                                                              identity=cf[:, CF_IDENT:CF_IDENT + 128]),
                 reads=[key_x1], writes=["psT2_%d" % half])
        P.act(lambda e, half=half: e.copy(out=W.x1T[:, half * 4:(half + 1) * 4, :],
                                          in_=psT2[half][:, :].rearrange("p (k t) -> p k t", k=4)),
              reads=["psT2_%d" % half], writes=["x1T"])
    for kc in range(8):
        P.pe(lambda e, kc=kc: e.matmul(psR[:, 0:36], lhsT=W.x1T[:, kc, :], rhs=g.wr[:, kc, :],
                                       start=(kc == 0), stop=(kc == 7)), reads=["x1T", "WR"], writes=["psR"])
    P.dve(lambda e: e.tensor_tensor(out=g.LALL[:, ti, :], in0=psR[:, 0:36], in1=g.pb[:, PB_RB:PB_RB + 36], op=ALU.add),
          reads=["psR", "PB"], writes=["LALL"])


class _RW:
    pass


def _router_work(ph):
    W = _RW()
    W.x1T = ph.sb([128, 8, 128], F32, "x1T")
    return W


def _post_mixer(ph, g, ti, r, key_r, W, bufs, psR, psT2, xdst):
    P = ph.P
    x1 = bufs.x1[ti % 2]
    kx1 = "x1_%d" % (0 if bufs.x1[0] is bufs.x1[1] else ti % 2)
    _ln_token_major(P, DVE, r, x1, g.pb[:, PB_LN1G:PB_LN1G + 1024], g.pb[:, PB_LN1B:PB_LN1B + 1024],
                    bufs.st, bufs.mv, bufs.rs, key_r, kx1, "ln1")
    P.dma(lambda e: e.dma_start(out=xdst[ti * 128:(ti + 1) * 128, :], in_=x1[:, :]), reads=[kx1], writes=["XB_%d" % ti])
    xb = bufs.x1bf[ti % 2]
    kxb = "x1bf_%d" % (0 if bufs.x1bf[0] is bufs.x1bf[1] else ti % 2)
    P.act(lambda e: e.copy(out=xb[:, :].rearrange("t (kk p) -> t kk p", p=128),
                           in_=x1[:, :].rearrange("t (p kk) -> t kk p", kk=8)), reads=[kx1], writes=[kxb])
    P.dma(lambda e: e.dma_start(out=g.xb16[ti * 128:(ti + 1) * 128, :], in_=xb[:, :]), reads=[kxb], writes=["xb16_%d" % ti])
    _router_tile(ph, g, ti, x1, kx1, W, psR, psT2)


def _load_layer_params(ph, g, li):
    P = ph.P
    P.dma(lambda e: e.dma_start(out=g.pb[:, :], in_=g.d_pbln[li]), writes=["PB"])
    P.dma(lambda e: e.dma_start(out=g.wr[:, :, :], in_=g.d_wr[li]), writes=["WR"])


def conv_phase(nc, g, li, xsrc, xdst):
    j = li // 2
    ph = Phase(nc, "cv%d" % li)
    P = ph.P
    cf, cb = g.cf, g.cb
    _load_layer_params(ph, g, li)
    W1b = ph.sb([128, 8, 2048], BF16, "W1")
    W2b = ph.sb([128, 8, 1024], BF16, "W2")
    pm = ph.sb([128, 1024], F32, "pm")
    pp = ph.sb([128, PP_N], F32, "pp")
    for q in range(4):
        P.dma(lambda e, q=q: e.dma_start(out=W1b[:, :, q * 512:(q + 1) * 512],
                                         in_=g.d_conv_w_pw1[j].rearrange("(k p) f -> p k f", p=128)[:, :, q * 512:(q + 1) * 512]),
              writes=["W1_%d" % q], q=POOL)
    P.dma(lambda e: e.dma_start(out=W2b[:, :, :], in_=g.d_conv_w_pw2[j].rearrange("(k p) f -> p k f", p=128)),
          writes=["W2"], q=POOL)
    P.dma(lambda e: e.dma_start(out=pm[:, :], in_=g.d_pbmix[li][:, 0:1024]), writes=["PM"])
    P.dma(lambda e: e.dma_start(out=pp[:, :], in_=g.d_pp[j]), writes=["PP"])

    xin = [ph.sb([128, 1024], F32, "xin") for _ in range(2)]
    xbf = ph.sb([128, 1024], BF16, "xbf")
    xT = ph.sb([128, 8, 512], BF16, "xT")
    hg = ph.sb([128, 8, 512 + CW - 1], BF16, "hg")
    NPE = NPE_TAPS
    diag = ph.sb([128, 8 * NPE, 128], BF16, "diag")
    for c in range(8):
        for jj in range(NPE):
            P.act(lambda e, c=c, jj=jj: e.activation(out=diag[:, c * NPE + jj, :], in_=cf[:, CF_IDENT:CF_IDENT + 128], func=AF.Copy,
                                                     scale=pp[:, PP_WDW + c * CW + jj:PP_WDW + c * CW + jj + 1]),
                  reads=["PP", "cf"], writes=["diag"])
    sig = [ph.sb([128, 512], F32, "sig")] * 2
    cv = ph.sb([128, 8, 512], F32, "cv")
    sq = [ph.sb([128, 512], F32, "sq")] * 2
    mean = ph.sb([128, 512], F32, "mean")
    rstd = ph.sb([128, 512], F32, "rstd")
    nmr = ph.sb([128, 512], F32, "nmr")
    hT = xT
    rbuf = [ph.sb([128, 1024], F32, "r") for _ in range(2)]
    bufs = _RW()
    bufs.x1 = [ph.sb([128, 1024], F32, "x1")] * 2
    bufs.x1bf = [ph.sb([128, 1024], BF16, "x1bf")] * 2
    bufs.st = ph.sb([128, 12], F32, "st")
    bufs.mv = ph.sb([128, 2], F32, "mv")
    bufs.rs = ph.sb([128, 2], F32, "rs")
    RWk = _router_work(ph)
    psT = ph.psum("psT")
    psA = [ph.psum("psA") for _ in range(2)]
    psG = [ph.psum("psG") for _ in range(2)]
    psR = ph.psum("psR")
    psT2 = [ph.psum("psT2") for _ in range(2)]
    psS = [psG[0], psG[1]]
    psT_bf = psT[:, :].bitcast(BF16)

    P.dve(lambda e: e.memset(hg[:, :, 0:CW - 1], 0.0), writes=["hg"])
    conv_eng = [DVE] * 8
    pending = []
    norm_eng = [DVE] * 8
    for st_i in range(NT // 4):
        for sub in range(4):
            ti = st_i * 4 + sub
            xi = xin[ti % 2]
            kxi = "xin_%d" % (ti % 2)
            P.dma(lambda e, xi=xi, ti=ti: e.dma_start(out=xi[:, :], in_=xsrc[ti * 128:(ti + 1) * 128, :]), writes=[kxi])
            P.act(lambda e, xi=xi: e.copy(out=xbf[:, :], in_=xi[:, :]), reads=[kxi], writes=["xbf"])
            for kc in range(8):
                P.pe(lambda e, kc=kc: e.transpose(out=psT_bf[:, kc * 128:(kc + 1) * 128], in_=xbf[:, kc * 128:(kc + 1) * 128],
                                                  identity=cb[:, CB_IDENT:CB_IDENT + 128]), reads=["xbf"], writes=["psT"])
            P.dve(lambda e, sub=sub: e.tensor_copy(out=xT[:, :, sub * 128:(sub + 1) * 128],
                                                   in_=psT_bf.rearrange("p (k t) -> p k t", k=8)), reads=["psT"], writes=["xT"])
        for c in range(8):
            pa, pg = psA[c % 2], psG[c % 2]
            ka, kg = "psA_%d" % (c % 2), "psG_%d" % (c % 2)
            for kc in range(8):
                P.pe(lambda e, kc=kc, c=c, pa=pa: e.matmul(pa[:, :], lhsT=W1b[:, kc, c * 128:(c + 1) * 128], rhs=xT[:, kc, :],
                                                           start=(kc == 0), stop=(kc == 7)),
                     reads=["xT", "W1_%d" % (c // 4)], writes=[ka])
            for kc in range(8):
                P.pe(lambda e, kc=kc, c=c, pg=pg: e.matmul(pg[:, :], lhsT=W1b[:, kc, 1024 + c * 128:1024 + (c + 1) * 128], rhs=xT[:, kc, :],
                                                           start=(kc == 0), stop=(kc == 7)),
                     reads=["xT", "W1_%d" % (2 + c // 4)], writes=[kg])
            sg = sig[c % 2]
            ks = "sig_0"
            P.act(lambda e, c=c, pg=pg, sg=sg: e.activation(out=sg[:, :], in_=pg[:, :], func=AF.Sigmoid,
                                                            bias=pp[:, PP_B1 + 8 + c:PP_B1 + 9 + c], scale=1.0),
                  reads=[kg, "PP"], writes=[ks])
            P.dve(lambda e, c=c, pa=pa, sg=sg: e.scalar_tensor_tensor(out=hg[:, c, CW - 1:CW - 1 + 512], in0=pa[:, :],
                                                                      scalar=pp[:, PP_B1 + c:PP_B1 + c + 1], in1=sg[:, :],
                                                                      op0=ALU.add, op1=ALU.mult),
                  reads=[ka, ks, "PP"], writes=["hg"])
        cps = [psA[0], psG[0], psA[1], psG[1]]
        cpk = ["psA_0", "psG_0", "psA_1", "psG_1"]
        for c in range(8):
            pc, kpc = cps[c % 4], cpk[c % 4]
            for jj in range(NPE):
                P.pe(lambda e, c=c, jj=jj, pc=pc: e.matmul(pc[:, :], lhsT=diag[:, c * NPE + jj, :], rhs=hg[:, c, jj:jj + 512],
                                                           start=(jj == 0), stop=(jj == NPE - 1)), reads=["hg", "diag"], writes=[kpc])
            P.act(lambda e, c=c, pc=pc: e.activation(out=cv[:, c, :], in_=pc[:, :], func=AF.Identity,
                                                     bias=pp[:, PP_BDW + c:PP_BDW + c + 1], scale=1.0),
                  reads=[kpc, "PP"], writes=["cv_%d" % c])
        for jj in range(NPE, CW):
            for c in range(8):
                P.dve(lambda e, c=c, jj=jj: e.scalar_tensor_tensor(out=cv[:, c, :], in0=hg[:, c, jj:jj + 512],
                                                                   scalar=pp[:, PP_WDW + c * CW + jj:PP_WDW + c * CW + jj + 1],
                                                                   in1=cv[:, c, :], op0=ALU.mult, op1=ALU.add),
                      reads=["hg", "PP", "cv_%d" % c], writes=["cv_%d" % c])
        P.act(lambda e: e.copy(out=hg[:, :, 0:CW - 1], in_=hg[:, :, 512:512 + CW - 1]), reads=["hg"], writes=["hg"])
        for c in range(8):
            s_ = sq[c % 2]
            ksq = "sq_0"
            P.act(lambda e, c=c, s_=s_: e.activation(out=s_[:, :], in_=cv[:, c, :], func=AF.Square), reads=["cv_%d" % c], writes=[ksq])
            P.pe(lambda e, c=c: e.matmul(psS[0][:, :], lhsT=cf[:, CF_ONES:CF_ONES + 128], rhs=cv[:, c, :], start=(c == 0), stop=(c == 7)),
                 reads=["cv_%d" % c], writes=["psG_0"])
            P.pe(lambda e, c=c, s_=s_: e.matmul(psS[1][:, :], lhsT=cf[:, CF_ONES:CF_ONES + 128], rhs=s_[:, :], start=(c == 0), stop=(c == 7)),
                 reads=[ksq], writes=["psG_1"])
        P.act(lambda e: e.mul(out=mean[:, :], in_=psS[0][:, :], mul=1.0 / D), reads=["psG_0"], writes=["mean"])
        P.dve(lambda e: e.tensor_tensor(out=nmr[:, :], in0=mean[:, :], in1=mean[:, :], op=ALU.mult), reads=["mean"], writes=["nmr"])
        P.dve(lambda e: e.scalar_tensor_tensor(out=rstd[:, :], in0=psS[1][:, :], scalar=1.0 / D, in1=nmr[:, :],
                                               op0=ALU.mult, op1=ALU.subtract), reads=["psG_1", "nmr"], writes=["rstd"])
        P.act(lambda e: e.activation(out=rstd[:, :], in_=rstd[:, :], func=AF.Sqrt, bias=EPS_AP[0], scale=1.0),
              reads=["rstd"], writes=["rstd"])
        P.dve(lambda e: e.reciprocal(out=rstd[:, :], in_=rstd[:, :]), reads=["rstd"], writes=["rstd"])
        P.dve(lambda e: e.scalar_tensor_tensor(out=nmr[:, :], in0=mean[:, :], scalar=-1.0, in1=rstd[:, :],
                                               op0=ALU.mult, op1=ALU.mult), reads=["mean", "rstd"], writes=["nmr"])
        for c in range(8):
            eng = norm_eng[c]
            P.ve(eng, lambda e, c=c: e.tensor_tensor(out=cv[:, c, :], in0=cv[:, c, :], in1=rstd[:, :], op=ALU.mult),
                 reads=["cv_%d" % c, "rstd"], writes=["cv_%d" % c])
        for c in range(8):
            eng = norm_eng[c]
            P.ve(eng, lambda e, c=c: e.tensor_tensor(out=cv[:, c, :], in0=cv[:, c, :], in1=nmr[:, :], op=ALU.add),
                 reads=["cv_%d" % c, "nmr"], writes=["cv_%d" % c])
        for c in range(8):
            P.act(lambda e, c=c: e.activation(out=hT[:, c, :], in_=cv[:, c, :], func=AF.Silu,
                                              bias=pp[:, PP_LNB + c:PP_LNB + c + 1], scale=pp[:, PP_LNG + c:PP_LNG + c + 1]),
                  reads=["cv_%d" % c, "PP"], writes=["xT"])
        for sub in range(4):
            ti = st_i * 4 + sub
            xi = xin[ti % 2]
            kxi = "xin_%d" % (ti % 2)
            P.dma(lambda e, xi=xi, ti=ti: e.dma_start(out=xi[:, :], in_=xsrc[ti * 128:(ti + 1) * 128, :]), writes=[kxi])
            r = rbuf[ti % 2]
            kr = "r_%d" % (ti % 2)
            for half in range(2):
                pa = (psA if sub % 2 == 0 else psG)[half]
                ka = ("psA_%d" if sub % 2 == 0 else "psG_%d") % half
                for c in range(8):
                    P.pe(lambda e, c=c, half=half, sub=sub, pa=pa: e.matmul(pa[:, :], lhsT=hT[:, c, sub * 128:(sub + 1) * 128],
                                                                            rhs=W2b[:, c, half * 512:(half + 1) * 512],
                                                                            start=(c == 0), stop=(c == 7)),
                         reads=["xT", "W2"], writes=[ka])
                P.dve(lambda e, half=half, pa=pa, xi=xi, r=r: e.scalar_tensor_tensor(out=r[:, half * 512:(half + 1) * 512],
                                                                                     in0=xi[:, half * 512:(half + 1) * 512], scalar=ALPHA,
                                                                                     in1=pa[:, :], op0=ALU.mult, op1=ALU.add),
                      reads=[ka, kxi], writes=[kr])
            P.dve(lambda e, r=r: e.tensor_tensor(out=r[:, :], in0=r[:, :], in1=pm[:, 0:1024], op=ALU.add), reads=[kr, "PM"], writes=[kr])
            if pending:
                pt_ = pending.pop(0)
                _post_mixer(ph, g, pt_, rbuf[pt_ % 2], "r_%d" % (pt_ % 2), RWk, bufs, psR, psT2, xdst)
            pending.append(ti)
    while pending:
        pt_ = pending.pop(0)
        _post_mixer(ph, g, pt_, rbuf[pt_ % 2], "r_%d" % (pt_ % 2), RWk, bufs, psR, psT2, xdst)
    ph.finish()


def offsets_scatter_phase(nc, g, li):
    ph = Phase(nc, "os%d" % li)
    P = ph.P
    cf, cb = g.cf, g.cb
    LA = g.LALL
    L4 = LA[:, :, 0:4]
    Lr = LA[:, :, 4:36]
    g.RE1 = ph.sb([128, NT, 32], F32, "RE1")
    g.RE2 = ph.sb([128, NT, 32], F32, "RE2")
    g.RPF = ph.sb([128, NT, 32], F32, "RPF")
    gm = ph.sb([128, NT], F32, "gm")
    d4 = ph.sb([128, NT, 4], F32, "d4")
    gs = ph.sb([128, NT], F32, "gs")
    pg = ph.sb([128, NT], F32, "pg")
    ml = ph.sb([128, NT, 32], F32, "ml")
    m1 = ph.sb([128, NT], F32, "m1")
    m2 = ph.sb([128, NT], F32, "m2")
    dd = ph.sb([128, NT], F32, "dd")
    ed = ph.sb([128, NT], F32, "ed")
    Mb = ph.sb([128, NT, 32], BF16, "Mb")
    cA = ph.sb([128, NT, 32], F32, "cA")
    cB = ph.sb([128, NT, 32], F32, "cB")
    psP = [ph.psum("psP") for _ in range(2)]
    psC = [ph.psum("psC") for _ in range(2)]
    P.dve(lambda e: e.tensor_reduce(out=gm[:, :], in_=L4, axis=AX.X, op=ALU.max), reads=["LALL"], writes=["gm"])
    P.dve(lambda e: e.tensor_tensor(out=d4[:, :, :], in0=L4, in1=gm[:, :].unsqueeze(2).to_broadcast([128, NT, 4]), op=ALU.subtract),
          reads=["LALL", "gm"], writes=["d4"])
    P.dve(lambda e: e.tensor_scalar(out=g.RE1[:, :, 0:4], in0=d4[:, :, :], scalar1=0.0, scalar2=None, op0=ALU.is_equal), reads=["d4"], writes=["RE1"])
    P.dve(lambda e: e.tensor_scalar(out=g.RE1[:, :, 0:4], in0=g.RE1[:, :, 0:4], scalar1=-1.0, scalar2=1e30, op0=ALU.add, op1=ALU.mult),
          reads=["RE1"], writes=["RE1"])
    P.act(lambda e: e.activation(out=d4[:, :, :], in_=d4[:, :, :], func=AF.Exp), reads=["d4"], writes=["d4"])
    P.dve(lambda e: e.tensor_reduce(out=gs[:, :], in_=d4[:, :, :], axis=AX.X, op=ALU.add), reads=["d4"], writes=["gs"])
    P.dve(lambda e: e.reciprocal(out=pg[:, :], in_=gs[:, :]), reads=["gs"], writes=["pg"])
    for gi in range(4):
        P.dve(lambda e, gi=gi: e.tensor_tensor(out=ml[:, :, gi * 8:(gi + 1) * 8], in0=Lr[:, :, gi * 8:(gi + 1) * 8],
                                               in1=g.RE1[:, :, gi:gi + 1].to_broadcast([128, NT, 8]), op=ALU.add),
              reads=["LALL", "RE1"], writes=["ml"])
    P.dve(lambda e: e.tensor_reduce(out=m1[:, :], in_=ml[:, :, :], axis=AX.X, op=ALU.max), reads=["ml"], writes=["m1"])
    P.dve(lambda e: e.tensor_tensor(out=g.RE1[:, :, :], in0=ml[:, :, :], in1=m1[:, :].unsqueeze(2).to_broadcast([128, NT, 32]), op=ALU.is_equal),
          reads=["ml", "m1"], writes=["RE1"])
    P.dve(lambda e: e.scalar_tensor_tensor(out=ml[:, :, :], in0=g.RE1[:, :, :], scalar=-1e30, in1=ml[:, :, :], op0=ALU.mult, op1=ALU.add),
          reads=["RE1", "ml"], writes=["ml"])
    P.dve(lambda e: e.tensor_reduce(out=m2[:, :], in_=ml[:, :, :], axis=AX.X, op=ALU.max), reads=["ml"], writes=["m2"])
    P.dve(lambda e: e.tensor_tensor(out=g.RE2[:, :, :], in0=ml[:, :, :], in1=m2[:, :].unsqueeze(2).to_broadcast([128, NT, 32]), op=ALU.is_equal),
          reads=["ml", "m2"], writes=["RE2"])
    P.dve(lambda e: e.tensor_tensor(out=dd[:, :], in0=m2[:, :], in1=m1[:, :], op=ALU.subtract), reads=["m1", "m2"], writes=["dd"])
    P.act(lambda e: e.activation(out=ed[:, :], in_=dd[:, :], func=AF.Exp), reads=["dd"], writes=["ed"])
    P.dve(lambda e: e.tensor_scalar(out=dd[:, :], in0=ed[:, :], scalar1=1.0, scalar2=None, op0=ALU.add), reads=["ed"], writes=["dd"])
    P.dve(lambda e: e.reciprocal(out=dd[:, :], in_=dd[:, :]), reads=["dd"], writes=["dd"])
    P.dve(lambda e: e.tensor_tensor(out=g.RG[:, :, 0], in0=dd[:, :], in1=pg[:, :], op=ALU.mult), reads=["dd", "pg"], writes=["RG"])
    P.dve(lambda e: e.tensor_tensor(out=g.RG[:, :, 1], in0=g.RG[:, :, 0], in1=ed[:, :], op=ALU.mult), reads=["RG", "ed"], writes=["RG"])
    P.dve(lambda e: e.tensor_tensor(out=Mb[:, :, :], in0=g.RE1[:, :, :], in1=g.RE2[:, :, :], op=ALU.add), reads=["RE1", "RE2"], writes=["Mb"])
    for ti in range(NT):
        hb, col = ti // 16, (ti % 16) * 32
        P.pe(lambda e, ti=ti, hb=hb, col=col: e.matmul(psP[hb][:, col:col + 32], lhsT=cb[:, CB_TRIU:CB_TRIU + 128], rhs=Mb[:, ti, :], start=True, stop=True),
             reads=["Mb"], writes=["psP_%d" % hb])
        P.pe(lambda e, ti=ti, hb=hb, col=col: e.matmul(psC[hb][:, col:col + 32], lhsT=cb[:, CB_ONES:CB_ONES + 128], rhs=Mb[:, ti, :], start=True, stop=True),
             reads=["Mb"], writes=["psC_%d" % hb])
    for hb in range(2):
        P.act(lambda e, hb=hb: e.copy(out=cA[:, hb * 16:(hb + 1) * 16, :], in_=psC[hb][:, :].rearrange("p (t x) -> p t x", x=32)),
              reads=["psC_%d" % hb], writes=["cA"])
    src, dst, ksrc, kdst = cA, cB, "cA", "cB"
    step = 1
    while step < NT:
        P.dve(lambda e, src=src, dst=dst, step=step: e.tensor_copy(out=dst[:, 0:step, :], in_=src[:, 0:step, :]), reads=[ksrc], writes=[kdst])
        P.dve(lambda e, src=src, dst=dst, step=step: e.tensor_tensor(out=dst[:, step:NT, :], in0=src[:, step:NT, :], in1=src[:, 0:NT - step, :], op=ALU.add),
              reads=[ksrc], writes=[kdst])
        src, dst, ksrc, kdst = dst, src, kdst, ksrc
        step *= 2
    inc, kinc = src, ksrc
    exc, kexc = dst, kdst
    P.dve(lambda e: e.memset(exc[:, 0:1, :], 0.0), reads=[kinc], writes=[kexc])
    P.dve(lambda e: e.tensor_copy(out=exc[:, 1:NT, :], in_=inc[:, 0:NT - 1, :]), reads=[kinc], writes=[kexc])
    P.dve(lambda e: e.tensor_copy(out=g.CAR[:, 0:32], in_=inc[:, NT - 1, :]), reads=[kinc], writes=["CAR"])
    for hb in range(2):
        P.dve(lambda e, hb=hb: e.tensor_tensor(out=g.RPF[:, hb * 16:(hb + 1) * 16, :], in0=psP[hb][:, :].rearrange("p (t x) -> p t x", x=32),
                                               in1=exc[:, hb * 16:(hb + 1) * 16, :], op=ALU.add),
              reads=["psP_%d" % hb, kexc], writes=["RPF"])
    cnt = g.CAR
    r_ = ph.sb([128, 32], F32, "r")
    nz = ph.sb([128, 32], F32, "nz")
    pad = ph.sb([128, 32], F32, "pad")
    ca = ph.sb([128, 32], F32, "ca")
    cbuf = ph.sb([128, 32], F32, "cb")
    off = ph.sb([128, 32], F32, "off")
    big = ph.sb([128, NT, 32], F32, "big")
    g.tmpbig = ph.sb([128, NT, 32], F32, "tmpbig")
    posf = ph.sb([128, NT, 2], F32, "posf")
    ej = ph.sb([128, NSLOT_T], F32, "ej")
    tmpw = ph.sb([128, NSLOT_T], F32, "tmpw")
    P.dve(lambda e: e.memset(nz[:, :], 0.0), writes=["nz"])
    for m in range(2 * SEQ // SLOT):
        P.dve(lambda e, m=m: e.scalar_tensor_tensor(out=nz[:, :], in0=cnt[:, 0:32], scalar=float(m * SLOT), in1=nz[:, :],
                                                    op0=ALU.is_gt, op1=ALU.add), reads=["CAR", "nz"], writes=["nz"])
    P.dve(lambda e: e.tensor_scalar(out=pad[:, :], in0=nz[:, :], scalar1=float(SLOT), scalar2=None, op0=ALU.mult), reads=["nz"], writes=["pad"])
    src, dst = pad, ca
    ksrc, kdst = "pad", "ca"
    step = 1
    while step < 32:
        P.dve(lambda e, src=src, dst=dst, step=step: e.tensor_copy(out=dst[:, 0:step], in_=src[:, 0:step]), reads=[ksrc], writes=[kdst])
        P.dve(lambda e, src=src, dst=dst, step=step: e.tensor_tensor(out=dst[:, step:32], in0=src[:, step:32], in1=src[:, 0:32 - step], op=ALU.add),
              reads=[ksrc], writes=[kdst])
        if dst is ca:
            src, dst, ksrc, kdst = ca, cbuf, "ca", "cb"
        else:
            src, dst, ksrc, kdst = cbuf, ca, "cb", "ca"
        step *= 2
    cum, kcum = src, ksrc
    P.dve(lambda e: e.tensor_tensor(out=off[:, :], in0=cum[:, :], in1=pad[:, :], op=ALU.subtract), reads=[kcum, "pad"], writes=["off"])
    P.dve(lambda e: e.tensor_tensor(out=big[:, :, :], in0=g.RPF[:, :, :], in1=off[:, :].unsqueeze(1).to_broadcast([128, NT, 32]), op=ALU.add),
          reads=["RPF", "off"], writes=["big"])
    for k, RE, kre in ((0, g.RE1, "RE1"), (1, g.RE2, "RE2")):
        P.dve(lambda e, RE=RE: e.tensor_tensor(out=g.tmpbig[:, :, :], in0=big[:, :, :], in1=RE[:, :, :], op=ALU.mult),
              reads=["big", kre], writes=["tmpbig"])
        P.dve(lambda e, k=k: e.tensor_reduce(out=posf[:, :, k], in_=g.tmpbig[:, :, :], axis=AX.X, op=ALU.add),
              reads=["tmpbig"], writes=["posf"])
    P.dve(lambda e: e.tensor_copy(out=g.POS[:, :, :], in_=posf[:, :, :]), reads=["posf"], writes=["POS"])
    P.dve(lambda e: e.memset(ej[:, :], 0.0), writes=["ej"])
    for ex in range(NE):
        P.dve(lambda e, ex=ex: e.scalar_tensor_tensor(out=ej[:, :], in0=cf[:, CF_THR:CF_THR + NSLOT_T], scalar=cum[:, ex:ex + 1], in1=ej[:, :],
                                                      op0=ALU.is_ge, op1=ALU.add), reads=[kcum, "ej"], writes=["ej"])
    P.dve(lambda e: e.tensor_scalar(out=ej[:, :], in0=ej[:, :], scalar1=float(NE - 1), scalar2=None, op0=ALU.min), reads=["ej"], writes=["ej"])
    P.dve(lambda e: e.tensor_scalar(out=tmpw[:, :], in0=ej[:, :], scalar1=128.0, scalar2=cf[:, CF_IOTA:CF_IOTA + 1], op0=ALU.mult, op1=ALU.add),
          reads=["ej"], writes=["tmpw"])
    P.dve(lambda e: e.tensor_scalar(out=tmpw[:, :], in0=tmpw[:, :], scalar1=float(li * NE * 128), scalar2=None, op0=ALU.add),
          reads=["tmpw"], writes=["tmpw"])
    P.dve(lambda e: e.tensor_copy(out=g.WG[:, :], in_=tmpw[:, :]), reads=["tmpw"], writes=["WG"])
    for c in range(4):
        P.dve(lambda e, c=c: e.tensor_scalar(out=tmpw[:, :], in0=ej[:, :], scalar1=512.0, scalar2=cf[:, CF_IOTA + 1 + c:CF_IOTA + 2 + c],
                                             op0=ALU.mult, op1=ALU.add), reads=["ej"], writes=["tmpw"])
        P.dve(lambda e: e.tensor_scalar(out=tmpw[:, :], in0=tmpw[:, :], scalar1=float(li * NE * FF), scalar2=None, op0=ALU.add),
              reads=["tmpw"], writes=["tmpw"])
        P.dve(lambda e, c=c: e.tensor_copy(out=g.WD[:, c, :], in_=tmpw[:, :]), reads=["tmpw"], writes=["WD"])
    xbt = [ph.sb([128, 1024], BF16, "xbt") for _ in range(4)]
    for ti in range(NT):
        xb = xbt[ti % 4]
        kx = "xbt_%d" % (ti % 4)
        P.dma(lambda e, xb=xb, ti=ti: e.dma_start(out=xb[:, :], in_=g.xb16[ti * 128:(ti + 1) * 128, :]), writes=[kx])
        for k in range(2):
            P.dma(lambda e, xb=xb, ti=ti, k=k: e.indirect_dma_start(out=g.xs, out_offset=bass.IndirectOffsetOnAxis(ap=g.POS[:, ti, k:k + 1], axis=0),
                                                                    in_=xb[:, :], in_offset=None),
                  reads=[kx, "POS"], writes=["xs_%d_%d" % (ti, k)], q=POOL)
    ph.finish()


def expert_phase(nc, g, li, use_dma_transpose=True):
    ph = Phase(nc, "ex%d" % li)
    P = ph.P
    cb = g.cb
    NB = 3
    Wg = [ph.sb([128, 8, FF], BF16, "Wg") for _ in range(NB)]
    Wu = [ph.sb([128, 8, FF], BF16, "Wu") for _ in range(NB)]
    Wd = [ph.sb([128, 4, D], BF16, "Wd") for _ in range(NB)]
    xTp = [ph.sb([128, 8, SLOT], BF16, "xTp") for _ in range(2)]
    xrow = [[ph.sb([128, D], BF16, "xrow") for _ in range(4)] for _ in range(2)]
    sgt = [ph.sb([128, SLOT], F32, "sg") for _ in range(2)]
    hT = [ph.sb([128, 4, SLOT], BF16, "hT") for _ in range(2)]
    ysb = [ph.sb([128, D], F32, "ysb") for _ in range(4)]
    psg = [ph.psum("psg") for _ in range(2)]
    psu = [ph.psum("psu") for _ in range(2)]
    psy = [ph.psum("psy") for _ in range(2)]
    pst = [ph.psum("pst") for _ in range(2)]
    pst_bf = [p[:, :].bitcast(BF16) for p in pst]
    wgv = g.d_moe_w_gate.rearrange("l e (p kk) f -> (l e p) (kk f)", kk=8)
    wuv = g.d_moe_w_up.rearrange("l e (p kk) f -> (l e p) (kk f)", kk=8)
    wdv = g.d_moe_w_down.rearrange("l e k d -> (l e k) d")

    def load(jt):
        b = jt % NB
        xb = jt % 2
        for sub in range(4):
            P.dma(lambda e, xb=xb, jt=jt, sub=sub: e.dma_start(out=xrow[xb][sub][:, :], in_=g.xs[jt * SLOT + sub * 128: jt * SLOT + (sub + 1) * 128, :]),
                  reads=["xs"], writes=["xrow_%d_%d" % (xb, sub)])
        P.dma(lambda e, b=b, jt=jt: e.indirect_dma_start(out=Wg[b][:, :, :].rearrange("p k f -> p (k f)"), out_offset=None, in_=wgv,
                                                         in_offset=bass.IndirectOffsetOnAxis(ap=g.WG[:, jt:jt + 1], axis=0)),
              reads=["WG"], writes=["Wg_%d" % b], q=POOL)
        P.dma(lambda e, b=b, jt=jt: e.indirect_dma_start(out=Wu[b][:, :, :].rearrange("p k f -> p (k f)"), out_offset=None, in_=wuv,
                                                         in_offset=bass.IndirectOffsetOnAxis(ap=g.WG[:, jt:jt + 1], axis=0)),
              reads=["WG"], writes=["Wu_%d" % b], q=POOL)
        for c in range(4):
            P.dma(lambda e, b=b, jt=jt, c=c: e.indirect_dma_start(out=Wd[b][:, c, :], out_offset=None, in_=wdv,
                                                                  in_offset=bass.IndirectOffsetOnAxis(ap=g.WD[:, c, jt:jt + 1], axis=0)),
                  reads=["WD"], writes=["Wd_%d_%d" % (b, c)], q=POOL)

    def transposes(jt):
        xb = jt % 2
        for sub in range(4):
            ptb = pst_bf[sub % 2]
            kpt = "pst_%d" % (sub % 2)
            xr = xrow[xb][sub]
            for kk in range(8):
                P.pe(lambda e, xr=xr, kk=kk, ptb=ptb: e.transpose(out=ptb[:, kk * 128:(kk + 1) * 128], in_=xr[:, kk * 128:(kk + 1) * 128],
                                                                  identity=cb[:, CB_IDENT:CB_IDENT + 128]),
                     reads=["xrow_%d_%d" % (xb, sub)], writes=[kpt])
            if sub % 2 == 0:
                P.act(lambda e, xb=xb, sub=sub, ptb=ptb: e.copy(out=xTp[xb][:, :, sub * 128:(sub + 1) * 128], in_=ptb.rearrange("p (k t) -> p k t", k=8)),
                      reads=[kpt], writes=["xTp_%d" % xb])
            else:
                P.dve(lambda e, xb=xb, sub=sub, ptb=ptb: e.tensor_copy(out=xTp[xb][:, :, sub * 128:(sub + 1) * 128], in_=ptb.rearrange("p (k t) -> p k t", k=8)),
                      reads=[kpt], writes=["xTp_%d" % xb])

    load(0)
    load(1)
    transposes(0)
    for jt in range(NSLOT_T):
        b = jt % NB
        xb = jt % 2
        if jt + 1 < NSLOT_T:
            transposes(jt + 1)
        if jt + 2 < NSLOT_T:
            load(jt + 2)
        h = hT[jt % 2]
        kh = "hT_%d" % (jt % 2)
        for fc in range(4):
            pg_, pu_ = psg[fc % 2], psu[fc % 2]
            kg, ku = "psg_%d" % (fc % 2), "psu_%d" % (fc % 2)
            for kk in range(8):
                P.pe(lambda e, b=b, xb=xb, kk=kk, fc=fc, pg_=pg_: e.matmul(pg_[:, :], lhsT=Wg[b][:, kk, fc * 128:(fc + 1) * 128], rhs=xTp[xb][:, kk, :],
                                                                           start=(kk == 0), stop=(kk == 7)),
                     reads=["Wg_%d" % b, "xTp_%d" % xb], writes=[kg])
            for kk in range(8):
                P.pe(lambda e, b=b, xb=xb, kk=kk, fc=fc, pu_=pu_: e.matmul(pu_[:, :], lhsT=Wu[b][:, kk, fc * 128:(fc + 1) * 128], rhs=xTp[xb][:, kk, :],
                                                                           start=(kk == 0), stop=(kk == 7)),
                     reads=["Wu_%d" % b, "xTp_%d" % xb], writes=[ku])
            sg = sgt[fc % 2]
            ksg = "sgt_%d" % (fc % 2)
            P.act(lambda e, pg_=pg_, sg=sg: e.activation(out=sg[:, :], in_=pg_[:, :], func=AF.Silu), reads=[kg], writes=[ksg])
            P.dve(lambda e, pu_=pu_, sg=sg, h=h, fc=fc: e.tensor_tensor(out=h[:, fc, :], in0=pu_[:, :], in1=sg[:, :], op=ALU.mult),
                  reads=[ku, ksg], writes=[kh])
        for sub in range(4):
            yb = ysb[sub]
            kyb = "ysb_%d" % sub
            for half in range(2):
                py = psy[half]
                kpy = "psy_%d" % half
                for fc in range(4):
                    P.pe(lambda e, b=b, fc=fc, sub=sub, half=half, py=py, h=h: e.matmul(py[:, :], lhsT=h[:, fc, sub * 128:(sub + 1) * 128],
                                                                                      rhs=Wd[b][:, fc, half * 512:(half + 1) * 512],
                                                                                      start=(fc == 0), stop=(fc == 3)),
                         reads=[kh] + ["Wd_%d_%d" % (b, c) for c in range(4)], writes=[kpy])
                if half == 0:
                    P.act(lambda e, py=py, yb=yb: e.copy(out=yb[:, 0:512], in_=py[:, :]), reads=[kpy], writes=[kyb + "a"])
                else:
                    P.dve(lambda e, py=py, yb=yb: e.tensor_copy(out=yb[:, 512:1024], in_=py[:, :]), reads=[kpy], writes=[kyb + "b"])
            P.dma(lambda e, yb=yb, jt=jt, sub=sub: e.dma_start(out=g.ys[jt * SLOT + sub * 128: jt * SLOT + (sub + 1) * 128, :], in_=yb[:, :]),
                  reads=[kyb + "a", kyb + "b"], writes=["ys_%d_%d" % (jt, sub)])
    ph.finish()


def combine_phase(nc, g, li, x1src, xdst):
    ph = Phase(nc, "cm%d" % li)
    P = ph.P
    NB = 3
    y1 = [ph.sb([128, D], F32, "y1") for _ in range(NB)]
    y2 = [ph.sb([128, D], F32, "y2") for _ in range(NB)]
    x1 = [ph.sb([128, D], F32, "x1") for _ in range(NB)]
    x2 = [ph.sb([128, D], F32, "x2") for _ in range(NB)]
    st = ph.sb([128, 12], F32, "st")
    mv = ph.sb([128, 2], F32, "mv")
    rs = ph.sb([128, 2], F32, "rs")

    def load(ti):
        b = ti % NB
        P.dma(lambda e, b=b, ti=ti: e.dma_start(out=x1[b][:, :], in_=x1src[ti * 128:(ti + 1) * 128, :]), reads=["XB"], writes=["x1_%d" % b])
        P.dma(lambda e, b=b, ti=ti: e.indirect_dma_start(out=y1[b][:, :], out_offset=None, in_=g.ys,
                                                         in_offset=bass.IndirectOffsetOnAxis(ap=g.POS[:, ti, 0:1], axis=0)),
              reads=["ys", "POS"], writes=["y1_%d" % b], q=POOL)
        P.dma(lambda e, b=b, ti=ti: e.indirect_dma_start(out=y2[b][:, :], out_offset=None, in_=g.ys,
                                                         in_offset=bass.IndirectOffsetOnAxis(ap=g.POS[:, ti, 1:2], axis=0)),
              reads=["ys", "POS"], writes=["y2_%d" % b], q=POOL)

    for ti in range(NB - 1):
        load(ti)
    for ti in range(NT):
        b = ti % NB
        if ti + NB - 1 < NT:
            load(ti + NB - 1)
        P.act(lambda e, b=b, ti=ti: e.activation(out=y1[b][:, :], in_=y1[b][:, :], func=AF.Copy, scale=g.RG[:, ti, 0:1]),
              reads=["y1_%d" % b, "RG"], writes=["y1_%d" % b])
        P.dve(lambda e, b=b, ti=ti: e.scalar_tensor_tensor(out=y2[b][:, :], in0=y2[b][:, :], scalar=g.RG[:, ti, 1:2], in1=y1[b][:, :],
                                                           op0=ALU.mult, op1=ALU.add), reads=["y2_%d" % b, "y1_%d" % b, "RG"], writes=["y2_%d" % b])
        P.dve(lambda e, b=b: e.scalar_tensor_tensor(out=x1[b][:, :], in0=x1[b][:, :], scalar=ALPHA, in1=y2[b][:, :],
                                                    op0=ALU.mult, op1=ALU.add), reads=["x1_%d" % b, "y2_%d" % b], writes=["x1_%d" % b])
        _ln_token_major(P, DVE, x1[b], x2[b], g.pb[:, PB_LN2G:PB_LN2G + 1024], g.pb[:, PB_LN2B:PB_LN2B + 1024],
                        st, mv, rs, "x1_%d" % b, "x2_%d" % b, "ln2")
        P.dma(lambda e, b=b, ti=ti: e.dma_start(out=xdst[ti * 128:(ti + 1) * 128, :], in_=x2[b][:, :]), reads=["x2_%d" % b], writes=["XOUT_%d" % ti])
    ph.finish()


def build_program(n_layers=DEPTH, stop_after=None, use_dma_transpose=True):
    nc = bass.Bass("TRN2", target_bir_lowering=False)
    g = G()

    def din(name, shape, dt):
        return nc.dram_tensor(name, list(shape), dt, kind="ExternalInput").ap()

    def dscr(name, shape, dt):
        return nc.dram_tensor(name, list(shape), dt, kind="Internal").ap()

    g.d_x = din("x", [SEQ, D], F32)
    g.d_posb = din("posb", [128, SEQ], I32)
    g.d_cf = din("constf", [128, CF_N], F32)
    g.d_cb = din("constb", [128, CB_N], BF16)
    g.d_conv_w_pw1 = din("conv_w_pw1", [2, D, 2 * D], F32)
    g.d_conv_w_pw2 = din("conv_w_pw2", [2, D, D], F32)
    g.d_ret_w_qkvg = din("ret_w_qkvg", [2, D, 6 * D], F32)
    g.d_ret_w_o = din("ret_w_o", [2, 2 * D, D], F32)
    g.d_moe_w_gate = din("moe_w_gate", [DEPTH, NE, D, FF], F32)
    g.d_moe_w_up = din("moe_w_up", [DEPTH, NE, D, FF], F32)
    g.d_moe_w_down = din("moe_w_down", [DEPTH, NE, FF, D], F32)
    g.d_pbln = din("pbln", [DEPTH, 128, PB_N], F32)
    g.d_pbmix = din("pbmix", [DEPTH, 128, PM_N], F32)
    g.d_pp = din("pp", [2, 128, PP_N], F32)
    g.d_wr = din("wr", [DEPTH, 128, 8, 36], F32)
    g.d_out = nc.dram_tensor("out", [SEQ, D], F32, kind="ExternalOutput").ap()
    g.XA = dscr("XA", [SEQ, D], F32)
    g.XB = dscr("XB", [SEQ, D], F32)
    g.xb16 = dscr("xb16", [SEQ, D], BF16)
    g.xs = dscr("xs", [NSLOT_T * SLOT, D], BF16)
    g.ys = dscr("ys", [NSLOT_T * SLOT, D], F32)
    g.QS = dscr("QS", [NT, 128, 8, 128], BF16)
    g.KS = dscr("KS", [NT, 128, 8, 128], BF16)
    g.VS = dscr("VS", [SEQ, 2 * D], BF16)
    g.SG = dscr("SG", [SEQ, 2 * D], BF16)

    with contextlib.ExitStack() as st:
        def sb(name, shape, dt):
            return st.enter_context(nc.sbuf_tensor(name, list(shape), dt))
        g.cf = sb("cf", [128, CF_N], F32)
        g.cb = sb("cb", [128, CB_N], BF16)
        g.pb = sb("pb", [128, PB_N], F32)
        g.wr = sb("wr_sb", [128, 8, 36], F32)
        g.LALL = sb("LALL", [128, NT, 36], F32)
        g.RG = sb("RG", [128, NT, 2], F32)
        g.POS = sb("POS", [128, NT, 2], I32)
        g.CAR = sb("CAR", [128, 32], F32)
        g.WG = sb("WG", [128, NSLOT_T], I32)
        g.WD = sb("WD", [128, 4, NSLOT_T], I32)

        EPS_AP[0] = g.cf[:, CF_EPS:CF_EPS + 1]
        ph = Phase(nc, "init")
        ph.P.dma(lambda e: e.dma_start(out=g.cf[:, :], in_=g.d_cf), writes=["cf"])
        ph.P.dma(lambda e: e.dma_start(out=g.cb[:, :], in_=g.d_cb), writes=["cb"])
        ph.finish()

        xcur = g.d_x
        for li in range(n_layers):
            if li % 2 == 0:
                conv_phase(nc, g, li, xcur, g.XB)
            else:
                retention_phase(nc, g, li, xcur, g.XB)
            if stop_after == ("mix", li):
                _copy_out(nc, g, g.XB)
                break
            offsets_scatter_phase(nc, g, li)
            expert_phase(nc, g, li, use_dma_transpose)
            last = (li == n_layers - 1)
            xnext = g.d_out if last else g.XA
            combine_phase(nc, g, li, g.XB, xnext)
            xcur = xnext
    return nc


def _copy_out(nc, g, src):
    ph = Phase(nc, "cpy")
    t = [ph.sb([128, D], F32, "t") for _ in range(2)]
    for ti in range(NT):
        b = ti % 2
        ph.P.dma(lambda e, b=b, ti=ti: e.dma_start(out=t[b][:, :], in_=src[ti * 128:(ti + 1) * 128, :]), reads=["src"], writes=["t%d" % b])
        ph.P.dma(lambda e, b=b, ti=ti: e.dma_start(out=g.d_out[ti * 128:(ti + 1) * 128, :], in_=t[b][:, :]), reads=["t%d" % b], writes=["o%d" % ti])
    ph.finish()


TWO_PI_HI = 6.28125
TWO_PI_LO = 2.0 * np.pi - 6.28125
PI = float(np.pi)


def retention_phase(nc, g, li, xsrc, xdst):
    _ret_pass1(nc, g, li, xsrc)
    _ret_pass2(nc, g, li, xsrc, xdst)


def _ret_pass1(nc, g, li, xsrc):
    j = li // 2
    ph = Phase(nc, "rp%d" % li)
    P = ph.P
    cf, cb = g.cf, g.cb
    _load_layer_params(ph, g, li)
    Wq = ph.sb([128, 8, 6 * D], BF16, "Wq")
    wsrc = g.d_ret_w_qkvg[j].rearrange("(k p) f -> p k f", p=128)
    for q in range(12):
        P.dma(lambda e, q=q: e.dma_start(out=Wq[:, :, q * 512:(q + 1) * 512], in_=wsrc[:, :, q * 512:(q + 1) * 512]),
              writes=["Wq_%d" % q], q=POOL)
    posi = ph.sb([128, 512], I32, "posi")
    posf = ph.sb([128, 512], F32, "posf")
    xin = [ph.sb([128, D], F32, "xin") for _ in range(2)]
    xbf = ph.sb([128, D], BF16, "xbf")
    xT = ph.sb([128, 8, 512], BF16, "xT")
    ang = ph.sb([128, 512], F32, "ang")
    ni = ph.sb([128, 512], I32, "ni")
    nf = ph.sb([128, 512], F32, "nf")
    rr = ph.sb([128, 512], F32, "rr")
    cc = ph.sb([128, 512], F32, "cc")
    mm = ph.sb([128, 512], F32, "mm")
    sinT = ph.sb([128, 512], F32, "sinT")
    cosT = ph.sb([128, 512], F32, "cosT")
    ta = [ph.sb([128, 512], F32, "ta")] * 2
    tb = [ph.sb([128, 512], F32, "tb")] * 2
    tc_ = [ph.sb([128, 512], F32, "tc")] * 2
    td = [ph.sb([128, 512], F32, "td")] * 2
    qkT = ph.sb([128, 16, 512], BF16, "qkT")
    vrow = [ph.sb([128, 2 * D], BF16, "vrow")] * 2
    srow = [ph.sb([128, 2 * D], BF16, "srow")] * 2
    psT = ph.psum("psT")
    psT_bf = psT[:, :].bitcast(BF16)
    psq = [ph.psum("psq") for _ in range(4)]
    psv = [ph.psum("psv") for _ in range(2)]
    invf = cf[:, CF_INVF:CF_INVF + 1]
    for st_i in range(NT // 4):
        t0 = st_i * 512
        for sub in range(4):
            ti = st_i * 4 + sub
            xi = xin[ti % 2]
            kxi = "xin_%d" % (ti % 2)
            P.dma(lambda e, xi=xi, ti=ti: e.dma_start(out=xi[:, :], in_=xsrc[ti * 128:(ti + 1) * 128, :]), writes=[kxi])
            P.act(lambda e, xi=xi: e.copy(out=xbf[:, :], in_=xi[:, :]), reads=[kxi], writes=["xbf"])
            for kc in range(8):
                P.pe(lambda e, kc=kc: e.transpose(out=psT_bf[:, kc * 128:(kc + 1) * 128], in_=xbf[:, kc * 128:(kc + 1) * 128],
                                                  identity=cb[:, CB_IDENT:CB_IDENT + 128]), reads=["xbf"], writes=["psT"])
            P.dve(lambda e, sub=sub: e.tensor_copy(out=xT[:, :, sub * 128:(sub + 1) * 128],
                                                   in_=psT_bf.rearrange("p (k t) -> p k t", k=8)), reads=["psT"], writes=["xT"])
        for sub in range(4):
            ti = st_i * 4 + sub
            vr, sr = vrow[ti % 2], srow[ti % 2]
            kv, ks = "vrow_0", "srow_0"
            for cbk in range(8):
                pv = psv[cbk % 2]
                kpv = "psv_%d" % (cbk % 2)
                c0 = 2048 + cbk * 512
                for kc in range(8):
                    P.pe(lambda e, kc=kc, c0=c0, pv=pv, sub=sub: e.matmul(pv[:, :], lhsT=xT[:, kc, sub * 128:(sub + 1) * 128], rhs=Wq[:, kc, c0:c0 + 512],
                                                                          start=(kc == 0), stop=(kc == 7)),
                         reads=["xT", "Wq_%d" % (c0 // 512)], writes=[kpv])
                if cbk < 4:
                    P.dve(lambda e, pv=pv, vr=vr, cbk=cbk: e.tensor_copy(out=vr[:, cbk * 512:(cbk + 1) * 512], in_=pv[:, :]), reads=[kpv], writes=[kv])
                else:
                    P.act(lambda e, pv=pv, sr=sr, cbk=cbk: e.activation(out=sr[:, (cbk - 4) * 512:(cbk - 3) * 512], in_=pv[:, :], func=AF.Silu),
                          reads=[kpv], writes=[ks])
            P.dma(lambda e, vr=vr, ti=ti: e.dma_start(out=g.VS[ti * 128:(ti + 1) * 128, :], in_=vr[:, :]), reads=[kv], writes=["VS_%d" % ti])
            P.dma(lambda e, sr=sr, ti=ti: e.dma_start(out=g.SG[ti * 128:(ti + 1) * 128, :], in_=sr[:, :]), reads=[ks], writes=["SG_%d" % ti])
        P.dma(lambda e, t0=t0: e.dma_start(out=posi[:, :], in_=g.d_posb[:, t0:t0 + 512]), writes=["posi"])
        P.dve(lambda e: e.tensor_copy(out=posf[:, :], in_=posi[:, :]), reads=["posi"], writes=["posf"])
        P.dve(lambda e: e.tensor_scalar(out=ang[:, :], in0=posf[:, :], scalar1=invf, scalar2=None, op0=ALU.mult),
               reads=["posf"], writes=["ang"])
        P.dve(lambda e: e.tensor_scalar(out=ni[:, :], in0=ang[:, :], scalar1=float(1.0 / (2.0 * np.pi)), scalar2=None, op0=ALU.mult),
               reads=["ang"], writes=["ni"])
        P.dve(lambda e: e.tensor_copy(out=nf[:, :], in_=ni[:, :]), reads=["ni"], writes=["nf"])
        P.dve(lambda e: e.scalar_tensor_tensor(out=rr[:, :], in0=nf[:, :], scalar=-TWO_PI_HI, in1=ang[:, :], op0=ALU.mult, op1=ALU.add),
              reads=["nf", "ang"], writes=["rr"])
        P.dve(lambda e: e.scalar_tensor_tensor(out=rr[:, :], in0=nf[:, :], scalar=-TWO_PI_LO, in1=rr[:, :], op0=ALU.mult, op1=ALU.add),
              reads=["nf", "rr"], writes=["rr"])
        P.dve(lambda e: e.tensor_scalar(out=mm[:, :], in0=rr[:, :], scalar1=PI, scalar2=-2.0 * PI, op0=ALU.is_gt, op1=ALU.mult),
               reads=["rr"], writes=["mm"])
        P.dve(lambda e: e.tensor_tensor(out=rr[:, :], in0=rr[:, :], in1=mm[:, :], op=ALU.add), reads=["rr", "mm"], writes=["rr"])
        P.dve(lambda e: e.tensor_scalar(out=mm[:, :], in0=rr[:, :], scalar1=-PI, scalar2=2.0 * PI, op0=ALU.is_lt, op1=ALU.mult),
               reads=["rr"], writes=["mm"])
        P.dve(lambda e: e.tensor_tensor(out=rr[:, :], in0=rr[:, :], in1=mm[:, :], op=ALU.add), reads=["rr", "mm"], writes=["rr"])
        P.dve(lambda e: e.tensor_scalar(out=cc[:, :], in0=rr[:, :], scalar1=0.5 * PI, scalar2=None, op0=ALU.add), reads=["rr"], writes=["cc"])
        P.dve(lambda e: e.tensor_scalar(out=mm[:, :], in0=cc[:, :], scalar1=PI, scalar2=-2.0 * PI, op0=ALU.is_gt, op1=ALU.mult),
               reads=["cc"], writes=["mm"])
        P.dve(lambda e: e.tensor_tensor(out=cc[:, :], in0=cc[:, :], in1=mm[:, :], op=ALU.add), reads=["cc", "mm"], writes=["cc"])
        P.act(lambda e: e.activation(out=sinT[:, :], in_=rr[:, :], func=AF.Sin), reads=["rr"], writes=["sinT"])
        P.act(lambda e: e.activation(out=cosT[:, :], in_=cc[:, :], func=AF.Sin), reads=["cc"], writes=["cosT"])
        for hp in range(8):
            b2 = hp % 2
            p1, p2 = psq[2 * b2], psq[2 * b2 + 1]
            k1, k2 = "psq_%d" % (2 * b2), "psq_%d" % (2 * b2 + 1)
            for half, pq, kq in ((0, p1, k1), (1, p2, k2)):
                c0 = (2 * hp + half) * 128
                for kc in range(8):
                    P.pe(lambda e, kc=kc, c0=c0, pq=pq: e.matmul(pq[:, :], lhsT=Wq[:, kc, c0:c0 + 128], rhs=xT[:, kc, :],
                                                                 start=(kc == 0), stop=(kc == 7)),
                         reads=["xT", "Wq_%d" % (c0 // 512)], writes=[kq])
            a_, b_, c_, d_ = ta[b2], tb[b2], tc_[b2], td[b2]
            sfx = "_0"
            P.dve(lambda e, p1=p1, a_=a_: e.tensor_tensor(out=a_[:, :], in0=p1[:, :], in1=cosT[:, :], op=ALU.mult), reads=[k1, "cosT"], writes=["ta" + sfx])
            P.dve(lambda e, p2=p2, b_=b_: e.tensor_tensor(out=b_[:, :], in0=p2[:, :], in1=sinT[:, :], op=ALU.mult), reads=[k2, "sinT"], writes=["tb" + sfx])
            P.dve(lambda e, p1=p1, c_=c_: e.tensor_tensor(out=c_[:, :], in0=p1[:, :], in1=sinT[:, :], op=ALU.mult), reads=[k1, "sinT"], writes=["tc" + sfx])
            P.dve(lambda e, p2=p2, d_=d_: e.tensor_tensor(out=d_[:, :], in0=p2[:, :], in1=cosT[:, :], op=ALU.mult), reads=[k2, "cosT"], writes=["td" + sfx])
            P.pool(lambda e, hp=hp, a_=a_, b_=b_: e.tensor_tensor(out=qkT[:, 2 * hp, :], in0=a_[:, :], in1=b_[:, :], op=ALU.subtract),
                   reads=["ta" + sfx, "tb" + sfx], writes=["qkT"])
            P.pool(lambda e, hp=hp, c_=c_, d_=d_: e.tensor_tensor(out=qkT[:, 2 * hp + 1, :], in0=c_[:, :], in1=d_[:, :], op=ALU.add),
                   reads=["tc" + sfx, "td" + sfx], writes=["qkT"])
        for sub in range(4):
            ti = st_i * 4 + sub
            P.dma(lambda e, ti=ti, sub=sub: e.dma_start(out=g.QS[ti], in_=qkT[:, 0:8, sub * 128:(sub + 1) * 128]), reads=["qkT"], writes=["QS_%d" % ti])
            P.dma(lambda e, ti=ti, sub=sub: e.dma_start(out=g.KS[ti], in_=qkT[:, 8:16, sub * 128:(sub + 1) * 128]), reads=["qkT"], writes=["KS_%d" % ti])
    ph.finish()


def _ret_pass2(nc, g, li, xsrc, xdst):
    j = li // 2
    ph = Phase(nc, "rq%d" % li)
    P = ph.P
    cf, cb = g.cf, g.cb
    Wo = ph.sb([128, 16, D], BF16, "Wo")
    wsrc = g.d_ret_w_o[j].rearrange("(k p) f -> p k f", p=128)
    for q in range(2):
        P.dma(lambda e, q=q: e.dma_start(out=Wo[:, q * 8:(q + 1) * 8, :], in_=wsrc[:, q * 8:(q + 1) * 8, :]), writes=["Wo"], q=POOL)
    pm = ph.sb([128, PM_N], F32, "pm")
    P.dma(lambda e: e.dma_start(out=pm[:, :], in_=g.d_pbmix[li]), writes=["PM"])
    qT = [ph.sb([128, 8, 128], BF16, "qT") for _ in range(2)]
    kT = [ph.sb([128, 8, 128], BF16, "kT") for _ in range(2)]
    vrow_ = [ph.sb([128, 2 * D], BF16, "vrow") for _ in range(2)]
    srow_ = [ph.sb([128, 2 * D], BF16, "srow") for _ in range(2)]
    xin = [ph.sb([128, D], F32, "xin") for _ in range(2)]
    S = ph.sb([128, RH, 2, 512], F32, "S")
    Sbf = ph.sb([128, RH, 2, 512], BF16, "Sbf")
    qxT = ph.sb([128, 8, 128], BF16, "qxT")
    kz = ph.sb([128, D], BF16, "kz")
    PT = [ph.sb([128, 128], BF16, "PT") for _ in range(2)]
    on = [ph.sb([128, 512], F32, "on") for _ in range(2)]
    y = ph.sb([128, 2 * D], BF16, "y")
    yT = ph.sb([128, 16, 128], BF16, "yT")
    rbuf = [ph.sb([128, D], F32, "r") for _ in range(2)]
    gst_ = ph.sb([128, RH, 6], F32, "gst")
    gmv_ = ph.sb([128, RH, 2], F32, "gmv")
    grs_ = ph.sb([128, RH, 1], F32, "grs")
    gnm_ = ph.sb([128, RH, 1], F32, "gnm")
    bufs = _RW()
    bufs.x1 = [ph.sb([128, D], F32, "x1") for _ in range(2)]
    bufs.x1bf = [ph.sb([128, D], BF16, "x1bf") for _ in range(2)]
    bufs.st = ph.sb([128, 12], F32, "st")
    bufs.mv = ph.sb([128, 2], F32, "mv")
    bufs.rs = ph.sb([128, 2], F32, "rs")
    RWk = _router_work(ph)
    pss = ph.psum("pss")
    pso = [ph.psum("pso") for _ in range(2)]
    pst = [ph.psum("pst") for _ in range(2)]
    psX = [ph.psum("psX") for _ in range(2)]
    psR = ph.psum("psR")
    psX_bf = [p[:, :].bitcast(BF16) for p in psX]
    P.dve(lambda e: e.memset(S[:, :, :, :], 0.0), writes=["S"])
    P.dve(lambda e: e.memset(Sbf[:, :, :, :], 0.0), writes=["Sbf"])
    def load2(ti):
        b = ti % 2
        P.dma(lambda e, b=b, ti=ti: e.dma_start(out=qT[b][:, :, :], in_=g.QS[ti]), writes=["qT_%d" % b])
        P.dma(lambda e, b=b, ti=ti: e.dma_start(out=kT[b][:, :, :], in_=g.KS[ti]), writes=["kT_%d" % b])
        P.dma(lambda e, b=b, ti=ti: e.dma_start(out=vrow_[b][:, :], in_=g.VS[ti * 128:(ti + 1) * 128, :]), writes=["vrow_%d" % b])
        P.dma(lambda e, b=b, ti=ti: e.dma_start(out=srow_[b][:, :], in_=g.SG[ti * 128:(ti + 1) * 128, :]), writes=["srow_%d" % b])
        P.dma(lambda e, b=b, ti=ti: e.dma_start(out=xin[b][:, :], in_=xsrc[ti * 128:(ti + 1) * 128, :]), writes=["xin_%d" % b])

    load2(0)
    for ti in range(NT):
        b = ti % 2
        kq, kk_ = "qT_%d" % b, "kT_%d" % b
        xi = xin[b]
        kxi = "xin_%d" % b
        vrow, srow = vrow_[b], srow_[b]
        kvr, ksr = "vrow_%d" % b, "srow_%d" % b
        if ti + 1 < NT:
            load2(ti + 1)
        P.dve(lambda e, b=b: e.tensor_tensor(out=qxT[:, :, :], in0=qT[b][:, :, :],
                                             in1=cb[:, CB_XI:CB_XI + 1024].rearrange("p (k t) -> p k t", k=8), op=ALU.mult),
              reads=[kq], writes=["qxT"])
        for kc in range(8):
            P.pe(lambda e, b=b, kc=kc: e.transpose(out=psX_bf[0][:, kc * 128:(kc + 1) * 128], in_=kT[b][:, kc, :],
                                                   identity=cb[:, CB_IDENT:CB_IDENT + 128]), reads=[kk_], writes=["psT2_0"])
        for h in range(RH):
            P.act(lambda e, h=h: e.activation(out=kz[:, h * 256:(h + 1) * 256], in_=psX_bf[0][:, h * 256:(h + 1) * 256], func=AF.Copy,
                                              scale=cf[:, CF_ZETA + h:CF_ZETA + h + 1]), reads=["psT2_0"], writes=["kz"])
        for h in range(RH):
            pb2 = h % 2
            po = pso[pb2]
            kpo = "pso_%d" % pb2
            for c in range(2):
                P.pe(lambda e, b=b, h=h, c=c: e.matmul(pss[:, (h % 4) * 128:(h % 4 + 1) * 128], lhsT=kT[b][:, 2 * h + c, :], rhs=qT[b][:, 2 * h + c, :],
                                                       start=(c == 0), stop=(c == 1)), reads=[kq, kk_], writes=["pss_%d" % h])
            pt = PT[pb2]
            kpt = "PT_%d" % pb2
            P.dve(lambda e, h=h, pt=pt: e.tensor_tensor(out=pt[:, :], in0=pss[:, (h % 4) * 128:(h % 4 + 1) * 128],
                                                        in1=cf[:, CF_MT + h * 128:CF_MT + (h + 1) * 128], op=ALU.mult),
                  reads=["pss_%d" % h], writes=[kpt])
            P.pe(lambda e, h=h, pt=pt, po=po, vrow=vrow: e.matmul(po[:, :], lhsT=pt[:, :], rhs=vrow[:, h * 512:(h + 1) * 512], start=True, stop=False),
                 reads=[kpt, kvr], writes=[kpo])
            for c in range(2):
                P.pe(lambda e, h=h, c=c, po=po: e.matmul(po[:, :], lhsT=qxT[:, 2 * h + c, :], rhs=Sbf[:, h, c, :], start=False, stop=(c == 1)),
                     reads=["qxT", "Sbf_%d" % h], writes=[kpo])
            for c in range(2):
                P.pe(lambda e, h=h, c=c, vrow=vrow: e.matmul(pst[c][:, :], lhsT=kz[:, h * 256 + c * 128:h * 256 + (c + 1) * 128], rhs=vrow[:, h * 512:(h + 1) * 512],
                                                  start=True, stop=True), reads=["kz", kvr], writes=["pst_%d" % c])
                P.dve(lambda e, h=h, c=c: e.scalar_tensor_tensor(out=S[:, h, c, :], in0=S[:, h, c, :], scalar=float(GAMMA[h] ** 128.0), in1=pst[c][:, :],
                                                                 op0=ALU.mult, op1=ALU.add), reads=["pst_%d" % c, "S"], writes=["S"])
                P.act(lambda e, h=h, c=c: e.copy(out=Sbf[:, h, c, :], in_=S[:, h, c, :]), reads=["S"], writes=["Sbf_%d" % h])
            gst, gmv, grs, gnm = gst_[:, h, :], gmv_[:, h, :], grs_[:, h, :], gnm_[:, h, :]
            kh_ = "_%d" % h
            P.dve(lambda e, po=po, gst=gst: e.bn_stats(out=gst[:, 0:6], in_=po[:, :]), reads=[kpo], writes=["gst" + kh_])
            P.dve(lambda e, gst=gst, gmv=gmv: e.bn_aggr(out=gmv[:, 0:2], in_=gst[:, 0:6]), reads=["gst" + kh_], writes=["gmv" + kh_])
            P.act(lambda e, grs=grs, gmv=gmv: e.activation(out=grs[:, 0:1], in_=gmv[:, 1:2], func=AF.Sqrt, bias=EPS_AP[0], scale=1.0),
                  reads=["gmv" + kh_], writes=["grs" + kh_])
            P.dve(lambda e, grs=grs: e.reciprocal(out=grs[:, 0:1], in_=grs[:, 0:1]), reads=["grs" + kh_], writes=["grs" + kh_])
            P.dve(lambda e, gnm=gnm, gmv=gmv, grs=grs: e.scalar_tensor_tensor(out=gnm[:, 0:1], in0=gmv[:, 0:1], scalar=-1.0, in1=grs[:, 0:1],
                                                                              op0=ALU.mult, op1=ALU.mult),
                  reads=["gmv" + kh_, "grs" + kh_], writes=["gnm" + kh_])
            o_ = on[pb2]
            kon = "on_%d" % pb2
            P.act(lambda e, po=po, o_=o_, gnm=gnm, grs=grs: e.activation(out=o_[:, :], in_=po[:, :], func=AF.Identity, bias=gnm[:, 0:1], scale=grs[:, 0:1]),
                  reads=[kpo, "grs" + kh_, "gnm" + kh_], writes=[kon])
            P.dve(lambda e, h=h, o_=o_: e.tensor_tensor(out=o_[:, :], in0=o_[:, :], in1=pm[:, h * 512:(h + 1) * 512], op=ALU.mult),
                  reads=[kon, "PM"], writes=[kon])
            P.dve(lambda e, h=h, o_=o_: e.tensor_tensor(out=o_[:, :], in0=o_[:, :], in1=pm[:, 2 * D + h * 512:2 * D + (h + 1) * 512], op=ALU.add),
                  reads=[kon, "PM"], writes=[kon])
            P.dve(lambda e, h=h, o_=o_, srow=srow: e.tensor_tensor(out=y[:, h * 512:(h + 1) * 512], in0=o_[:, :], in1=srow[:, h * 512:(h + 1) * 512], op=ALU.mult),
                  reads=[kon, ksr], writes=["y"])
        for half in range(2):
            for q in range(8):
                kc = half * 8 + q
                P.pe(lambda e, kc=kc, q=q, half=half: e.transpose(out=psX_bf[half][:, q * 128:(q + 1) * 128], in_=y[:, kc * 128:(kc + 1) * 128],
                                                                  identity=cb[:, CB_IDENT:CB_IDENT + 128]), reads=["y"], writes=["psT2_%d" % half])
            if half == 0:
                P.act(lambda e: e.copy(out=yT[:, 0:8, :], in_=psX_bf[0].rearrange("p (k t) -> p k t", k=8)), reads=["psT2_0"], writes=["yT"])
            else:
                P.dve(lambda e: e.tensor_copy(out=yT[:, 8:16, :], in_=psX_bf[1].rearrange("p (k t) -> p k t", k=8)), reads=["psT2_1"], writes=["yT"])
        r = rbuf[b]
        kr = "r_%d" % b
        for half in range(2):
            po = pso[half]
            kpo = "pso_%d" % half
            for kc in range(16):
                P.pe(lambda e, kc=kc, half=half, po=po: e.matmul(po[:, :], lhsT=yT[:, kc, :], rhs=Wo[:, kc, half * 512:(half + 1) * 512],
                                                                 start=(kc == 0), stop=(kc == 15)), reads=["yT", "Wo"], writes=[kpo])
            P.dve(lambda e, half=half, po=po, xi=xi, r=r: e.scalar_tensor_tensor(out=r[:, half * 512:(half + 1) * 512], in0=xi[:, half * 512:(half + 1) * 512],
                                                                                 scalar=ALPHA, in1=po[:, :], op0=ALU.mult, op1=ALU.add),
                  reads=[kpo, kxi], writes=[kr])
        if ti > 0:
            _post_mixer(ph, g, ti - 1, rbuf[(ti - 1) % 2], "r_%d" % ((ti - 1) % 2), RWk, bufs, psR, psX, xdst)
    _post_mixer(ph, g, NT - 1, rbuf[(NT - 1) % 2], "r_%d" % ((NT - 1) % 2), RWk, bufs, psR, psX, xdst)
    ph.finish()


def _rep(v, n=128):
    return np.ascontiguousarray(np.broadcast_to(np.asarray(v, np.float32).reshape(1, -1), (n, np.asarray(v).size)))


def prepare_shared(inputs):
    cf, cb, _ = _host_consts()
    sh = {"constf": cf, "constb": cb}
    for k in ("conv_w_pw1", "conv_w_pw2", "ret_w_qkvg", "ret_w_o", "moe_w_gate", "moe_w_up", "moe_w_down"):
        sh[k] = np.ascontiguousarray(inputs[k], dtype=np.float32)
    pbln = np.zeros((DEPTH, 128, PB_N), np.float32)
    pbmix = np.zeros((DEPTH, 128, PM_N), np.float32)
    wr = np.zeros((DEPTH, 128, 8, 36), np.float32)
    for i in range(DEPTH):
        pbln[i, :, PB_LN1G:PB_LN1G + D] = _rep(inputs["ln1_g"][i])
        pbln[i, :, PB_LN1B:PB_LN1B + D] = _rep(inputs["ln1_b"][i])
        pbln[i, :, PB_LN2G:PB_LN2G + D] = _rep(inputs["ln2_g"][i])
        pbln[i, :, PB_LN2B:PB_LN2B + D] = _rep(inputs["ln2_b"][i])
        pbln[i, :, PB_RB:PB_RB + 4] = _rep(inputs["moe_b_grp"][i])
        pbln[i, :, PB_RB + 4:PB_RB + 36] = _rep(inputs["moe_b_route"][i])
        wcat = np.concatenate([inputs["moe_w_grp"][i], inputs["moe_w_route"][i]], axis=1)
        wr[i] = wcat.reshape(8, 128, 36).transpose(1, 0, 2)
        j = i // 2
        if i % 2 == 0:
            pbmix[i, :, 0:D] = _rep(inputs["conv_b_pw2"][j])
        else:
            pbmix[i, :, 0:2 * D] = _rep(inputs["ret_gn_g"][j])
            pbmix[i, :, 2 * D:4 * D] = _rep(inputs["ret_gn_b"][j])
    pp = np.zeros((2, 128, PP_N), np.float32)
    for j in range(2):
        pp[j, :, PP_B1:PP_B1 + 16] = np.asarray(inputs["conv_b_pw1"][j]).reshape(16, 128).T
        wdw = np.asarray(inputs["conv_w_dw"][j])
        pp[j, :, PP_WDW:PP_WDW + 248] = wdw.reshape(CW, 8, 128).transpose(2, 1, 0).reshape(128, 248)
        pp[j, :, PP_BDW:PP_BDW + 8] = np.asarray(inputs["conv_b_dw"][j]).reshape(8, 128).T
        pp[j, :, PP_LNG:PP_LNG + 8] = np.asarray(inputs["conv_ln_g"][j]).reshape(8, 128).T
        pp[j, :, PP_LNB:PP_LNB + 8] = np.asarray(inputs["conv_ln_b"][j]).reshape(8, 128).T
    sh["pbln"], sh["pbmix"], sh["pp"], sh["wr"] = pbln, pbmix, pp, wr
    return sh


_NC_CACHE = {}


def kernel(**inputs):
    x = np.asarray(inputs["x"], np.float32)
    pos = np.asarray(inputs["positions"], np.int32)
    sh = prepare_shared(inputs)
    if "nc" not in _NC_CACHE:
        _NC_CACHE["nc"] = build_program()
    nc = _NC_CACHE["nc"]
    in_maps = []
    for c in range(8):
        m = dict(sh)
        m["x"] = np.ascontiguousarray(x[c])
        m["posb"] = np.ascontiguousarray(np.broadcast_to(pos[c][None, :], (128, SEQ)))
        in_maps.append(m)
    res = run_bass_kernel_spmd(nc, in_maps, core_ids=list(range(8)))
    return np.stack([np.asarray(r["out"], np.float32) for r in res.results], axis=0)
```

```python
import contextlib
import numpy as np
import ml_dtypes
import concourse.bass as bass
import concourse.mybir as mybir
from concourse.bass_utils import run_bass_kernel_spmd

F32 = mybir.dt.float32
BF16 = mybir.dt.bfloat16
I32 = mybir.dt.int32
ALU = mybir.AluOpType
AF = mybir.ActivationFunctionType
AX = mybir.AxisListType

PE, ACT, DVE, POOL, SP = "pe", "act", "dve", "pool", "sp"
ENGS = (PE, ACT, DVE, POOL, SP)

D = 1024
SEQ = 4096
NT = SEQ // 128
DEPTH = 4
NE = 32
FF = 512
SLOT = 512
NSLOT_T = 2 * SEQ // SLOT + NE
ALPHA = (2.0 * DEPTH) ** 0.25
EPS = 1e-5
CW = 31
NPE_TAPS = 24
RH = 4


class _Op:
    __slots__ = ("eng", "fn", "deps", "dma", "sig", "dsem", "dval", "dprev", "pos", "need_sig")


class Prog:
    def __init__(self, nc, n_dma_sems=8, same_eng_dist=3):
        self.nc = nc
        self.ops = []
        self.last_writer = {}
        self.readers = {}
        self.eng_count = {e: 0 for e in ENGS}
        self.n_dma_sems = n_dma_sems
        self.same_eng_dist = same_eng_dist

    def add(self, eng, fn, reads=(), writes=(), dma=False):
        op = _Op()
        op.eng, op.fn, op.dma = eng, fn, dma
        op.sig = None
        op.need_sig = False
        op.pos = self.eng_count[eng]
        self.eng_count[eng] += 1
        idx = len(self.ops)
        deps = set()
        for k in reads:
            w = self.last_writer.get(k)
            if w is not None:
                deps.add((w, True))
        for k in writes:
            w = self.last_writer.get(k)
            if w is not None:
                deps.add((w, False))
            for r in self.readers.get(k, ()):
                if r != idx:
                    deps.add((r, False))
        for k in reads:
            self.readers.setdefault(k, []).append(idx)
        for k in writes:
            self.last_writer[k] = idx
            self.readers[k] = []
        real = {}
        for d, raw in deps:
            dop = self.ops[d]
            if dop.dma or dop.eng != eng or dma:
                real[d] = True
            elif raw and eng != PE and (op.pos - dop.pos) < self.same_eng_dist:
                real[d] = True
        latest = {}
        keep = []
        for d in real:
            dop = self.ops[d]
            if dop.dma:
                keep.append(d)
            elif d > latest.get(dop.eng, -1):
                latest[dop.eng] = d
        op.deps = sorted(keep + list(latest.values()))
        for d in op.deps:
            if not self.ops[d].dma:
                self.ops[d].need_sig = True
        self.ops.append(op)
        return idx

    def pe(self, fn, reads=(), writes=()):
        return self.add(PE, fn, reads, writes)

    def act(self, fn, reads=(), writes=()):
        return self.add(ACT, fn, reads, writes)

    def dve(self, fn, reads=(), writes=()):
        return self.add(DVE, fn, reads, writes)

    def pool(self, fn, reads=(), writes=()):
        return self.add(POOL, fn, reads, writes)

    def ve(self, eng, fn, reads=(), writes=()):
        return self.add(eng, fn, reads, writes)

    def dma(self, fn, reads=(), writes=(), q=SP):
        return self.add(q, fn, reads, writes, dma=True)

    def emit(self):
        nc = self.nc
        ops = self.ops
        pool = _sem_pool(nc, self.n_dma_sems)
        sigc = dict(pool["eval"])
        sig0 = dict(pool["eval"])
        dcount = dict(pool["dcount"])
        dsem_val = dict(pool["dval"])
        used_d = set()
        last_of = {}
        for i, op in enumerate(ops):
            last_of[op.eng] = i
        for e, i in last_of.items():
            if not ops[i].dma:
                ops[i].need_sig = True
        for op in ops:
            if op.dma:
                j = dcount[op.eng] % self.n_dma_sems
                dcount[op.eng] += 1
                key = (op.eng, j)
                prev = dsem_val.get(key, 0)
                op.dsem, op.dprev, op.dval = key, prev, prev + 16
                dsem_val[key] = op.dval
                used_d.add(key)
            elif op.need_sig:
                sigc[op.eng] += 1
                op.sig = sigc[op.eng]

        esem = pool["esem"]
        dsem = pool["dsem"]
        pool["eval"] = dict(sigc)
        pool["dcount"] = dict(dcount)
        pool["dval"] = dict(dsem_val)
        with contextlib.ExitStack() as st:
            block = st.enter_context(nc.Block())

            def run_engine(ename, eh):
                waited = {}

                def wait(sem_key, semh, val):
                    if waited.get(sem_key, 0) >= val:
                        return
                    eh.wait_ge(semh, val)
                    waited[sem_key] = val

                for op in ops:
                    if op.eng != ename:
                        continue
                    for d in op.deps:
                        dop = ops[d]
                        if dop.dma:
                            wait(dop.dsem, dsem[dop.dsem], dop.dval)
                        else:
                            wait(dop.eng, esem[dop.eng], dop.sig)
                    if op.dma:
                        if op.dprev > 0:
                            wait(op.dsem, dsem[op.dsem], op.dprev)
                        op.fn(eh).then_inc(dsem[op.dsem], 16)
                    else:
                        ins = op.fn(eh)
                        if op.sig is not None:
                            ins.then_inc(esem[ename], 1)
                for key in sorted(used_d):
                    wait(key, dsem[key], dsem_val[key])
                for e in ENGS:
                    if sigc[e] > sig0[e]:
                        wait(e, esem[e], sigc[e])

            @block.tensor
            def _(eh):
                run_engine(PE, eh)

            @block.scalar
            def _(eh):
                run_engine(ACT, eh)

            @block.vector
            def _(eh):
                run_engine(DVE, eh)

            @block.gpsimd
            def _(eh):
                run_engine(POOL, eh)

            @block.sync
            def _(eh):
                run_engine(SP, eh)


_SEM_POOLS = {}


def _sem_pool(nc, n_dma_sems):
    key = id(nc)
    if key not in _SEM_POOLS:
        pool = {"esem": {e: nc.alloc_semaphore(name="s_" + e) for e in ENGS}, "dsem": {}, "dval": {}}
        pool["eval"] = {e: 0 for e in ENGS}
        pool["dcount"] = {e: 0 for e in ENGS}
        for e in (SP, ACT, POOL):
            for j in range(n_dma_sems):
                pool["dsem"][(e, j)] = nc.alloc_semaphore(name="d_%s_%d" % (e, j))
        _SEM_POOLS[key] = pool
    return _SEM_POOLS[key]


class Phase:
    def __init__(self, nc, name):
        self.nc = nc
        self.name = name
        self.st = contextlib.ExitStack()
        self.P = Prog(nc)
        self.n = 0

    def sb(self, shape, dt, tag="t"):
        self.n += 1
        return self.st.enter_context(self.nc.sbuf_tensor("%s_%s%d" % (self.name, tag, self.n), list(shape), dt))

    def psum(self, tag="ps"):
        self.n += 1
        return self.st.enter_context(self.nc.psum_tensor("%s_%s%d" % (self.name, tag, self.n), [128, 512], F32))

    def finish(self):
        self.P.emit()
        self.st.close()


CF_IDENT = 0
CF_ONES = 128
CF_MT = 256
CF_THR = 768
CF_IOTA = 816
CF_INVF = 824
CF_ZETA = 825
CF_EPS = 829
CF_N = 832
CB_IDENT = 0
CB_ONES = 128
CB_TRIU = 256
CB_XI = 384
CB_N = 384 + 1024


def _host_consts():
    cf = np.zeros((128, CF_N), np.float32)
    cf[:, CF_IDENT:CF_IDENT + 128] = np.eye(128, dtype=np.float32)
    cf[:, CF_ONES:CF_ONES + 128] = 1.0
    gam = 1.0 - 2.0 ** (-5.0 - np.arange(RH, dtype=np.float64))
    i = np.arange(128)
    for h in range(RH):
        ci, cj = i[:, None] // 64, i[None, :] // 64
        dist = (i[:, None] - i[None, :]).astype(np.float64)
        M = np.where(ci == cj, gam[h] ** np.abs(dist), np.where(ci > cj, gam[h] ** dist, 0.0))
        cf[:, CF_MT + h * 128: CF_MT + (h + 1) * 128] = (M.T * (256.0 ** -0.5)).astype(np.float32)
        cf[:, CF_ZETA + h] = (gam[h] ** (127.0 - i) * (256.0 ** -0.5)).astype(np.float32)
    cf[:, CF_THR:CF_THR + NSLOT_T] = (np.arange(NSLOT_T) * SLOT).astype(np.float32)[None, :]
    cf[:, CF_IOTA] = i
    cf[:, CF_EPS] = EPS
    for c in range(4):
        cf[:, CF_IOTA + 1 + c] = c * 128 + i
    cf[:, CF_INVF] = (np.float32(10000.0) ** (-(np.arange(128, dtype=np.float32)) / np.float32(128))).astype(np.float32)
    cb = np.zeros((128, CB_N), np.float32)
    cb[:, CB_IDENT:CB_IDENT + 128] = np.eye(128)
    cb[:, CB_ONES:CB_ONES + 128] = 1.0
    cb[:, CB_TRIU:CB_TRIU + 128] = (i[:, None] < i[None, :]).astype(np.float32)
    for kc in range(8):
        h = kc // 2
        cb[:, CB_XI + kc * 128: CB_XI + (kc + 1) * 128] = (gam[h] ** (i + 1.0))[None, :]
    return cf, cb.astype(ml_dtypes.bfloat16), gam


GAMMA = 1.0 - 2.0 ** (-5.0 - np.arange(RH, dtype=np.float64))

PB_LN1G, PB_LN1B, PB_LN2G, PB_LN2B, PB_RB = 0, 1024, 2048, 3072, 4096
PB_N = 4160
PM_N = 4096
PP_B1, PP_WDW, PP_BDW, PP_LNG, PP_LNB = 0, 16, 16 + 248, 16 + 248 + 8, 16 + 248 + 16
PP_N = 16 + 248 + 24


class G:
    pass


EPS_AP = [None]


def _ln_token_major(P, eng2, r, x1, g_ap, b_ap, tmp_stats, tmp_mv, tmp_rs, key_r, key_out, tagk):
    P.dve(lambda e: e.bn_stats(out=tmp_stats[:, 0:6], in_=r[:, 0:512]), reads=[key_r], writes=[tagk + "st"])
    P.dve(lambda e: e.bn_stats(out=tmp_stats[:, 6:12], in_=r[:, 512:1024]), reads=[key_r], writes=[tagk + "st"])
    P.dve(lambda e: e.bn_aggr(out=tmp_mv[:, 0:2], in_=tmp_stats[:, 0:12]), reads=[tagk + "st"], writes=[tagk + "mv"])
    P.act(lambda e: e.activation(out=tmp_rs[:, 0:1], in_=tmp_mv[:, 1:2], func=AF.Sqrt, bias=EPS_AP[0], scale=1.0),
          reads=[tagk + "mv"], writes=[tagk + "rs"])
    P.dve(lambda e: e.reciprocal(out=tmp_rs[:, 0:1], in_=tmp_rs[:, 0:1]), reads=[tagk + "rs"], writes=[tagk + "rs"])
    P.dve(lambda e: e.scalar_tensor_tensor(out=tmp_rs[:, 1:2], in0=tmp_mv[:, 0:1], scalar=-1.0, in1=tmp_rs[:, 0:1], op0=ALU.mult, op1=ALU.mult),
          reads=[tagk + "mv", tagk + "rs"], writes=[tagk + "rs"])
    P.act(lambda e: e.activation(out=r[:, :], in_=r[:, :], func=AF.Identity, bias=tmp_rs[:, 1:2], scale=tmp_rs[:, 0:1]),
          reads=[key_r, tagk + "rs"], writes=[key_r])
    P.ve(eng2, lambda e: e.tensor_tensor(out=r[:, :], in0=r[:, :], in1=g_ap, op=ALU.mult), reads=[key_r, "PB"], writes=[key_r])
    P.ve(eng2, lambda e: e.tensor_tensor(out=x1[:, :], in0=r[:, :], in1=b_ap, op=ALU.add), reads=[key_r, "PB"], writes=[key_out])


def _router_tile(ph, g, ti, x1, key_x1, W, psR, psT2):
    P = ph.P
    cf = g.cf
    for half in range(2):
        for q in range(4):
            kc = half * 4 + q
            P.pe(lambda e, kc=kc, q=q, half=half: e.transpose(out=psT2[half][:, q * 128:(q + 1) * 128],
                                                              in_=x1[:, kc * 128:(kc + 1) * 128],
                                                              identity=cf[:, CF_IDENT:CF_IDENT + 128]),
                 reads=[key_x1], writes=["psT2_%d" % half])
        P.act(lambda e, half=half: e.copy(out=W.x1T[:, half * 4:(half + 1) * 4, :],
                                          in_=psT2[half][:, :].rearrange("p (k t) -> p k t", k=4)),
              reads=["psT2_%d" % half], writes=["x1T"])
    for kc in range(8):
        P.pe(lambda e, kc=kc: e.matmul(psR[:, 0:36], lhsT=W.x1T[:, kc, :], rhs=g.wr[:, kc, :],
                                       start=(kc == 0), stop=(kc == 7)), reads=["x1T", "WR"], writes=["psR"])
    P.dve(lambda e: e.tensor_tensor(out=g.LALL[:, ti, :], in0=psR[:, 0:36], in1=g.pb[:, PB_RB:PB_RB + 36], op=ALU.add),
          reads=["psR", "PB"], writes=["LALL"])


class _RW:
    pass


def _router_work(ph):
    W = _RW()
    W.x1T = ph.sb([128, 8, 128], F32, "x1T")
    return W


def _post_mixer(ph, g, ti, r, key_r, W, bufs, psR, psT2, xdst):
    P = ph.P
    x1 = bufs.x1[ti % 2]
    kx1 = "x1_%d" % (0 if bufs.x1[0] is bufs.x1[1] else ti % 2)
    _ln_token_major(P, DVE, r, x1, g.pb[:, PB_LN1G:PB_LN1G + 1024], g.pb[:, PB_LN1B:PB_LN1B + 1024],
                    bufs.st, bufs.mv, bufs.rs, key_r, kx1, "ln1")
    P.dma(lambda e: e.dma_start(out=xdst[ti * 128:(ti + 1) * 128, :], in_=x1[:, :]), reads=[kx1], writes=["XB_%d" % ti])
    xb = bufs.x1bf[ti % 2]
    kxb = "x1bf_%d" % (0 if bufs.x1bf[0] is bufs.x1bf[1] else ti % 2)
    P.act(lambda e: e.copy(out=xb[:, :].rearrange("t (kk p) -> t kk p", p=128),
                           in_=x1[:, :].rearrange("t (p kk) -> t kk p", kk=8)), reads=[kx1], writes=[kxb])
    P.dma(lambda e: e.dma_start(out=g.xb16[ti * 128:(ti + 1) * 128, :], in_=xb[:, :]), reads=[kxb], writes=["xb16_%d" % ti])
    _router_tile(ph, g, ti, x1, kx1, W, psR, psT2)


def _load_layer_params(ph, g, li):
    P = ph.P
    P.dma(lambda e: e.dma_start(out=g.pb[:, :], in_=g.d_pbln[li]), writes=["PB"])
    P.dma(lambda e: e.dma_start(out=g.wr[:, :, :], in_=g.d_wr[li]), writes=["WR"])


def conv_phase(nc, g, li, xsrc, xdst):
    j = li // 2
    ph = Phase(nc, "cv%d" % li)
    P = ph.P
    cf, cb = g.cf, g.cb
    _load_layer_params(ph, g, li)
    W1b = ph.sb([128, 8, 2048], BF16, "W1")
    W2b = ph.sb([128, 8, 1024], BF16, "W2")
    pm = ph.sb([128, 1024], F32, "pm")
    pp = ph.sb([128, PP_N], F32, "pp")
    for q in range(4):
        P.dma(lambda e, q=q: e.dma_start(out=W1b[:, :, q * 512:(q + 1) * 512],
                                         in_=g.d_conv_w_pw1[j].rearrange("(k p) f -> p k f", p=128)[:, :, q * 512:(q + 1) * 512]),
              writes=["W1_%d" % q], q=POOL)
    P.dma(lambda e: e.dma_start(out=W2b[:, :, :], in_=g.d_conv_w_pw2[j].rearrange("(k p) f -> p k f", p=128)),
          writes=["W2"], q=POOL)
    P.dma(lambda e: e.dma_start(out=pm[:, :], in_=g.d_pbmix[li][:, 0:1024]), writes=["PM"])
    P.dma(lambda e: e.dma_start(out=pp[:, :], in_=g.d_pp[j]), writes=["PP"])

    xin = [ph.sb([128, 1024], F32, "xin") for _ in range(2)]
    xbf = ph.sb([128, 1024], BF16, "xbf")
    xT = ph.sb([128, 8, 512], BF16, "xT")
    hg = ph.sb([128, 8, 512 + CW - 1], BF16, "hg")
    NPE = NPE_TAPS
    diag = ph.sb([128, 8 * NPE, 128], BF16, "diag")
    for c in range(8):
        for jj in range(NPE):
            P.act(lambda e, c=c, jj=jj: e.activation(out=diag[:, c * NPE + jj, :], in_=cf[:, CF_IDENT:CF_IDENT + 128], func=AF.Copy,
                                                     scale=pp[:, PP_WDW + c * CW + jj:PP_WDW + c * CW + jj + 1]),
                  reads=["PP", "cf"], writes=["diag"])
    sig = [ph.sb([128, 512], F32, "sig")] * 2
    cv = ph.sb([128, 8, 512], F32, "cv")
    sq = [ph.sb([128, 512], F32, "sq")] * 2
    mean = ph.sb([128, 512], F32, "mean")
    rstd = ph.sb([128, 512], F32, "rstd")
    nmr = ph.sb([128, 512], F32, "nmr")
    hT = xT
    rbuf = [ph.sb([128, 1024], F32, "r") for _ in range(2)]
    bufs = _RW()
    bufs.x1 = [ph.sb([128, 1024], F32, "x1")] * 2
    bufs.x1bf = [ph.sb([128, 1024], BF16, "x1bf")] * 2
    bufs.st = ph.sb([128, 12], F32, "st")
    bufs.mv = ph.sb([128, 2], F32, "mv")
    bufs.rs = ph.sb([128, 2], F32, "rs")
    RWk = _router_work(ph)
    psT = ph.psum("psT")
    psA = [ph.psum("psA") for _ in range(2)]
    psG = [ph.psum("psG") for _ in range(2)]
    psR = ph.psum("psR")
    psT2 = [ph.psum("psT2") for _ in range(2)]
    psS = [psG[0], psG[1]]
    psT_bf = psT[:, :].bitcast(BF16)

    P.dve(lambda e: e.memset(hg[:, :, 0:CW - 1], 0.0), writes=["hg"])
    conv_eng = [DVE] * 8
    pending = []
    norm_eng = [DVE] * 8
    for st_i in range(NT // 4):
        for sub in range(4):
            ti = st_i * 4 + sub
            xi = xin[ti % 2]
            kxi = "xin_%d" % (ti % 2)
            P.dma(lambda e, xi=xi, ti=ti: e.dma_start(out=xi[:, :], in_=xsrc[ti * 128:(ti + 1) * 128, :]), writes=[kxi])
            P.act(lambda e, xi=xi: e.copy(out=xbf[:, :], in_=xi[:, :]), reads=[kxi], writes=["xbf"])
            for kc in range(8):
                P.pe(lambda e, kc=kc: e.transpose(out=psT_bf[:, kc * 128:(kc + 1) * 128], in_=xbf[:, kc * 128:(kc + 1) * 128],
                                                  identity=cb[:, CB_IDENT:CB_IDENT + 128]), reads=["xbf"], writes=["psT"])
            P.dve(lambda e, sub=sub: e.tensor_copy(out=xT[:, :, sub * 128:(sub + 1) * 128],
                                                   in_=psT_bf.rearrange("p (k t) -> p k t", k=8)), reads=["psT"], writes=["xT"])
        for c in range(8):
            pa, pg = psA[c % 2], psG[c % 2]
            ka, kg = "psA_%d" % (c % 2), "psG_%d" % (c % 2)
            for kc in range(8):
                P.pe(lambda e, kc=kc, c=c, pa=pa: e.matmul(pa[:, :], lhsT=W1b[:, kc, c * 128:(c + 1) * 128], rhs=xT[:, kc, :],
                                                           start=(kc == 0), stop=(kc == 7)),
                     reads=["xT", "W1_%d" % (c // 4)], writes=[ka])
            for kc in range(8):
                P.pe(lambda e, kc=kc, c=c, pg=pg: e.matmul(pg[:, :], lhsT=W1b[:, kc, 1024 + c * 128:1024 + (c + 1) * 128], rhs=xT[:, kc, :],
                                                           start=(kc == 0), stop=(kc == 7)),
                     reads=["xT", "W1_%d" % (2 + c // 4)], writes=[kg])
            sg = sig[c % 2]
            ks = "sig_0"
            P.act(lambda e, c=c, pg=pg, sg=sg: e.activation(out=sg[:, :], in_=pg[:, :], func=AF.Sigmoid,
                                                            bias=pp[:, PP_B1 + 8 + c:PP_B1 + 9 + c], scale=1.0),
                  reads=[kg, "PP"], writes=[ks])
            P.dve(lambda e, c=c, pa=pa, sg=sg: e.scalar_tensor_tensor(out=hg[:, c, CW - 1:CW - 1 + 512], in0=pa[:, :],
                                                                      scalar=pp[:, PP_B1 + c:PP_B1 + c + 1], in1=sg[:, :],
                                                                      op0=ALU.add, op1=ALU.mult),
                  reads=[ka, ks, "PP"], writes=["hg"])
        cps = [psA[0], psG[0], psA[1], psG[1]]
        cpk = ["psA_0", "psG_0", "psA_1", "psG_1"]
        for c in range(8):
            pc, kpc = cps[c % 4], cpk[c % 4]
            for jj in range(NPE):
                P.pe(lambda e, c=c, jj=jj, pc=pc: e.matmul(pc[:, :], lhsT=diag[:, c * NPE + jj, :], rhs=hg[:, c, jj:jj + 512],
                                                           start=(jj == 0), stop=(jj == NPE - 1)), reads=["hg", "diag"], writes=[kpc])
            P.act(lambda e, c=c, pc=pc: e.activation(out=cv[:, c, :], in_=pc[:, :], func=AF.Identity,
                                                     bias=pp[:, PP_BDW + c:PP_BDW + c + 1], scale=1.0),
                  reads=[kpc, "PP"], writes=["cv_%d" % c])
        for jj in range(NPE, CW):
            for c in range(8):
                P.dve(lambda e, c=c, jj=jj: e.scalar_tensor_tensor(out=cv[:, c, :], in0=hg[:, c, jj:jj + 512],
                                                                   scalar=pp[:, PP_WDW + c * CW + jj:PP_WDW + c * CW + jj + 1],
                                                                   in1=cv[:, c, :], op0=ALU.mult, op1=ALU.add),
                      reads=["hg", "PP", "cv_%d" % c], writes=["cv_%d" % c])
        P.act(lambda e: e.copy(out=hg[:, :, 0:CW - 1], in_=hg[:, :, 512:512 + CW - 1]), reads=["hg"], writes=["hg"])
        for c in range(8):
            s_ = sq[c % 2]
            ksq = "sq_0"
            P.act(lambda e, c=c, s_=s_: e.activation(out=s_[:, :], in_=cv[:, c, :], func=AF.Square), reads=["cv_%d" % c], writes=[ksq])
            P.pe(lambda e, c=c: e.matmul(psS[0][:, :], lhsT=cf[:, CF_ONES:CF_ONES + 128], rhs=cv[:, c, :], start=(c == 0), stop=(c == 7)),
                 reads=["cv_%d" % c], writes=["psG_0"])
            P.pe(lambda e, c=c, s_=s_: e.matmul(psS[1][:, :], lhsT=cf[:, CF_ONES:CF_ONES + 128], rhs=s_[:, :], start=(c == 0), stop=(c == 7)),
                 reads=[ksq], writes=["psG_1"])
        P.act(lambda e: e.mul(out=mean[:, :], in_=psS[0][:, :], mul=1.0 / D), reads=["psG_0"], writes=["mean"])
        P.dve(lambda e: e.tensor_tensor(out=nmr[:, :], in0=mean[:, :], in1=mean[:, :], op=ALU.mult), reads=["mean"], writes=["nmr"])
        P.dve(lambda e: e.scalar_tensor_tensor(out=rstd[:, :], in0=psS[1][:, :], scalar=1.0 / D, in1=nmr[:, :],
                                               op0=ALU.mult, op1=ALU.subtract), reads=["psG_1", "nmr"], writes=["rstd"])
        P.act(lambda e: e.activation(out=rstd[:, :], in_=rstd[:, :], func=AF.Sqrt, bias=EPS_AP[0], scale=1.0),
              reads=["rstd"], writes=["rstd"])
        P.dve(lambda e: e.reciprocal(out=rstd[:, :], in_=rstd[:, :]), reads=["rstd"], writes=["rstd"])
        P.dve(lambda e: e.scalar_tensor_tensor(out=nmr[:, :], in0=mean[:, :], scalar=-1.0, in1=rstd[:, :],
                                               op0=ALU.mult, op1=ALU.mult), reads=["mean", "rstd"], writes=["nmr"])
        for c in range(8):
            eng = norm_eng[c]
            P.ve(eng, lambda e, c=c: e.tensor_tensor(out=cv[:, c, :], in0=cv[:, c, :], in1=rstd[:, :], op=ALU.mult),
                 reads=["cv_%d" % c, "rstd"], writes=["cv_%d" % c])
        for c in range(8):
            eng = norm_eng[c]
            P.ve(eng, lambda e, c=c: e.tensor_tensor(out=cv[:, c, :], in0=cv[:, c, :], in1=nmr[:, :], op=ALU.add),
                 reads=["cv_%d" % c, "nmr"], writes=["cv_%d" % c])
        for c in range(8):
            P.act(lambda e, c=c: e.activation(out=hT[:, c, :], in_=cv[:, c, :], func=AF.Silu,
                                              bias=pp[:, PP_LNB + c:PP_LNB + c + 1], scale=pp[:, PP_LNG + c:PP_LNG + c + 1]),
                  reads=["cv_%d" % c, "PP"], writes=["xT"])
        for sub in range(4):
            ti = st_i * 4 + sub
            xi = xin[ti % 2]
            kxi = "xin_%d" % (ti % 2)
            P.dma(lambda e, xi=xi, ti=ti: e.dma_start(out=xi[:, :], in_=xsrc[ti * 128:(ti + 1) * 128, :]), writes=[kxi])
            r = rbuf[ti % 2]
            kr = "r_%d" % (ti % 2)
            for half in range(2):
                pa = (psA if sub % 2 == 0 else psG)[half]
                ka = ("psA_%d" if sub % 2 == 0 else "psG_%d") % half
                for c in range(8):
                    P.pe(lambda e, c=c, half=half, sub=sub, pa=pa: e.matmul(pa[:, :], lhsT=hT[:, c, sub * 128:(sub + 1) * 128],
                                                                            rhs=W2b[:, c, half * 512:(half + 1) * 512],
                                                                            start=(c == 0), stop=(c == 7)),
                         reads=["xT", "W2"], writes=[ka])
                P.dve(lambda e, half=half, pa=pa, xi=xi, r=r: e.scalar_tensor_tensor(out=r[:, half * 512:(half + 1) * 512],
                                                                                     in0=xi[:, half * 512:(half + 1) * 512], scalar=ALPHA,
                                                                                     in1=pa[:, :], op0=ALU.mult, op1=ALU.add),
                      reads=[ka, kxi], writes=[kr])
            P.dve(lambda e, r=r: e.tensor_tensor(out=r[:, :], in0=r[:, :], in1=pm[:, 0:1024], op=ALU.add), reads=[kr, "PM"], writes=[kr])
            if pending:
                pt_ = pending.pop(0)
                _post_mixer(ph, g, pt_, rbuf[pt_ % 2], "r_%d" % (pt_ % 2), RWk, bufs, psR, psT2, xdst)
            pending.append(ti)
    while pending:
        pt_ = pending.pop(0)
        _post_mixer(ph, g, pt_, rbuf[pt_ % 2], "r_%d" % (pt_ % 2), RWk, bufs, psR, psT2, xdst)
    ph.finish()


def offsets_scatter_phase(nc, g, li):
    ph = Phase(nc, "os%d" % li)
    P = ph.P
    cf, cb = g.cf, g.cb
    LA = g.LALL
    L4 = LA[:, :, 0:4]
    Lr = LA[:, :, 4:36]
    g.RE1 = ph.sb([128, NT, 32], F32, "RE1")
    g.RE2 = ph.sb([128, NT, 32], F32, "RE2")
    g.RPF = ph.sb([128, NT, 32], F32, "RPF")
    gm = ph.sb([128, NT], F32, "gm")
    d4 = ph.sb([128, NT, 4], F32, "d4")
    gs = ph.sb([128, NT], F32, "gs")
    pg = ph.sb([128, NT], F32, "pg")
    ml = ph.sb([128, NT, 32], F32, "ml")
    m1 = ph.sb([128, NT], F32, "m1")
    m2 = ph.sb([128, NT], F32, "m2")
    dd = ph.sb([128, NT], F32, "dd")
    ed = ph.sb([128, NT], F32, "ed")
    Mb = ph.sb([128, NT, 32], BF16, "Mb")
    cA = ph.sb([128, NT, 32], F32, "cA")
    cB = ph.sb([128, NT, 32], F32, "cB")
    psP = [ph.psum("psP") for _ in range(2)]
    psC = [ph.psum("psC") for _ in range(2)]
    P.dve(lambda e: e.tensor_reduce(out=gm[:, :], in_=L4, axis=AX.X, op=ALU.max), reads=["LALL"], writes=["gm"])
    P.dve(lambda e: e.tensor_tensor(out=d4[:, :, :], in0=L4, in1=gm[:, :].unsqueeze(2).to_broadcast([128, NT, 4]), op=ALU.subtract),
          reads=["LALL", "gm"], writes=["d4"])
    P.dve(lambda e: e.tensor_scalar(out=g.RE1[:, :, 0:4], in0=d4[:, :, :], scalar1=0.0, scalar2=None, op0=ALU.is_equal), reads=["d4"], writes=["RE1"])
    P.dve(lambda e: e.tensor_scalar(out=g.RE1[:, :, 0:4], in0=g.RE1[:, :, 0:4], scalar1=-1.0, scalar2=1e30, op0=ALU.add, op1=ALU.mult),
          reads=["RE1"], writes=["RE1"])
    P.act(lambda e: e.activation(out=d4[:, :, :], in_=d4[:, :, :], func=AF.Exp), reads=["d4"], writes=["d4"])
    P.dve(lambda e: e.tensor_reduce(out=gs[:, :], in_=d4[:, :, :], axis=AX.X, op=ALU.add), reads=["d4"], writes=["gs"])
    P.dve(lambda e: e.reciprocal(out=pg[:, :], in_=gs[:, :]), reads=["gs"], writes=["pg"])
    for gi in range(4):
        P.dve(lambda e, gi=gi: e.tensor_tensor(out=ml[:, :, gi * 8:(gi + 1) * 8], in0=Lr[:, :, gi * 8:(gi + 1) * 8],
                                               in1=g.RE1[:, :, gi:gi + 1].to_broadcast([128, NT, 8]), op=ALU.add),
              reads=["LALL", "RE1"], writes=["ml"])
    P.dve(lambda e: e.tensor_reduce(out=m1[:, :], in_=ml[:, :, :], axis=AX.X, op=ALU.max), reads=["ml"], writes=["m1"])
    P.dve(lambda e: e.tensor_tensor(out=g.RE1[:, :, :], in0=ml[:, :, :], in1=m1[:, :].unsqueeze(2).to_broadcast([128, NT, 32]), op=ALU.is_equal),
          reads=["ml", "m1"], writes=["RE1"])
    P.dve(lambda e: e.scalar_tensor_tensor(out=ml[:, :, :], in0=g.RE1[:, :, :], scalar=-1e30, in1=ml[:, :, :], op0=ALU.mult, op1=ALU.add),
          reads=["RE1", "ml"], writes=["ml"])
    P.dve(lambda e: e.tensor_reduce(out=m2[:, :], in_=ml[:, :, :], axis=AX.X, op=ALU.max), reads=["ml"], writes=["m2"])
    P.dve(lambda e: e.tensor_tensor(out=g.RE2[:, :, :], in0=ml[:, :, :], in1=m2[:, :].unsqueeze(2).to_broadcast([128, NT, 32]), op=ALU.is_equal),
          reads=["ml", "m2"], writes=["RE2"])
    P.dve(lambda e: e.tensor_tensor(out=dd[:, :], in0=m2[:, :], in1=m1[:, :], op=ALU.subtract), reads=["m1", "m2"], writes=["dd"])
    P.act(lambda e: e.activation(out=ed[:, :], in_=dd[:, :], func=AF.Exp), reads=["dd"], writes=["ed"])
    P.dve(lambda e: e.tensor_scalar(out=dd[:, :], in0=ed[:, :], scalar1=1.0, scalar2=None, op0=ALU.add), reads=["ed"], writes=["dd"])
    P.dve(lambda e: e.reciprocal(out=dd[:, :], in_=dd[:, :]), reads=["dd"], writes=["dd"])
    P.dve(lambda e: e.tensor_tensor(out=g.RG[:, :, 0], in0=dd[:, :], in1=pg[:, :], op=ALU.mult), reads=["dd", "pg"], writes=["RG"])
    P.dve(lambda e: e.tensor_tensor(out=g.RG[:, :, 1], in0=g.RG[:, :, 0], in1=ed[:, :], op=ALU.mult), reads=["RG", "ed"], writes=["RG"])
    P.dve(lambda e: e.tensor_tensor(out=Mb[:, :, :], in0=g.RE1[:, :, :], in1=g.RE2[:, :, :], op=ALU.add), reads=["RE1", "RE2"], writes=["Mb"])
    for ti in range(NT):
        hb, col = ti // 16, (ti % 16) * 32
        P.pe(lambda e, ti=ti, hb=hb, col=col: e.matmul(psP[hb][:, col:col + 32], lhsT=cb[:, CB_TRIU:CB_TRIU + 128], rhs=Mb[:, ti, :], start=True, stop=True),
             reads=["Mb"], writes=["psP_%d" % hb])
        P.pe(lambda e, ti=ti, hb=hb, col=col: e.matmul(psC[hb][:, col:col + 32], lhsT=cb[:, CB_ONES:CB_ONES + 128], rhs=Mb[:, ti, :], start=True, stop=True),
             reads=["Mb"], writes=["psC_%d" % hb])
    for hb in range(2):
        P.act(lambda e, hb=hb: e.copy(out=cA[:, hb * 16:(hb + 1) * 16, :], in_=psC[hb][:, :].rearrange("p (t x) -> p t x", x=32)),
              reads=["psC_%d" % hb], writes=["cA"])
    src, dst, ksrc, kdst = cA, cB, "cA", "cB"
    step = 1
    while step < NT:
        P.dve(lambda e, src=src, dst=dst, step=step: e.tensor_copy(out=dst[:, 0:step, :], in_=src[:, 0:step, :]), reads=[ksrc], writes=[kdst])
        P.dve(lambda e, src=src, dst=dst, step=step: e.tensor_tensor(out=dst[:, step:NT, :], in0=src[:, step:NT, :], in1=src[:, 0:NT - step, :], op=ALU.add),
              reads=[ksrc], writes=[kdst])
        src, dst, ksrc, kdst = dst, src, kdst, ksrc
        step *= 2
    inc, kinc = src, ksrc
    exc, kexc = dst, kdst
    P.dve(lambda e: e.memset(exc[:, 0:1, :], 0.0), reads=[kinc], writes=[kexc])
    P.dve(lambda e: e.tensor_copy(out=exc[:, 1:NT, :], in_=inc[:, 0:NT - 1, :]), reads=[kinc], writes=[kexc])
    P.dve(lambda e: e.tensor_copy(out=g.CAR[:, 0:32], in_=inc[:, NT - 1, :]), reads=[kinc], writes=["CAR"])
    for hb in range(2):
        P.dve(lambda e, hb=hb: e.tensor_tensor(out=g.RPF[:, hb * 16:(hb + 1) * 16, :], in0=psP[hb][:, :].rearrange("p (t x) -> p t x", x=32),
                                               in1=exc[:, hb * 16:(hb + 1) * 16, :], op=ALU.add),
              reads=["psP_%d" % hb, kexc], writes=["RPF"])
    cnt = g.CAR
    r_ = ph.sb([128, 32], F32, "r")
    nz = ph.sb([128, 32], F32, "nz")
    pad = ph.sb([128, 32], F32, "pad")
    ca = ph.sb([128, 32], F32, "ca")
    cbuf = ph.sb([128, 32], F32, "cb")
    off = ph.sb([128, 32], F32, "off")
    big = ph.sb([128, NT, 32], F32, "big")
    g.tmpbig = ph.sb([128, NT, 32], F32, "tmpbig")
    posf = ph.sb([128, NT, 2], F32, "posf")
    ej = ph.sb([128, NSLOT_T], F32, "ej")
    tmpw = ph.sb([128, NSLOT_T], F32, "tmpw")
    P.dve(lambda e: e.memset(nz[:, :], 0.0), writes=["nz"])
    for m in range(2 * SEQ // SLOT):
        P.dve(lambda e, m=m: e.scalar_tensor_tensor(out=nz[:, :], in0=cnt[:, 0:32], scalar=float(m * SLOT), in1=nz[:, :],
                                                    op0=ALU.is_gt, op1=ALU.add), reads=["CAR", "nz"], writes=["nz"])
    P.dve(lambda e: e.tensor_scalar(out=pad[:, :], in0=nz[:, :], scalar1=float(SLOT), scalar2=None, op0=ALU.mult), reads=["nz"], writes=["pad"])
    src, dst = pad, ca
    ksrc, kdst = "pad", "ca"
    step = 1
    while step < 32:
        P.dve(lambda e, src=src, dst=dst, step=step: e.tensor_copy(out=dst[:, 0:step], in_=src[:, 0:step]), reads=[ksrc], writes=[kdst])
        P.dve(lambda e, src=src, dst=dst, step=step: e.tensor_tensor(out=dst[:, step:32], in0=src[:, step:32], in1=src[:, 0:32 - step], op=ALU.add),
              reads=[ksrc], writes=[kdst])
        if dst is ca:
            src, dst, ksrc, kdst = ca, cbuf, "ca", "cb"
        else:
            src, dst, ksrc, kdst = cbuf, ca, "cb", "ca"
        step *= 2
    cum, kcum = src, ksrc
    P.dve(lambda e: e.tensor_tensor(out=off[:, :], in0=cum[:, :], in1=pad[:, :], op=ALU.subtract), reads=[kcum, "pad"], writes=["off"])
    P.dve(lambda e: e.tensor_tensor(out=big[:, :, :], in0=g.RPF[:, :, :], in1=off[:, :].unsqueeze(1).to_broadcast([128, NT, 32]), op=ALU.add),
          reads=["RPF", "off"], writes=["big"])
    for k, RE, kre in ((0, g.RE1, "RE1"), (1, g.RE2, "RE2")):
        P.dve(lambda e, RE=RE: e.tensor_tensor(out=g.tmpbig[:, :, :], in0=big[:, :, :], in1=RE[:, :, :], op=ALU.mult),
              reads=["big", kre], writes=["tmpbig"])
        P.dve(lambda e, k=k: e.tensor_reduce(out=posf[:, :, k], in_=g.tmpbig[:, :, :], axis=AX.X, op=ALU.add),
              reads=["tmpbig"], writes=["posf"])
    P.dve(lambda e: e.tensor_copy(out=g.POS[:, :, :], in_=posf[:, :, :]), reads=["posf"], writes=["POS"])
    P.dve(lambda e: e.memset(ej[:, :], 0.0), writes=["ej"])
    for ex in range(NE):
        P.dve(lambda e, ex=ex: e.scalar_tensor_tensor(out=ej[:, :], in0=cf[:, CF_THR:CF_THR + NSLOT_T], scalar=cum[:, ex:ex + 1], in1=ej[:, :],
                                                      op0=ALU.is_ge, op1=ALU.add), reads=[kcum, "ej"], writes=["ej"])
    P.dve(lambda e: e.tensor_scalar(out=ej[:, :], in0=ej[:, :], scalar1=float(NE - 1), scalar2=None, op0=ALU.min), reads=["ej"], writes=["ej"])
    P.dve(lambda e: e.tensor_scalar(out=tmpw[:, :], in0=ej[:, :], scalar1=128.0, scalar2=cf[:, CF_IOTA:CF_IOTA + 1], op0=ALU.mult, op1=ALU.add),
          reads=["ej"], writes=["tmpw"])
    P.dve(lambda e: e.tensor_scalar(out=tmpw[:, :], in0=tmpw[:, :], scalar1=float(li * NE * 128), scalar2=None, op0=ALU.add),
          reads=["tmpw"], writes=["tmpw"])
    P.dve(lambda e: e.tensor_copy(out=g.WG[:, :], in_=tmpw[:, :]), reads=["tmpw"], writes=["WG"])
    for c in range(4):
        P.dve(lambda e, c=c: e.tensor_scalar(out=tmpw[:, :], in0=ej[:, :], scalar1=512.0, scalar2=cf[:, CF_IOTA + 1 + c:CF_IOTA + 2 + c],
                                             op0=ALU.mult, op1=ALU.add), reads=["ej"], writes=["tmpw"])
        P.dve(lambda e: e.tensor_scalar(out=tmpw[:, :], in0=tmpw[:, :], scalar1=float(li * NE * FF), scalar2=None, op0=ALU.add),
              reads=["tmpw"], writes=["tmpw"])
        P.dve(lambda e, c=c: e.tensor_copy(out=g.WD[:, c, :], in_=tmpw[:, :]), reads=["tmpw"], writes=["WD"])
    xbt = [ph.sb([128, 1024], BF16, "xbt") for _ in range(4)]
    for ti in range(NT):
        xb = xbt[ti % 4]
        kx = "xbt_%d" % (ti % 4)
        P.dma(lambda e, xb=xb, ti=ti: e.dma_start(out=xb[:, :], in_=g.xb16[ti * 128:(ti + 1) * 128, :]), writes=[kx])
        for k in range(2):
            P.dma(lambda e, xb=xb, ti=ti, k=k: e.indirect_dma_start(out=g.xs, out_offset=bass.IndirectOffsetOnAxis(ap=g.POS[:, ti, k:k + 1], axis=0),
                                                                    in_=xb[:, :], in_offset=None),
                  reads=[kx, "POS"], writes=["xs_%d_%d" % (ti, k)], q=POOL)
    ph.finish()


def expert_phase(nc, g, li, use_dma_transpose=True):
    ph = Phase(nc, "ex%d" % li)
    P = ph.P
    cb = g.cb
    NB = 3
    Wg = [ph.sb([128, 8, FF], BF16, "Wg") for _ in range(NB)]
    Wu = [ph.sb([128, 8, FF], BF16, "Wu") for _ in range(NB)]
    Wd = [ph.sb([128, 4, D], BF16, "Wd") for _ in range(NB)]
    xTp = [ph.sb([128, 8, SLOT], BF16, "xTp") for _ in range(2)]
    xrow = [[ph.sb([128, D], BF16, "xrow") for _ in range(4)] for _ in range(2)]
    sgt = [ph.sb([128, SLOT], F32, "sg") for _ in range(2)]
    hT = [ph.sb([128, 4, SLOT], BF16, "hT") for _ in range(2)]
    ysb = [ph.sb([128, D], F32, "ysb") for _ in range(4)]
    psg = [ph.psum("psg") for _ in range(2)]
    psu = [ph.psum("psu") for _ in range(2)]
    psy = [ph.psum("psy") for _ in range(2)]
    pst = [ph.psum("pst") for _ in range(2)]
    pst_bf = [p[:, :].bitcast(BF16) for p in pst]
    wgv = g.d_moe_w_gate.rearrange("l e (p kk) f -> (l e p) (kk f)", kk=8)
    wuv = g.d_moe_w_up.rearrange("l e (p kk) f -> (l e p) (kk f)", kk=8)
    wdv = g.d_moe_w_down.rearrange("l e k d -> (l e k) d")

    def load(jt):
        b = jt % NB
        xb = jt % 2
        for sub in range(4):
            P.dma(lambda e, xb=xb, jt=jt, sub=sub: e.dma_start(out=xrow[xb][sub][:, :], in_=g.xs[jt * SLOT + sub * 128: jt * SLOT + (sub + 1) * 128, :]),
                  reads=["xs"], writes=["xrow_%d_%d" % (xb, sub)])
        P.dma(lambda e, b=b, jt=jt: e.indirect_dma_start(out=Wg[b][:, :, :].rearrange("p k f -> p (k f)"), out_offset=None, in_=wgv,
                                                         in_offset=bass.IndirectOffsetOnAxis(ap=g.WG[:, jt:jt + 1], axis=0)),
              reads=["WG"], writes=["Wg_%d" % b], q=POOL)
        P.dma(lambda e, b=b, jt=jt: e.indirect_dma_start(out=Wu[b][:, :, :].rearrange("p k f -> p (k f)"), out_offset=None, in_=wuv,
                                                         in_offset=bass.IndirectOffsetOnAxis(ap=g.WG[:, jt:jt + 1], axis=0)),
              reads=["WG"], writes=["Wu_%d" % b], q=POOL)
        for c in range(4):
            P.dma(lambda e, b=b, jt=jt, c=c: e.indirect_dma_start(out=Wd[b][:, c, :], out_offset=None, in_=wdv,
                                                                  in_offset=bass.IndirectOffsetOnAxis(ap=g.WD[:, c, jt:jt + 1], axis=0)),
                  reads=["WD"], writes=["Wd_%d_%d" % (b, c)], q=POOL)

    def transposes(jt):
        xb = jt % 2
        for sub in range(4):
            ptb = pst_bf[sub % 2]
            kpt = "pst_%d" % (sub % 2)
            xr = xrow[xb][sub]
            for kk in range(8):
                P.pe(lambda e, xr=xr, kk=kk, ptb=ptb: e.transpose(out=ptb[:, kk * 128:(kk + 1) * 128], in_=xr[:, kk * 128:(kk + 1) * 128],
                                                                  identity=cb[:, CB_IDENT:CB_IDENT + 128]),
                     reads=["xrow_%d_%d" % (xb, sub)], writes=[kpt])
            if sub % 2 == 0:
                P.act(lambda e, xb=xb, sub=sub, ptb=ptb: e.copy(out=xTp[xb][:, :, sub * 128:(sub + 1) * 128], in_=ptb.rearrange("p (k t) -> p k t", k=8)),
                      reads=[kpt], writes=["xTp_%d" % xb])
            else:
                P.dve(lambda e, xb=xb, sub=sub, ptb=ptb: e.tensor_copy(out=xTp[xb][:, :, sub * 128:(sub + 1) * 128], in_=ptb.rearrange("p (k t) -> p k t", k=8)),
                      reads=[kpt], writes=["xTp_%d" % xb])

    load(0)
    load(1)
    transposes(0)
    for jt in range(NSLOT_T):
        b = jt % NB
        xb = jt % 2
        if jt + 1 < NSLOT_T:
            transposes(jt + 1)
        if jt + 2 < NSLOT_T:
            load(jt + 2)
        h = hT[jt % 2]
        kh = "hT_%d" % (jt % 2)
        for fc in range(4):
            pg_, pu_ = psg[fc % 2], psu[fc % 2]
            kg, ku = "psg_%d" % (fc % 2), "psu_%d" % (fc % 2)
            for kk in range(8):
                P.pe(lambda e, b=b, xb=xb, kk=kk, fc=fc, pg_=pg_: e.matmul(pg_[:, :], lhsT=Wg[b][:, kk, fc * 128:(fc + 1) * 128], rhs=xTp[xb][:, kk, :],
                                                                           start=(kk == 0), stop=(kk == 7)),
                     reads=["Wg_%d" % b, "xTp_%d" % xb], writes=[kg])
            for kk in range(8):
                P.pe(lambda e, b=b, xb=xb, kk=kk, fc=fc, pu_=pu_: e.matmul(pu_[:, :], lhsT=Wu[b][:, kk, fc * 128:(fc + 1) * 128], rhs=xTp[xb][:, kk, :],
                                                                           start=(kk == 0), stop=(kk == 7)),
                     reads=["Wu_%d" % b, "xTp_%d" % xb], writes=[ku])
            sg = sgt[fc % 2]
            ksg = "sgt_%d" % (fc % 2)
            P.act(lambda e, pg_=pg_, sg=sg: e.activation(out=sg[:, :], in_=pg_[:, :], func=AF.Silu), reads=[kg], writes=[ksg])
            P.dve(lambda e, pu_=pu_, sg=sg, h=h, fc=fc: e.tensor_tensor(out=h[:, fc, :], in0=pu_[:, :], in1=sg[:, :], op=ALU.mult),
                  reads=[ku, ksg], writes=[kh])
        for sub in range(4):
            yb = ysb[sub]
            kyb = "ysb_%d" % sub
            for half in range(2):
                py = psy[half]
                kpy = "psy_%d" % half
                for fc in range(4):
                    P.pe(lambda e, b=b, fc=fc, sub=sub, half=half, py=py, h=h: e.matmul(py[:, :], lhsT=h[:, fc, sub * 128:(sub + 1) * 128],
                                                                                      rhs=Wd[b][:, fc, half * 512:(half + 1) * 512],
                                                                                      start=(fc == 0), stop=(fc == 3)),
                         reads=[kh] + ["Wd_%d_%d" % (b, c) for c in range(4)], writes=[kpy])
                if half == 0:
                    P.act(lambda e, py=py, yb=yb: e.copy(out=yb[:, 0:512], in_=py[:, :]), reads=[kpy], writes=[kyb + "a"])
                else:
                    P.dve(lambda e, py=py, yb=yb: e.tensor_copy(out=yb[:, 512:1024], in_=py[:, :]), reads=[kpy], writes=[kyb + "b"])
            P.dma(lambda e, yb=yb, jt=jt, sub=sub: e.dma_start(out=g.ys[jt * SLOT + sub * 128: jt * SLOT + (sub + 1) * 128, :], in_=yb[:, :]),
                  reads=[kyb + "a", kyb + "b"], writes=["ys_%d_%d" % (jt, sub)])
    ph.finish()


def combine_phase(nc, g, li, x1src, xdst):
    ph = Phase(nc, "cm%d" % li)
    P = ph.P
    NB = 3
    y1 = [ph.sb([128, D], F32, "y1") for _ in range(NB)]
    y2 = [ph.sb([128, D], F32, "y2") for _ in range(NB)]
    x1 = [ph.sb([128, D], F32, "x1") for _ in range(NB)]
    x2 = [ph.sb([128, D], F32, "x2") for _ in range(NB)]
    st = ph.sb([128, 12], F32, "st")
    mv = ph.sb([128, 2], F32, "mv")
    rs = ph.sb([128, 2], F32, "rs")

    def load(ti):
        b = ti % NB
        P.dma(lambda e, b=b, ti=ti: e.dma_start(out=x1[b][:, :], in_=x1src[ti * 128:(ti + 1) * 128, :]), reads=["XB"], writes=["x1_%d" % b])
        P.dma(lambda e, b=b, ti=ti: e.indirect_dma_start(out=y1[b][:, :], out_offset=None, in_=g.ys,
                                                         in_offset=bass.IndirectOffsetOnAxis(ap=g.POS[:, ti, 0:1], axis=0)),
              reads=["ys", "POS"], writes=["y1_%d" % b], q=POOL)
        P.dma(lambda e, b=b, ti=ti: e.indirect_dma_start(out=y2[b][:, :], out_offset=None, in_=g.ys,
                                                         in_offset=bass.IndirectOffsetOnAxis(ap=g.POS[:, ti, 1:2], axis=0)),
              reads=["ys", "POS"], writes=["y2_%d" % b], q=POOL)

    for ti in range(NB - 1):
        load(ti)
    for ti in range(NT):
        b = ti % NB
        if ti + NB - 1 < NT:
            load(ti + NB - 1)
        P.act(lambda e, b=b, ti=ti: e.activation(out=y1[b][:, :], in_=y1[b][:, :], func=AF.Copy, scale=g.RG[:, ti, 0:1]),
              reads=["y1_%d" % b, "RG"], writes=["y1_%d" % b])
        P.dve(lambda e, b=b, ti=ti: e.scalar_tensor_tensor(out=y2[b][:, :], in0=y2[b][:, :], scalar=g.RG[:, ti, 1:2], in1=y1[b][:, :],
                                                           op0=ALU.mult, op1=ALU.add), reads=["y2_%d" % b, "y1_%d" % b, "RG"], writes=["y2_%d" % b])
        P.dve(lambda e, b=b: e.scalar_tensor_tensor(out=x1[b][:, :], in0=x1[b][:, :], scalar=ALPHA, in1=y2[b][:, :],
                                                    op0=ALU.mult, op1=ALU.add), reads=["x1_%d" % b, "y2_%d" % b], writes=["x1_%d" % b])
        _ln_token_major(P, DVE, x1[b], x2[b], g.pb[:, PB_LN2G:PB_LN2G + 1024], g.pb[:, PB_LN2B:PB_LN2B + 1024],
                        st, mv, rs, "x1_%d" % b, "x2_%d" % b, "ln2")
        P.dma(lambda e, b=b, ti=ti: e.dma_start(out=xdst[ti * 128:(ti + 1) * 128, :], in_=x2[b][:, :]), reads=["x2_%d" % b], writes=["XOUT_%d" % ti])
    ph.finish()


def build_program(n_layers=DEPTH, stop_after=None, use_dma_transpose=True):
    nc = bass.Bass("TRN2", target_bir_lowering=False)
    g = G()

    def din(name, shape, dt):
        return nc.dram_tensor(name, list(shape), dt, kind="ExternalInput").ap()

    def dscr(name, shape, dt):
        return nc.dram_tensor(name, list(shape), dt, kind="Internal").ap()

    g.d_x = din("x", [SEQ, D], F32)
    g.d_posb = din("posb", [128, SEQ], I32)
    g.d_cf = din("constf", [128, CF_N], F32)
    g.d_cb = din("constb", [128, CB_N], BF16)
    g.d_conv_w_pw1 = din("conv_w_pw1", [2, D, 2 * D], F32)
    g.d_conv_w_pw2 = din("conv_w_pw2", [2, D, D], F32)
    g.d_ret_w_qkvg = din("ret_w_qkvg", [2, D, 6 * D], F32)
    g.d_ret_w_o = din("ret_w_o", [2, 2 * D, D], F32)
    g.d_moe_w_gate = din("moe_w_gate", [DEPTH, NE, D, FF], F32)
    g.d_moe_w_up = din("moe_w_up", [DEPTH, NE, D, FF], F32)
    g.d_moe_w_down = din("moe_w_down", [DEPTH, NE, FF, D], F32)
    g.d_pbln = din("pbln", [DEPTH, 128, PB_N], F32)
    g.d_pbmix = din("pbmix", [DEPTH, 128, PM_N], F32)
    g.d_pp = din("pp", [2, 128, PP_N], F32)
    g.d_wr = din("wr", [DEPTH, 128, 8, 36], F32)
    g.d_out = nc.dram_tensor("out", [SEQ, D], F32, kind="ExternalOutput").ap()
    g.XA = dscr("XA", [SEQ, D], F32)
    g.XB = dscr("XB", [SEQ, D], F32)
    g.xb16 = dscr("xb16", [SEQ, D], BF16)
    g.xs = dscr("xs", [NSLOT_T * SLOT, D], BF16)
    g.ys = dscr("ys", [NSLOT_T * SLOT, D], F32)
    g.QS = dscr("QS", [NT, 128, 8, 128], BF16)
    g.KS = dscr("KS", [NT, 128, 8, 128], BF16)
    g.VS = dscr("VS", [SEQ, 2 * D], BF16)
    g.SG = dscr("SG", [SEQ, 2 * D], BF16)

    with contextlib.ExitStack() as st:
        def sb(name, shape, dt):
            return st.enter_context(nc.sbuf_tensor(name, list(shape), dt))
        g.cf = sb("cf", [128, CF_N], F32)
        g.cb = sb("cb", [128, CB_N], BF16)
        g.pb = sb("pb", [128, PB_N], F32)
        g.wr = sb("wr_sb", [128, 8, 36], F32)
        g.LALL = sb("LALL", [128, NT, 36], F32)
        g.RG = sb("RG", [128, NT, 2], F32)
        g.POS = sb("POS", [128, NT, 2], I32)
        g.CAR = sb("CAR", [128, 32], F32)
        g.WG = sb("WG", [128, NSLOT_T], I32)
        g.WD = sb("WD", [128, 4, NSLOT_T], I32)

        EPS_AP[0] = g.cf[:, CF_EPS:CF_EPS + 1]
        ph = Phase(nc, "init")
        ph.P.dma(lambda e: e.dma_start(out=g.cf[:, :], in_=g.d_cf), writes=["cf"])
        ph.P.dma(lambda e: e.dma_start(out=g.cb[:, :], in_=g.d_cb), writes=["cb"])
        ph.finish()

        xcur = g.d_x
        for li in range(n_layers):
            if li % 2 == 0:
                conv_phase(nc, g, li, xcur, g.XB)
            else:
                retention_phase(nc, g, li, xcur, g.XB)
            if stop_after == ("mix", li):
                _copy_out(nc, g, g.XB)
                break
            offsets_scatter_phase(nc, g, li)
            expert_phase(nc, g, li, use_dma_transpose)
            last = (li == n_layers - 1)
            xnext = g.d_out if last else g.XA
            combine_phase(nc, g, li, g.XB, xnext)
            xcur = xnext
    return nc


def _copy_out(nc, g, src):
    ph = Phase(nc, "cpy")
    t = [ph.sb([128, D], F32, "t") for _ in range(2)]
    for ti in range(NT):
        b = ti % 2
        ph.P.dma(lambda e, b=b, ti=ti: e.dma_start(out=t[b][:, :], in_=src[ti * 128:(ti + 1) * 128, :]), reads=["src"], writes=["t%d" % b])
        ph.P.dma(lambda e, b=b, ti=ti: e.dma_start(out=g.d_out[ti * 128:(ti + 1) * 128, :], in_=t[b][:, :]), reads=["t%d" % b], writes=["o%d" % ti])
    ph.finish()


TWO_PI_HI = 6.28125
TWO_PI_LO = 2.0 * np.pi - 6.28125
PI = float(np.pi)


def retention_phase(nc, g, li, xsrc, xdst):
    _ret_pass1(nc, g, li, xsrc)
    _ret_pass2(nc, g, li, xsrc, xdst)


def _ret_pass1(nc, g, li, xsrc):
    j = li // 2
    ph = Phase(nc, "rp%d" % li)
    P = ph.P
    cf, cb = g.cf, g.cb
    _load_layer_params(ph, g, li)
    Wq = ph.sb([128, 8, 6 * D], BF16, "Wq")
    wsrc = g.d_ret_w_qkvg[j].rearrange("(k p) f -> p k f", p=128)
    for q in range(12):
        P.dma(lambda e, q=q: e.dma_start(out=Wq[:, :, q * 512:(q + 1) * 512], in_=wsrc[:, :, q * 512:(q + 1) * 512]),
              writes=["Wq_%d" % q], q=POOL)
    posi = ph.sb([128, 512], I32, "posi")
    posf = ph.sb([128, 512], F32, "posf")
    xin = [ph.sb([128, D], F32, "xin") for _ in range(2)]
    xbf = ph.sb([128, D], BF16, "xbf")
    xT = ph.sb([128, 8, 512], BF16, "xT")
    ang = ph.sb([128, 512], F32, "ang")
    ni = ph.sb([128, 512], I32, "ni")
    nf = ph.sb([128, 512], F32, "nf")
    rr = ph.sb([128, 512], F32, "rr")
    cc = ph.sb([128, 512], F32, "cc")
    mm = ph.sb([128, 512], F32, "mm")
    sinT = ph.sb([128, 512], F32, "sinT")
    cosT = ph.sb([128, 512], F32, "cosT")
    ta = [ph.sb([128, 512], F32, "ta")] * 2
    tb = [ph.sb([128, 512], F32, "tb")] * 2
    tc_ = [ph.sb([128, 512], F32, "tc")] * 2
    td = [ph.sb([128, 512], F32, "td")] * 2
    qkT = ph.sb([128, 16, 512], BF16, "qkT")
    vrow = [ph.sb([128, 2 * D], BF16, "vrow")] * 2
    srow = [ph.sb([128, 2 * D], BF16, "srow")] * 2
    psT = ph.psum("psT")
    psT_bf = psT[:, :].bitcast(BF16)
    psq = [ph.psum("psq") for _ in range(4)]
    psv = [ph.psum("psv") for _ in range(2)]
    invf = cf[:, CF_INVF:CF_INVF + 1]
    for st_i in range(NT // 4):
        t0 = st_i * 512
        for sub in range(4):
            ti = st_i * 4 + sub
            xi = xin[ti % 2]
            kxi = "xin_%d" % (ti % 2)
            P.dma(lambda e, xi=xi, ti=ti: e.dma_start(out=xi[:, :], in_=xsrc[ti * 128:(ti + 1) * 128, :]), writes=[kxi])
            P.act(lambda e, xi=xi: e.copy(out=xbf[:, :], in_=xi[:, :]), reads=[kxi], writes=["xbf"])
            for kc in range(8):
                P.pe(lambda e, kc=kc: e.transpose(out=psT_bf[:, kc * 128:(kc + 1) * 128], in_=xbf[:, kc * 128:(kc + 1) * 128],
                                                  identity=cb[:, CB_IDENT:CB_IDENT + 128]), reads=["xbf"], writes=["psT"])
            P.dve(lambda e, sub=sub: e.tensor_copy(out=xT[:, :, sub * 128:(sub + 1) * 128],
                                                   in_=psT_bf.rearrange("p (k t) -> p k t", k=8)), reads=["psT"], writes=["xT"])
        for sub in range(4):
            ti = st_i * 4 + sub
            vr, sr = vrow[ti % 2], srow[ti % 2]
            kv, ks = "vrow_0", "srow_0"
            for cbk in range(8):
                pv = psv[cbk % 2]
                kpv = "psv_%d" % (cbk % 2)
                c0 = 2048 + cbk * 512
                for kc in range(8):
                    P.pe(lambda e, kc=kc, c0=c0, pv=pv, sub=sub: e.matmul(pv[:, :], lhsT=xT[:, kc, sub * 128:(sub + 1) * 128], rhs=Wq[:, kc, c0:c0 + 512],
                                                                          start=(kc == 0), stop=(kc == 7)),
                         reads=["xT", "Wq_%d" % (c0 // 512)], writes=[kpv])
                if cbk < 4:
                    P.dve(lambda e, pv=pv, vr=vr, cbk=cbk: e.tensor_copy(out=vr[:, cbk * 512:(cbk + 1) * 512], in_=pv[:, :]), reads=[kpv], writes=[kv])
                else:
                    P.act(lambda e, pv=pv, sr=sr, cbk=cbk: e.activation(out=sr[:, (cbk - 4) * 512:(cbk - 3) * 512], in_=pv[:, :], func=AF.Silu),
                          reads=[kpv], writes=[ks])
            P.dma(lambda e, vr=vr, ti=ti: e.dma_start(out=g.VS[ti * 128:(ti + 1) * 128, :], in_=vr[:, :]), reads=[kv], writes=["VS_%d" % ti])
            P.dma(lambda e, sr=sr, ti=ti: e.dma_start(out=g.SG[ti * 128:(ti + 1) * 128, :], in_=sr[:, :]), reads=[ks], writes=["SG_%d" % ti])
        P.dma(lambda e, t0=t0: e.dma_start(out=posi[:, :], in_=g.d_posb[:, t0:t0 + 512]), writes=["posi"])
        P.dve(lambda e: e.tensor_copy(out=posf[:, :], in_=posi[:, :]), reads=["posi"], writes=["posf"])
        P.dve(lambda e: e.tensor_scalar(out=ang[:, :], in0=posf[:, :], scalar1=invf, scalar2=None, op0=ALU.mult),
               reads=["posf"], writes=["ang"])
        P.dve(lambda e: e.tensor_scalar(out=ni[:, :], in0=ang[:, :], scalar1=float(1.0 / (2.0 * np.pi)), scalar2=None, op0=ALU.mult),
               reads=["ang"], writes=["ni"])
        P.dve(lambda e: e.tensor_copy(out=nf[:, :], in_=ni[:, :]), reads=["ni"], writes=["nf"])
        P.dve(lambda e: e.scalar_tensor_tensor(out=rr[:, :], in0=nf[:, :], scalar=-TWO_PI_HI, in1=ang[:, :], op0=ALU.mult, op1=ALU.add),
              reads=["nf", "ang"], writes=["rr"])
        P.dve(lambda e: e.scalar_tensor_tensor(out=rr[:, :], in0=nf[:, :], scalar=-TWO_PI_LO, in1=rr[:, :], op0=ALU.mult, op1=ALU.add),
              reads=["nf", "rr"], writes=["rr"])
        P.dve(lambda e: e.tensor_scalar(out=mm[:, :], in0=rr[:, :], scalar1=PI, scalar2=-2.0 * PI, op0=ALU.is_gt, op1=ALU.mult),
               reads=["rr"], writes=["mm"])
        P.dve(lambda e: e.tensor_tensor(out=rr[:, :], in0=rr[:, :], in1=mm[:, :], op=ALU.add), reads=["rr", "mm"], writes=["rr"])
        P.dve(lambda e: e.tensor_scalar(out=mm[:, :], in0=rr[:, :], scalar1=-PI, scalar2=2.0 * PI, op0=ALU.is_lt, op1=ALU.mult),
               reads=["rr"], writes=["mm"])
        P.dve(lambda e: e.tensor_tensor(out=rr[:, :], in0=rr[:, :], in1=mm[:, :], op=ALU.add), reads=["rr", "mm"], writes=["rr"])
        P.dve(lambda e: e.tensor_scalar(out=cc[:, :], in0=rr[:, :], scalar1=0.5 * PI, scalar2=None, op0=ALU.add), reads=["rr"], writes=["cc"])
        P.dve(lambda e: e.tensor_scalar(out=mm[:, :], in0=cc[:, :], scalar1=PI, scalar2=-2.0 * PI, op0=ALU.is_gt, op1=ALU.mult),
               reads=["cc"], writes=["mm"])
        P.dve(lambda e: e.tensor_tensor(out=cc[:, :], in0=cc[:, :], in1=mm[:, :], op=ALU.add), reads=["cc", "mm"], writes=["cc"])
        P.act(lambda e: e.activation(out=sinT[:, :], in_=rr[:, :], func=AF.Sin), reads=["rr"], writes=["sinT"])
        P.act(lambda e: e.activation(out=cosT[:, :], in_=cc[:, :], func=AF.Sin), reads=["cc"], writes=["cosT"])
        for hp in range(8):
            b2 = hp % 2
            p1, p2 = psq[2 * b2], psq[2 * b2 + 1]
            k1, k2 = "psq_%d" % (2 * b2), "psq_%d" % (2 * b2 + 1)
            for half, pq, kq in ((0, p1, k1), (1, p2, k2)):
                c0 = (2 * hp + half) * 128
                for kc in range(8):
                    P.pe(lambda e, kc=kc, c0=c0, pq=pq: e.matmul(pq[:, :], lhsT=Wq[:, kc, c0:c0 + 128], rhs=xT[:, kc, :],
                                                                 start=(kc == 0), stop=(kc == 7)),
                         reads=["xT", "Wq_%d" % (c0 // 512)], writes=[kq])
            a_, b_, c_, d_ = ta[b2], tb[b2], tc_[b2], td[b2]
            sfx = "_0"
            P.dve(lambda e, p1=p1, a_=a_: e.tensor_tensor(out=a_[:, :], in0=p1[:, :], in1=cosT[:, :], op=ALU.mult), reads=[k1, "cosT"], writes=["ta" + sfx])
            P.dve(lambda e, p2=p2, b_=b_: e.tensor_tensor(out=b_[:, :], in0=p2[:, :], in1=sinT[:, :], op=ALU.mult), reads=[k2, "sinT"], writes=["tb" + sfx])
            P.dve(lambda e, p1=p1, c_=c_: e.tensor_tensor(out=c_[:, :], in0=p1[:, :], in1=sinT[:, :], op=ALU.mult), reads=[k1, "sinT"], writes=["tc" + sfx])
            P.dve(lambda e, p2=p2, d_=d_: e.tensor_tensor(out=d_[:, :], in0=p2[:, :], in1=cosT[:, :], op=ALU.mult), reads=[k2, "cosT"], writes=["td" + sfx])
            P.pool(lambda e, hp=hp, a_=a_, b_=b_: e.tensor_tensor(out=qkT[:, 2 * hp, :], in0=a_[:, :], in1=b_[:, :], op=ALU.subtract),
                   reads=["ta" + sfx, "tb" + sfx], writes=["qkT"])
            P.pool(lambda e, hp=hp, c_=c_, d_=d_: e.tensor_tensor(out=qkT[:, 2 * hp + 1, :], in0=c_[:, :], in1=d_[:, :], op=ALU.add),
                   reads=["tc" + sfx, "td" + sfx], writes=["qkT"])
        for sub in range(4):
            ti = st_i * 4 + sub
            P.dma(lambda e, ti=ti, sub=sub: e.dma_start(out=g.QS[ti], in_=qkT[:, 0:8, sub * 128:(sub + 1) * 128]), reads=["qkT"], writes=["QS_%d" % ti])
            P.dma(lambda e, ti=ti, sub=sub: e.dma_start(out=g.KS[ti], in_=qkT[:, 8:16, sub * 128:(sub + 1) * 128]), reads=["qkT"], writes=["KS_%d" % ti])
    ph.finish()


def _ret_pass2(nc, g, li, xsrc, xdst):
    j = li // 2
    ph = Phase(nc, "rq%d" % li)
    P = ph.P
    cf, cb = g.cf, g.cb
    Wo = ph.sb([128, 16, D], BF16, "Wo")
    wsrc = g.d_ret_w_o[j].rearrange("(k p) f -> p k f", p=128)
    for q in range(2):
        P.dma(lambda e, q=q: e.dma_start(out=Wo[:, q * 8:(q + 1) * 8, :], in_=wsrc[:, q * 8:(q + 1) * 8, :]), writes=["Wo"], q=POOL)
    pm = ph.sb([128, PM_N], F32, "pm")
    P.dma(lambda e: e.dma_start(out=pm[:, :], in_=g.d_pbmix[li]), writes=["PM"])
    qT = [ph.sb([128, 8, 128], BF16, "qT") for _ in range(2)]
    kT = [ph.sb([128, 8, 128], BF16, "kT") for _ in range(2)]
    vrow_ = [ph.sb([128, 2 * D], BF16, "vrow") for _ in range(2)]
    srow_ = [ph.sb([128, 2 * D], BF16, "srow") for _ in range(2)]
    xin = [ph.sb([128, D], F32, "xin") for _ in range(2)]
    S = ph.sb([128, RH, 2, 512], F32, "S")
    Sbf = ph.sb([128, RH, 2, 512], BF16, "Sbf")
    qxT = ph.sb([128, 8, 128], BF16, "qxT")
    kz = ph.sb([128, D], BF16, "kz")
    PT = [ph.sb([128, 128], BF16, "PT") for _ in range(2)]
    on = [ph.sb([128, 512], F32, "on") for _ in range(2)]
    y = ph.sb([128, 2 * D], BF16, "y")
    yT = ph.sb([128, 16, 128], BF16, "yT")
    rbuf = [ph.sb([128, D], F32, "r") for _ in range(2)]
    gst_ = ph.sb([128, RH, 6], F32, "gst")
    gmv_ = ph.sb([128, RH, 2], F32, "gmv")
    grs_ = ph.sb([128, RH, 1], F32, "grs")
    gnm_ = ph.sb([128, RH, 1], F32, "gnm")
    bufs = _RW()
    bufs.x1 = [ph.sb([128, D], F32, "x1") for _ in range(2)]
    bufs.x1bf = [ph.sb([128, D], BF16, "x1bf") for _ in range(2)]
    bufs.st = ph.sb([128, 12], F32, "st")
    bufs.mv = ph.sb([128, 2], F32, "mv")
    bufs.rs = ph.sb([128, 2], F32, "rs")
    RWk = _router_work(ph)
    pss = ph.psum("pss")
    pso = [ph.psum("pso") for _ in range(2)]
    pst = [ph.psum("pst") for _ in range(2)]
    psX = [ph.psum("psX") for _ in range(2)]
    psR = ph.psum("psR")
    psX_bf = [p[:, :].bitcast(BF16) for p in psX]
    P.dve(lambda e: e.memset(S[:, :, :, :], 0.0), writes=["S_%d_%d" % (h_, c_) for h_ in range(RH) for c_ in range(2)])
    P.dve(lambda e: e.memset(Sbf[:, :, :, :], 0.0), writes=["Sbf"])
    def load2(ti):
        b = ti % 2
        P.dma(lambda e, b=b, ti=ti: e.dma_start(out=qT[b][:, :, :], in_=g.QS[ti]), writes=["qT_%d" % b])
        P.dma(lambda e, b=b, ti=ti: e.dma_start(out=kT[b][:, :, :], in_=g.KS[ti]), writes=["kT_%d" % b])
        P.dma(lambda e, b=b, ti=ti: e.dma_start(out=vrow_[b][:, :], in_=g.VS[ti * 128:(ti + 1) * 128, :]), writes=["vrow_%d" % b])
        P.dma(lambda e, b=b, ti=ti: e.dma_start(out=srow_[b][:, :], in_=g.SG[ti * 128:(ti + 1) * 128, :]), writes=["srow_%d" % b])
        P.dma(lambda e, b=b, ti=ti: e.dma_start(out=xin[b][:, :], in_=xsrc[ti * 128:(ti + 1) * 128, :]), writes=["xin_%d" % b])

    load2(0)
    for ti in range(NT):
        b = ti % 2
        kq, kk_ = "qT_%d" % b, "kT_%d" % b
        xi = xin[b]
        kxi = "xin_%d" % b
        vrow, srow = vrow_[b], srow_[b]
        kvr, ksr = "vrow_%d" % b, "srow_%d" % b
        if ti + 1 < NT:
            load2(ti + 1)
        P.dve(lambda e, b=b: e.tensor_tensor(out=qxT[:, :, :], in0=qT[b][:, :, :],
                                             in1=cb[:, CB_XI:CB_XI + 1024].rearrange("p (k t) -> p k t", k=8), op=ALU.mult),
              reads=[kq], writes=["qxT"])
        for kc in range(8):
            P.pe(lambda e, b=b, kc=kc: e.transpose(out=psX_bf[0][:, kc * 128:(kc + 1) * 128], in_=kT[b][:, kc, :],
                                                   identity=cb[:, CB_IDENT:CB_IDENT + 128]), reads=[kk_], writes=["psT2_0"])
        for h in range(RH):
            P.act(lambda e, h=h: e.activation(out=kz[:, h * 256:(h + 1) * 256], in_=psX_bf[0][:, h * 256:(h + 1) * 256], func=AF.Copy,
                                              scale=cf[:, CF_ZETA + h:CF_ZETA + h + 1]), reads=["psT2_0"], writes=["kz"])
        for h in range(RH):
            pb2 = h % 2
            po = pso[pb2]
            kpo = "pso_%d" % pb2
            for c in range(2):
                P.pe(lambda e, b=b, h=h, c=c: e.matmul(pss[:, (h % 4) * 128:(h % 4 + 1) * 128], lhsT=kT[b][:, 2 * h + c, :], rhs=qT[b][:, 2 * h + c, :],
                                                       start=(c == 0), stop=(c == 1)), reads=[kq, kk_], writes=["pss_%d" % h])
            pt = PT[pb2]
            kpt = "PT_%d" % pb2
            P.dve(lambda e, h=h, pt=pt: e.tensor_tensor(out=pt[:, :], in0=pss[:, (h % 4) * 128:(h % 4 + 1) * 128],
                                                        in1=cf[:, CF_MT + h * 128:CF_MT + (h + 1) * 128], op=ALU.mult),
                  reads=["pss_%d" % h], writes=[kpt])
            P.pe(lambda e, h=h, pt=pt, po=po, vrow=vrow: e.matmul(po[:, :], lhsT=pt[:, :], rhs=vrow[:, h * 512:(h + 1) * 512], start=True, stop=False),
                 reads=[kpt, kvr], writes=[kpo])
            for c in range(2):
                P.pe(lambda e, h=h, c=c, po=po: e.matmul(po[:, :], lhsT=qxT[:, 2 * h + c, :], rhs=Sbf[:, h, c, :], start=False, stop=(c == 1)),
                     reads=["qxT", "Sbf_%d" % h], writes=[kpo])
            for c in range(2):
                P.pe(lambda e, h=h, c=c, vrow=vrow: e.matmul(pst[c][:, :], lhsT=kz[:, h * 256 + c * 128:h * 256 + (c + 1) * 128], rhs=vrow[:, h * 512:(h + 1) * 512],
                                                  start=True, stop=True), reads=["kz", kvr], writes=["pst_%d" % c])
                P.dve(lambda e, h=h, c=c: e.scalar_tensor_tensor(out=S[:, h, c, :], in0=S[:, h, c, :], scalar=float(GAMMA[h] ** 128.0), in1=pst[c][:, :],
                                                                 op0=ALU.mult, op1=ALU.add), reads=["pst_%d" % c, "S_%d_%d" % (h, c)], writes=["S_%d_%d" % (h, c)])
                P.act(lambda e, h=h, c=c: e.copy(out=Sbf[:, h, c, :], in_=S[:, h, c, :]), reads=["S_%d_%d" % (h, c)], writes=["Sbf_%d" % h])
            gst, gmv, grs, gnm = gst_[:, h, :], gmv_[:, h, :], grs_[:, h, :], gnm_[:, h, :]
            kh_ = "_%d" % h
            P.dve(lambda e, po=po, gst=gst: e.bn_stats(out=gst[:, 0:6], in_=po[:, :]), reads=[kpo], writes=["gst" + kh_])
            P.dve(lambda e, gst=gst, gmv=gmv: e.bn_aggr(out=gmv[:, 0:2], in_=gst[:, 0:6]), reads=["gst" + kh_], writes=["gmv" + kh_])
            P.act(lambda e, grs=grs, gmv=gmv: e.activation(out=grs[:, 0:1], in_=gmv[:, 1:2], func=AF.Sqrt, bias=EPS_AP[0], scale=1.0),
                  reads=["gmv" + kh_], writes=["grs" + kh_])
            P.dve(lambda e, grs=grs: e.reciprocal(out=grs[:, 0:1], in_=grs[:, 0:1]), reads=["grs" + kh_], writes=["grs" + kh_])
            P.dve(lambda e, gnm=gnm, gmv=gmv, grs=grs: e.scalar_tensor_tensor(out=gnm[:, 0:1], in0=gmv[:, 0:1], scalar=-1.0, in1=grs[:, 0:1],
                                                                              op0=ALU.mult, op1=ALU.mult),
                  reads=["gmv" + kh_, "grs" + kh_], writes=["gnm" + kh_])
            o_ = on[pb2]
            kon = "on_%d" % pb2
            P.act(lambda e, po=po, o_=o_, gnm=gnm, grs=grs: e.activation(out=o_[:, :], in_=po[:, :], func=AF.Identity, bias=gnm[:, 0:1], scale=grs[:, 0:1]),
                  reads=[kpo, "grs" + kh_, "gnm" + kh_], writes=[kon])
            P.dve(lambda e, h=h, o_=o_: e.tensor_tensor(out=o_[:, :], in0=o_[:, :], in1=pm[:, h * 512:(h + 1) * 512], op=ALU.mult),
                  reads=[kon, "PM"], writes=[kon])
            P.dve(lambda e, h=h, o_=o_: e.tensor_tensor(out=o_[:, :], in0=o_[:, :], in1=pm[:, 2 * D + h * 512:2 * D + (h + 1) * 512], op=ALU.add),
                  reads=[kon, "PM"], writes=[kon])
            P.dve(lambda e, h=h, o_=o_, srow=srow: e.tensor_tensor(out=y[:, h * 512:(h + 1) * 512], in0=o_[:, :], in1=srow[:, h * 512:(h + 1) * 512], op=ALU.mult),
                  reads=[kon, ksr], writes=["y"])
        for half in range(2):
            for q in range(8):
                kc = half * 8 + q
                P.pe(lambda e, kc=kc, q=q, half=half: e.transpose(out=psX_bf[half][:, q * 128:(q + 1) * 128], in_=y[:, kc * 128:(kc + 1) * 128],
                                                                  identity=cb[:, CB_IDENT:CB_IDENT + 128]), reads=["y"], writes=["psT2_%d" % half])
            if half == 0:
                P.act(lambda e: e.copy(out=yT[:, 0:8, :], in_=psX_bf[0].rearrange("p (k t) -> p k t", k=8)), reads=["psT2_0"], writes=["yT"])
            else:
                P.dve(lambda e: e.tensor_copy(out=yT[:, 8:16, :], in_=psX_bf[1].rearrange("p (k t) -> p k t", k=8)), reads=["psT2_1"], writes=["yT"])
        r = rbuf[b]
        kr = "r_%d" % b
        for half in range(2):
            po = pso[half]
            kpo = "pso_%d" % half
            for kc in range(16):
                P.pe(lambda e, kc=kc, half=half, po=po: e.matmul(po[:, :], lhsT=yT[:, kc, :], rhs=Wo[:, kc, half * 512:(half + 1) * 512],
                                                                 start=(kc == 0), stop=(kc == 15)), reads=["yT", "Wo"], writes=[kpo])
            P.dve(lambda e, half=half, po=po, xi=xi, r=r: e.scalar_tensor_tensor(out=r[:, half * 512:(half + 1) * 512], in0=xi[:, half * 512:(half + 1) * 512],
                                                                                 scalar=ALPHA, in1=po[:, :], op0=ALU.mult, op1=ALU.add),
                  reads=[kpo, kxi], writes=[kr])
        if ti > 0:
            _post_mixer(ph, g, ti - 1, rbuf[(ti - 1) % 2], "r_%d" % ((ti - 1) % 2), RWk, bufs, psR, psX, xdst)
    _post_mixer(ph, g, NT - 1, rbuf[(NT - 1) % 2], "r_%d" % ((NT - 1) % 2), RWk, bufs, psR, psX, xdst)
    ph.finish()


def _rep(v, n=128):
    return np.ascontiguousarray(np.broadcast_to(np.asarray(v, np.float32).reshape(1, -1), (n, np.asarray(v).size)))


def prepare_shared(inputs):
    cf, cb, _ = _host_consts()
    sh = {"constf": cf, "constb": cb}
    for k in ("conv_w_pw1", "conv_w_pw2", "ret_w_qkvg", "ret_w_o", "moe_w_gate", "moe_w_up", "moe_w_down"):
        sh[k] = np.ascontiguousarray(inputs[k], dtype=np.float32)
    pbln = np.zeros((DEPTH, 128, PB_N), np.float32)
    pbmix = np.zeros((DEPTH, 128, PM_N), np.float32)
    wr = np.zeros((DEPTH, 128, 8, 36), np.float32)
    for i in range(DEPTH):
        pbln[i, :, PB_LN1G:PB_LN1G + D] = _rep(inputs["ln1_g"][i])
        pbln[i, :, PB_LN1B:PB_LN1B + D] = _rep(inputs["ln1_b"][i])
        pbln[i, :, PB_LN2G:PB_LN2G + D] = _rep(inputs["ln2_g"][i])
        pbln[i, :, PB_LN2B:PB_LN2B + D] = _rep(inputs["ln2_b"][i])
        pbln[i, :, PB_RB:PB_RB + 4] = _rep(inputs["moe_b_grp"][i])
        pbln[i, :, PB_RB + 4:PB_RB + 36] = _rep(inputs["moe_b_route"][i])
        wcat = np.concatenate([inputs["moe_w_grp"][i], inputs["moe_w_route"][i]], axis=1)
        wr[i] = wcat.reshape(8, 128, 36).transpose(1, 0, 2)
        j = i // 2
        if i % 2 == 0:
            pbmix[i, :, 0:D] = _rep(inputs["conv_b_pw2"][j])
        else:
            pbmix[i, :, 0:2 * D] = _rep(inputs["ret_gn_g"][j])
            pbmix[i, :, 2 * D:4 * D] = _rep(inputs["ret_gn_b"][j])
    pp = np.zeros((2, 128, PP_N), np.float32)
    for j in range(2):
        pp[j, :, PP_B1:PP_B1 + 16] = np.asarray(inputs["conv_b_pw1"][j]).reshape(16, 128).T
        wdw = np.asarray(inputs["conv_w_dw"][j])
        pp[j, :, PP_WDW:PP_WDW + 248] = wdw.reshape(CW, 8, 128).transpose(2, 1, 0).reshape(128, 248)
        pp[j, :, PP_BDW:PP_BDW + 8] = np.asarray(inputs["conv_b_dw"][j]).reshape(8, 128).T
        pp[j, :, PP_LNG:PP_LNG + 8] = np.asarray(inputs["conv_ln_g"][j]).reshape(8, 128).T
        pp[j, :, PP_LNB:PP_LNB + 8] = np.asarray(inputs["conv_ln_b"][j]).reshape(8, 128).T
    sh["pbln"], sh["pbmix"], sh["pp"], sh["wr"] = pbln, pbmix, pp, wr
    return sh


_NC_CACHE = {}


def kernel(**inputs):
    x = np.asarray(inputs["x"], np.float32)
    pos = np.asarray(inputs["positions"], np.int32)
    sh = prepare_shared(inputs)
    if "nc" not in _NC_CACHE:
        _NC_CACHE["nc"] = build_program()
    nc = _NC_CACHE["nc"]
    in_maps = []
    for c in range(8):
        m = dict(sh)
        m["x"] = np.ascontiguousarray(x[c])
        m["posb"] = np.ascontiguousarray(np.broadcast_to(pos[c][None, :], (128, SEQ)))
        in_maps.append(m)
    res = run_bass_kernel_spmd(nc, in_maps, core_ids=list(range(8)))
    return np.stack([np.asarray(r["out"], np.float32) for r in res.results], axis=0)
```
